# Optimizing a Trainium2 kernel written in Bass

```python
import math
import jax, jax.numpy as jnp
from jax import lax
import numpy as np

D_MODEL = 1024
BATCH = 4
SEQ = 4096
DEPTH = 2

GRID_W = 64
CTX_LEN = 256
EPS = 1e-6
ROPE_THETA = 10000.0
Q_BLOCK = 128
CHUNK = 64

MIX_WIDTH = D_MODEL
GROUP_WIDTH = MIX_WIDTH // 4
HEAD_DIM = 64
DIFF_HEADS = GROUP_WIDTH // HEAD_DIM
DIFF_QK_DIM = HEAD_DIM // 2
GQA_HEADS = GROUP_WIDTH // HEAD_DIM
GQA_KV_HEADS = 2
MLSTM_HEADS = GROUP_WIDTH // HEAD_DIM
GDN_HEADS = GROUP_WIDTH // HEAD_DIM
GDN_CONV = 5
D_FF = 2816
N_EXPERTS = 8
TOP_K = 2
D_FF_EXPERT = 3584

SPLIT_SIZES = (
    GROUP_WIDTH, GROUP_WIDTH, GROUP_WIDTH,
    GROUP_WIDTH, GQA_KV_HEADS * HEAD_DIM, GQA_KV_HEADS * HEAD_DIM,
    GROUP_WIDTH, GROUP_WIDTH, GROUP_WIDTH, GROUP_WIDTH, 4 * MLSTM_HEADS,
    3 * GROUP_WIDTH, GROUP_WIDTH, 4 * GDN_HEADS,
)
N_IN = sum(SPLIT_SIZES)

kernel_name = 'hybrid_dit_parallel_head_groups'


def rms_norm(x, gain):
    xf = x.astype(jnp.float32)
    y = xf * lax.rsqrt(jnp.mean(xf * xf, axis=-1, keepdims=True) + EPS)
    return (y * gain.astype(jnp.float32)).astype(x.dtype)


def head_layer_norm(h, gain):
    mu = jnp.mean(h, axis=-1, keepdims=True)
    var = jnp.mean(jnp.square(h - mu), axis=-1, keepdims=True)
    return (h - mu) * lax.rsqrt(var + EPS) * gain.astype(jnp.float32).reshape(h.shape[-2:])


def l2_normalize(x):
    return x * lax.rsqrt(jnp.sum(x * x, axis=-1, keepdims=True) + EPS)


def modulate(h, shift, scale):
    return h * (1 + scale) + shift


def axial_rope_angles(n, dim):
    rows = n // GRID_W
    t = jnp.arange(rows * GRID_W)
    row = (t // GRID_W).astype(jnp.float32)
    col = (t % GRID_W).astype(jnp.float32)
    n_freq = dim // 4
    inv_freq = ROPE_THETA ** (-jnp.arange(n_freq, dtype=jnp.float32) / n_freq)
    ang = jnp.stack([row[:, None] * inv_freq, col[:, None] * inv_freq], axis=1)
    return jnp.cos(ang), jnp.sin(ang)


def apply_axial_rope(x, cos, sin):
    shp = x.shape
    xr = x.astype(jnp.float32).reshape(shp[:-1] + (2, 2, shp[-1] // 4))
    x1, x2 = xr[..., 0, :], xr[..., 1, :]
    c, s = cos[:, None], sin[:, None]
    y = jnp.stack([x1 * c - x2 * s, x2 * c + x1 * s], axis=-2)
    return y.reshape(shp).astype(x.dtype)


def softmax_f32(s):
    return jax.nn.softmax(s.astype(jnp.float32), axis=-1)


def sweep_query_blocks(core, q, q_axis, out_axis):
    n = q.shape[q_axis]
    nb = n // Q_BLOCK
    qb = q.reshape(q.shape[:q_axis] + (nb, Q_BLOCK) + q.shape[q_axis + 1:])
    out = lax.map(core, jnp.moveaxis(qb, q_axis, 0))
    out = jnp.moveaxis(out, 0, out_axis)
    return out.reshape(out.shape[:out_axis] + (n,) + out.shape[out_axis + 2:])


def diff_attn_core(q, k, v, lam):
    p = softmax_f32(jnp.einsum('bhjqd,bhjkd->bhjqk', q, k) * DIFF_QK_DIM ** -0.5)
    p = p[:, :, 0] - lam * p[:, :, 1]
    return jnp.einsum('bhqk,bhkd->bhqd', p.astype(v.dtype), v)


def gqa_core(q, k, v):
    p = softmax_f32(jnp.einsum('bhgqd,bhkd->bhgqk', q, k) * HEAD_DIM ** -0.5)
    return jnp.einsum('bhgqk,bhkd->bhgqd', p.astype(v.dtype), v)


def diff_attention(q, k, v, qc, kc, vc, qk_gain, lam_vec, subln, lambda_init, rope, with_ctx):
    H, dq = DIFF_HEADS, DIFF_QK_DIM
    maps = lambda t: t.reshape(t.shape[0], t.shape[1], 2 * H, dq)
    tqk = lambda t: t.reshape(t.shape[0], t.shape[1], H, 2, dq).transpose(0, 2, 3, 1, 4)
    tv = lambda t: t.reshape(t.shape[0], t.shape[1], H, 2 * dq).transpose(0, 2, 1, 3)
    q = apply_axial_rope(rms_norm(maps(q), qk_gain[0]), *rope)
    k = apply_axial_rope(rms_norm(maps(k), qk_gain[1]), *rope)
    kc = tqk(rms_norm(maps(kc), qk_gain[1]))
    vc = tv(vc)
    lam_vec = lam_vec.astype(jnp.float32)
    lam = jnp.exp(jnp.sum(lam_vec[0] * lam_vec[1])) - jnp.exp(jnp.sum(lam_vec[2] * lam_vec[3])) + lambda_init
    k_all = jnp.concatenate([kc, tqk(k)], axis=3)
    v_all = jnp.concatenate([vc, tv(v)], axis=2)
    o = sweep_query_blocks(lambda qb: diff_attn_core(qb, k_all, v_all, lam), tqk(q), 3, 2)

    def post(o):
        o = rms_norm(o, subln) * (1 - lambda_init)
        return o.transpose(0, 2, 1, 3).reshape(o.shape[0], o.shape[2], GROUP_WIDTH)

    if not with_ctx:
        return post(o), None
    qc = tqk(rms_norm(maps(qc), qk_gain[0]))
    return post(o), post(diff_attn_core(qc, kc, vc, lam))


def gqa_attention(q, k, v, qc, kc, vc, qk_gain, rope, with_ctx):
    G = GQA_HEADS // GQA_KV_HEADS
    heads = lambda t, h: t.reshape(t.shape[0], t.shape[1], h, HEAD_DIM)
    tq = lambda t: t.reshape(t.shape[0], t.shape[1], GQA_KV_HEADS, G, HEAD_DIM).transpose(0, 2, 3, 1, 4)
    tk = lambda t: t.transpose(0, 2, 1, 3)
    q = apply_axial_rope(rms_norm(heads(q, GQA_HEADS), qk_gain[0]), *rope)
    k = apply_axial_rope(rms_norm(heads(k, GQA_KV_HEADS), qk_gain[1]), *rope)
    kc = tk(rms_norm(heads(kc, GQA_KV_HEADS), qk_gain[1]))
    vc = tk(heads(vc, GQA_KV_HEADS))
    k_all = jnp.concatenate([kc, tk(k)], axis=2)
    v_all = jnp.concatenate([vc, tk(heads(v, GQA_KV_HEADS))], axis=2)
    o = sweep_query_blocks(lambda qb: gqa_core(qb, k_all, v_all), tq(q), 3, 3)
    post = lambda o: o.transpose(0, 3, 1, 2, 4).reshape(o.shape[0], o.shape[3], GROUP_WIDTH)
    if not with_ctx:
        return post(o), None
    qc = tq(rms_norm(heads(qc, GQA_HEADS), qk_gain[0]))
    return post(o), post(gqa_core(qc, kc, vc))


def mlstm_chunkwise(q, k, v, i_pre, f_pre, state):
    B, H, T, d = q.shape
    nc = T // CHUNK
    chunks = lambda a: jnp.moveaxis(a.reshape(a.shape[:2] + (nc, CHUNK) + a.shape[3:]), 2, 0)
    idx = jnp.arange(CHUNK)
    causal = idx[:, None] >= idx[None, :]

    def step(carry, xs):
        C, n, m = carry
        qc, kc, vc, ic, fc = xs
        b = jnp.cumsum(fc, axis=-1)
        inter = b + m[..., None]
        dmat = jnp.where(causal, b[..., :, None] - b[..., None, :] + ic[..., None, :], -jnp.inf)
        m_t = jnp.maximum(inter, jnp.max(dmat, axis=-1))
        w_inter = jnp.exp(inter - m_t)
        s = jnp.einsum('bhtd,bhsd->bhts', qc, kc) * jnp.exp(dmat - m_t[..., None])
        num = w_inter[..., None] * jnp.einsum('bhvk,bhtk->bhtv', C, qc) + jnp.einsum('bhts,bhsv->bhtv', s, vc)
        den = w_inter * jnp.einsum('bhk,bhtk->bht', n, qc) + jnp.sum(s, axis=-1)
        h = num / jnp.maximum(jnp.abs(den), jnp.exp(-m_t))[..., None]
        b_last = b[..., -1]
        g = b_last[..., None] - b + ic
        m_new = jnp.maximum(b_last + m, jnp.max(g, axis=-1))
        a_prev = jnp.exp(b_last + m - m_new)
        a_s = jnp.exp(g - m_new[..., None])
        C = a_prev[..., None, None] * C + jnp.einsum('bhs,bhsv,bhsk->bhvk', a_s, vc, kc)
        n = a_prev[..., None] * n + jnp.einsum('bhs,bhsk->bhk', a_s, kc)
        return (C, n, m_new), h

    xs = tuple(chunks(a) for a in (q, k, v, i_pre, jax.nn.log_sigmoid(f_pre)))
    state, hs = lax.scan(step, state, xs)
    return jnp.moveaxis(hs, 0, 2).reshape(B, H, T, d), state


def mlstm_mixer(lat, ctx, gate_bias, norm_gain, with_ctx):
    f32 = jnp.float32

    def prep(q, k, v, gates):
        bsz, t_len, _ = q.shape
        heads = lambda a: a.reshape(bsz, t_len, MLSTM_HEADS, HEAD_DIM).transpose(0, 2, 1, 3).astype(f32)
        gt = gates.reshape(bsz, t_len, 4, MLSTM_HEADS).astype(f32) + gate_bias.astype(f32)
        return heads(q), heads(k) * HEAD_DIM ** -0.5, heads(v), gt.transpose(2, 0, 3, 1)

    ql, kl, vl, gl = prep(lat[0], lat[1], lat[2], lat[4])
    qc, kc, vc, gc = prep(ctx[0], ctx[1], ctx[2], ctx[4])
    bsz = ql.shape[0]
    state0 = (jnp.zeros((bsz, MLSTM_HEADS, HEAD_DIM, HEAD_DIM), f32),
              jnp.zeros((bsz, MLSTM_HEADS, HEAD_DIM), f32),
              jnp.zeros((bsz, MLSTM_HEADS), f32))
    h_lat, h_ctx = [], []
    for d in range(2):
        rev = (lambda a: jnp.flip(a, axis=2)) if d == 1 else (lambda a: a)
        hc_d, st = mlstm_chunkwise(rev(qc), rev(kc), rev(vc), rev(gc[2 * d]), rev(gc[2 * d + 1]), state0)
        hl_d, _ = mlstm_chunkwise(rev(ql), rev(kl), rev(vl), rev(gl[2 * d]), rev(gl[2 * d + 1]), st)
        h_lat.append(rev(hl_d))
        h_ctx.append(rev(hc_d))

    def post(h, o):
        bsz_, t_len, _ = o.shape
        h = head_layer_norm(h.transpose(0, 2, 1, 3), norm_gain).reshape(bsz_, t_len, GROUP_WIDTH)
        return (jax.nn.sigmoid(o.astype(f32)) * h).astype(o.dtype)

    y_lat = post(h_lat[0] + h_lat[1], lat[3])
    return y_lat, (post(h_ctx[0] + h_ctx[1], ctx[3]) if with_ctx else None)


def short_conv(x, w):
    kw = w.shape[0]
    return lax.conv_general_dilated(x, w[:, None, :].astype(x.dtype), window_strides=(1,),
                                    padding=[(kw // 2, kw // 2)],
                                    dimension_numbers=('NWC', 'WIO', 'NWC'),
                                    feature_group_count=x.shape[-1])


def gated_delta_chunkwise(q, k, v, beta, g, state):
    B, H, T, dk = q.shape
    dv = v.shape[-1]
    nc = T // CHUNK
    ch = lambda a: a.reshape(a.shape[:2] + (nc, CHUNK) + a.shape[3:])
    q, k, v, beta, g = (ch(a) for a in (q, k, v, beta, g))
    gam = jnp.cumsum(g, axis=-1)
    idx = jnp.arange(CHUNK)
    causal = idx[:, None] >= idx[None, :]
    strict = idx[:, None] > idx[None, :]
    decay = jnp.exp(jnp.where(causal, gam[..., :, None] - gam[..., None, :], -jnp.inf))
    kb = k * beta[..., None]
    a = jnp.where(strict, jnp.einsum('bhcid,bhcjd->bhcij', kb, k) * decay, 0.0)
    m = a + jnp.eye(CHUNK, dtype=a.dtype)
    rhs = jnp.concatenate([v * beta[..., None], kb * jnp.exp(gam)[..., None]], axis=-1)
    sol = lax.linalg.triangular_solve(m, rhs, left_side=True, lower=True, unit_diagonal=True)
    u, w = sol[..., :dv], sol[..., dv:]
    attn = jnp.einsum('bhcid,bhcjd->bhcij', q, k) * decay

    def step(S, xs):
        qi, ki, ui, wi, gi, ai = xs
        v_new = ui - jnp.einsum('bhid,bhdv->bhiv', wi, S)
        o = jnp.einsum('bhid,bhdv->bhiv', qi * jnp.exp(gi)[..., None], S) + jnp.einsum('bhij,bhjv->bhiv', ai, v_new)
        gl = gi[..., -1:]
        S = S * jnp.exp(gl)[..., None] + jnp.einsum('bhid,bhiv->bhdv', ki * jnp.exp(gl - gi)[..., None], v_new)
        return S, o

    xs = tuple(jnp.moveaxis(t, 2, 0) for t in (q, k, u, w, gam, attn))
    state, o = lax.scan(step, state, xs)
    return jnp.moveaxis(o, 0, 2).reshape(B, H, T, dv), state


def gdn_mixer(lat, ctx, conv_w, a_log, dt_bias, norm_gain, with_ctx):
    f32 = jnp.float32

    def prep(qkv, gates):
        bsz, t_len, _ = qkv.shape
        t = jax.nn.silu(short_conv(qkv, conv_w))
        q, k, v = jnp.split(t, 3, axis=-1)
        heads = lambda a: a.reshape(bsz, t_len, GDN_HEADS, HEAD_DIM).transpose(0, 2, 1, 3).astype(f32)
        q = l2_normalize(heads(q)) * HEAD_DIM ** -0.5
        k = l2_normalize(heads(k))
        gt = gates.reshape(bsz, t_len, 4, GDN_HEADS).astype(f32).transpose(2, 0, 3, 1)
        beta = jax.nn.sigmoid(gt[0::2])
        logdecay = -jnp.exp(a_log.astype(f32))[:, None, :, None] * jax.nn.softplus(
            gt[1::2] + dt_bias.astype(f32)[:, None, :, None])
        return q, k, heads(v), beta, logdecay

    ql, kl, vl, bl, gl = prep(lat[0], lat[2])
    qc, kc, vc, bc, gc = prep(ctx[0], ctx[2])
    state0 = jnp.zeros(ql.shape[:2] + (HEAD_DIM, HEAD_DIM), f32)
    o_lat, o_ctx = [], []
    for d in range(2):
        rev = (lambda a: jnp.flip(a, axis=2)) if d == 1 else (lambda a: a)
        oc, st = gated_delta_chunkwise(rev(qc), rev(kc), rev(vc), rev(bc[d]), rev(gc[d]), state0)
        ol, _ = gated_delta_chunkwise(rev(ql), rev(kl), rev(vl), rev(bl[d]), rev(gl[d]), st)
        o_lat.append(rev(ol))
        o_ctx.append(rev(oc))

    def post(o, z):
        bsz, t_len, _ = z.shape
        o = rms_norm(o.transpose(0, 2, 1, 3), norm_gain)
        zh = z.reshape(bsz, t_len, GDN_HEADS, HEAD_DIM).astype(f32)
        return (o * jax.nn.silu(zh)).reshape(bsz, t_len, GROUP_WIDTH).astype(z.dtype)

    y_lat = post(o_lat[0] + o_lat[1], lat[1])
    return y_lat, (post(o_ctx[0] + o_ctx[1], ctx[1]) if with_ctx else None)


def swiglu(h, wg, wu, wd):
    return (jax.nn.silu(h @ wg) * (h @ wu)) @ wd


def moe_swiglu(h, router, wg, wu, wd):
    logits = (h @ router).astype(jnp.float32)
    top_val, top_idx = lax.top_k(logits, TOP_K)
    top_w = jax.nn.softmax(top_val, axis=-1)
    gates = jnp.sum(jax.nn.one_hot(top_idx, N_EXPERTS, dtype=jnp.float32) * top_w[..., None], axis=-2)
    out = jnp.zeros_like(h)
    for e in range(N_EXPERTS):
        out = out + gates[..., e:e + 1].astype(h.dtype) * swiglu(h, wg[e], wu[e], wd[e])
    return out


def setup_inputs(seed: int = 0) -> dict:
    key = jax.random.key(seed)
    ks = iter(jax.random.split(key, 40))
    D = D_MODEL
    n_dense = (DEPTH + 1) // 2
    n_moe = DEPTH // 2
    nrm = lambda shape, scale: jax.random.normal(next(ks), shape, jnp.float32) * scale
    gain = lambda shape: 1.0 + nrm(shape, 0.02)
    forget_lin = jnp.linspace(3.0, 6.0, MLSTM_HEADS, dtype=jnp.float32)
    gate_base = jnp.zeros((4, MLSTM_HEADS), jnp.float32).at[1].set(forget_lin).at[3].set(forget_lin)
    dt = jnp.exp(jax.random.uniform(next(ks), (DEPTH, 2, GDN_HEADS), jnp.float32,
                                    minval=math.log(1e-3), maxval=math.log(1e-1)))
    return {
        'x': nrm((BATCH, SEQ, D), 1.0),
        'c': nrm((BATCH, D), 1.0),
        'ctx': nrm((BATCH, CTX_LEN, D), 1.0),
        'c_ctx': nrm((D,), 1.0),
        'w_mod': nrm((DEPTH, D, 6 * D), 0.5 * D ** -0.5),
        'b_mod': nrm((DEPTH, 6 * D), 0.02),
        'norm1': gain((DEPTH, D)),
        'norm2': gain((DEPTH, D)),
        'w_in': nrm((DEPTH, D, N_IN), D ** -0.5),
        'w_out': nrm((DEPTH, MIX_WIDTH, D), MIX_WIDTH ** -0.5),
        'diff_qk_gain': gain((DEPTH, 2, DIFF_QK_DIM)),
        'diff_lambda': nrm((DEPTH, 4, DIFF_QK_DIM), 0.1),
        'diff_subln': gain((DEPTH, 2 * DIFF_QK_DIM)),
        'gqa_qk_gain': gain((DEPTH, 2, HEAD_DIM)),
        'mlstm_gate_bias': gate_base + nrm((DEPTH, 4, MLSTM_HEADS), 0.1),
        'mlstm_norm': gain((DEPTH, GROUP_WIDTH)),
        'gdn_conv': nrm((DEPTH, GDN_CONV, 3 * GROUP_WIDTH), GDN_CONV ** -0.5),
        'gdn_a_log': jnp.log(jax.random.uniform(next(ks), (DEPTH, 2, GDN_HEADS), jnp.float32, minval=1.0, maxval=16.0)),
        'gdn_dt_bias': dt + jnp.log(-jnp.expm1(-dt)),
        'gdn_norm': gain((DEPTH, HEAD_DIM)),
        'ffn_w_gate': nrm((n_dense, D, D_FF), D ** -0.5),
        'ffn_w_up': nrm((n_dense, D, D_FF), D ** -0.5),
        'ffn_w_down': nrm((n_dense, D_FF, D), D_FF ** -0.5),
        'moe_router': nrm((n_moe, D, N_EXPERTS), D ** -0.5),
        'moe_w_gate': nrm((n_moe, N_EXPERTS, D, D_FF_EXPERT), D ** -0.5),
        'moe_w_up': nrm((n_moe, N_EXPERTS, D, D_FF_EXPERT), D ** -0.5),
        'moe_w_down': nrm((n_moe, N_EXPERTS, D_FF_EXPERT, D), D_FF_EXPERT ** -0.5),
    }


def reference(x, c, ctx, c_ctx, w_mod, b_mod, norm1, norm2, w_in, w_out,
              diff_qk_gain, diff_lambda, diff_subln, gqa_qk_gain,
              mlstm_gate_bias, mlstm_norm, gdn_conv, gdn_a_log, gdn_dt_bias, gdn_norm,
              ffn_w_gate, ffn_w_up, ffn_w_down, moe_router, moe_w_gate, moe_w_up, moe_w_down):
    n = x.shape[1]
    rope_diff = axial_rope_angles(n, DIFF_QK_DIM)
    rope_gqa = axial_rope_angles(n, HEAD_DIM)
    offsets = np.cumsum(SPLIT_SIZES)[:-1].tolist()
    xc = ctx
    for li in range(DEPTH):
        with_ctx = li < DEPTH - 1
        lambda_init = 0.8 - 0.6 * math.exp(-0.3 * li)
        mod = jnp.einsum('bd,de->be', jax.nn.silu(c), w_mod[li]) + b_mod[li]
        sh1, sc1, g1, sh2, sc2, g2 = jnp.split(mod[:, None, :], 6, axis=-1)
        csh1, csc1, cg1, csh2, csc2, cg2 = jnp.split(jax.nn.silu(c_ctx) @ w_mod[li] + b_mod[li], 6, axis=-1)
        u = jnp.split(modulate(rms_norm(x, norm1[li]), sh1, sc1) @ w_in[li], offsets, axis=-1)
        uc = jnp.split(modulate(rms_norm(xc, norm1[li]), csh1, csc1) @ w_in[li], offsets, axis=-1)
        ya, ya_c = diff_attention(u[0], u[1], u[2], uc[0], uc[1], uc[2], diff_qk_gain[li], diff_lambda[li],
                                  diff_subln[li], lambda_init, rope_diff, with_ctx)
        yb, yb_c = gqa_attention(u[3], u[4], u[5], uc[3], uc[4], uc[5], gqa_qk_gain[li], rope_gqa, with_ctx)
        yc, yc_c = mlstm_mixer(u[6:11], uc[6:11], mlstm_gate_bias[li], mlstm_norm[li], with_ctx)
        yd, yd_c = gdn_mixer(u[11:14], uc[11:14], gdn_conv[li], gdn_a_log[li], gdn_dt_bias[li], gdn_norm[li], with_ctx)
        x = x + g1 * (jnp.concatenate([ya, yb, yc, yd], axis=-1) @ w_out[li])
        if with_ctx:
            xc = xc + cg1 * (jnp.concatenate([ya_c, yb_c, yc_c, yd_c], axis=-1) @ w_out[li])
        j = li // 2
        if li % 2 == 0:
            ffn = lambda h: swiglu(h, ffn_w_gate[j], ffn_w_up[j], ffn_w_down[j])
        else:
            ffn = lambda h: moe_swiglu(h, moe_router[j], moe_w_gate[j], moe_w_up[j], moe_w_down[j])
        x = x + g2 * ffn(modulate(rms_norm(x, norm2[li]), sh2, sc2))
        if with_ctx:
            xc = xc + cg2 * ffn(modulate(rms_norm(xc, norm2[li]), csh2, csc2))
    return x
```

```python
import math
from contextlib import ExitStack
import numpy as np
import concourse.bass as bass
import concourse.mybir as mybir
from concourse.bass_utils import run_bass_kernel_spmd

F32 = mybir.dt.float32
BF16 = mybir.dt.bfloat16
AF = mybir.ActivationFunctionType
ALU = mybir.AluOpType
AX = mybir.AxisListType

D = 1024
NCTX = 256
NLAT = 4096
T = NCTX + NLAT
NT = T // 128
EPS = 1e-6
N_IN = 3360
D_FF = 2816
D_FFE = 3584
NE = 8


class Dep:
    __slots__ = ("w", "r", "excl")

    def __init__(self, excl=False):
        self.w = {}
        self.r = {}
        self.excl = excl


class Prog:
    def __init__(self):
        self.nc = bass.Bass("TRN2", target_bir_lowering=False)
        nc = self.nc
        self.h = {"pe": nc.tensor, "act": nc.scalar, "dve": nc.vector, "pool": nc.gpsimd, "sp": nc.sync}
        self.sem = {}
        self.cnt = {}
        self.semobj = {}
        self.seen = {e: {} for e in self.h}
        self.nsem = 0
        for e in self.h:
            self._newsem(e)
        self.NS = 12
        self.slots = {q: [nc.alloc_semaphore(f"dq_{q}_{i}") for i in range(self.NS)] for q in ("sp", "pool", "act")}
        self.dcnt = {q: 0 for q in self.slots}
        for q in self.slots:
            for s in self.slots[q]:
                self.semobj[id(s)] = s

    def _newsem(self, e):
        s = self.nc.alloc_semaphore(f"s_{e}_{self.nsem}")
        self.nsem += 1
        self.sem[e] = s
        self.cnt[e] = 0
        if not hasattr(self, "semobj"):
            self.semobj = {}
        self.semobj[id(s)] = s

    def _wait(self, e, tok):
        sid, val = tok
        if self.seen[e].get(sid, 0) >= val:
            return
        self.seen[e][sid] = val
        self.h[e].wait_ge(self.semobj[sid], val)

    def _deps(self, e, r, w):
        need = {}
        for d in r:
            for sid, v in d.w.items():
                need[sid] = max(need.get(sid, 0), v)
            if d.excl:
                for sid, v in d.r.items():
                    need[sid] = max(need.get(sid, 0), v)
        for d in w:
            for sid, v in d.w.items():
                need[sid] = max(need.get(sid, 0), v)
            for sid, v in d.r.items():
                need[sid] = max(need.get(sid, 0), v)
        own = id(self.sem[e])
        for sid, v in need.items():
            if e == "pe" and sid == own:
                continue
            self._wait(e, (sid, v))

    def _mark(self, tok, r, w):
        sid, v = tok
        for d in r:
            d.r[sid] = max(d.r.get(sid, 0), v)
        for d in w:
            d.w = {sid: v}
            d.r = {}

    def op(self, e, fn, r=(), w=()):
        self._deps(e, r, w)
        if self.cnt[e] >= 30000:
            self._newsem(e)
        ins = fn(self.h[e])
        self.cnt[e] += 1
        ins.then_inc(self.sem[e], 1)
        self._mark((id(self.sem[e]), self.cnt[e]), r, w)

    def dma(self, q, out, in_, r=(), w=(), **kw):
        self._deps(q, r, w)
        i = self.dcnt[q]
        self.dcnt[q] += 1
        s = self.slots[q][i % self.NS]
        val = 16 * (i // self.NS + 1)
        self._wait(q, (id(s), val - 16))
        self.h[q].dma_start(out=out, in_=in_, **kw).then_inc(s, 16)
        self._mark((id(s), val), r, w)

    def eps_ap(self, eps, n):
        assert abs(eps - EPS) < 1e-12
        return self.epst[0:n, 0:1]

    def barrier(self):
        toks = []
        for e in self.h:
            if self.cnt[e] > 0:
                toks.append((id(self.sem[e]), self.cnt[e]))
        for q in self.slots:
            n = self.dcnt[q]
            for j in range(min(n, self.NS)):
                i = n - 1 - j
                toks.append((id(self.slots[q][i % self.NS]), 16 * (i // self.NS + 1)))
        for e in self.h:
            for t in toks:
                self._wait(e, t)


class SB:
    N = 0

    def __init__(self, p):
        self.p = p
        self.es = ExitStack()
        self.n = 0

    def __enter__(self):
        self.es.__enter__()
        return self

    def __exit__(self, *a):
        self.p.barrier()
        return self.es.__exit__(*a)

    def t(self, shape, dt=F32, name="t"):
        SB.N += 1
        return self.es.enter_context(self.p.nc.sbuf_tensor(f"{name}_{SB.N}", list(shape), dt)), Dep()

    def ps(self, shape=(128, 512), dt=F32, name="ps"):
        SB.N += 1
        return self.es.enter_context(self.p.nc.psum_tensor(f"{name}_{SB.N}", list(shape), dt)), Dep(excl=True)


CA_Q, CA_K, CA_V = 0, 256, 512
CB_Q, CB_K, CB_V = 768, 1024, 1152
CC_Q, CC_K, CC_V, CC_O, CC_G = 1280, 1536, 1792, 2048, 2304
CD_QKV, CD_Z, CD_G = 2320, 3088, 3344


def dram(p, name, shape, dt, kind="Internal"):
    return p.nc.dram_tensor(name, list(shape), dt, kind=kind).ap()


def phase_mod(p, l, I, S):
    nc = p.nc
    modT, d_modT = S["modT"]
    gsh, d_gsh = S["gsh"]
    grow, d_grow = S["grow"]
    with SB(p) as sb:
        cc, d_cc = sb.t((128, 8, 2))
        sc, d_sc = sb.t((128, 8, 2))
        ones, d_ones = sb.t((128, 128))
        rep, d_rep = sb.t((128, 2, 8, 128))
        bmT, d_bmT = sb.t((128, 48))
        nrm, d_nrm = sb.t((128, 2, 8))
        wm = [sb.t((128, 8, 512)) for _ in range(2)]
        psm, d_psm = sb.ps((128, 512))
        psr = [sb.ps((128, 512)) for _ in range(2)]
        p.dma("sp", cc[:], I["cc"][:, :, :], w=[d_cc])
        p.dma("sp", bmT[:], I["bmodT"][:, l, :], w=[d_bmT])
        p.dma("sp", nrm[:, 0, :], I["norm1T"][:, l, :], w=[d_nrm])
        p.dma("sp", nrm[:, 1, :], I["norm2T"][:, l, :], w=[d_nrm])
        for j in range(2):
            for which, c0 in ((0, 2048), (1, 5120)):
                p.dma("sp", grow[:, which, j, :], I["b_mod"][l:l + 1, c0:c0 + 1024].partition_broadcast(128), w=[d_grow])
        p.op("act", lambda h: h.activation(out=sc[:], in_=cc[:], func=AF.Sigmoid), r=[d_cc], w=[d_sc])
        p.op("dve", lambda h: h.tensor_tensor(out=sc[:], in0=sc[:], in1=cc[:], op=ALU.mult), r=[d_cc, d_sc], w=[d_sc])
        p.op("dve", lambda h: h.memset(ones[:], 1.0), w=[d_ones])
        for j in range(2):
            for k in range(8):
                p.op("dve", lambda h, j=j, k=k: h.tensor_scalar(out=rep[:, j, k, :], in0=ones[:], scalar1=sc[:, k, j:j + 1],
                                                                 scalar2=None, op0=ALU.mult), r=[d_ones, d_sc], w=[d_rep])
        wsrc = I["w_mod"][l].rearrange("(k q) n -> q k n", q=128)
        for blk in range(12):
            wt, d_wt = wm[blk % 2]
            p.dma("sp", wt[:], wsrc[:, :, blk * 512:(blk + 1) * 512], w=[d_wt])
            for fc in range(4):
                for k in range(8):
                    p.op("pe", lambda h, fc=fc, k=k, wt=wt: h.matmul(psm[:, fc * 2:fc * 2 + 2], lhsT=wt[:, k, fc * 128:(fc + 1) * 128],
                                                                    rhs=sc[:, k, :], start=(k == 0), stop=(k == 7)),
                         r=[d_wt, d_sc], w=[d_psm])
            p.op("dve", lambda h, blk=blk: h.tensor_copy(out=modT[:, blk * 4:(blk + 1) * 4, :],
                                                         in_=psm[:, 0:8].rearrange("q (f j) -> q f j", j=2)),
                 r=[d_psm], w=[d_modT])
            if blk in (4, 5, 10, 11):
                which = 0 if blk < 6 else 1
                half = blk % 2
                for j in range(2):
                    pr, d_pr = psr[j]
                    for k in range(8):
                        p.op("pe", lambda h, j=j, k=k, wt=wt, pr=pr: h.matmul(pr[:, :], lhsT=rep[:, j, k, :], rhs=wt[:, k, :],
                                                                              start=(k == 0), stop=(k == 7)),
                             r=[d_wt, d_rep], w=[d_pr])
                    dst = grow[:, which, j, half * 512:(half + 1) * 512]
                    p.op("dve", lambda h, dst=dst, pr=pr: h.tensor_tensor(out=dst, in0=pr[:, :], in1=dst, op=ALU.add),
                         r=[d_pr, d_grow], w=[d_grow])
        for j in range(2):
            p.op("dve", lambda h, j=j: h.tensor_tensor(out=modT[:, :, j], in0=modT[:, :, j], in1=bmT[:], op=ALU.add),
                 r=[d_bmT, d_modT], w=[d_modT])
        for j in range(2):
            for n_i, (c_sh, c_sc) in enumerate(((0, 8), (24, 32))):
                p.op("dve", lambda h, j=j, n_i=n_i, c_sc=c_sc: h.scalar_tensor_tensor(
                    out=gsh[:, 2 * n_i, :, j], in0=modT[:, c_sc:c_sc + 8, j], scalar=1.0, in1=nrm[:, n_i, :],
                    op0=ALU.add, op1=ALU.mult), r=[d_modT, d_nrm], w=[d_gsh])
                p.op("dve", lambda h, j=j, n_i=n_i, c_sh=c_sh: h.tensor_copy(out=gsh[:, 2 * n_i + 1, :, j], in_=modT[:, c_sh:c_sh + 8, j]),
                     r=[d_modT], w=[d_gsh])


def rstd(p, out, in_, tmp, scale, deps, eps=EPS):
    p.op("act", lambda h: h.activation(out=tmp, in_=in_, func=AF.Sqrt, scale=scale, bias=p.eps_ap(eps, in_.shape[0])), r=deps + [p.d_eps], w=deps)
    p.op("dve", lambda h: h.reciprocal(out=out, in_=tmp), r=deps, w=deps)


def norm_tile(p, sb_objs, xt, d_xt, gsh, d_gsh, which, j, xnT, d_xnT, tok0):
    (junk, d_junk, ss, d_ss, xs, d_xs, tps, ident, d_ident) = sb_objs
    p.op("act", lambda h: h.activation(out=junk[:], in_=xt[:], func=AF.Square, accum_out=ss[:, 0:1]), r=[d_xt], w=[d_junk, d_ss])
    rstd(p, ss[:, 2:3], ss[:, 0:1], ss[:, 1:2], 1.0 / D, [d_ss])
    p.op("dve", lambda h: h.tensor_scalar(out=xs[:], in0=xt[:], scalar1=ss[:, 2:3], scalar2=None, op0=ALU.mult),
         r=[d_xt, d_ss], w=[d_xs])
    for c in range(8):
        tp, d_tp = tps[c // 4]
        p.op("pe", lambda h, c=c, tp=tp: h.transpose(tp[:, (c % 4) * 128:(c % 4 + 1) * 128], xs[:, c * 128:(c + 1) * 128], ident[:]),
             r=[d_xs, d_ident], w=[d_tp])
    for c in range(8):
        tp, d_tp = tps[c // 4]
        p.op("act", lambda h, c=c, tp=tp: h.activation(out=xnT[:, c, tok0:tok0 + 128], in_=tp[:, (c % 4) * 128:(c % 4 + 1) * 128],
                                                       func=AF.Identity, scale=gsh[:, 2 * which, c, j:j + 1],
                                                       bias=gsh[:, 2 * which + 1, c, j:j + 1]),
             r=[d_tp, d_gsh], w=[d_xnT])


def qk_norm_rope(p, sbo, src, d_src, ncols, dim, gain, d_gain, rope, d_rope, dst, d_dst):
    (sq, d_sq, st, d_st, t1, d_t1, t2, d_t2) = sbo
    ng = ncols // dim
    p.op("act", lambda h: h.activation(out=sq[:, 0:ncols], in_=src, func=AF.Square), r=[d_src], w=[d_sq])
    p.op("dve", lambda h: h.tensor_reduce(out=st[:, 0:ng], in_=sq[:, 0:ncols].rearrange("q (g d) -> q g d", d=dim), axis=AX.X, op=ALU.add),
         r=[d_sq], w=[d_st])
    rstd(p, st[:, 0:ng], st[:, 0:ng], st[:, 0:ng], 1.0 / dim, [d_st])
    tgt = t1 if rope is not None else dst
    d_tgt = d_t1 if rope is not None else d_dst
    p.op("dve", lambda h: h.tensor_tensor(out=tgt[:, 0:ncols].rearrange("q (g d) -> q g d", d=dim),
                                          in0=src.rearrange("q (g d) -> q g d", d=dim),
                                          in1=st[:, 0:ng].unsqueeze(2).to_broadcast([128, ng, dim]), op=ALU.mult),
         r=[d_src, d_st], w=[d_tgt])
    p.op("dve", lambda h: h.tensor_tensor(out=tgt[:, 0:ncols], in0=tgt[:, 0:ncols], in1=gain[:, 0:ncols], op=ALU.mult),
         r=[d_gain, d_tgt], w=[d_tgt])
    if rope is None:
        return
    q4 = dim // 4
    v = lambda a: a[:, 0:ncols].rearrange("q (g a s f) -> q g a s f", a=2, s=2, f=q4)
    cb = rope[:, 0, :].rearrange("q (a s f) -> q a s f", a=2, s=2).unsqueeze(1).to_broadcast([128, ng, 2, 2, q4])
    sbv = rope[:, 1, :].rearrange("q (a s f) -> q a s f", a=2, s=2)
    for s_ in range(2):
        p.op("dve", lambda h, s_=s_: h.tensor_tensor(out=v(t2)[:, :, :, s_, :], in0=v(t1)[:, :, :, 1 - s_, :],
                                                     in1=sbv[:, :, s_, :].unsqueeze(1).to_broadcast([128, ng, 2, q4]), op=ALU.mult),
             r=[d_t1, d_rope], w=[d_t2])
    p.op("dve", lambda h: h.tensor_tensor(out=v(t1), in0=v(t1), in1=cb, op=ALU.mult), r=[d_rope, d_t1], w=[d_t1])
    p.op("dve", lambda h: h.tensor_tensor(out=dst[:, 0:ncols], in0=t1[:, 0:ncols], in1=t2[:, 0:ncols], op=ALU.add),
         r=[d_t1, d_t2], w=[d_dst])


def phase_inproj(p, l, I, S, Z, xsrc):
    gsh, d_gsh = S["gsh"]
    ident, d_ident = S["ident"]
    with SB(p) as sb:
        w, d_w = sb.t((128, 8, N_IN), BF16, "win")
        for k in range(8):
            p.dma("pool", w[:, k, :], I["w_in"][l, k * 128:(k + 1) * 128, :], w=[d_w])
        gainA, d_gainA = sb.t((128, 512))
        gainB, d_gainB = sb.t((128, 384))
        for m in range(8):
            p.dma("sp", gainA[:, m * 32:(m + 1) * 32], I["diff_qk_gain"][l, 0:1, :].partition_broadcast(128), w=[d_gainA])
            p.dma("sp", gainA[:, 256 + m * 32:256 + (m + 1) * 32], I["diff_qk_gain"][l, 1:2, :].partition_broadcast(128), w=[d_gainA])
        for m in range(6):
            p.dma("sp", gainB[:, m * 64:(m + 1) * 64], I["gqa_qk_gain"][l, (0 if m < 4 else 1):(1 if m < 4 else 2), :].partition_broadcast(128),
                  w=[d_gainB])
        xts = [sb.t((128, D)) for _ in range(2)]
        junk, d_junk = sb.t((128, D))
        xs, d_xs = sb.t((128, D))
        ss, d_ss = sb.t((128, 4))
        tps = [sb.ps() for _ in range(2)]
        nobj = (junk, d_junk, ss, d_ss, xs, d_xs, tps, ident, d_ident)
        xnT, d_xnT = sb.t((128, 8, 512), BF16, "xnT")
        sq, d_sq = sb.t((128, 512))
        st, d_st = sb.t((128, 16))
        t1, d_t1 = sb.t((128, 512))
        t2, d_t2 = sb.t((128, 512))
        qko = (sq, d_sq, st, d_st, t1, d_t1, t2, d_t2)
        qkn, d_qkn = sb.t((128, 512))
        ropeA, d_ropeA = sb.t((128, 2, 32))
        ropeB, d_ropeB = sb.t((128, 2, 64))
        ps_tm = [sb.ps() for _ in range(2)]
        ps_fm = [sb.ps() for _ in range(2)]
        ps_tr, d_ps_tr = sb.ps()
        stA, d_stA = sb.t((128, 4, 512), BF16)
        stB, d_stB = sb.t((128, 3, 512), BF16)
        stv, d_stv = sb.t((128, 512), BF16)
        stf, d_stf = sb.t((128, 784))
        stfm = [sb.t((128, 512)) for _ in range(2)]
        blocks = [(0, 2)] + [(2 + 4 * i, 4) for i in range(8)]
        ntm = 0
        nfm = 0
        for (t0, ntl) in blocks:
            ntok = ntl * 128
            tokb = t0 * 128
            j = 1 if t0 < 2 else 0
            for ti in range(ntl):
                tt = t0 + ti
                xt, d_xt = xts[tt % 2]
                p.dma("sp", xt[:], xsrc[tt * 128:(tt + 1) * 128, :], w=[d_xt])
                norm_tile(p, nobj, xt, d_xt, gsh, d_gsh, 0, j, xnT, d_xnT, ti * 128)
            for ti in range(ntl):
                tt = t0 + ti
                tk = slice(ti * 128, (ti + 1) * 128)
                rows = slice(tt * 128, (tt + 1) * 128)
                lat = tt >= 2
                if lat:
                    p.dma("sp", ropeA[:], I["ropeA"][(tt - 2) * 128:(tt - 1) * 128, :, :], w=[d_ropeA])
                    p.dma("sp", ropeB[:], I["ropeB"][(tt - 2) * 128:(tt - 1) * 128, :, :], w=[d_ropeB])

                def tm_mm(c0, ncol):
                    nonlocal ntm
                    ps, d_ps = ps_tm[ntm % 2]
                    ntm += 1
                    for k in range(8):
                        p.op("pe", lambda h, k=k, ps=ps: h.matmul(ps[:, 0:ncol], lhsT=xnT[:, k, tk], rhs=w[:, k, c0:c0 + ncol],
                                                                  start=(k == 0), stop=(k == 7)), r=[d_xnT, d_w], w=[d_ps])
                    return ps, d_ps
                ps, d_ps = tm_mm(CA_Q, 512)
                qk_norm_rope(p, qko, ps[:, 0:512], d_ps, 512, 32, gainA, d_gainA, ropeA if lat else None, d_ropeA, qkn, d_qkn)
                for c in range(4):
                    p.op("pe", lambda h, c=c: h.transpose(ps_tr[:, c * 128:(c + 1) * 128], qkn[:, c * 128:(c + 1) * 128], ident[:]),
                         r=[d_qkn, d_ident], w=[d_ps_tr])
                p.op("act", lambda h: h.activation(out=stA[:, :, tk], in_=ps_tr[:, :].rearrange("q (c t) -> q c t", c=4), func=AF.Copy),
                     r=[d_ps_tr], w=[d_stA])
                ps, d_ps = tm_mm(CA_V, 256)
                p.op("act", lambda h, ps=ps: h.activation(out=stv[:, 0:256], in_=ps[:, 0:256], func=AF.Copy), r=[d_ps], w=[d_stv])
                p.dma("sp", Z["vA"][rows, :], stv[:, 0:256], r=[d_stv])
                ps, d_ps = tm_mm(CB_Q, 512)
                p.op("act", lambda h, ps=ps: h.activation(out=stv[:, 256:384], in_=ps[:, 384:512], func=AF.Copy), r=[d_ps], w=[d_stv])
                p.dma("sp", Z["vB"][rows, :], stv[:, 256:384], r=[d_stv])
                qk_norm_rope(p, qko, ps[:, 0:384], d_ps, 384, 64, gainB, d_gainB, ropeB if lat else None, d_ropeB, qkn, d_qkn)
                for c in range(3):
                    p.op("pe", lambda h, c=c: h.transpose(ps_tr[:, c * 128:(c + 1) * 128], qkn[:, c * 128:(c + 1) * 128], ident[:]),
                         r=[d_qkn, d_ident], w=[d_ps_tr])
                p.op("act", lambda h: h.activation(out=stB[:, :, tk], in_=ps_tr[:, 0:384].rearrange("q (c t) -> q c t", c=3), func=AF.Copy),
                     r=[d_ps_tr], w=[d_stB])
                ps, d_ps = tm_mm(CC_V, 512)
                p.op("act", lambda h, ps=ps: h.activation(out=stf[:, 0:512], in_=ps[:, 0:512], func=AF.Copy), r=[d_ps], w=[d_stf])
                p.dma("sp", Z["vC"][rows, :], stf[:, 0:256], r=[d_stf])
                p.dma("sp", Z["oC"][rows, :], stf[:, 256:512], r=[d_stf])
                ps, d_ps = tm_mm(CD_Z, 272)
                p.op("act", lambda h, ps=ps: h.activation(out=stf[:, 512:784], in_=ps[:, 0:272], func=AF.Copy), r=[d_ps], w=[d_stf])
                p.dma("sp", Z["zD"][rows, :], stf[:, 512:768], r=[d_stf])
                p.dma("sp", Z["gD"][rows, :], stf[:, 768:784], r=[d_stf])
                ps, d_ps = tm_mm(CC_K, 256)
                p.op("act", lambda h, ps=ps: h.activation(out=stf[:, 0:256], in_=ps[:, 0:256], func=AF.Copy), r=[d_ps], w=[d_stf])
                p.dma("sp", Z["kC"][rows, :], stf[:, 0:256], r=[d_stf])
                ps, d_ps = tm_mm(CC_G, 16)
                p.op("dve", lambda h, ps=ps: h.tensor_copy(out=st[:, 0:16], in_=ps[:, 0:16]), r=[d_ps], w=[d_st])
                p.dma("sp", Z["gC"][rows, :], st[:, 0:16], r=[d_st])
            tb = slice(tokb, tokb + ntok)
            for c in range(2):
                p.dma("sp", Z["qTa"][c * 128:(c + 1) * 128, tb], stA[:, c, 0:ntok], r=[d_stA])
                p.dma("sp", Z["kTa"][c * 128:(c + 1) * 128, tb], stA[:, 2 + c, 0:ntok], r=[d_stA])
                p.dma("sp", Z["qTb"][c * 128:(c + 1) * 128, tb], stB[:, c, 0:ntok], r=[d_stB])
            p.dma("sp", Z["kTb"][:, tb], stB[:, 2, 0:ntok], r=[d_stB])
            for ci in range(10):
                c0 = CC_Q + ci * 128 if ci < 4 else CD_QKV + (ci - 4) * 128
                ps, d_ps = ps_fm[nfm % 2]
                so, d_so = stfm[nfm % 2]
                nfm += 1
                for k in range(8):
                    p.op("pe", lambda h, k=k, ps=ps, c0=c0: h.matmul(ps[:, 0:ntok], lhsT=w[:, k, c0:c0 + 128], rhs=xnT[:, k, 0:ntok],
                                                                     start=(k == 0), stop=(k == 7)), r=[d_xnT, d_w], w=[d_ps])
                p.op("act", lambda h, ps=ps, so=so: h.activation(out=so[:, 0:ntok], in_=ps[:, 0:ntok], func=AF.Copy), r=[d_ps], w=[d_so])
                if ci < 2:
                    dst = Z["qTc"][ci * 128:(ci + 1) * 128, tb]
                elif ci < 4:
                    dst = Z["kTc"][(ci - 2) * 128:(ci - 1) * 128, tb]
                else:
                    dst = Z["qkvT"][(ci - 4) * 128:(ci - 3) * 128, tb]
                p.dma("sp", dst, so[:, 0:ntok], r=[d_so])


def rope_table(dim):
    nf = dim // 4
    t = np.arange(NLAT)
    row = (t // 64).astype(np.float32)
    col = (t % 64).astype(np.float32)
    inv = (np.float32(10000.0) ** (-np.arange(nf, dtype=np.float32) / np.float32(nf))).astype(np.float32)
    ang = np.stack([row[:, None] * inv, col[:, None] * inv], axis=1).astype(np.float32)
    c, s = np.cos(ang).astype(np.float32), np.sin(ang).astype(np.float32)
    C = np.stack([c, c], axis=2)
    Sp = np.stack([-s, s], axis=2)
    return np.ascontiguousarray(np.stack([C.reshape(NLAT, dim), Sp.reshape(NLAT, dim)], axis=1)).astype(np.float32)


IN_SPECS = {
    "xin": ([T, D], F32), "cc": ([128, 8, 2], F32), "flags": ([128, 2], F32),
    "bmodT": ([128, 2, 48], F32), "norm1T": ([128, 2, 8], F32), "norm2T": ([128, 2, 8], F32),
    "b_mod": ([2, 6 * D], F32), "w_mod": ([2, D, 6 * D], F32), "w_in": ([2, D, N_IN], F32), "w_out": ([2, D, D], F32),
    "diff_qk_gain": ([2, 2, 32], F32), "diff_lambda": ([2, 4, 32], F32), "diff_subln": ([2, 64], F32),
    "gqa_qk_gain": ([2, 2, 64], F32), "mlstm_gate_bias": ([2, 16], F32), "mlstm_norm": ([2, 256], F32),
    "gdn_convT": ([128, 2, 6, 5], F32), "gdn_a_log": ([2, 8], F32), "gdn_dt_bias": ([2, 8], F32), "gdn_norm": ([2, 64], F32),
    "ffn_w_gate": ([D, D_FF], F32), "ffn_w_up": ([D, D_FF], F32), "ffn_w_down": ([D_FF, D], F32),
    "moe_router": ([D, NE], F32), "moe_w_gate": ([NE, D, D_FFE], F32), "moe_w_up": ([NE, D, D_FFE], F32),
    "moe_w_down": ([NE, D_FFE, D], F32),
    "ropeA": ([NLAT, 2, 32], F32), "ropeB": ([NLAT, 2, 64], F32), "ident": ([128, 128], F32),
    "cmask": ([128, 2, 128], F32),
}


def host_inputs(inp, core):
    b, hh = core // 2, core % 2
    f = lambda a: np.ascontiguousarray(np.asarray(a, dtype=np.float32))
    colsT = lambda v, n: f(np.asarray(v).reshape(v.shape[0], n, 128).transpose(2, 0, 1))
    m = {}
    m["xin"] = f(np.concatenate([inp["ctx"][b], inp["x"][b]], axis=0))
    m["cc"] = f(np.stack([np.asarray(inp["c"][b]).reshape(8, 128).T, np.asarray(inp["c_ctx"]).reshape(8, 128).T], axis=2))
    fl = np.zeros((128, 2), np.float32)
    fl[:, hh] = 1.0
    m["flags"] = fl
    m["bmodT"] = colsT(inp["b_mod"], 48)
    m["norm1T"] = colsT(inp["norm1"], 8)
    m["norm2T"] = colsT(inp["norm2"], 8)
    for k in ("b_mod", "w_mod", "w_in", "w_out", "diff_qk_gain", "diff_lambda", "diff_subln", "gqa_qk_gain", "mlstm_norm",
              "gdn_norm"):
        m[k] = f(inp[k])
    m["mlstm_gate_bias"] = f(np.asarray(inp["mlstm_gate_bias"]).reshape(2, 16))
    m["gdn_a_log"] = f(np.asarray(inp["gdn_a_log"]).reshape(2, 8))
    m["gdn_dt_bias"] = f(np.asarray(inp["gdn_dt_bias"]).reshape(2, 8))
    m["gdn_convT"] = f(np.asarray(inp["gdn_conv"]).reshape(2, 5, 6, 128).transpose(3, 0, 2, 1))
    m["ffn_w_gate"] = f(inp["ffn_w_gate"][0])
    m["ffn_w_up"] = f(inp["ffn_w_up"][0])
    m["ffn_w_down"] = f(inp["ffn_w_down"][0])
    m["moe_router"] = f(inp["moe_router"][0])
    m["moe_w_gate"] = f(inp["moe_w_gate"][0])
    m["moe_w_up"] = f(inp["moe_w_up"][0])
    m["moe_w_down"] = f(inp["moe_w_down"][0])
    m["ropeA"] = rope_table(32)
    m["ropeB"] = rope_table(64)
    m["ident"] = np.eye(128, dtype=np.float32)
    i = np.arange(128)
    low = (i[:, None] >= i[None, :]).astype(np.float32)
    m["cmask"] = f(np.stack([low, low.T], axis=1))
    return m


SCRATCH = {
    "xres": ([T, D], F32),
    "qTa": ([256, T], BF16), "kTa": ([256, T], BF16), "vA": ([T, 256], BF16),
    "qTb": ([256, T], BF16), "kTb": ([128, T], BF16), "vB": ([T, 128], BF16),
    "qTc": ([256, T], F32), "kTc": ([256, T], F32), "vC": ([T, 256], F32), "oC": ([T, 256], F32), "gC": ([T, 16], F32),
    "qkvT": ([768, T], F32), "zD": ([T, 256], F32), "gD": ([T, 16], F32), "kC": ([T, 256], F32),
    "gqT": ([256, T], F32), "gkT": ([256, T], F32), "gk": ([T, 256], F32), "gv": ([T, 256], F32),
    "y": ([T, D], F32),
}


def build(debug=None, upto="all"):
    p = Prog()
    nc = p.nc
    I = {k: nc.dram_tensor(k, sh, dt, kind="ExternalInput").ap() for k, (sh, dt) in IN_SPECS.items()}
    Z = {}
    for k, (sh, dt) in SCRATCH.items():
        kind = "ExternalOutput" if (debug and k in debug) else "Internal"
        Z[k] = nc.dram_tensor("z_" + k, sh, dt, kind=kind).ap()
    out = nc.dram_tensor("out", [NLAT // 2, D], F32, kind="ExternalOutput").ap()
    with SB(p) as gsb:
        S = {"modT": gsb.t((128, 48, 2)), "gsh": gsb.t((128, 4, 8, 2)), "grow": gsb.t((128, 2, 2, D)), "ident": gsb.t((128, 128))}
        p.dma("sp", S["ident"][0][:], I["ident"][:, :], w=[S["ident"][1]])
        epst, p.d_eps = gsb.t((128, 1))
        p.epst = epst
        p.op("dve", lambda h: h.memset(epst[:], EPS), w=[p.d_eps])
        if debug and "modT" in debug:
            dbg_mod = nc.dram_tensor("z_modT", [128, 48, 2], F32, kind="ExternalOutput").ap()
            dbg_grow = nc.dram_tensor("z_grow", [128, 2, 2, D], F32, kind="ExternalOutput").ap()
        for l in range(2):
            phase_mod(p, l, I, S)
            if debug and "modT" in debug and l == 0:
                p.dma("sp", dbg_mod[:, :, :], S["modT"][0][:], r=[S["modT"][1]])
                p.dma("sp", dbg_grow[:, :, :, :], S["grow"][0][:], r=[S["grow"][1]])
            phase_inproj(p, l, I, S, Z, I["xin"] if l == 0 else Z["xres"])
            if upto == "inproj":
                break
            if "noattn" not in upto:
                phase_attn(p, l, I, S, Z, l == 0)
            if upto == "attn":
                break
            phase_chunk(p, l, I, S, Z, do_c=("noc" not in upto), do_d=("nod" not in upto), upto=upto)
            if upto.startswith("chunk"):
                break
            phase_outproj(p, l, I, S, Z, I["xin"] if l == 0 else Z["xres"])
            if l == 0:
                phase_ffn_dense(p, l, I, S, Z)
                if upto == "layer0":
                    break
            else:
                phase_moe(p, l, I, S, Z, out)
        p.barrier()
    return p


def bcast_load(p, sb, src_row_ap, n, name="bc"):
    t, d = sb.t((128, n), F32, name)
    p.dma("sp", t[:], src_row_ap.partition_broadcast(128), w=[d])
    return t, d


def phase_attn(p, l, I, S, Z, with_ctx):
    ident, d_ident = S["ident"]
    lambda_init = 0.8 - 0.6 * math.exp(-0.3 * l)
    with SB(p) as sb:
        lamv, d_lamv = sb.t((128, 4, 32))
        p.dma("sp", lamv[:], I["diff_lambda"][l:l + 1, :, :].partition_broadcast(128), w=[d_lamv])
        cst, d_cst = sb.t((128, 16))
        tmp32, d_tmp32 = sb.t((128, 2, 32))
        p.op("dve", lambda h: h.tensor_tensor(out=tmp32[:], in0=lamv[:, 0:4:2, :], in1=lamv[:, 1:4:2, :], op=ALU.mult),
             r=[d_lamv], w=[d_tmp32])
        p.op("dve", lambda h: h.tensor_reduce(out=cst[:, 0:2], in_=tmp32[:], axis=AX.X, op=ALU.add), r=[d_tmp32], w=[d_cst])
        p.op("act", lambda h: h.activation(out=cst[:, 2:4], in_=cst[:, 0:2], func=AF.Exp), r=[d_cst], w=[d_cst])
        p.op("dve", lambda h: h.tensor_tensor(out=cst[:, 4:5], in0=cst[:, 3:4], in1=cst[:, 2:3], op=ALU.subtract), r=[d_cst], w=[d_cst])
        p.op("dve", lambda h: h.tensor_scalar(out=cst[:, 4:5], in0=cst[:, 4:5], scalar1=-lambda_init, scalar2=None, op0=ALU.add),
             r=[d_cst], w=[d_cst])
        gA, d_gA = sb.t((128, 2, 32))
        gB, d_gB = sb.t((128, 2, 64))
        p.dma("sp", gA[:], I["diff_qk_gain"][l:l + 1, :, :].partition_broadcast(128), w=[d_gA])
        p.dma("sp", gB[:], I["gqa_qk_gain"][l:l + 1, :, :].partition_broadcast(128), w=[d_gB])
        p.op("dve", lambda h: h.tensor_reduce(out=cst[:, 6:8], in_=gA[:], axis=AX.X, op=ALU.max, apply_absolute_value=True),
             r=[d_gA], w=[d_cst])
        p.op("dve", lambda h: h.tensor_reduce(out=cst[:, 8:10], in_=gB[:], axis=AX.X, op=ALU.max, apply_absolute_value=True),
             r=[d_gB], w=[d_cst])
        p.op("dve", lambda h: h.scalar_tensor_tensor(out=cst[:, 10:11], in0=cst[:, 6:7], scalar=-math.sqrt(32.0), in1=cst[:, 7:8],
                                                     op0=ALU.mult, op1=ALU.mult), r=[d_cst], w=[d_cst])
        p.op("dve", lambda h: h.scalar_tensor_tensor(out=cst[:, 11:12], in0=cst[:, 8:9], scalar=-8.0, in1=cst[:, 9:10],
                                                     op0=ALU.mult, op1=ALU.mult), r=[d_cst], w=[d_cst])
        subg, d_subg = bcast_load(p, sb, I["diff_subln"][l:l + 1, :], 64)
        p.op("dve", lambda h: h.tensor_scalar(out=subg[:], in0=subg[:], scalar1=1.0 - lambda_init, scalar2=None, op0=ALU.mult),
             r=[d_subg], w=[d_subg])
        kT, d_kT = sb.t((64, T), BF16, "kT")
        va, d_va = sb.t((128, NT, 65), BF16, "vaug")
        qTs = [sb.t((64, 512), BF16, "qT") for _ in range(2)]
        pTs = [sb.t((128, 512), BF16, "pT") for _ in range(3)]
        ps_s = [sb.ps() for _ in range(3)]
        ps_o = [sb.ps() for _ in range(2)]
        ps_t, d_ps_t = sb.ps()
        osb = [sb.t((65, 512), F32, "osb") for _ in range(2)]
        on = [sb.t((128, 64), F32, "on") for _ in range(2)]
        od, d_od = sb.t((128, 64))
        junk, d_junk = sb.t((128, 64))
        st, d_st = sb.t((128, 4))
        ystage, d_ys = sb.t((128, 4, 64))
        rc, d_rc = sb.t((128, 2))
        qblocks = ([(0, 256, [0, 1])] if with_ctx else []) + [(256 + 512 * i, 512, list(range(NT))) for i in range(8)]
        cnt = {"s": 0, "q": 0}

        def run_head(kind, qsrc_rows, nmaps, scale, negB, ycol):
            for (q0, qn, kts) in qblocks:
                qT, d_qT = qTs[cnt["q"] % 2]
                cnt["q"] += 1
                p.dma("sp", qT[:, 0:qn], qsrc_rows[:, q0:q0 + qn], w=[d_qT])
                for j in range(nmaps):
                    kr = slice(32 * j, 32 * j + 32) if kind == "A" else slice(0, 64)
                    po, d_po = ps_o[j]
                    for ki, kt in enumerate(kts):
                        i = cnt["s"]
                        cnt["s"] += 1
                        ps, d_ps = ps_s[i % 3]
                        pT, d_pT = pTs[i % 3]
                        p.op("pe", lambda h, ps=ps, kt=kt, kr=kr, qT=qT: h.matmul(ps[:, 0:qn], lhsT=kT[kr, kt * 128:(kt + 1) * 128],
                                                                                 rhs=qT[kr, 0:qn], start=True, stop=True),
                             r=[d_kT, d_qT], w=[d_ps])
                        p.op("act", lambda h, ps=ps, pT=pT: h.activation(out=pT[:, 0:qn], in_=ps[:, 0:qn], func=AF.Exp, scale=scale,
                                                                          bias=negB), r=[d_ps, d_cst], w=[d_pT])
                        p.op("pe", lambda h, po=po, pT=pT, kt=kt, ki=ki: h.matmul(po[0:65, 0:qn], lhsT=va[:, kt, :], rhs=pT[:, 0:qn],
                                                                                 start=(ki == 0), stop=(ki == len(kts) - 1)),
                             r=[d_va, d_pT], w=[d_po])
                    ob, d_ob = osb[j]
                    p.op("act", lambda h, ob=ob, po=po: h.activation(out=ob[:, 0:qn], in_=po[0:65, 0:qn], func=AF.Copy), r=[d_po], w=[d_ob])
                nsub = qn // 128
                for s_ in range(nsub):
                    for j in range(nmaps):
                        ob, d_ob = osb[j]
                        p.op("pe", lambda h, ob=ob, j=j, s_=s_: h.transpose(ps_t[:, j * 128:j * 128 + 65], ob[0:65, s_ * 128:(s_ + 1) * 128],
                                                                            ident[0:65, 0:65]), r=[d_ob, d_ident], w=[d_ps_t])
                    for j in range(nmaps):
                        o_, d_o = on[j]
                        p.op("dve", lambda h, j=j: h.reciprocal(out=rc[:, j:j + 1], in_=ps_t[:, j * 128 + 64:j * 128 + 65]),
                             r=[d_ps_t], w=[d_rc])
                        p.op("dve", lambda h, o_=o_, j=j: h.tensor_scalar(out=o_[:], in0=ps_t[:, j * 128:j * 128 + 64],
                                                                          scalar1=rc[:, j:j + 1], scalar2=None,
                                                                          op0=ALU.mult), r=[d_ps_t, d_rc], w=[d_o])
                    if kind == "A":
                        p.op("dve", lambda h: h.scalar_tensor_tensor(out=od[:], in0=on[1][0][:], scalar=cst[:, 4:5], in1=on[0][0][:],
                                                                     op0=ALU.mult, op1=ALU.add), r=[on[0][1], on[1][1], d_cst], w=[d_od])
                        p.op("act", lambda h: h.activation(out=junk[:], in_=od[:], func=AF.Square, accum_out=st[:, 0:1]),
                             r=[d_od], w=[d_junk, d_st])
                        rstd(p, st[:, 2:3], st[:, 0:1], st[:, 1:2], 1.0 / 64, [d_st])
                        p.op("dve", lambda h, s_=s_: h.scalar_tensor_tensor(out=ystage[:, s_, :], in0=od[:], scalar=st[:, 2:3], in1=subg[:],
                                                                            op0=ALU.mult, op1=ALU.mult), r=[d_od, d_st, d_subg], w=[d_ys])
                    else:
                        p.op("dve", lambda h, s_=s_: h.tensor_copy(out=ystage[:, s_, :], in_=on[0][0][:]), r=[on[0][1]], w=[d_ys])
                p.dma("sp", Z["y"][q0:q0 + qn, ycol:ycol + 64].rearrange("(s q) d -> q s d", q=128), ystage[:, 0:nsub, :], r=[d_ys])

        def load_kv(ksrc_rows, vsrc_cols):
            p.dma("sp", kT[:, :], ksrc_rows, w=[d_kT])
            p.dma("sp", va[:, :, 0:64], vsrc_cols.rearrange("(n q) d -> q n d", q=128), w=[d_va])
            p.op("dve", lambda h: h.memset(va[:, :, 64:65], 1.0), w=[d_va])

        for hd in range(4):
            load_kv(Z["kTa"][64 * hd:64 * hd + 64, :], Z["vA"][:, 64 * hd:64 * hd + 64])
            run_head("A", Z["qTa"][64 * hd:64 * hd + 64, :], 2, 1.0 / math.sqrt(32.0), cst[:, 10:11], 64 * hd)
        for kv in range(2):
            load_kv(Z["kTb"][64 * kv:64 * kv + 64, :], Z["vB"][:, 64 * kv:64 * kv + 64])
            for g in (2 * kv, 2 * kv + 1):
                run_head("B", Z["qTb"][64 * g:64 * g + 64, :], 1, 0.125, cst[:, 11:12], 256 + 64 * g)


ORDER = [list(range(NT)), [1, 0] + list(range(NT - 1, 1, -1))]
NGC = 40


class PsumPool:
    def __init__(self, sb, nbanks=8):
        self.q = []
        banks = [sb.ps() for b in range(nbanks)]
        for k in range(4):
            for (t, d) in banks:
                self.q.append((t[:, k * 128:(k + 1) * 128], d))
        self.i = 0

    def get(self):
        r = self.q[self.i % len(self.q)]
        self.i += 1
        return r


def phase_gdn_prep(p, l, I, S, Z):
    ident, d_ident = S["ident"]
    W = 2 + 256 + 4 + 4096 + 2
    with SB(p) as sb:
        cw, d_cw = sb.t((128, 6, 5))
        p.dma("sp", cw[:], I["gdn_convT"][:, l, :, :], w=[d_cw])
        bones, d_bones = sb.t((128, 128))
        p.op("dve", lambda h: h.memset(bones[:], 0.0), w=[d_bones])
        p.op("dve", lambda h: h.memset(bones[0:64, 0:64], 1.0), w=[d_bones])
        p.op("dve", lambda h: h.memset(bones[64:128, 64:128], 1.0), w=[d_bones])
        X, d_X = sb.t((128, W))
        acc, d_acc = sb.t((128, W))
        sq, d_sq = sb.t((128, 512))
        rs, d_rs = sb.t((128, 512))
        pss = [sb.ps() for _ in range(2)]
        pst = [sb.ps() for _ in range(2)]
        tst = [sb.t((128, 128)) for _ in range(2)]
        p.op("dve", lambda h: h.memset(X[:], 0.0), w=[d_X])
        nb = 0
        for fc in range(6):
            p.dma("sp", X[:, 2:258], Z["qkvT"][fc * 128:(fc + 1) * 128, 0:256], w=[d_X])
            p.dma("sp", X[:, 262:4358], Z["qkvT"][fc * 128:(fc + 1) * 128, 256:T], w=[d_X])
            lo, hi = 2, 4358
            p.op("dve", lambda h, fc=fc: h.tensor_scalar(out=acc[:, lo:hi], in0=X[:, lo - 2:hi - 2], scalar1=cw[:, fc, 0:1], scalar2=None,
                                                         op0=ALU.mult), r=[d_X, d_cw], w=[d_acc])
            for tap in range(1, 5):
                eng = "dve"
                p.op(eng, lambda h, fc=fc, tap=tap: h.scalar_tensor_tensor(out=acc[:, lo:hi], in0=X[:, lo + tap - 2:hi + tap - 2],
                                                                             scalar=cw[:, fc, tap:tap + 1], in1=acc[:, lo:hi],
                                                                             op0=ALU.mult, op1=ALU.add), r=[d_X, d_cw, d_acc], w=[d_acc])
            p.op("act", lambda h: h.activation(out=X[:, lo:hi], in_=acc[:, lo:hi], func=AF.Sigmoid), r=[d_acc], w=[d_X])
            p.op("dve", lambda h: h.tensor_tensor(out=acc[:, lo:hi], in0=acc[:, lo:hi], in1=X[:, lo:hi], op=ALU.mult), r=[d_X, d_acc], w=[d_acc])
            segs = [(2, 256, 0)] + [(262 + 512 * i, 512, 256 + 512 * i) for i in range(8)]
            if fc < 4:
                for (c0, n, t0) in segs:
                    ps, d_ps = pss[nb % 2]
                    nb += 1
                    p.op("act", lambda h: h.activation(out=sq[:, 0:n], in_=acc[:, c0:c0 + n], func=AF.Square), r=[d_acc], w=[d_sq])
                    p.op("pe", lambda h, ps=ps: h.matmul(ps[:, 0:n], lhsT=bones[:], rhs=sq[:, 0:n], start=True, stop=True),
                         r=[d_bones, d_sq], w=[d_ps])
                    p.op("act", lambda h, ps=ps: h.activation(out=rs[:, 0:n], in_=ps[:, 0:n], func=AF.Sqrt, scale=1.0, bias=p.eps_ap(EPS, 128)),
                         r=[d_ps, p.d_eps], w=[d_rs])
                    p.op("dve", lambda h: h.reciprocal(out=rs[:, 0:n], in_=rs[:, 0:n]), r=[d_rs], w=[d_rs])
                    p.op("dve", lambda h: h.scalar_tensor_tensor(out=acc[:, c0:c0 + n], in0=acc[:, c0:c0 + n], scalar=(0.125 if fc < 2 else 1.0),
                                                                 in1=rs[:, 0:n], op0=ALU.mult, op1=ALU.mult), r=[d_acc, d_rs], w=[d_acc])
                dst = Z["gqT"] if fc < 2 else Z["gkT"]
                r0 = (fc % 2) * 128
                p.dma("sp", dst[r0:r0 + 128, 0:256], acc[:, 2:258], r=[d_acc])
                p.dma("sp", dst[r0:r0 + 128, 256:T], acc[:, 262:4358], r=[d_acc])
            if fc >= 2:
                dst = Z["gk"] if fc < 4 else Z["gv"]
                r0 = (fc % 2) * 128
                for tt in range(NT):
                    c0 = 2 + tt * 128 if tt < 2 else 262 + (tt - 2) * 128
                    ps, d_ps = pst[tt % 2]
                    ts_, d_ts = tst[tt % 2]
                    p.op("pe", lambda h, ps=ps, c0=c0: h.transpose(ps[:, 0:128], acc[:, c0:c0 + 128], ident[:]), r=[d_acc, d_ident], w=[d_ps])
                    p.op("act", lambda h, ps=ps, ts_=ts_: h.activation(out=ts_[:], in_=ps[:, 0:128], func=AF.Copy), r=[d_ps], w=[d_ts])
                    p.dma("sp", dst[tt * 128:(tt + 1) * 128, r0:r0 + 128], ts_[:], r=[d_ts])


def softplus_parts(p, z, d_z, tmp, d_tmp, n):
    p.op("dve", lambda h: h.tensor_scalar(out=tmp[:, n:2 * n], in0=z, scalar1=-1.0, scalar2=None, op0=ALU.mult), r=[d_z], w=[d_tmp])
    p.op("dve", lambda h: h.tensor_tensor(out=tmp[:, n:2 * n], in0=tmp[:, n:2 * n], in1=z, op=ALU.min), r=[d_z, d_tmp], w=[d_tmp])
    p.op("act", lambda h: h.activation(out=tmp[:, n:2 * n], in_=tmp[:, n:2 * n], func=AF.Exp), r=[d_tmp], w=[d_tmp])
    p.op("act", lambda h: h.activation(out=tmp[:, 0:n], in_=tmp[:, n:2 * n], func=AF.Ln, scale=1.0, bias=p.ones1[:, 0:1]),
         r=[d_tmp, p.d_ones1], w=[d_tmp])


def phase_gates(p, l, I, S, Z, tab, d_tab, cm, d_cm, ones, d_ones):
    ident, d_ident = S["ident"]
    with SB(p) as sb:
        pp = PsumPool(sb, 4)
        biasC, d_biasC = bcast_load(p, sb, I["mlstm_gate_bias"][l:l + 1, :], 16)
        dtb, d_dtb = bcast_load(p, sb, I["gdn_dt_bias"][l:l + 1, :], 8)
        nea, d_nea = bcast_load(p, sb, I["gdn_a_log"][l:l + 1, :], 8)
        p.op("act", lambda h: h.activation(out=nea[:], in_=nea[:], func=AF.Exp), r=[d_nea], w=[d_nea])
        p.op("dve", lambda h: h.tensor_scalar(out=nea[:], in0=nea[:], scalar1=-1.0, scalar2=None, op0=ALU.mult), r=[d_nea], w=[d_nea])
        gts = [sb.t((128, 32)) for _ in range(2)]
        for d in range(2):
            Bprev, d_B = sb.t((128, 4))
            R, d_R = sb.t((128, 4))
            p.op("dve", lambda h: h.memset(Bprev[:], 0.0), w=[d_B])
            p.op("dve", lambda h: h.memset(R[:], 0.0), w=[d_R])
            for s_, tt in enumerate(ORDER[d]):
                g, d_g = gts[s_ % 2]
                p.dma("sp", g[:, 0:16], Z["gC"][tt * 128:(tt + 1) * 128, :], w=[d_g])
                p.dma("sp", g[:, 16:32], Z["gD"][tt * 128:(tt + 1) * 128, :], w=[d_g])
                wk, d_wk = S["gwk"][s_ % 2]
                lhs_cum = cm[:, 1 - d, :]
                T_ = lambda a, b: tab[:, tt, d, a:b]
                xf = wk[:, 0:4]
                ig = wk[:, 4:8]
                p.op("dve", lambda h: h.tensor_tensor(out=xf, in0=g[:, 8 * d + 4:8 * d + 8], in1=biasC[:, 8 * d + 4:8 * d + 8], op=ALU.add),
                     r=[d_g, d_biasC], w=[d_wk])
                p.op("dve", lambda h: h.tensor_tensor(out=ig, in0=g[:, 8 * d:8 * d + 4], in1=biasC[:, 8 * d:8 * d + 4], op=ALU.add),
                     r=[d_g, d_biasC], w=[d_wk])
                softplus_parts(p, xf, d_wk, wk[:, 8:16], d_wk, 4)
                logf = wk[:, 16:20]
                p.op("dve", lambda h: h.scalar_tensor_tensor(out=logf, in0=xf, scalar=0.0, in1=wk[:, 8:12], op0=ALU.min, op1=ALU.subtract),
                     r=[d_wk], w=[d_wk])
                pcs, d_pcs = pp.get()
                ptot, d_ptot = pp.get()
                p.op("pe", lambda h: h.matmul(pcs[:, 0:4], lhsT=lhs_cum, rhs=logf, start=True, stop=True), r=[d_cm, d_wk], w=[d_pcs])
                p.op("pe", lambda h: h.matmul(ptot[:, 0:4], lhsT=ones[:], rhs=logf, start=True, stop=True), r=[d_ones, d_wk], w=[d_ptot])
                Bv = wk[:, 20:24]
                av = wk[:, 24:28]
                p.op("dve", lambda h: h.tensor_tensor(out=Bv, in0=pcs[:, 0:4], in1=Bprev[:], op=ALU.add), r=[d_pcs, d_B], w=[d_wk])
                p.op("dve", lambda h: h.tensor_tensor(out=av, in0=ig, in1=Bv, op=ALU.subtract), r=[d_wk], w=[d_wk])
                p.op("dve", lambda h: h.tensor_tensor(out=Bprev[:], in0=ptot[:, 0:4], in1=Bprev[:], op=ALU.add), r=[d_ptot, d_B], w=[d_B])
                ptr, d_ptr = pp.get()
                p.op("pe", lambda h: h.transpose(ptr[0:4, 0:128], av, ident[:]), r=[d_wk, d_ident], w=[d_ptr])
                am, d_am = S["gam4"]
                p.op("dve", lambda h: h.tensor_reduce(out=am[0:4, 0:1], in_=ptr[0:4, 0:128], axis=AX.X, op=ALU.max), r=[d_ptr], w=[d_am])
                p.op("dve", lambda h: h.tensor_scalar(out=am[0:4, 4:8], in0=ident[0:4, 0:4], scalar1=am[0:4, 0:1], scalar2=None, op0=ALU.mult),
                     r=[d_am, d_ident], w=[d_am])
                pam, d_pam = pp.get()
                p.op("pe", lambda h: h.matmul(pam[:, 0:4], lhsT=ones[0:4, :], rhs=am[0:4, 4:8], start=True, stop=True),
                     r=[d_ones, d_am], w=[d_pam])
                Mc = wk[:, 28:32]
                p.op("dve", lambda h: h.tensor_tensor(out=Mc, in0=pam[:, 0:4], in1=R[:], op=ALU.max), r=[d_pam, d_R], w=[d_wk])
                p.op("dve", lambda h: h.tensor_tensor(out=wk[:, 32:36], in0=R[:], in1=Mc, op=ALU.subtract), r=[d_R, d_wk], w=[d_wk])
                p.op("dve", lambda h: h.tensor_tensor(out=wk[:, 36:40], in0=av, in1=Mc, op=ALU.subtract), r=[d_wk], w=[d_wk])
                p.op("dve", lambda h: h.tensor_tensor(out=wk[:, 40:44], in0=Bv, in1=Mc, op=ALU.add), r=[d_wk], w=[d_wk])
                p.op("dve", lambda h: h.tensor_copy(out=R[:], in_=Mc), r=[d_wk], w=[d_R])
                p.op("act", lambda h: h.activation(out=T_(8, 12), in_=wk[:, 32:36], func=AF.Exp), r=[d_wk], w=[d_tab])
                p.op("act", lambda h: h.activation(out=T_(0, 4), in_=wk[:, 36:40], func=AF.Exp), r=[d_wk], w=[d_tab])
                p.op("act", lambda h: h.activation(out=T_(4, 8), in_=wk[:, 40:44], func=AF.Exp, scale=-1.0), r=[d_wk], w=[d_tab])
                z = wk[:, 44:48]
                p.op("dve", lambda h: h.tensor_tensor(out=z, in0=g[:, 16 + 8 * d + 4:16 + 8 * d + 8], in1=dtb[:, 4 * d:4 * d + 4], op=ALU.add),
                     r=[d_g, d_dtb], w=[d_wk])
                softplus_parts(p, z, d_wk, wk[:, 48:56], d_wk, 4)
                gg = wk[:, 56:60]
                p.op("dve", lambda h: h.scalar_tensor_tensor(out=gg, in0=z, scalar=0.0, in1=wk[:, 48:52], op0=ALU.max, op1=ALU.add),
                     r=[d_wk], w=[d_wk])
                p.op("dve", lambda h: h.tensor_tensor(out=gg, in0=gg, in1=nea[:, 4 * d:4 * d + 4], op=ALU.mult), r=[d_wk, d_nea], w=[d_wk])
                p.op("act", lambda h: h.activation(out=T_(12, 16), in_=g[:, 16 + 8 * d:16 + 8 * d + 4], func=AF.Sigmoid), r=[d_g], w=[d_tab])
                p.op("dve", lambda h: h.tensor_scalar(out=T_(16, 20), in0=T_(12, 16), scalar1=-1.0, scalar2=None, op0=ALU.mult),
                     r=[d_tab], w=[d_tab])
                pgm, d_pgm = pp.get()
                pgl, d_pgl = pp.get()
                p.op("pe", lambda h: h.matmul(pgm[:, 0:4], lhsT=lhs_cum, rhs=gg, start=True, stop=True), r=[d_cm, d_wk], w=[d_pgm])
                p.op("pe", lambda h: h.matmul(pgl[:, 0:4], lhsT=ones[:], rhs=gg, start=True, stop=True), r=[d_ones, d_wk], w=[d_pgl])
                p.op("dve", lambda h: h.tensor_copy(out=T_(20, 24), in_=pgm[:, 0:4]), r=[d_pgm], w=[d_tab])
                p.op("dve", lambda h: h.tensor_tensor(out=wk[:, 60:64], in0=pgl[:, 0:4], in1=T_(20, 24), op=ALU.subtract),
                     r=[d_pgl, d_tab], w=[d_wk])
                p.op("act", lambda h: h.activation(out=T_(24, 28), in_=T_(20, 24), func=AF.Exp), r=[d_tab], w=[d_tab])
                p.op("act", lambda h: h.activation(out=T_(28, 32), in_=wk[:, 60:64], func=AF.Exp), r=[d_wk], w=[d_tab])
                p.op("act", lambda h: h.activation(out=T_(32, 36), in_=pgl[:, 0:4], func=AF.Exp), r=[d_pgl], w=[d_tab])
                p.op("dve", lambda h: h.tensor_tensor(out=T_(36, 40), in0=T_(12, 16), in1=T_(24, 28), op=ALU.mult), r=[d_tab], w=[d_tab])


def phase_chunk(p, l, I, S, Z, do_c=True, do_d=True, upto=""):
    ident, d_ident = S["ident"]
    phase_gdn_prep(p, l, I, S, Z)
    if "stopprep" in upto:
        return
    with SB(p) as sb:
        tab, d_tab = sb.t((128, NT, 2, NGC), F32, "gtab")
        cm, d_cm = sb.t((128, 2, 128), F32, "cmask")
        p.dma("sp", cm[:], I["cmask"][:, :, :], w=[d_cm])
        ones, d_ones = sb.t((128, 128))
        p.op("dve", lambda h: h.memset(ones[:], 1.0), w=[d_ones])
        ones1, p.d_ones1 = sb.t((128, 1))
        p.ones1 = ones1
        p.op("dve", lambda h: h.memset(ones1[:], 1.0), w=[p.d_ones1])
        S["gwk"] = [sb.t((128, 64)) for _ in range(2)]
        S["gam4"] = sb.t((8, 8))
        strict, d_strict = sb.t((128, 2, 128))
        for d in range(2):
            p.op("dve", lambda h, d=d: h.tensor_tensor(out=strict[:, d, :], in0=cm[:, d, :], in1=ident[:], op=ALU.subtract),
                 r=[d_cm, d_ident], w=[d_strict])
        phase_gates(p, l, I, S, Z, tab, d_tab, cm, d_cm, ones, d_ones)
        if "stopgates" in upto:
            return
        acc, d_acc = sb.t((128, NT, 256), F32, "hacc")
        if do_c:
            p.op("dve", lambda h: h.memset(acc[:], 0.0), w=[d_acc])
            chunk_c(p, l, I, S, Z, sb, tab, d_tab, cm, d_cm, acc, d_acc)
            p.barrier()
        if do_d:
            p.op("dve", lambda h: h.memset(acc[:], 0.0), w=[d_acc])
            chunk_d(p, l, I, S, Z, sb, tab, d_tab, cm, d_cm, strict, d_strict, ones, d_ones, acc, d_acc)


def chunk_c(p, l, I, S, Z, sbo, tab, d_tab, cm, d_cm, acc, d_acc):
    ident, d_ident = S["ident"]
    with SB(p) as sb:
        pp = PsumPool(sb, 6)
        qT, d_qT = sb.t((64, T), F32, "cqT")
        kT, d_kT = sb.t((64, T), F32, "ckT")
        ktm, d_ktm = sb.t((128, NT, 64), F32, "cktm")
        vtm, d_vtm = sb.t((128, NT, 66), F32, "cvtm")
        Cs = [sb.t((64, 66), F32, "cst") for _ in range(2)]
        Cd = [sb.t((64, 66), F32, "cdec") for _ in range(2)]
        PTs = [sb.t((128, 128), F32, "cPT") for _ in range(3)]
        vps = [sb.t((128, 66), F32, "cvp") for _ in range(3)]
        rcs = [sb.t((128, 2), F32, "crc") for _ in range(3)]
        for hd in range(4):
            p.dma("sp", qT[:, :], Z["qTc"][64 * hd:64 * hd + 64, :], w=[d_qT])
            p.dma("sp", kT[:, :], Z["kTc"][64 * hd:64 * hd + 64, :], w=[d_kT])
            p.dma("sp", ktm[:, :, :], Z["kC"][:, 64 * hd:64 * hd + 64].rearrange("(n q) d -> q n d", q=128), w=[d_ktm])
            p.dma("sp", vtm[:, :, 0:64], Z["vC"][:, 64 * hd:64 * hd + 64].rearrange("(n q) d -> q n d", q=128), w=[d_vtm])
            p.op("dve", lambda h: h.memset(vtm[:, :, 64:65], 1.0), w=[d_vtm])
            p.op("dve", lambda h: h.memset(vtm[:, :, 65:66], 0.0), w=[d_vtm])
            p.op("pool", lambda h: h.tensor_scalar(out=ktm[:, :, :], in0=ktm[:, :, :], scalar1=0.125, scalar2=None, op0=ALU.mult),
                 r=[d_ktm], w=[d_ktm])
            for d in range(2):
                p.op("dve", lambda h, d=d: h.memset(Cs[d][0][:], 0.0), w=[Cs[d][1]])
            for s_ in range(NT):
                for d in range(2):
                    tt = ORDER[d][s_]
                    tk = slice(tt * 128, (tt + 1) * 128)
                    Cst, d_Cst = Cs[d]
                    Cdc, d_Cdc = Cd[d]
                    i3 = (2 * s_ + d) % 3
                    PT, d_PT = PTs[i3]
                    vp, d_vp = vps[i3]
                    rc, d_rc = rcs[i3]
                    pst, d_pst = pp.get()
                    p.op("pe", lambda h, pst=pst, tk=tk: h.matmul(pst[:, 0:128], lhsT=kT[:, tk], rhs=qT[:, tk], start=True, stop=True),
                         r=[d_kT, d_qT], w=[d_pst])
                    p.op("dve", lambda h, pst=pst, PT=PT, d=d: h.scalar_tensor_tensor(out=PT[:], in0=pst[:, 0:128], scalar=0.125,
                                                                                   in1=cm[:, 1 - d, :], op0=ALU.mult, op1=ALU.mult),
                         r=[d_pst, d_cm], w=[d_PT])
                    p.op("pool", lambda h, vp=vp, tt=tt, d=d, hd=hd: h.tensor_scalar(out=vp[:], in0=vtm[:, tt, :],
                                                                                  scalar1=tab[:, tt, d, hd:hd + 1], scalar2=None,
                                                                                  op0=ALU.mult), r=[d_vtm, d_tab], w=[d_vp])
                    p.op("dve", lambda h, Cdc=Cdc, Cst=Cst, tt=tt, d=d, hd=hd: h.tensor_scalar(out=Cdc[:], in0=Cst[:],
                                                                                            scalar1=tab[0:64, tt, d, 8 + hd:9 + hd],
                                                                                            scalar2=None, op0=ALU.mult),
                         r=[d_Cst, d_tab], w=[d_Cdc])
                    po, d_po = pp.get()
                    p.op("pe", lambda h, po=po, PT=PT, vp=vp: h.matmul(po[:, 0:66], lhsT=PT[:], rhs=vp[:], start=True, stop=False),
                         r=[d_PT, d_vp], w=[d_po])
                    p.op("pe", lambda h, po=po, Cdc=Cdc, tk=tk: h.matmul(po[:, 0:66], lhsT=qT[:, tk], rhs=Cdc[:], start=False, stop=True),
                         r=[d_qT, d_Cdc], w=[d_po])
                    pc, d_pc = pp.get()
                    p.op("pe", lambda h, pc=pc, tt=tt, vp=vp: h.matmul(pc[0:64, 0:66], lhsT=ktm[:, tt, :], rhs=vp[:], start=True, stop=True),
                         r=[d_ktm, d_vp], w=[d_pc])
                    p.op("dve", lambda h, pc=pc, Cst=Cst, Cdc=Cdc: h.tensor_tensor(out=Cst[:], in0=pc[0:64, 0:66], in1=Cdc[:], op=ALU.add),
                         r=[d_pc, d_Cdc], w=[d_Cst])
                    p.op("act", lambda h, po=po, rc=rc: h.activation(out=rc[:, 0:1], in_=po[:, 64:65], func=AF.Abs), r=[d_po], w=[d_rc])
                    p.op("dve", lambda h, rc=rc, tt=tt, d=d, hd=hd: h.tensor_tensor(out=rc[:, 0:1], in0=rc[:, 0:1],
                                                                                 in1=tab[:, tt, d, 4 + hd:5 + hd], op=ALU.max),
                         r=[d_rc, d_tab], w=[d_rc])
                    p.op("dve", lambda h, rc=rc: h.reciprocal(out=rc[:, 1:2], in_=rc[:, 0:1]), r=[d_rc], w=[d_rc])
                    dst = acc[:, tt, 64 * hd:64 * hd + 64]
                    p.op("dve", lambda h, po=po, rc=rc, dst=dst: h.scalar_tensor_tensor(out=dst, in0=po[:, 0:64], scalar=rc[:, 1:2], in1=dst,
                                                                                        op0=ALU.mult, op1=ALU.add),
                         r=[d_po, d_rc, d_acc], w=[d_acc])
        gain, d_gain = bcast_load(p, sb, I["mlstm_norm"][l:l + 1, :], 256)
        ots = [sb.t((128, 256)) for _ in range(2)]
        xcs = [sb.t((128, 256)) for _ in range(2)]
        sqs = [sb.t((128, 256)) for _ in range(2)]
        sts = [sb.t((128, 12)) for _ in range(2)]
        for tt in range(NT):
            ot, d_ot = ots[tt % 2]
            xc, d_xc = xcs[tt % 2]
            sq, d_sq = sqs[tt % 2]
            st, d_st = sts[tt % 2]
            p.dma("sp", ot[:], Z["oC"][tt * 128:(tt + 1) * 128, :], w=[d_ot])
            p.op("act", lambda h, ot=ot: h.activation(out=ot[:], in_=ot[:], func=AF.Sigmoid), r=[d_ot], w=[d_ot])
            hv = acc[:, tt, :].rearrange("q (g e) -> q g e", e=64)
            v3 = lambda a: a[:, :].rearrange("q (g e) -> q g e", e=64)
            p.op("dve", lambda h, st=st, hv=hv: h.tensor_reduce(out=st[:, 0:4], in_=hv, axis=AX.X, op=ALU.add), r=[d_acc], w=[d_st])
            p.op("dve", lambda h, st=st: h.tensor_scalar(out=st[:, 0:4], in0=st[:, 0:4], scalar1=1.0 / 64, scalar2=None, op0=ALU.mult),
                 r=[d_st], w=[d_st])
            p.op("dve", lambda h, st=st, hv=hv, xc=xc: h.tensor_tensor(out=v3(xc), in0=hv, in1=st[:, 0:4].unsqueeze(2).to_broadcast([128, 4, 64]),
                                                                       op=ALU.subtract), r=[d_acc, d_st], w=[d_xc])
            p.op("act", lambda h, sq=sq, xc=xc: h.activation(out=sq[:], in_=xc[:], func=AF.Square), r=[d_xc], w=[d_sq])
            p.op("dve", lambda h, st=st, sq=sq: h.tensor_reduce(out=st[:, 4:8], in_=v3(sq), axis=AX.X, op=ALU.add), r=[d_sq], w=[d_st])
            rstd(p, st[:, 8:12], st[:, 4:8], st[:, 4:8], 1.0 / 64, [d_st])
            p.op("dve", lambda h, st=st, xc=xc: h.tensor_tensor(out=v3(xc), in0=v3(xc), in1=st[:, 8:12].unsqueeze(2).to_broadcast([128, 4, 64]),
                                                                op=ALU.mult), r=[d_st, d_xc], w=[d_xc])
            p.op("dve", lambda h, xc=xc: h.tensor_tensor(out=xc[:], in0=xc[:], in1=gain[:], op=ALU.mult), r=[d_gain, d_xc], w=[d_xc])
            p.op("dve", lambda h, xc=xc, ot=ot: h.tensor_tensor(out=xc[:], in0=xc[:], in1=ot[:], op=ALU.mult), r=[d_ot, d_xc], w=[d_xc])
            p.dma("sp", Z["y"][tt * 128:(tt + 1) * 128, 512:768], xc[:], r=[d_xc])


def chunk_d(p, l, I, S, Z, sbo, tab, d_tab, cm, d_cm, strict, d_strict, ones, d_ones, acc, d_acc):
    ident, d_ident = S["ident"]
    with SB(p) as sb:
        pp = PsumPool(sb, 7)
        zero1, d_zero1 = sb.t((128, 1))
        p.op("dve", lambda h: h.memset(zero1[:], 0.0), w=[d_zero1])
        qT, d_qT = sb.t((64, T), F32, "dqT")
        kT, d_kT = sb.t((64, T), F32, "dkT")
        kT2, d_kT2 = sb.t((64, T), F32, "dkT2")
        ktm, d_ktm = sb.t((128, NT, 64), F32, "dktm")
        vtm, d_vtm = sb.t((128, NT, 64), F32, "dvtm")
        Ss = [sb.t((64, 64), F32, "dS") for _ in range(2)]
        NB = 2
        mk = lambda shape, nm: [sb.t(shape, F32, nm) for _ in range(NB)]
        diag_, E1_, E2_, decT_, decS_ = mk((128, 128), "diag"), mk((128, 128), "E1"), mk((128, 128), "E2"), mk((128, 128), "decT"), mk((128, 128), "decS")
        Pb = [mk((128, 128), "P") for _ in range(2)]
        Qb = [mk((128, 128), "Q") for _ in range(2)]
        TTb = [mk((128, 128), "TT") for _ in range(2)]
        Ru_, Rw_, u_, kdec_, vnew_, attnT_ = mk((128, 64), "Ru"), mk((128, 64), "Rw"), mk((128, 64), "u"), mk((128, 64), "kdec"), mk((128, 64), "vnew"), mk((128, 128), "attnT")
        wT_ = mk((64, 128), "wT")
        import os
        STG = float(os.environ.get("GDN_STAGE", "9"))
        for hd in range(int(os.environ.get("GDN_NH", "4"))):
            p.dma("sp", qT[:, :], Z["gqT"][64 * hd:64 * hd + 64, :], w=[d_qT])
            p.dma("sp", kT[:, :], Z["gkT"][64 * hd:64 * hd + 64, :], w=[d_kT])
            p.dma("sp", kT2[:, :], Z["gkT"][64 * hd:64 * hd + 64, :], w=[d_kT2])
            p.dma("sp", ktm[:, :, :], Z["gk"][:, 64 * hd:64 * hd + 64].rearrange("(n q) d -> q n d", q=128), w=[d_ktm])
            p.dma("sp", vtm[:, :, :], Z["gv"][:, 64 * hd:64 * hd + 64].rearrange("(n q) d -> q n d", q=128), w=[d_vtm])
            for d in range(2):
                p.op("dve", lambda h, d=d: h.memset(Ss[d][0][:], 0.0), w=[Ss[d][1]])
            for s_ in range(int(os.environ.get("GDN_NS", str(NT)))):
                for d in range(2):
                    tt = ORDER[d][s_]
                    tk = slice(tt * 128, (tt + 1) * 128)
                    b = d
                    col = lambda g: tab[:, tt, d, 4 * g + hd:4 * g + hd + 1]
                    beta, negbeta, gam, egam, ekd, egl, bg = col(3), col(4), col(5), col(6), col(7), col(8), col(9)
                    Sst, d_S = Ss[d]
                    diag, d_diag = diag_[b]
                    E1, d_E1 = E1_[b]
                    E2, d_E2 = E2_[b]
                    decT, d_decT = decT_[b]
                    decS, d_decS = decS_[b]
                    p.op("dve", lambda h, diag=diag, gam=gam: h.tensor_scalar(out=diag[:], in0=ident[:], scalar1=gam, scalar2=None, op0=ALU.mult),
                         r=[d_ident, d_tab], w=[d_diag])
                    pg, d_pg = pp.get()
                    p.op("pe", lambda h, pg=pg, diag=diag: h.matmul(pg, lhsT=ones[:], rhs=diag[:], start=True, stop=True),
                         r=[d_ones, d_diag], w=[d_pg])
                    p.op("dve", lambda h, pg=pg, E1=E1, gam=gam: h.tensor_scalar(out=E1[:], in0=pg, scalar1=gam, scalar2=zero1[:, 0:1], op0=ALU.subtract,
                                                                                 op1=ALU.min), r=[d_pg, d_tab, d_zero1], w=[d_E1])
                    p.op("dve", lambda h, pg=pg, E2=E2, gam=gam: h.tensor_scalar(out=E2[:], in0=pg, scalar1=gam, scalar2=zero1[:, 0:1], op0=ALU.subtract,
                                                                                 op1=ALU.max), r=[d_pg, d_tab, d_zero1], w=[d_E2])
                    p.op("act", lambda h, E1=E1: h.activation(out=E1[:], in_=E1[:], func=AF.Exp), r=[d_E1], w=[d_E1])
                    p.op("act", lambda h, E2=E2: h.activation(out=E2[:], in_=E2[:], func=AF.Exp, scale=-1.0), r=[d_E2], w=[d_E2])
                    p.op("dve", lambda h, E1=E1, decT=decT, d=d: h.tensor_tensor(out=decT[:], in0=E1[:], in1=cm[:, 1 - d, :], op=ALU.mult),
                         r=[d_E1, d_cm], w=[d_decT])
                    p.op("dve", lambda h, E2=E2, decS=decS, d=d: h.tensor_tensor(out=decS[:], in0=E2[:], in1=strict[:, d, :], op=ALU.mult),
                         r=[d_E2, d_strict], w=[d_decS])
                    if STG <= 1:
                        continue
                    pG, d_pG = pp.get()
                    p.op("pe", lambda h, pG=pG, tk=tk: h.matmul(pG, lhsT=kT[:, tk], rhs=kT2[:, tk], start=True, stop=True), r=[d_kT, d_kT2], w=[d_pG])
                    Pc, d_Pc = Pb[0][b]
                    Qc, d_Qc = Qb[0][b]
                    TT, d_TT = TTb[0][b]
                    if STG <= 1.25:
                        continue
                    p.op("dve", lambda h, pG=pG, Pc=Pc, negbeta=negbeta, decS=decS: h.scalar_tensor_tensor(
                        out=Pc[:], in0=pG, scalar=negbeta, in1=decS[:], op0=ALU.mult, op1=ALU.mult), r=[d_pG, d_tab, d_decS], w=[d_Pc])
                    if STG <= 1.5:
                        continue
                    pq, d_pq = pp.get()
                    p.op("pe", lambda h, pq=pq, Pc=Pc: h.transpose(pq, Pc[:], ident[:]), r=[d_Pc, d_ident], w=[d_pq])
                    if STG <= 1.75:
                        continue
                    p.op("act", lambda h, pq=pq, Qc=Qc: h.activation(out=Qc[:], in_=pq, func=AF.Copy), r=[d_pq], w=[d_Qc])
                    if STG <= 1.8:
                        continue
                    p.op("dve", lambda h, pq=pq, TT=TT: h.tensor_tensor(out=TT[:], in0=pq, in1=ident[:], op=ALU.add), r=[d_pq, d_ident], w=[d_TT])
                    if STG <= 2:
                        continue
                    for k in range(1, 7):
                        Pn, d_Pn = Pb[k % 2][b]
                        Qn, d_Qn = Qb[k % 2][b]
                        TTn, d_TTn = TTb[k % 2][b]
                        pP, d_pP = pp.get()
                        p.op("pe", lambda h, pP=pP, Qc=Qc, Pc=Pc: h.matmul(pP, lhsT=Qc[:], rhs=Pc[:], start=True, stop=True),
                             r=[d_Qc, d_Pc], w=[d_pP])
                        p.op("act", lambda h, pP=pP, Pn=Pn: h.activation(out=Pn[:], in_=pP, func=AF.Copy), r=[d_pP], w=[d_Pn])
                        if k < 6:
                            pQ, d_pQ = pp.get()
                            p.op("pe", lambda h, pQ=pQ, Qc=Qc, Pc=Pc: h.matmul(pQ, lhsT=Pc[:], rhs=Qc[:], start=True, stop=True),
                                 r=[d_Qc, d_Pc], w=[d_pQ])
                            p.op("dve", lambda h, pQ=pQ, Qn=Qn: h.tensor_copy(out=Qn[:], in_=pQ), r=[d_pQ], w=[d_Qn])
                        pT, d_pT = pp.get()
                        p.op("pe", lambda h, pT=pT, Pn=Pn, TT=TT: h.matmul(pT, lhsT=Pn[:], rhs=TT[:], start=True, stop=False),
                             r=[d_Pn, d_TT], w=[d_pT])
                        p.op("pe", lambda h, pT=pT, TT=TT: h.matmul(pT, lhsT=ident[:], rhs=TT[:], start=False, stop=True),
                             r=[d_ident, d_TT], w=[d_pT])
                        p.op("dve" if k % 2 else "act", (lambda h, pT=pT, TTn=TTn: h.tensor_copy(out=TTn[:], in_=pT)) if k % 2 else
                             (lambda h, pT=pT, TTn=TTn: h.activation(out=TTn[:], in_=pT, func=AF.Copy)), r=[d_pT], w=[d_TTn])
                        Pc, d_Pc, Qc, d_Qc, TT, d_TT = Pn, d_Pn, Qn, d_Qn, TTn, d_TTn
                    if STG <= 3:
                        continue
                    Ru, d_Ru = Ru_[b]
                    Rw, d_Rw = Rw_[b]
                    u, d_u = u_[b]
                    wT, d_wT = wT_[b]
                    kdec, d_kdec = kdec_[b]
                    vnew, d_vnew = vnew_[b]
                    attnT, d_attnT = attnT_[b]
                    p.op("dve", lambda h, Ru=Ru, tt=tt, beta=beta: h.tensor_scalar(out=Ru[:], in0=vtm[:, tt, :], scalar1=beta, scalar2=None,
                                                                                    op0=ALU.mult), r=[d_vtm, d_tab], w=[d_Ru])
                    p.op("dve", lambda h, Rw=Rw, tt=tt, bg=bg: h.tensor_scalar(out=Rw[:], in0=ktm[:, tt, :], scalar1=bg, scalar2=None,
                                                                                op0=ALU.mult), r=[d_ktm, d_tab], w=[d_Rw])
                    p.op("dve", lambda h, kdec=kdec, tt=tt, ekd=ekd: h.tensor_scalar(out=kdec[:], in0=ktm[:, tt, :], scalar1=ekd, scalar2=None,
                                                                                      op0=ALU.mult), r=[d_ktm, d_tab], w=[d_kdec])
                    pu, d_pu = pp.get()
                    p.op("pe", lambda h, pu=pu, TT=TT, Ru=Ru: h.matmul(pu[:, 0:64], lhsT=TT[:], rhs=Ru[:], start=True, stop=True),
                         r=[d_TT, d_Ru], w=[d_pu])
                    p.op("act", lambda h, pu=pu, u=u: h.activation(out=u[:], in_=pu[:, 0:64], func=AF.Copy), r=[d_pu], w=[d_u])
                    pw, d_pw = pp.get()
                    p.op("pe", lambda h, pw=pw, TT=TT, Rw=Rw: h.matmul(pw[0:64, :], lhsT=Rw[:], rhs=TT[:], start=True, stop=True),
                         r=[d_TT, d_Rw], w=[d_pw])
                    p.op("dve", lambda h, pw=pw, wT=wT: h.tensor_copy(out=wT[:], in_=pw[0:64, :]), r=[d_pw], w=[d_wT])
                    pS, d_pS = pp.get()
                    p.op("pe", lambda h, pS=pS, tk=tk: h.matmul(pS, lhsT=kT[:, tk], rhs=qT[:, tk], start=True, stop=True),
                         r=[d_kT, d_qT], w=[d_pS])
                    p.op("dve", lambda h, pS=pS, attnT=attnT, decT=decT: h.tensor_tensor(out=attnT[:], in0=pS, in1=decT[:], op=ALU.mult),
                         r=[d_pS, d_decT], w=[d_attnT])
                    if STG <= 4:
                        continue
                    pv, d_pv = pp.get()
                    p.op("pe", lambda h, pv=pv, wT=wT, Sst=Sst: h.matmul(pv[:, 0:64], lhsT=wT[:], rhs=Sst[:], start=True, stop=True),
                         r=[d_wT, d_S], w=[d_pv])
                    p.op("dve", lambda h, pv=pv, u=u, vnew=vnew: h.tensor_tensor(out=vnew[:], in0=u[:], in1=pv[:, 0:64], op=ALU.subtract),
                         r=[d_pv, d_u], w=[d_vnew])
                    po1, d_po1 = pp.get()
                    po2, d_po2 = pp.get()
                    p.op("pe", lambda h, po1=po1, attnT=attnT, vnew=vnew: h.matmul(po1[:, 0:64], lhsT=attnT[:], rhs=vnew[:], start=True, stop=True),
                         r=[d_attnT, d_vnew], w=[d_po1])
                    p.op("pe", lambda h, po2=po2, tk=tk, Sst=Sst: h.matmul(po2[:, 0:64], lhsT=qT[:, tk], rhs=Sst[:], start=True, stop=True),
                         r=[d_qT, d_S], w=[d_po2])
                    dst = acc[:, tt, 64 * hd:64 * hd + 64]
                    p.op("dve", lambda h, po1=po1, dst=dst: h.tensor_tensor(out=dst, in0=po1[:, 0:64], in1=dst, op=ALU.add),
                         r=[d_po1, d_acc], w=[d_acc])
                    p.op("dve", lambda h, po2=po2, dst=dst, egam=egam: h.scalar_tensor_tensor(out=dst, in0=po2[:, 0:64], scalar=egam, in1=dst,
                                                                                            op0=ALU.mult, op1=ALU.add),
                         r=[d_po2, d_tab, d_acc], w=[d_acc])
                    pn, d_pn = pp.get()
                    p.op("pe", lambda h, pn=pn, kdec=kdec, vnew=vnew: h.matmul(pn[0:64, 0:64], lhsT=kdec[:], rhs=vnew[:], start=True, stop=True),
                         r=[d_kdec, d_vnew], w=[d_pn])
                    p.op("dve", lambda h, pn=pn, Sst=Sst, tt=tt, d=d, hd=hd: h.scalar_tensor_tensor(
                        out=Sst[:], in0=Sst[:], scalar=tab[0:64, tt, d, 32 + hd:33 + hd], in1=pn[0:64, 0:64], op0=ALU.mult, op1=ALU.add),
                         r=[d_pn, d_tab, d_S], w=[d_S])
        gain, d_gain = sb.t((128, 256))
        for g4 in range(4):
            p.dma("sp", gain[:, 64 * g4:64 * g4 + 64], I["gdn_norm"][l:l + 1, :].partition_broadcast(128), w=[d_gain])
        zts = [sb.t((128, 256)) for _ in range(2)]
        sgs = [sb.t((128, 256)) for _ in range(2)]
        sqs = [sb.t((128, 256)) for _ in range(2)]
        sts = [sb.t((128, 8)) for _ in range(2)]
        v3 = lambda a: a[:, :].rearrange("q (g e) -> q g e", e=64)
        for tt in range(NT):
            zt, d_zt = zts[tt % 2]
            sg, d_sg = sgs[tt % 2]
            sq, d_sq = sqs[tt % 2]
            st, d_st = sts[tt % 2]
            p.dma("sp", zt[:], Z["zD"][tt * 128:(tt + 1) * 128, :], w=[d_zt])
            p.op("act", lambda h, zt=zt, sg=sg: h.activation(out=sg[:], in_=zt[:], func=AF.Sigmoid), r=[d_zt], w=[d_sg])
            p.op("dve", lambda h, zt=zt, sg=sg: h.tensor_tensor(out=sg[:], in0=sg[:], in1=zt[:], op=ALU.mult), r=[d_zt, d_sg], w=[d_sg])
            ov = acc[:, tt, :]
            p.op("act", lambda h, sq=sq, ov=ov: h.activation(out=sq[:], in_=ov, func=AF.Square), r=[d_acc], w=[d_sq])
            p.op("dve", lambda h, st=st, sq=sq: h.tensor_reduce(out=st[:, 0:4], in_=v3(sq), axis=AX.X, op=ALU.add), r=[d_sq], w=[d_st])
            rstd(p, st[:, 4:8], st[:, 0:4], st[:, 0:4], 1.0 / 64, [d_st])
            p.op("dve", lambda h, sq=sq, st=st, ov=ov: h.tensor_tensor(out=v3(sq), in0=ov.rearrange("q (g e) -> q g e", e=64),
                                                                       in1=st[:, 4:8].unsqueeze(2).to_broadcast([128, 4, 64]), op=ALU.mult),
                 r=[d_acc, d_st], w=[d_sq])
            p.op("dve", lambda h, sq=sq: h.tensor_tensor(out=sq[:], in0=sq[:], in1=gain[:], op=ALU.mult), r=[d_gain, d_sq], w=[d_sq])
            p.op("dve", lambda h, sq=sq, sg=sg: h.tensor_tensor(out=sq[:], in0=sq[:], in1=sg[:], op=ALU.mult), r=[d_sg, d_sq], w=[d_sq])
            p.dma("sp", Z["y"][tt * 128:(tt + 1) * 128, 768:1024], sq[:], r=[d_sq])


def phase_outproj(p, l, I, S, Z, xsrc):
    ident, d_ident = S["ident"]
    grow, d_grow = S["grow"]
    with SB(p) as sb:
        w, d_w = sb.t((128, 8, D), BF16, "wout")
        for k in range(8):
            p.dma("pool", w[:, k, :], I["w_out"][l, k * 128:(k + 1) * 128, :], w=[d_w])
        yts = [sb.t((128, D)) for _ in range(2)]
        xts = [sb.t((128, D)) for _ in range(2)]
        yTs = [sb.t((128, 8, 128), BF16) for _ in range(2)]
        tps = [sb.ps() for _ in range(2)]
        pos = [sb.ps() for _ in range(2)]
        tiles = range(NT) if l == 0 else range(2, NT)
        for tt in tiles:
            j = 1 if tt < 2 else 0
            rows = slice(tt * 128, (tt + 1) * 128)
            yt, d_yt = yts[tt % 2]
            xt, d_xt = xts[tt % 2]
            yT, d_yT = yTs[tt % 2]
            p.dma("sp", yt[:], Z["y"][rows, :], w=[d_yt])
            p.dma("sp", xt[:], xsrc[rows, :], w=[d_xt])
            for c in range(8):
                tp, d_tp = tps[c // 4]
                p.op("pe", lambda h, c=c, tp=tp, yt=yt: h.transpose(tp[:, (c % 4) * 128:(c % 4 + 1) * 128], yt[:, c * 128:(c + 1) * 128], ident[:]),
                     r=[d_yt, d_ident], w=[d_tp])
            for hh in range(2):
                tp, d_tp = tps[hh]
                p.op("act", lambda h, hh=hh, tp=tp, yT=yT: h.activation(out=yT[:, 4 * hh:4 * hh + 4, :],
                                                                       in_=tp[:, :].rearrange("q (c t) -> q c t", c=4), func=AF.Copy),
                     r=[d_tp], w=[d_yT])
            for hh in range(2):
                po, d_po = pos[hh]
                for k in range(8):
                    p.op("pe", lambda h, k=k, po=po, yT=yT, hh=hh: h.matmul(po[:, :], lhsT=yT[:, k, :], rhs=w[:, k, hh * 512:(hh + 1) * 512],
                                                                          start=(k == 0), stop=(k == 7)), r=[d_yT, d_w], w=[d_po])
                p.op("dve", lambda h, po=po, yt=yt, hh=hh, j=j: h.tensor_tensor(out=yt[:, hh * 512:(hh + 1) * 512], in0=po[:, :],
                                                                             in1=grow[:, 0, j, hh * 512:(hh + 1) * 512], op=ALU.mult),
                     r=[d_po, d_grow], w=[d_yt])
            p.op("dve", lambda h, xt=xt, yt=yt: h.tensor_tensor(out=xt[:], in0=xt[:], in1=yt[:], op=ALU.add), r=[d_yt, d_xt], w=[d_xt])
            p.dma("sp", Z["xres"][rows, :], xt[:], r=[d_xt])


def norm_objs(sb, S):
    junk, d_junk = sb.t((128, D))
    xs, d_xs = sb.t((128, D))
    ss, d_ss = sb.t((128, 4))
    tps = [sb.ps() for _ in range(2)]
    return (junk, d_junk, ss, d_ss, xs, d_xs, tps, S["ident"][0], S["ident"][1])


def phase_ffn_dense(p, l, I, S, Z):
    gsh, d_gsh = S["gsh"]
    grow, d_grow = S["grow"]
    NF = D_FF // 128
    with SB(p) as sb:
        wg, d_wg = sb.t((128, 8, D_FF), BF16, "wg")
        wu, d_wu = sb.t((128, 8, D_FF), BF16, "wu")
        wd, d_wd = sb.t((128, NF, D), BF16, "wd")
        for k in range(8):
            p.dma("pool", wg[:, k, :], I["ffn_w_gate"][k * 128:(k + 1) * 128, :], w=[d_wg])
            p.dma("pool", wu[:, k, :], I["ffn_w_up"][k * 128:(k + 1) * 128, :], w=[d_wu])
        for k in range(NF):
            p.dma("pool", wd[:, k, :], I["ffn_w_down"][k * 128:(k + 1) * 128, :], w=[d_wd])
        nobj = norm_objs(sb, S)
        xb, d_xb = sb.t((128, 2, D), F32, "xblk")
        xnT, d_xnT = sb.t((128, 8, 256), BF16, "xnT")
        hT, d_hT = sb.t((128, NF, 256), BF16, "hT")
        sgs = [sb.t((128, 256)) for _ in range(2)]
        pgs = [sb.ps() for _ in range(2)]
        pus = [sb.ps() for _ in range(2)]
        pos = [sb.ps() for _ in range(2)]
        ot, d_ot = sb.t((128, 512))
        blocks = [(2 * i, 2) for i in range(17)]
        n = 0
        for (t0, ntl) in blocks:
            ntok = ntl * 128
            j = 1 if t0 < 2 else 0
            for ti in range(ntl):
                tt = t0 + ti
                p.dma("sp", xb[:, ti, :], Z["xres"][tt * 128:(tt + 1) * 128, :], w=[d_xb])
            for ti in range(ntl):
                norm_tile(p, nobj, xb[:, ti, :], d_xb, gsh, d_gsh, 1, j, xnT, d_xnT, ti * 128)
            for fc in range(NF):
                pg, d_pg = pgs[fc % 2]
                pu, d_pu = pus[fc % 2]
                sg, d_sg = sgs[fc % 2]
                for k in range(8):
                    p.op("pe", lambda h, k=k, pg=pg, fc=fc: h.matmul(pg[:, 0:ntok], lhsT=wg[:, k, fc * 128:(fc + 1) * 128], rhs=xnT[:, k, 0:ntok],
                                                                    start=(k == 0), stop=(k == 7)), r=[d_wg, d_xnT], w=[d_pg])
                for k in range(8):
                    p.op("pe", lambda h, k=k, pu=pu, fc=fc: h.matmul(pu[:, 0:ntok], lhsT=wu[:, k, fc * 128:(fc + 1) * 128], rhs=xnT[:, k, 0:ntok],
                                                                    start=(k == 0), stop=(k == 7)), r=[d_wu, d_xnT], w=[d_pu])
                p.op("act", lambda h, pg=pg, sg=sg: h.activation(out=sg[:, 0:ntok], in_=pg[:, 0:ntok], func=AF.Sigmoid), r=[d_pg], w=[d_sg])
                p.op("dve", lambda h, pg=pg, sg=sg: h.tensor_tensor(out=sg[:, 0:ntok], in0=pg[:, 0:ntok], in1=sg[:, 0:ntok], op=ALU.mult),
                     r=[d_pg, d_sg], w=[d_sg])
                p.op("dve", lambda h, pu=pu, sg=sg, fc=fc: h.tensor_tensor(out=hT[:, fc, 0:ntok], in0=pu[:, 0:ntok], in1=sg[:, 0:ntok], op=ALU.mult),
                     r=[d_pu, d_sg], w=[d_hT])
            for ti in range(ntl):
                tt = t0 + ti
                for hh in range(2):
                    po, d_po = pos[n % 2]
                    n += 1
                    for fc in range(NF):
                        p.op("pe", lambda h, fc=fc, po=po, ti=ti, hh=hh: h.matmul(po[:, :], lhsT=hT[:, fc, ti * 128:(ti + 1) * 128],
                                                                                rhs=wd[:, fc, hh * 512:(hh + 1) * 512],
                                                                                start=(fc == 0), stop=(fc == NF - 1)), r=[d_hT, d_wd], w=[d_po])
                    p.op("dve", lambda h, po=po, hh=hh, j=j: h.tensor_tensor(out=ot[:], in0=po[:, :], in1=grow[:, 1, j, hh * 512:(hh + 1) * 512],
                                                                          op=ALU.mult), r=[d_po, d_grow], w=[d_ot])
                    p.op("dve", lambda h, ti=ti, hh=hh: h.tensor_tensor(out=xb[:, ti, hh * 512:(hh + 1) * 512], in0=xb[:, ti, hh * 512:(hh + 1) * 512],
                                                                         in1=ot[:], op=ALU.add), r=[d_ot, d_xb], w=[d_xb])
                p.dma("sp", Z["xres"][tt * 128:(tt + 1) * 128, :], xb[:, ti, :], r=[d_xb])


def phase_moe(p, l, I, S, Z, out):
    gsh, d_gsh = S["gsh"]
    grow, d_grow = S["grow"]
    NTM = 16
    SLAB = 512
    NSL = D_FFE // SLAB
    with SB(p) as sb:
        fl, d_fl = sb.t((128, 2))
        p.dma("sp", fl[:], I["flags"][:, :], w=[d_fl])
        xm, d_xm = sb.t((128, NTM, D), F32, "xm")
        xnT, d_xnT = sb.t((128, 8, NTM * 128), BF16, "xnTm")
        gates, d_gates = sb.t((128, NTM, NE), F32, "gates")
        rt, d_rt = sb.t((128, 8, NE), F32, "router")
        p.dma("sp", rt[:], I["moe_router"].rearrange("(k q) e -> q k e", q=128), w=[d_rt])
        with SB(p) as sb2:
            nobj = norm_objs(sb2, S)
            (junk, d_junk, ss, d_ss, xs, d_xs, tps, ident, d_ident) = nobj
            xa = [sb2.t((128, D)) for _ in range(2)]
            xnf, d_xnf = sb2.t((128, 8, 128), F32, "xnf")
            plg, d_plg = sb2.ps()
            lg, d_lg = sb2.t((128, 8))
            mx, d_mx = sb2.t((128, 8))
            wk, d_wk = sb2.t((128, 32))
            for jt in range(NTM):
                a_, d_a = xa[0]
                b_, d_b = xa[1]
                p.dma("sp", a_[:], Z["xres"][256 + jt * 128:256 + (jt + 1) * 128, :], w=[d_a])
                p.dma("sp", b_[:], Z["xres"][256 + 2048 + jt * 128:256 + 2048 + (jt + 1) * 128, :], w=[d_b])
                p.op("dve", lambda h, jt=jt: h.tensor_scalar(out=xm[:, jt, :], in0=a_[:], scalar1=fl[:, 0:1], scalar2=None, op0=ALU.mult),
                     r=[d_a, d_fl], w=[d_xm])
                p.op("dve", lambda h, jt=jt: h.scalar_tensor_tensor(out=xm[:, jt, :], in0=b_[:], scalar=fl[:, 1:2], in1=xm[:, jt, :],
                                                                    op0=ALU.mult, op1=ALU.add), r=[d_b, d_fl, d_xm], w=[d_xm])
                norm_tile(p, nobj, xm[:, jt, :], d_xm, gsh, d_gsh, 1, 0, xnT, d_xnT, jt * 128)
                for c in range(8):
                    tp, d_tp = tps[c // 4]
                    p.op("act", lambda h, c=c, tp=tp: h.activation(out=xnf[:, c, :], in_=tp[:, (c % 4) * 128:(c % 4 + 1) * 128],
                                                                   func=AF.Identity, scale=gsh[:, 2, c, 0:1], bias=gsh[:, 3, c, 0:1]),
                         r=[d_tp, d_gsh], w=[d_xnf])
                for k in range(8):
                    p.op("pe", lambda h, k=k: h.matmul(plg[:, 0:NE], lhsT=xnf[:, k, :], rhs=rt[:, k, :], start=(k == 0), stop=(k == 7)),
                         r=[d_xnf, d_rt], w=[d_plg])
                p.op("dve", lambda h: h.tensor_copy(out=lg[:], in_=plg[:, 0:NE]), r=[d_plg], w=[d_lg])
                p.op("dve", lambda h: h.max(out=mx[:], in_=lg[:]), r=[d_lg], w=[d_mx])
                p.op("dve", lambda h: h.tensor_tensor(out=wk[:, 0:1], in0=mx[:, 1:2], in1=mx[:, 0:1], op=ALU.subtract), r=[d_mx], w=[d_wk])
                p.op("act", lambda h: h.activation(out=wk[:, 1:2], in_=wk[:, 0:1], func=AF.Exp), r=[d_wk], w=[d_wk])
                p.op("dve", lambda h: h.tensor_scalar(out=wk[:, 1:2], in0=wk[:, 1:2], scalar1=1.0, scalar2=None, op0=ALU.add), r=[d_wk], w=[d_wk])
                p.op("dve", lambda h: h.reciprocal(out=wk[:, 2:3], in_=wk[:, 1:2]), r=[d_wk], w=[d_wk])
                p.op("dve", lambda h: h.tensor_scalar(out=wk[:, 3:4], in0=wk[:, 2:3], scalar1=-1.0, scalar2=1.0, op0=ALU.mult, op1=ALU.add),
                     r=[d_wk], w=[d_wk])
                p.op("dve", lambda h: h.tensor_scalar(out=wk[:, 8:16], in0=lg[:], scalar1=mx[:, 0:1], scalar2=wk[:, 2:3], op0=ALU.is_equal,
                                                      op1=ALU.mult), r=[d_lg, d_mx, d_wk], w=[d_wk])
                p.op("dve", lambda h: h.tensor_scalar(out=wk[:, 16:24], in0=lg[:], scalar1=mx[:, 1:2], scalar2=wk[:, 3:4], op0=ALU.is_equal,
                                                      op1=ALU.mult), r=[d_lg, d_mx, d_wk], w=[d_wk])
                p.op("dve", lambda h, jt=jt: h.tensor_tensor(out=gates[:, jt, :], in0=wk[:, 8:16], in1=wk[:, 16:24], op=ALU.add),
                     r=[d_wk], w=[d_gates])
        wgs = [sb.t((128, 8, SLAB), BF16, "wgs") for _ in range(2)]
        wus = [sb.t((128, 8, SLAB), BF16, "wus") for _ in range(2)]
        wds = [sb.t((128, 4, D), BF16, "wds") for _ in range(2)]
        hTs = [sb.t((128, 4, 512), BF16, "hTs") for _ in range(2)]
        sgs = [sb.t((128, 512)) for _ in range(2)]
        pgs = [sb.ps() for _ in range(2)]
        pus = [sb.ps() for _ in range(2)]
        pos = [sb.ps() for _ in range(2)]
        ns = 0
        nh = 0
        nf = 0
        no = 0
        for e in range(NE):
            for sl in range(NSL):
                wg, d_wg = wgs[ns % 2]
                wu, d_wu = wus[ns % 2]
                wd, d_wd = wds[ns % 2]
                ns += 1
                c0 = sl * SLAB
                p.dma("pool", wg[:], I["moe_w_gate"][e].rearrange("(k q) f -> q k f", q=128)[:, :, c0:c0 + SLAB], w=[d_wg])
                p.dma("pool", wu[:], I["moe_w_up"][e].rearrange("(k q) f -> q k f", q=128)[:, :, c0:c0 + SLAB], w=[d_wu])
                p.dma("pool", wd[:], I["moe_w_down"][e, c0:c0 + SLAB, :].rearrange("(k q) d -> q k d", q=128), w=[d_wd])
                for k in range(4):
                    p.op("dve", lambda h, k=k, wd=wd: h.tensor_tensor(out=wd[:, k, :], in0=wd[:, k, :], in1=grow[:, 1, 0, :], op=ALU.mult),
                         r=[d_grow, d_wd], w=[d_wd])
                for tb in range(NTM // 4):
                    hT, d_hT = hTs[nh % 2]
                    nh += 1
                    toks = slice(tb * 512, (tb + 1) * 512)
                    for fc in range(4):
                        pg, d_pg = pgs[nf % 2]
                        pu, d_pu = pus[nf % 2]
                        sg, d_sg = sgs[nf % 2]
                        nf += 1
                        for k in range(8):
                            p.op("pe", lambda h, k=k, pg=pg, fc=fc, wg=wg: h.matmul(pg[:, :], lhsT=wg[:, k, fc * 128:(fc + 1) * 128], rhs=xnT[:, k, toks],
                                                                                  start=(k == 0), stop=(k == 7)), r=[d_wg, d_xnT], w=[d_pg])
                        for k in range(8):
                            p.op("pe", lambda h, k=k, pu=pu, fc=fc, wu=wu: h.matmul(pu[:, :], lhsT=wu[:, k, fc * 128:(fc + 1) * 128], rhs=xnT[:, k, toks],
                                                                                  start=(k == 0), stop=(k == 7)), r=[d_wu, d_xnT], w=[d_pu])
                        p.op("act", lambda h, pg=pg, sg=sg: h.activation(out=sg[:], in_=pg[:, :], func=AF.Sigmoid), r=[d_pg], w=[d_sg])
                        p.op("dve", lambda h, pg=pg, sg=sg: h.tensor_tensor(out=sg[:], in0=pg[:, :], in1=sg[:], op=ALU.mult), r=[d_pg, d_sg], w=[d_sg])
                        p.op("dve", lambda h, pu=pu, sg=sg, fc=fc, hT=hT: h.tensor_tensor(out=hT[:, fc, :], in0=pu[:, :], in1=sg[:], op=ALU.mult),
                             r=[d_pu, d_sg], w=[d_hT])
                    for ti in range(4):
                        jt = tb * 4 + ti
                        for hh in range(2):
                            po, d_po = pos[no % 2]
                            no += 1
                            for fc in range(4):
                                p.op("pe", lambda h, fc=fc, po=po, ti=ti, hh=hh, hT=hT, wd=wd: h.matmul(
                                    po[:, :], lhsT=hT[:, fc, ti * 128:(ti + 1) * 128], rhs=wd[:, fc, hh * 512:(hh + 1) * 512],
                                    start=(fc == 0), stop=(fc == 3)), r=[d_hT, d_wd], w=[d_po])
                            dst = xm[:, jt, hh * 512:(hh + 1) * 512]
                            p.op("dve", lambda h, po=po, dst=dst, jt=jt, e=e: h.scalar_tensor_tensor(out=dst, in0=po[:, :], scalar=gates[:, jt, e:e + 1],
                                                                                                  in1=dst, op0=ALU.mult, op1=ALU.add),
                                 r=[d_po, d_gates, d_xm], w=[d_xm])
        for jt in range(NTM):
            p.dma("sp", out[jt * 128:(jt + 1) * 128, :], xm[:, jt, :], r=[d_xm])


_CACHE = {}


def kernel(**inputs):
    inp = {k: np.asarray(v) for k, v in inputs.items()}
    if "p" not in _CACHE:
        _CACHE["p"] = build()
    p = _CACHE["p"]
    in_maps = [host_inputs(inp, c) for c in range(8)]
    res = run_bass_kernel_spmd(p.nc, in_maps, core_ids=list(range(8)))
    out = np.zeros((4, NLAT, D), np.float32)
    for c in range(8):
        b, hh = c // 2, c % 2
        out[b, hh * 2048:(hh + 1) * 2048, :] = np.asarray(res.results[c]["out"], dtype=np.float32)
    return out
```

```python
import math
from contextlib import ExitStack
import numpy as np
import concourse.bass as bass
import concourse.mybir as mybir
from concourse.bass_utils import run_bass_kernel_spmd

F32 = mybir.dt.float32
BF16 = mybir.dt.bfloat16
AF = mybir.ActivationFunctionType
ALU = mybir.AluOpType
AX = mybir.AxisListType

D = 1024
NCTX = 256
NLAT = 4096
T = NCTX + NLAT
NT = T // 128
EPS = 1e-6
N_IN = 3360
D_FF = 2816
D_FFE = 3584
NE = 8


class Dep:
    __slots__ = ("w", "r", "excl")

    def __init__(self, excl=False):
        self.w = {}
        self.r = {}
        self.excl = excl


class Prog:
    def __init__(self):
        self.nc = bass.Bass("TRN2", target_bir_lowering=False)
        nc = self.nc
        self.h = {"pe": nc.tensor, "act": nc.scalar, "dve": nc.vector, "pool": nc.gpsimd, "sp": nc.sync}
        self.sem = {}
        self.cnt = {}
        self.semobj = {}
        self.seen = {e: {} for e in self.h}
        self.nsem = 0
        for e in self.h:
            self._newsem(e)
        self.NS = 12
        self.slots = {q: [nc.alloc_semaphore(f"dq_{q}_{i}") for i in range(self.NS)] for q in ("sp", "pool", "act")}
        self.dcnt = {q: 0 for q in self.slots}
        for q in self.slots:
            for s in self.slots[q]:
                self.semobj[id(s)] = s

    def _newsem(self, e):
        s = self.nc.alloc_semaphore(f"s_{e}_{self.nsem}")
        self.nsem += 1
        self.sem[e] = s
        self.cnt[e] = 0
        if not hasattr(self, "semobj"):
            self.semobj = {}
        self.semobj[id(s)] = s

    def _wait(self, e, tok):
        sid, val = tok
        if self.seen[e].get(sid, 0) >= val:
            return
        self.seen[e][sid] = val
        self.h[e].wait_ge(self.semobj[sid], val)

    def _deps(self, e, r, w):
        need = {}
        for d in r:
            for sid, v in d.w.items():
                need[sid] = max(need.get(sid, 0), v)
            if d.excl:
                for sid, v in d.r.items():
                    need[sid] = max(need.get(sid, 0), v)
        for d in w:
            for sid, v in d.w.items():
                need[sid] = max(need.get(sid, 0), v)
            for sid, v in d.r.items():
                need[sid] = max(need.get(sid, 0), v)
        own = id(self.sem[e])
        for sid, v in need.items():
            if e == "pe" and sid == own:
                continue
            self._wait(e, (sid, v))

    def _mark(self, tok, r, w):
        sid, v = tok
        for d in r:
            d.r[sid] = max(d.r.get(sid, 0), v)
        for d in w:
            d.w = {sid: v}
            d.r = {}

    def op(self, e, fn, r=(), w=()):
        self._deps(e, r, w)
        if self.cnt[e] >= 30000:
            self._newsem(e)
        ins = fn(self.h[e])
        self.cnt[e] += 1
        ins.then_inc(self.sem[e], 1)
        self._mark((id(self.sem[e]), self.cnt[e]), r, w)

    def dma(self, q, out, in_, r=(), w=(), **kw):
        self._deps(q, r, w)
        i = self.dcnt[q]
        self.dcnt[q] += 1
        s = self.slots[q][i % self.NS]
        val = 16 * (i // self.NS + 1)
        self._wait(q, (id(s), val - 16))
        self.h[q].dma_start(out=out, in_=in_, **kw).then_inc(s, 16)
        self._mark((id(s), val), r, w)

    def eps_ap(self, eps, n):
        assert abs(eps - EPS) < 1e-12
        return self.epst[0:n, 0:1]

    def barrier(self):
        toks = []
        for e in self.h:
            if self.cnt[e] > 0:
                toks.append((id(self.sem[e]), self.cnt[e]))
        for q in self.slots:
            n = self.dcnt[q]
            for j in range(min(n, self.NS)):
                i = n - 1 - j
                toks.append((id(self.slots[q][i % self.NS]), 16 * (i // self.NS + 1)))
        for e in self.h:
            for t in toks:
                self._wait(e, t)


class SB:
    N = 0

    def __init__(self, p):
        self.p = p
        self.es = ExitStack()
        self.n = 0

    def __enter__(self):
        self.es.__enter__()
        return self

    def __exit__(self, *a):
        self.p.barrier()
        return self.es.__exit__(*a)

    def t(self, shape, dt=F32, name="t"):
        SB.N += 1
        return self.es.enter_context(self.p.nc.sbuf_tensor(f"{name}_{SB.N}", list(shape), dt)), Dep()

    def ps(self, shape=(128, 512), dt=F32, name="ps"):
        SB.N += 1
        return self.es.enter_context(self.p.nc.psum_tensor(f"{name}_{SB.N}", list(shape), dt)), Dep(excl=True)


CA_Q, CA_K, CA_V = 0, 256, 512
CB_Q, CB_K, CB_V = 768, 1024, 1152
CC_Q, CC_K, CC_V, CC_O, CC_G = 1280, 1536, 1792, 2048, 2304
CD_QKV, CD_Z, CD_G = 2320, 3088, 3344


def dram(p, name, shape, dt, kind="Internal"):
    return p.nc.dram_tensor(name, list(shape), dt, kind=kind).ap()


def phase_mod(p, l, I, S):
    nc = p.nc
    modT, d_modT = S["modT"]
    gsh, d_gsh = S["gsh"]
    grow, d_grow = S["grow"]
    with SB(p) as sb:
        cc, d_cc = sb.t((128, 8, 2))
        sc, d_sc = sb.t((128, 8, 2))
        ones, d_ones = sb.t((128, 128))
        rep, d_rep = sb.t((128, 2, 8, 128))
        bmT, d_bmT = sb.t((128, 48))
        nrm, d_nrm = sb.t((128, 2, 8))
        wm = [sb.t((128, 8, 512)) for _ in range(2)]
        psm, d_psm = sb.ps((128, 512))
        psr = [sb.ps((128, 512)) for _ in range(2)]
        p.dma("sp", cc[:], I["cc"][:, :, :], w=[d_cc])
        p.dma("sp", bmT[:], I["bmodT"][:, l, :], w=[d_bmT])
        p.dma("sp", nrm[:, 0, :], I["norm1T"][:, l, :], w=[d_nrm])
        p.dma("sp", nrm[:, 1, :], I["norm2T"][:, l, :], w=[d_nrm])
        for j in range(2):
            for which, c0 in ((0, 2048), (1, 5120)):
                p.dma("sp", grow[:, which, j, :], I["b_mod"][l:l + 1, c0:c0 + 1024].partition_broadcast(128), w=[d_grow])
        p.op("act", lambda h: h.activation(out=sc[:], in_=cc[:], func=AF.Sigmoid), r=[d_cc], w=[d_sc])
        p.op("dve", lambda h: h.tensor_tensor(out=sc[:], in0=sc[:], in1=cc[:], op=ALU.mult), r=[d_cc, d_sc], w=[d_sc])
        p.op("dve", lambda h: h.memset(ones[:], 1.0), w=[d_ones])
        for j in range(2):
            for k in range(8):
                p.op("dve", lambda h, j=j, k=k: h.tensor_scalar(out=rep[:, j, k, :], in0=ones[:], scalar1=sc[:, k, j:j + 1],
                                                                 scalar2=None, op0=ALU.mult), r=[d_ones, d_sc], w=[d_rep])
        wsrc = I["w_mod"][l].rearrange("(k q) n -> q k n", q=128)
        for blk in range(12):
            wt, d_wt = wm[blk % 2]
            p.dma("sp", wt[:], wsrc[:, :, blk * 512:(blk + 1) * 512], w=[d_wt])
            for fc in range(4):
                for k in range(8):
                    p.op("pe", lambda h, fc=fc, k=k, wt=wt: h.matmul(psm[:, fc * 2:fc * 2 + 2], lhsT=wt[:, k, fc * 128:(fc + 1) * 128],
                                                                    rhs=sc[:, k, :], start=(k == 0), stop=(k == 7)),
                         r=[d_wt, d_sc], w=[d_psm])
            p.op("dve", lambda h, blk=blk: h.tensor_copy(out=modT[:, blk * 4:(blk + 1) * 4, :],
                                                         in_=psm[:, 0:8].rearrange("q (f j) -> q f j", j=2)),
                 r=[d_psm], w=[d_modT])
            if blk in (4, 5, 10, 11):
                which = 0 if blk < 6 else 1
                half = blk % 2
                for j in range(2):
                    pr, d_pr = psr[j]
                    for k in range(8):
                        p.op("pe", lambda h, j=j, k=k, wt=wt, pr=pr: h.matmul(pr[:, :], lhsT=rep[:, j, k, :], rhs=wt[:, k, :],
                                                                              start=(k == 0), stop=(k == 7)),
                             r=[d_wt, d_rep], w=[d_pr])
                    dst = grow[:, which, j, half * 512:(half + 1) * 512]
                    p.op("dve", lambda h, dst=dst, pr=pr: h.tensor_tensor(out=dst, in0=pr[:, :], in1=dst, op=ALU.add),
                         r=[d_pr, d_grow], w=[d_grow])
        for j in range(2):
            p.op("dve", lambda h, j=j: h.tensor_tensor(out=modT[:, :, j], in0=modT[:, :, j], in1=bmT[:], op=ALU.add),
                 r=[d_bmT, d_modT], w=[d_modT])
        for j in range(2):
            for n_i, (c_sh, c_sc) in enumerate(((0, 8), (24, 32))):
                p.op("dve", lambda h, j=j, n_i=n_i, c_sc=c_sc: h.scalar_tensor_tensor(
                    out=gsh[:, 2 * n_i, :, j], in0=modT[:, c_sc:c_sc + 8, j], scalar=1.0, in1=nrm[:, n_i, :],
                    op0=ALU.add, op1=ALU.mult), r=[d_modT, d_nrm], w=[d_gsh])
                p.op("dve", lambda h, j=j, n_i=n_i, c_sh=c_sh: h.tensor_copy(out=gsh[:, 2 * n_i + 1, :, j], in_=modT[:, c_sh:c_sh + 8, j]),
                     r=[d_modT], w=[d_gsh])


def rstd(p, out, in_, tmp, scale, deps, eps=EPS):
    p.op("act", lambda h: h.activation(out=tmp, in_=in_, func=AF.Sqrt, scale=scale, bias=p.eps_ap(eps, in_.shape[0])), r=deps + [p.d_eps], w=deps)
    p.op("dve", lambda h: h.reciprocal(out=out, in_=tmp), r=deps, w=deps)


def norm_tile(p, sb_objs, xt, d_xt, gsh, d_gsh, which, j, xnT, d_xnT, tok0):
    (junk, d_junk, ss, d_ss, xs, d_xs, tps, ident, d_ident) = sb_objs
    p.op("act", lambda h: h.activation(out=junk[:], in_=xt[:], func=AF.Square, accum_out=ss[:, 0:1]), r=[d_xt], w=[d_junk, d_ss])
    rstd(p, ss[:, 2:3], ss[:, 0:1], ss[:, 1:2], 1.0 / D, [d_ss])
    p.op("dve", lambda h: h.tensor_scalar(out=xs[:], in0=xt[:], scalar1=ss[:, 2:3], scalar2=None, op0=ALU.mult),
         r=[d_xt, d_ss], w=[d_xs])
    for c in range(8):
        tp, d_tp = tps[c // 4]
        p.op("pe", lambda h, c=c, tp=tp: h.transpose(tp[:, (c % 4) * 128:(c % 4 + 1) * 128], xs[:, c * 128:(c + 1) * 128], ident[:]),
             r=[d_xs, d_ident], w=[d_tp])
    for c in range(8):
        tp, d_tp = tps[c // 4]
        p.op("act", lambda h, c=c, tp=tp: h.activation(out=xnT[:, c, tok0:tok0 + 128], in_=tp[:, (c % 4) * 128:(c % 4 + 1) * 128],
                                                       func=AF.Identity, scale=gsh[:, 2 * which, c, j:j + 1],
                                                       bias=gsh[:, 2 * which + 1, c, j:j + 1]),
             r=[d_tp, d_gsh], w=[d_xnT])


def qk_norm_rope(p, sbo, src, d_src, ncols, dim, gain, d_gain, rope, d_rope, dst, d_dst):
    (sq, d_sq, st, d_st, t1, d_t1, t2, d_t2) = sbo
    ng = ncols // dim
    p.op("act", lambda h: h.activation(out=sq[:, 0:ncols], in_=src, func=AF.Square), r=[d_src], w=[d_sq])
    p.op("dve", lambda h: h.tensor_reduce(out=st[:, 0:ng], in_=sq[:, 0:ncols].rearrange("q (g d) -> q g d", d=dim), axis=AX.X, op=ALU.add),
         r=[d_sq], w=[d_st])
    rstd(p, st[:, 0:ng], st[:, 0:ng], st[:, 0:ng], 1.0 / dim, [d_st])
    tgt = t1 if rope is not None else dst
    d_tgt = d_t1 if rope is not None else d_dst
    p.op("dve", lambda h: h.tensor_tensor(out=tgt[:, 0:ncols].rearrange("q (g d) -> q g d", d=dim),
                                          in0=src.rearrange("q (g d) -> q g d", d=dim),
                                          in1=st[:, 0:ng].unsqueeze(2).to_broadcast([128, ng, dim]), op=ALU.mult),
         r=[d_src, d_st], w=[d_tgt])
    p.op("dve", lambda h: h.tensor_tensor(out=tgt[:, 0:ncols], in0=tgt[:, 0:ncols], in1=gain[:, 0:ncols], op=ALU.mult),
         r=[d_gain, d_tgt], w=[d_tgt])
    if rope is None:
        return
    q4 = dim // 4
    v = lambda a: a[:, 0:ncols].rearrange("q (g a s f) -> q g a s f", a=2, s=2, f=q4)
    cb = rope[:, 0, :].rearrange("q (a s f) -> q a s f", a=2, s=2).unsqueeze(1).to_broadcast([128, ng, 2, 2, q4])
    sbv = rope[:, 1, :].rearrange("q (a s f) -> q a s f", a=2, s=2)
    for s_ in range(2):
        p.op("dve", lambda h, s_=s_: h.tensor_tensor(out=v(t2)[:, :, :, s_, :], in0=v(t1)[:, :, :, 1 - s_, :],
                                                     in1=sbv[:, :, s_, :].unsqueeze(1).to_broadcast([128, ng, 2, q4]), op=ALU.mult),
             r=[d_t1, d_rope], w=[d_t2])
    p.op("dve", lambda h: h.tensor_tensor(out=v(t1), in0=v(t1), in1=cb, op=ALU.mult), r=[d_rope, d_t1], w=[d_t1])
    p.op("dve", lambda h: h.tensor_tensor(out=dst[:, 0:ncols], in0=t1[:, 0:ncols], in1=t2[:, 0:ncols], op=ALU.add),
         r=[d_t1, d_t2], w=[d_dst])


def phase_inproj(p, l, I, S, Z, xsrc):
    gsh, d_gsh = S["gsh"]
    ident, d_ident = S["ident"]
    with SB(p) as sb:
        w, d_w = sb.t((128, 8, N_IN), BF16, "win")
        for k in range(8):
            p.dma("pool", w[:, k, :], I["w_in"][l, k * 128:(k + 1) * 128, :], w=[d_w])
        gainA, d_gainA = sb.t((128, 512))
        gainB, d_gainB = sb.t((128, 384))
        for m in range(8):
            p.dma("sp", gainA[:, m * 32:(m + 1) * 32], I["diff_qk_gain"][l, 0:1, :].partition_broadcast(128), w=[d_gainA])
            p.dma("sp", gainA[:, 256 + m * 32:256 + (m + 1) * 32], I["diff_qk_gain"][l, 1:2, :].partition_broadcast(128), w=[d_gainA])
        for m in range(6):
            p.dma("sp", gainB[:, m * 64:(m + 1) * 64], I["gqa_qk_gain"][l, (0 if m < 4 else 1):(1 if m < 4 else 2), :].partition_broadcast(128),
                  w=[d_gainB])
        xts = [sb.t((128, D)) for _ in range(2)]
        junk, d_junk = sb.t((128, D))
        xs, d_xs = sb.t((128, D))
        ss, d_ss = sb.t((128, 4))
        tps = [sb.ps() for _ in range(2)]
        nobj = (junk, d_junk, ss, d_ss, xs, d_xs, tps, ident, d_ident)
        xnT, d_xnT = sb.t((128, 8, 512), BF16, "xnT")
        sq, d_sq = sb.t((128, 512))
        st, d_st = sb.t((128, 16))
        t1, d_t1 = sb.t((128, 512))
        t2, d_t2 = sb.t((128, 512))
        qko = (sq, d_sq, st, d_st, t1, d_t1, t2, d_t2)
        qkn, d_qkn = sb.t((128, 512))
        ropeA, d_ropeA = sb.t((128, 2, 32))
        ropeB, d_ropeB = sb.t((128, 2, 64))
        ps_tm = [sb.ps() for _ in range(2)]
        ps_fm = [sb.ps() for _ in range(2)]
        ps_tr, d_ps_tr = sb.ps()
        stA, d_stA = sb.t((128, 4, 512), BF16)
        stB, d_stB = sb.t((128, 3, 512), BF16)
        stv, d_stv = sb.t((128, 512), BF16)
        stf, d_stf = sb.t((128, 784))
        stfm = [sb.t((128, 512)) for _ in range(2)]
        blocks = [(0, 2)] + [(2 + 4 * i, 4) for i in range(8)]
        ntm = 0
        nfm = 0
        for (t0, ntl) in blocks:
            ntok = ntl * 128
            tokb = t0 * 128
            j = 1 if t0 < 2 else 0
            for ti in range(ntl):
                tt = t0 + ti
                xt, d_xt = xts[tt % 2]
                p.dma("sp", xt[:], xsrc[tt * 128:(tt + 1) * 128, :], w=[d_xt])
                norm_tile(p, nobj, xt, d_xt, gsh, d_gsh, 0, j, xnT, d_xnT, ti * 128)
            for ti in range(ntl):
                tt = t0 + ti
                tk = slice(ti * 128, (ti + 1) * 128)
                rows = slice(tt * 128, (tt + 1) * 128)
                lat = tt >= 2
                if lat:
                    p.dma("sp", ropeA[:], I["ropeA"][(tt - 2) * 128:(tt - 1) * 128, :, :], w=[d_ropeA])
                    p.dma("sp", ropeB[:], I["ropeB"][(tt - 2) * 128:(tt - 1) * 128, :, :], w=[d_ropeB])

                def tm_mm(c0, ncol):
                    nonlocal ntm
                    ps, d_ps = ps_tm[ntm % 2]
                    ntm += 1
                    for k in range(8):
                        p.op("pe", lambda h, k=k, ps=ps: h.matmul(ps[:, 0:ncol], lhsT=xnT[:, k, tk], rhs=w[:, k, c0:c0 + ncol],
                                                                  start=(k == 0), stop=(k == 7)), r=[d_xnT, d_w], w=[d_ps])
                    return ps, d_ps
                ps, d_ps = tm_mm(CA_Q, 512)
                qk_norm_rope(p, qko, ps[:, 0:512], d_ps, 512, 32, gainA, d_gainA, ropeA if lat else None, d_ropeA, qkn, d_qkn)
                for c in range(4):
                    p.op("pe", lambda h, c=c: h.transpose(ps_tr[:, c * 128:(c + 1) * 128], qkn[:, c * 128:(c + 1) * 128], ident[:]),
                         r=[d_qkn, d_ident], w=[d_ps_tr])
                p.op("act", lambda h: h.activation(out=stA[:, :, tk], in_=ps_tr[:, :].rearrange("q (c t) -> q c t", c=4), func=AF.Copy),
                     r=[d_ps_tr], w=[d_stA])
                ps, d_ps = tm_mm(CA_V, 256)
                p.op("act", lambda h, ps=ps: h.activation(out=stv[:, 0:256], in_=ps[:, 0:256], func=AF.Copy), r=[d_ps], w=[d_stv])
                p.dma("sp", Z["vA"][rows, :], stv[:, 0:256], r=[d_stv])
                ps, d_ps = tm_mm(CB_Q, 512)
                p.op("act", lambda h, ps=ps: h.activation(out=stv[:, 256:384], in_=ps[:, 384:512], func=AF.Copy), r=[d_ps], w=[d_stv])
                p.dma("sp", Z["vB"][rows, :], stv[:, 256:384], r=[d_stv])
                qk_norm_rope(p, qko, ps[:, 0:384], d_ps, 384, 64, gainB, d_gainB, ropeB if lat else None, d_ropeB, qkn, d_qkn)
                for c in range(3):
                    p.op("pe", lambda h, c=c: h.transpose(ps_tr[:, c * 128:(c + 1) * 128], qkn[:, c * 128:(c + 1) * 128], ident[:]),
                         r=[d_qkn, d_ident], w=[d_ps_tr])
                p.op("act", lambda h: h.activation(out=stB[:, :, tk], in_=ps_tr[:, 0:384].rearrange("q (c t) -> q c t", c=3), func=AF.Copy),
                     r=[d_ps_tr], w=[d_stB])
                ps, d_ps = tm_mm(CC_V, 512)
                p.op("act", lambda h, ps=ps: h.activation(out=stf[:, 0:512], in_=ps[:, 0:512], func=AF.Copy), r=[d_ps], w=[d_stf])
                p.dma("sp", Z["vC"][rows, :], stf[:, 0:256], r=[d_stf])
                p.dma("sp", Z["oC"][rows, :], stf[:, 256:512], r=[d_stf])
                ps, d_ps = tm_mm(CD_Z, 272)
                p.op("act", lambda h, ps=ps: h.activation(out=stf[:, 512:784], in_=ps[:, 0:272], func=AF.Copy), r=[d_ps], w=[d_stf])
                p.dma("sp", Z["zD"][rows, :], stf[:, 512:768], r=[d_stf])
                p.dma("sp", Z["gD"][rows, :], stf[:, 768:784], r=[d_stf])
                ps, d_ps = tm_mm(CC_K, 256)
                p.op("act", lambda h, ps=ps: h.activation(out=stf[:, 0:256], in_=ps[:, 0:256], func=AF.Copy), r=[d_ps], w=[d_stf])
                p.dma("sp", Z["kC"][rows, :], stf[:, 0:256], r=[d_stf])
                ps, d_ps = tm_mm(CC_G, 16)
                p.op("dve", lambda h, ps=ps: h.tensor_copy(out=st[:, 0:16], in_=ps[:, 0:16]), r=[d_ps], w=[d_st])
                p.dma("sp", Z["gC"][rows, :], st[:, 0:16], r=[d_st])
            tb = slice(tokb, tokb + ntok)
            for c in range(2):
                p.dma("sp", Z["qTa"][c * 128:(c + 1) * 128, tb], stA[:, c, 0:ntok], r=[d_stA])
                p.dma("sp", Z["kTa"][c * 128:(c + 1) * 128, tb], stA[:, 2 + c, 0:ntok], r=[d_stA])
                p.dma("sp", Z["qTb"][c * 128:(c + 1) * 128, tb], stB[:, c, 0:ntok], r=[d_stB])
            p.dma("sp", Z["kTb"][:, tb], stB[:, 2, 0:ntok], r=[d_stB])
            for ci in range(10):
                c0 = CC_Q + ci * 128 if ci < 4 else CD_QKV + (ci - 4) * 128
                ps, d_ps = ps_fm[nfm % 2]
                so, d_so = stfm[nfm % 2]
                nfm += 1
                for k in range(8):
                    p.op("pe", lambda h, k=k, ps=ps, c0=c0: h.matmul(ps[:, 0:ntok], lhsT=w[:, k, c0:c0 + 128], rhs=xnT[:, k, 0:ntok],
                                                                     start=(k == 0), stop=(k == 7)), r=[d_xnT, d_w], w=[d_ps])
                p.op("act", lambda h, ps=ps, so=so: h.activation(out=so[:, 0:ntok], in_=ps[:, 0:ntok], func=AF.Copy), r=[d_ps], w=[d_so])
                if ci < 2:
                    dst = Z["qTc"][ci * 128:(ci + 1) * 128, tb]
                elif ci < 4:
                    dst = Z["kTc"][(ci - 2) * 128:(ci - 1) * 128, tb]
                else:
                    dst = Z["qkvT"][(ci - 4) * 128:(ci - 3) * 128, tb]
                p.dma("sp", dst, so[:, 0:ntok], r=[d_so])


def rope_table(dim):
    nf = dim // 4
    t = np.arange(NLAT)
    row = (t // 64).astype(np.float32)
    col = (t % 64).astype(np.float32)
    inv = (np.float32(10000.0) ** (-np.arange(nf, dtype=np.float32) / np.float32(nf))).astype(np.float32)
    ang = np.stack([row[:, None] * inv, col[:, None] * inv], axis=1).astype(np.float32)
    c, s = np.cos(ang).astype(np.float32), np.sin(ang).astype(np.float32)
    C = np.stack([c, c], axis=2)
    Sp = np.stack([-s, s], axis=2)
    return np.ascontiguousarray(np.stack([C.reshape(NLAT, dim), Sp.reshape(NLAT, dim)], axis=1)).astype(np.float32)


IN_SPECS = {
    "xin": ([T, D], F32), "cc": ([128, 8, 2], F32), "flags": ([128, 2], F32),
    "bmodT": ([128, 2, 48], F32), "norm1T": ([128, 2, 8], F32), "norm2T": ([128, 2, 8], F32),
    "b_mod": ([2, 6 * D], F32), "w_mod": ([2, D, 6 * D], F32), "w_in": ([2, D, N_IN], F32), "w_out": ([2, D, D], F32),
    "diff_qk_gain": ([2, 2, 32], F32), "diff_lambda": ([2, 4, 32], F32), "diff_subln": ([2, 64], F32),
    "gqa_qk_gain": ([2, 2, 64], F32), "mlstm_gate_bias": ([2, 16], F32), "mlstm_norm": ([2, 256], F32),
    "gdn_convT": ([128, 2, 6, 5], F32), "gdn_a_log": ([2, 8], F32), "gdn_dt_bias": ([2, 8], F32), "gdn_norm": ([2, 64], F32),
    "ffn_w_gate": ([D, D_FF], F32), "ffn_w_up": ([D, D_FF], F32), "ffn_w_down": ([D_FF, D], F32),
    "moe_router": ([D, NE], F32), "moe_w_gate": ([NE, D, D_FFE], F32), "moe_w_up": ([NE, D, D_FFE], F32),
    "moe_w_down": ([NE, D_FFE, D], F32),
    "ropeA": ([NLAT, 2, 32], F32), "ropeB": ([NLAT, 2, 64], F32), "ident": ([128, 128], F32),
    "cmask": ([128, 2, 128], F32),
}


def host_inputs(inp, core):
    b, hh = core // 2, core % 2
    f = lambda a: np.ascontiguousarray(np.asarray(a, dtype=np.float32))
    colsT = lambda v, n: f(np.asarray(v).reshape(v.shape[0], n, 128).transpose(2, 0, 1))
    m = {}
    m["xin"] = f(np.concatenate([inp["ctx"][b], inp["x"][b]], axis=0))
    m["cc"] = f(np.stack([np.asarray(inp["c"][b]).reshape(8, 128).T, np.asarray(inp["c_ctx"]).reshape(8, 128).T], axis=2))
    fl = np.zeros((128, 2), np.float32)
    fl[:, hh] = 1.0
    m["flags"] = fl
    m["bmodT"] = colsT(inp["b_mod"], 48)
    m["norm1T"] = colsT(inp["norm1"], 8)
    m["norm2T"] = colsT(inp["norm2"], 8)
    for k in ("b_mod", "w_mod", "w_in", "w_out", "diff_qk_gain", "diff_lambda", "diff_subln", "gqa_qk_gain", "mlstm_norm",
              "gdn_norm"):
        m[k] = f(inp[k])
    m["mlstm_gate_bias"] = f(np.asarray(inp["mlstm_gate_bias"]).reshape(2, 16))
    m["gdn_a_log"] = f(np.asarray(inp["gdn_a_log"]).reshape(2, 8))
    m["gdn_dt_bias"] = f(np.asarray(inp["gdn_dt_bias"]).reshape(2, 8))
    m["gdn_convT"] = f(np.asarray(inp["gdn_conv"]).reshape(2, 5, 6, 128).transpose(3, 0, 2, 1))
    m["ffn_w_gate"] = f(inp["ffn_w_gate"][0])
    m["ffn_w_up"] = f(inp["ffn_w_up"][0])
    m["ffn_w_down"] = f(inp["ffn_w_down"][0])
    m["moe_router"] = f(inp["moe_router"][0])
    m["moe_w_gate"] = f(inp["moe_w_gate"][0])
    m["moe_w_up"] = f(inp["moe_w_up"][0])
    m["moe_w_down"] = f(inp["moe_w_down"][0])
    m["ropeA"] = rope_table(32)
    m["ropeB"] = rope_table(64)
    m["ident"] = np.eye(128, dtype=np.float32)
    i = np.arange(128)
    low = (i[:, None] >= i[None, :]).astype(np.float32)
    m["cmask"] = f(np.stack([low, low.T], axis=1))
    return m


SCRATCH = {
    "xres": ([T, D], F32),
    "qTa": ([256, T], BF16), "kTa": ([256, T], BF16), "vA": ([T, 256], BF16),
    "qTb": ([256, T], BF16), "kTb": ([128, T], BF16), "vB": ([T, 128], BF16),
    "qTc": ([256, T], F32), "kTc": ([256, T], F32), "vC": ([T, 256], F32), "oC": ([T, 256], F32), "gC": ([T, 16], F32),
    "qkvT": ([768, T], F32), "zD": ([T, 256], F32), "gD": ([T, 16], F32), "kC": ([T, 256], F32),
    "gqT": ([256, T], F32), "gkT": ([256, T], F32), "gk": ([T, 256], F32), "gv": ([T, 256], F32),
    "y": ([T, D], F32),
}


def build(debug=None, upto="all"):
    p = Prog()
    nc = p.nc
    I = {k: nc.dram_tensor(k, sh, dt, kind="ExternalInput").ap() for k, (sh, dt) in IN_SPECS.items()}
    Z = {}
    for k, (sh, dt) in SCRATCH.items():
        kind = "ExternalOutput" if (debug and k in debug) else "Internal"
        Z[k] = nc.dram_tensor("z_" + k, sh, dt, kind=kind).ap()
    out = nc.dram_tensor("out", [NLAT // 2, D], F32, kind="ExternalOutput").ap()
    with SB(p) as gsb:
        S = {"modT": gsb.t((128, 48, 2)), "gsh": gsb.t((128, 4, 8, 2)), "grow": gsb.t((128, 2, 2, D)), "ident": gsb.t((128, 128))}
        p.dma("sp", S["ident"][0][:], I["ident"][:, :], w=[S["ident"][1]])
        epst, p.d_eps = gsb.t((128, 1))
        p.epst = epst
        p.op("dve", lambda h: h.memset(epst[:], EPS), w=[p.d_eps])
        if debug and "modT" in debug:
            dbg_mod = nc.dram_tensor("z_modT", [128, 48, 2], F32, kind="ExternalOutput").ap()
            dbg_grow = nc.dram_tensor("z_grow", [128, 2, 2, D], F32, kind="ExternalOutput").ap()
        for l in range(2):
            phase_mod(p, l, I, S)
            if debug and "modT" in debug and l == 0:
                p.dma("sp", dbg_mod[:, :, :], S["modT"][0][:], r=[S["modT"][1]])
                p.dma("sp", dbg_grow[:, :, :, :], S["grow"][0][:], r=[S["grow"][1]])
            phase_inproj(p, l, I, S, Z, I["xin"] if l == 0 else Z["xres"])
            if upto == "inproj":
                break
            if "noattn" not in upto:
                phase_attn(p, l, I, S, Z, l == 0)
            if upto == "attn":
                break
            phase_chunk(p, l, I, S, Z, do_c=("noc" not in upto), do_d=("nod" not in upto), upto=upto)
            if upto.startswith("chunk"):
                break
            phase_outproj(p, l, I, S, Z, I["xin"] if l == 0 else Z["xres"])
            if l == 0:
                phase_ffn_dense(p, l, I, S, Z)
                if upto == "layer0":
                    break
            else:
                phase_moe(p, l, I, S, Z, out)
        p.barrier()
    return p


def bcast_load(p, sb, src_row_ap, n, name="bc"):
    t, d = sb.t((128, n), F32, name)
    p.dma("sp", t[:], src_row_ap.partition_broadcast(128), w=[d])
    return t, d


def phase_attn(p, l, I, S, Z, with_ctx):
    ident, d_ident = S["ident"]
    lambda_init = 0.8 - 0.6 * math.exp(-0.3 * l)
    with SB(p) as sb:
        lamv, d_lamv = sb.t((128, 4, 32))
        p.dma("sp", lamv[:], I["diff_lambda"][l:l + 1, :, :].partition_broadcast(128), w=[d_lamv])
        cst, d_cst = sb.t((128, 16))
        tmp32, d_tmp32 = sb.t((128, 2, 32))
        p.op("dve", lambda h: h.tensor_tensor(out=tmp32[:], in0=lamv[:, 0:4:2, :], in1=lamv[:, 1:4:2, :], op=ALU.mult),
             r=[d_lamv], w=[d_tmp32])
        p.op("dve", lambda h: h.tensor_reduce(out=cst[:, 0:2], in_=tmp32[:], axis=AX.X, op=ALU.add), r=[d_tmp32], w=[d_cst])
        p.op("act", lambda h: h.activation(out=cst[:, 2:4], in_=cst[:, 0:2], func=AF.Exp), r=[d_cst], w=[d_cst])
        p.op("dve", lambda h: h.tensor_tensor(out=cst[:, 4:5], in0=cst[:, 3:4], in1=cst[:, 2:3], op=ALU.subtract), r=[d_cst], w=[d_cst])
        p.op("dve", lambda h: h.tensor_scalar(out=cst[:, 4:5], in0=cst[:, 4:5], scalar1=-lambda_init, scalar2=None, op0=ALU.add),
             r=[d_cst], w=[d_cst])
        gA, d_gA = sb.t((128, 2, 32))
        gB, d_gB = sb.t((128, 2, 64))
        p.dma("sp", gA[:], I["diff_qk_gain"][l:l + 1, :, :].partition_broadcast(128), w=[d_gA])
        p.dma("sp", gB[:], I["gqa_qk_gain"][l:l + 1, :, :].partition_broadcast(128), w=[d_gB])
        p.op("dve", lambda h: h.tensor_reduce(out=cst[:, 6:8], in_=gA[:], axis=AX.X, op=ALU.max, apply_absolute_value=True),
             r=[d_gA], w=[d_cst])
        p.op("dve", lambda h: h.tensor_reduce(out=cst[:, 8:10], in_=gB[:], axis=AX.X, op=ALU.max, apply_absolute_value=True),
             r=[d_gB], w=[d_cst])
        p.op("dve", lambda h: h.scalar_tensor_tensor(out=cst[:, 10:11], in0=cst[:, 6:7], scalar=-math.sqrt(32.0), in1=cst[:, 7:8],
                                                     op0=ALU.mult, op1=ALU.mult), r=[d_cst], w=[d_cst])
        p.op("dve", lambda h: h.scalar_tensor_tensor(out=cst[:, 11:12], in0=cst[:, 8:9], scalar=-8.0, in1=cst[:, 9:10],
                                                     op0=ALU.mult, op1=ALU.mult), r=[d_cst], w=[d_cst])
        subg, d_subg = bcast_load(p, sb, I["diff_subln"][l:l + 1, :], 64)
        p.op("dve", lambda h: h.tensor_scalar(out=subg[:], in0=subg[:], scalar1=1.0 - lambda_init, scalar2=None, op0=ALU.mult),
             r=[d_subg], w=[d_subg])
        kT, d_kT = sb.t((64, T), BF16, "kT")
        va, d_va = sb.t((128, NT, 65), BF16, "vaug")
        qTs = [sb.t((64, 512), BF16, "qT") for _ in range(2)]
        pTs = [sb.t((128, 512), BF16, "pT") for _ in range(3)]
        ps_s = [sb.ps() for _ in range(3)]
        ps_o = [sb.ps() for _ in range(2)]
        ps_t, d_ps_t = sb.ps()
        osb = [sb.t((65, 512), F32, "osb") for _ in range(2)]
        on = [sb.t((128, 64), F32, "on") for _ in range(2)]
        od, d_od = sb.t((128, 64))
        junk, d_junk = sb.t((128, 64))
        st, d_st = sb.t((128, 4))
        ystage, d_ys = sb.t((128, 4, 64))
        rc, d_rc = sb.t((128, 2))
        qblocks = ([(0, 256, [0, 1])] if with_ctx else []) + [(256 + 512 * i, 512, list(range(NT))) for i in range(8)]
        cnt = {"s": 0, "q": 0}

        def run_head(kind, qsrc_rows, nmaps, scale, negB, ycol):
            for (q0, qn, kts) in qblocks:
                qT, d_qT = qTs[cnt["q"] % 2]
                cnt["q"] += 1
                p.dma("sp", qT[:, 0:qn], qsrc_rows[:, q0:q0 + qn], w=[d_qT])
                for j in range(nmaps):
                    kr = slice(32 * j, 32 * j + 32) if kind == "A" else slice(0, 64)
                    po, d_po = ps_o[j]
                    LA = 2
                    pend = []
                    for ki in range(len(kts) + LA):
                        if ki < len(kts):
                            kt = kts[ki]
                            i = cnt["s"]
                            cnt["s"] += 1
                            ps, d_ps = ps_s[i % 3]
                            pT, d_pT = pTs[i % 3]
                            p.op("pe", lambda h, ps=ps, kt=kt, kr=kr, qT=qT: h.matmul(ps[:, 0:qn], lhsT=kT[kr, kt * 128:(kt + 1) * 128],
                                                                                     rhs=qT[kr, 0:qn], start=True, stop=True),
                                 r=[d_kT, d_qT], w=[d_ps])
                            p.op("act", lambda h, ps=ps, pT=pT: h.activation(out=pT[:, 0:qn], in_=ps[:, 0:qn], func=AF.Exp, scale=scale,
                                                                              bias=negB), r=[d_ps, d_cst], w=[d_pT])
                            pend.append((kt, ki, pT, d_pT))
                        if ki >= LA:
                            kt2, ki2, pT2, d_pT2 = pend.pop(0)
                            p.op("pe", lambda h, po=po, pT2=pT2, kt2=kt2, ki2=ki2: h.matmul(po[0:65, 0:qn], lhsT=va[:, kt2, :], rhs=pT2[:, 0:qn],
                                                                                          start=(ki2 == 0), stop=(ki2 == len(kts) - 1)),
                                 r=[d_va, d_pT2], w=[d_po])
                    ob, d_ob = osb[j]
                    p.op("act", lambda h, ob=ob, po=po: h.activation(out=ob[:, 0:qn], in_=po[0:65, 0:qn], func=AF.Copy), r=[d_po], w=[d_ob])
                nsub = qn // 128
                for s_ in range(nsub):
                    for j in range(nmaps):
                        ob, d_ob = osb[j]
                        p.op("pe", lambda h, ob=ob, j=j, s_=s_: h.transpose(ps_t[:, j * 128:j * 128 + 65], ob[0:65, s_ * 128:(s_ + 1) * 128],
                                                                            ident[0:65, 0:65]), r=[d_ob, d_ident], w=[d_ps_t])
                    for j in range(nmaps):
                        o_, d_o = on[j]
                        p.op("dve", lambda h, j=j: h.reciprocal(out=rc[:, j:j + 1], in_=ps_t[:, j * 128 + 64:j * 128 + 65]),
                             r=[d_ps_t], w=[d_rc])
                        p.op("dve", lambda h, o_=o_, j=j: h.tensor_scalar(out=o_[:], in0=ps_t[:, j * 128:j * 128 + 64],
                                                                          scalar1=rc[:, j:j + 1], scalar2=None,
                                                                          op0=ALU.mult), r=[d_ps_t, d_rc], w=[d_o])
                    if kind == "A":
                        p.op("dve", lambda h: h.scalar_tensor_tensor(out=od[:], in0=on[1][0][:], scalar=cst[:, 4:5], in1=on[0][0][:],
                                                                     op0=ALU.mult, op1=ALU.add), r=[on[0][1], on[1][1], d_cst], w=[d_od])
                        p.op("act", lambda h: h.activation(out=junk[:], in_=od[:], func=AF.Square, accum_out=st[:, 0:1]),
                             r=[d_od], w=[d_junk, d_st])
                        rstd(p, st[:, 2:3], st[:, 0:1], st[:, 1:2], 1.0 / 64, [d_st])
                        p.op("dve", lambda h, s_=s_: h.scalar_tensor_tensor(out=ystage[:, s_, :], in0=od[:], scalar=st[:, 2:3], in1=subg[:],
                                                                            op0=ALU.mult, op1=ALU.mult), r=[d_od, d_st, d_subg], w=[d_ys])
                    else:
                        p.op("dve", lambda h, s_=s_: h.tensor_copy(out=ystage[:, s_, :], in_=on[0][0][:]), r=[on[0][1]], w=[d_ys])
                p.dma("sp", Z["y"][q0:q0 + qn, ycol:ycol + 64].rearrange("(s q) d -> q s d", q=128), ystage[:, 0:nsub, :], r=[d_ys])

        def load_kv(ksrc_rows, vsrc_cols):
            p.dma("sp", kT[:, :], ksrc_rows, w=[d_kT])
            p.dma("sp", va[:, :, 0:64], vsrc_cols.rearrange("(n q) d -> q n d", q=128), w=[d_va])
            p.op("dve", lambda h: h.memset(va[:, :, 64:65], 1.0), w=[d_va])

        for hd in range(4):
            load_kv(Z["kTa"][64 * hd:64 * hd + 64, :], Z["vA"][:, 64 * hd:64 * hd + 64])
            run_head("A", Z["qTa"][64 * hd:64 * hd + 64, :], 2, 1.0 / math.sqrt(32.0), cst[:, 10:11], 64 * hd)
        for kv in range(2):
            load_kv(Z["kTb"][64 * kv:64 * kv + 64, :], Z["vB"][:, 64 * kv:64 * kv + 64])
            for g in (2 * kv, 2 * kv + 1):
                run_head("B", Z["qTb"][64 * g:64 * g + 64, :], 1, 0.125, cst[:, 11:12], 256 + 64 * g)


ORDER = [list(range(NT)), [1, 0] + list(range(NT - 1, 1, -1))]
NGC = 40


class PsumPool:
    def __init__(self, sb, nbanks=8):
        self.q = []
        banks = [sb.ps() for b in range(nbanks)]
        for k in range(4):
            for (t, d) in banks:
                self.q.append((t[:, k * 128:(k + 1) * 128], d))
        self.i = 0

    def get(self):
        r = self.q[self.i % len(self.q)]
        self.i += 1
        return r


def phase_gdn_prep(p, l, I, S, Z):
    ident, d_ident = S["ident"]
    W = 2 + 256 + 4 + 4096 + 2
    with SB(p) as sb:
        cw, d_cw = sb.t((128, 6, 5))
        p.dma("sp", cw[:], I["gdn_convT"][:, l, :, :], w=[d_cw])
        bones, d_bones = sb.t((128, 128))
        p.op("dve", lambda h: h.memset(bones[:], 0.0), w=[d_bones])
        p.op("dve", lambda h: h.memset(bones[0:64, 0:64], 1.0), w=[d_bones])
        p.op("dve", lambda h: h.memset(bones[64:128, 64:128], 1.0), w=[d_bones])
        X, d_X = sb.t((128, W))
        acc, d_acc = sb.t((128, W))
        sq, d_sq = sb.t((128, 512))
        rs, d_rs = sb.t((128, 512))
        pss = [sb.ps() for _ in range(2)]
        pst = [sb.ps() for _ in range(2)]
        tst = [sb.t((128, 128)) for _ in range(2)]
        p.op("dve", lambda h: h.memset(X[:], 0.0), w=[d_X])
        nb = 0
        for fc in range(6):
            p.dma("sp", X[:, 2:258], Z["qkvT"][fc * 128:(fc + 1) * 128, 0:256], w=[d_X])
            p.dma("sp", X[:, 262:4358], Z["qkvT"][fc * 128:(fc + 1) * 128, 256:T], w=[d_X])
            lo, hi = 2, 4358
            p.op("dve", lambda h, fc=fc: h.tensor_scalar(out=acc[:, lo:hi], in0=X[:, lo - 2:hi - 2], scalar1=cw[:, fc, 0:1], scalar2=None,
                                                         op0=ALU.mult), r=[d_X, d_cw], w=[d_acc])
            for tap in range(1, 5):
                eng = "dve"
                p.op(eng, lambda h, fc=fc, tap=tap: h.scalar_tensor_tensor(out=acc[:, lo:hi], in0=X[:, lo + tap - 2:hi + tap - 2],
                                                                             scalar=cw[:, fc, tap:tap + 1], in1=acc[:, lo:hi],
                                                                             op0=ALU.mult, op1=ALU.add), r=[d_X, d_cw, d_acc], w=[d_acc])
            p.op("act", lambda h: h.activation(out=X[:, lo:hi], in_=acc[:, lo:hi], func=AF.Sigmoid), r=[d_acc], w=[d_X])
            p.op("dve", lambda h: h.tensor_tensor(out=acc[:, lo:hi], in0=acc[:, lo:hi], in1=X[:, lo:hi], op=ALU.mult), r=[d_X, d_acc], w=[d_acc])
            segs = [(2, 256, 0)] + [(262 + 512 * i, 512, 256 + 512 * i) for i in range(8)]
            if fc < 4:
                for (c0, n, t0) in segs:
                    ps, d_ps = pss[nb % 2]
                    nb += 1
                    p.op("act", lambda h: h.activation(out=sq[:, 0:n], in_=acc[:, c0:c0 + n], func=AF.Square), r=[d_acc], w=[d_sq])
                    p.op("pe", lambda h, ps=ps: h.matmul(ps[:, 0:n], lhsT=bones[:], rhs=sq[:, 0:n], start=True, stop=True),
                         r=[d_bones, d_sq], w=[d_ps])
                    p.op("act", lambda h, ps=ps: h.activation(out=rs[:, 0:n], in_=ps[:, 0:n], func=AF.Sqrt, scale=1.0, bias=p.eps_ap(EPS, 128)),
                         r=[d_ps, p.d_eps], w=[d_rs])
                    p.op("dve", lambda h: h.reciprocal(out=rs[:, 0:n], in_=rs[:, 0:n]), r=[d_rs], w=[d_rs])
                    p.op("dve", lambda h: h.scalar_tensor_tensor(out=acc[:, c0:c0 + n], in0=acc[:, c0:c0 + n], scalar=(0.125 if fc < 2 else 1.0),
                                                                 in1=rs[:, 0:n], op0=ALU.mult, op1=ALU.mult), r=[d_acc, d_rs], w=[d_acc])
                dst = Z["gqT"] if fc < 2 else Z["gkT"]
                r0 = (fc % 2) * 128
                p.dma("sp", dst[r0:r0 + 128, 0:256], acc[:, 2:258], r=[d_acc])
                p.dma("sp", dst[r0:r0 + 128, 256:T], acc[:, 262:4358], r=[d_acc])
            if fc >= 2:
                dst = Z["gk"] if fc < 4 else Z["gv"]
                r0 = (fc % 2) * 128
                for tt in range(NT):
                    c0 = 2 + tt * 128 if tt < 2 else 262 + (tt - 2) * 128
                    ps, d_ps = pst[tt % 2]
                    ts_, d_ts = tst[tt % 2]
                    p.op("pe", lambda h, ps=ps, c0=c0: h.transpose(ps[:, 0:128], acc[:, c0:c0 + 128], ident[:]), r=[d_acc, d_ident], w=[d_ps])
                    p.op("act", lambda h, ps=ps, ts_=ts_: h.activation(out=ts_[:], in_=ps[:, 0:128], func=AF.Copy), r=[d_ps], w=[d_ts])
                    p.dma("sp", dst[tt * 128:(tt + 1) * 128, r0:r0 + 128], ts_[:], r=[d_ts])


def softplus_parts(p, z, d_z, tmp, d_tmp, n):
    p.op("dve", lambda h: h.tensor_scalar(out=tmp[:, n:2 * n], in0=z, scalar1=-1.0, scalar2=None, op0=ALU.mult), r=[d_z], w=[d_tmp])
    p.op("dve", lambda h: h.tensor_tensor(out=tmp[:, n:2 * n], in0=tmp[:, n:2 * n], in1=z, op=ALU.min), r=[d_z, d_tmp], w=[d_tmp])
    p.op("act", lambda h: h.activation(out=tmp[:, n:2 * n], in_=tmp[:, n:2 * n], func=AF.Exp), r=[d_tmp], w=[d_tmp])
    p.op("act", lambda h: h.activation(out=tmp[:, 0:n], in_=tmp[:, n:2 * n], func=AF.Ln, scale=1.0, bias=p.ones1[:, 0:1]),
         r=[d_tmp, p.d_ones1], w=[d_tmp])


def phase_gates(p, l, I, S, Z, tab, d_tab, cm, d_cm, ones, d_ones):
    ident, d_ident = S["ident"]
    with SB(p) as sb:
        pp = PsumPool(sb, 4)
        biasC, d_biasC = bcast_load(p, sb, I["mlstm_gate_bias"][l:l + 1, :], 16)
        dtb, d_dtb = bcast_load(p, sb, I["gdn_dt_bias"][l:l + 1, :], 8)
        nea, d_nea = bcast_load(p, sb, I["gdn_a_log"][l:l + 1, :], 8)
        p.op("act", lambda h: h.activation(out=nea[:], in_=nea[:], func=AF.Exp), r=[d_nea], w=[d_nea])
        p.op("dve", lambda h: h.tensor_scalar(out=nea[:], in0=nea[:], scalar1=-1.0, scalar2=None, op0=ALU.mult), r=[d_nea], w=[d_nea])
        gts = [sb.t((128, 32)) for _ in range(2)]
        for d in range(2):
            Bprev, d_B = sb.t((128, 4))
            R, d_R = sb.t((128, 4))
            p.op("dve", lambda h: h.memset(Bprev[:], 0.0), w=[d_B])
            p.op("dve", lambda h: h.memset(R[:], 0.0), w=[d_R])
            for s_, tt in enumerate(ORDER[d]):
                g, d_g = gts[s_ % 2]
                p.dma("sp", g[:, 0:16], Z["gC"][tt * 128:(tt + 1) * 128, :], w=[d_g])
                p.dma("sp", g[:, 16:32], Z["gD"][tt * 128:(tt + 1) * 128, :], w=[d_g])
                wk, d_wk = S["gwk"][s_ % 2]
                lhs_cum = cm[:, 1 - d, :]
                T_ = lambda a, b: tab[:, tt, d, a:b]
                xf = wk[:, 0:4]
                ig = wk[:, 4:8]
                p.op("dve", lambda h: h.tensor_tensor(out=xf, in0=g[:, 8 * d + 4:8 * d + 8], in1=biasC[:, 8 * d + 4:8 * d + 8], op=ALU.add),
                     r=[d_g, d_biasC], w=[d_wk])
                p.op("dve", lambda h: h.tensor_tensor(out=ig, in0=g[:, 8 * d:8 * d + 4], in1=biasC[:, 8 * d:8 * d + 4], op=ALU.add),
                     r=[d_g, d_biasC], w=[d_wk])
                softplus_parts(p, xf, d_wk, wk[:, 8:16], d_wk, 4)
                logf = wk[:, 16:20]
                p.op("dve", lambda h: h.scalar_tensor_tensor(out=logf, in0=xf, scalar=0.0, in1=wk[:, 8:12], op0=ALU.min, op1=ALU.subtract),
                     r=[d_wk], w=[d_wk])
                pcs, d_pcs = pp.get()
                ptot, d_ptot = pp.get()
                p.op("pe", lambda h: h.matmul(pcs[:, 0:4], lhsT=lhs_cum, rhs=logf, start=True, stop=True), r=[d_cm, d_wk], w=[d_pcs])
                p.op("pe", lambda h: h.matmul(ptot[:, 0:4], lhsT=ones[:], rhs=logf, start=True, stop=True), r=[d_ones, d_wk], w=[d_ptot])
                Bv = wk[:, 20:24]
                av = wk[:, 24:28]
                p.op("dve", lambda h: h.tensor_tensor(out=Bv, in0=pcs[:, 0:4], in1=Bprev[:], op=ALU.add), r=[d_pcs, d_B], w=[d_wk])
                p.op("dve", lambda h: h.tensor_tensor(out=av, in0=ig, in1=Bv, op=ALU.subtract), r=[d_wk], w=[d_wk])
                p.op("dve", lambda h: h.tensor_tensor(out=Bprev[:], in0=ptot[:, 0:4], in1=Bprev[:], op=ALU.add), r=[d_ptot, d_B], w=[d_B])
                ptr, d_ptr = pp.get()
                p.op("pe", lambda h: h.transpose(ptr[0:4, 0:128], av, ident[:]), r=[d_wk, d_ident], w=[d_ptr])
                am, d_am = S["gam4"]
                p.op("dve", lambda h: h.tensor_reduce(out=am[0:4, 0:1], in_=ptr[0:4, 0:128], axis=AX.X, op=ALU.max), r=[d_ptr], w=[d_am])
                p.op("dve", lambda h: h.tensor_scalar(out=am[0:4, 4:8], in0=ident[0:4, 0:4], scalar1=am[0:4, 0:1], scalar2=None, op0=ALU.mult),
                     r=[d_am, d_ident], w=[d_am])
                pam, d_pam = pp.get()
                p.op("pe", lambda h: h.matmul(pam[:, 0:4], lhsT=ones[0:4, :], rhs=am[0:4, 4:8], start=True, stop=True),
                     r=[d_ones, d_am], w=[d_pam])
                Mc = wk[:, 28:32]
                p.op("dve", lambda h: h.tensor_tensor(out=Mc, in0=pam[:, 0:4], in1=R[:], op=ALU.max), r=[d_pam, d_R], w=[d_wk])
                p.op("dve", lambda h: h.tensor_tensor(out=wk[:, 32:36], in0=R[:], in1=Mc, op=ALU.subtract), r=[d_R, d_wk], w=[d_wk])
                p.op("dve", lambda h: h.tensor_tensor(out=wk[:, 36:40], in0=av, in1=Mc, op=ALU.subtract), r=[d_wk], w=[d_wk])
                p.op("dve", lambda h: h.tensor_tensor(out=wk[:, 40:44], in0=Bv, in1=Mc, op=ALU.add), r=[d_wk], w=[d_wk])
                p.op("dve", lambda h: h.tensor_copy(out=R[:], in_=Mc), r=[d_wk], w=[d_R])
                p.op("act", lambda h: h.activation(out=T_(8, 12), in_=wk[:, 32:36], func=AF.Exp), r=[d_wk], w=[d_tab])
                p.op("act", lambda h: h.activation(out=T_(0, 4), in_=wk[:, 36:40], func=AF.Exp), r=[d_wk], w=[d_tab])
                p.op("act", lambda h: h.activation(out=T_(4, 8), in_=wk[:, 40:44], func=AF.Exp, scale=-1.0), r=[d_wk], w=[d_tab])
                z = wk[:, 44:48]
                p.op("dve", lambda h: h.tensor_tensor(out=z, in0=g[:, 16 + 8 * d + 4:16 + 8 * d + 8], in1=dtb[:, 4 * d:4 * d + 4], op=ALU.add),
                     r=[d_g, d_dtb], w=[d_wk])
                softplus_parts(p, z, d_wk, wk[:, 48:56], d_wk, 4)
                gg = wk[:, 56:60]
                p.op("dve", lambda h: h.scalar_tensor_tensor(out=gg, in0=z, scalar=0.0, in1=wk[:, 48:52], op0=ALU.max, op1=ALU.add),
                     r=[d_wk], w=[d_wk])
                p.op("dve", lambda h: h.tensor_tensor(out=gg, in0=gg, in1=nea[:, 4 * d:4 * d + 4], op=ALU.mult), r=[d_wk, d_nea], w=[d_wk])
                p.op("act", lambda h: h.activation(out=T_(12, 16), in_=g[:, 16 + 8 * d:16 + 8 * d + 4], func=AF.Sigmoid), r=[d_g], w=[d_tab])
                p.op("dve", lambda h: h.tensor_scalar(out=T_(16, 20), in0=T_(12, 16), scalar1=-1.0, scalar2=None, op0=ALU.mult),
                     r=[d_tab], w=[d_tab])
                pgm, d_pgm = pp.get()
                pgl, d_pgl = pp.get()
                p.op("pe", lambda h: h.matmul(pgm[:, 0:4], lhsT=lhs_cum, rhs=gg, start=True, stop=True), r=[d_cm, d_wk], w=[d_pgm])
                p.op("pe", lambda h: h.matmul(pgl[:, 0:4], lhsT=ones[:], rhs=gg, start=True, stop=True), r=[d_ones, d_wk], w=[d_pgl])
                p.op("dve", lambda h: h.tensor_copy(out=T_(20, 24), in_=pgm[:, 0:4]), r=[d_pgm], w=[d_tab])
                p.op("dve", lambda h: h.tensor_tensor(out=wk[:, 60:64], in0=pgl[:, 0:4], in1=T_(20, 24), op=ALU.subtract),
                     r=[d_pgl, d_tab], w=[d_wk])
                p.op("act", lambda h: h.activation(out=T_(24, 28), in_=T_(20, 24), func=AF.Exp), r=[d_tab], w=[d_tab])
                p.op("act", lambda h: h.activation(out=T_(28, 32), in_=wk[:, 60:64], func=AF.Exp), r=[d_wk], w=[d_tab])
                p.op("act", lambda h: h.activation(out=T_(32, 36), in_=pgl[:, 0:4], func=AF.Exp), r=[d_pgl], w=[d_tab])
                p.op("dve", lambda h: h.tensor_tensor(out=T_(36, 40), in0=T_(12, 16), in1=T_(24, 28), op=ALU.mult), r=[d_tab], w=[d_tab])


def phase_chunk(p, l, I, S, Z, do_c=True, do_d=True, upto=""):
    ident, d_ident = S["ident"]
    phase_gdn_prep(p, l, I, S, Z)
    if "stopprep" in upto:
        return
    with SB(p) as sb:
        tab, d_tab = sb.t((128, NT, 2, NGC), F32, "gtab")
        cm, d_cm = sb.t((128, 2, 128), F32, "cmask")
        p.dma("sp", cm[:], I["cmask"][:, :, :], w=[d_cm])
        ones, d_ones = sb.t((128, 128))
        p.op("dve", lambda h: h.memset(ones[:], 1.0), w=[d_ones])
        ones1, p.d_ones1 = sb.t((128, 1))
        p.ones1 = ones1
        p.op("dve", lambda h: h.memset(ones1[:], 1.0), w=[p.d_ones1])
        S["gwk"] = [sb.t((128, 64)) for _ in range(2)]
        S["gam4"] = sb.t((8, 8))
        strict, d_strict = sb.t((128, 2, 128))
        for d in range(2):
            p.op("dve", lambda h, d=d: h.tensor_tensor(out=strict[:, d, :], in0=cm[:, d, :], in1=ident[:], op=ALU.subtract),
                 r=[d_cm, d_ident], w=[d_strict])
        phase_gates(p, l, I, S, Z, tab, d_tab, cm, d_cm, ones, d_ones)
        if "stopgates" in upto:
            return
        acc, d_acc = sb.t((128, NT, 256), F32, "hacc")
        if do_c:
            p.op("dve", lambda h: h.memset(acc[:], 0.0), w=[d_acc])
            chunk_c(p, l, I, S, Z, sb, tab, d_tab, cm, d_cm, acc, d_acc)
            p.barrier()
        if do_d:
            p.op("dve", lambda h: h.memset(acc[:], 0.0), w=[d_acc])
            chunk_d(p, l, I, S, Z, sb, tab, d_tab, cm, d_cm, strict, d_strict, ones, d_ones, acc, d_acc)


def chunk_c(p, l, I, S, Z, sbo, tab, d_tab, cm, d_cm, acc, d_acc):
    ident, d_ident = S["ident"]
    with SB(p) as sb:
        pp = PsumPool(sb, 6)
        qT, d_qT = sb.t((64, T), F32, "cqT")
        kT, d_kT = sb.t((64, T), F32, "ckT")
        ktm, d_ktm = sb.t((128, NT, 64), F32, "cktm")
        vtm, d_vtm = sb.t((128, NT, 66), F32, "cvtm")
        Cs = [sb.t((64, 66), F32, "cst") for _ in range(2)]
        Cd = [sb.t((64, 66), F32, "cdec") for _ in range(2)]
        PTs = [sb.t((128, 128), F32, "cPT") for _ in range(3)]
        vps = [sb.t((128, 66), F32, "cvp") for _ in range(3)]
        rcs = [sb.t((128, 2), F32, "crc") for _ in range(3)]
        for hd in range(4):
            p.dma("sp", qT[:, :], Z["qTc"][64 * hd:64 * hd + 64, :], w=[d_qT])
            p.dma("sp", kT[:, :], Z["kTc"][64 * hd:64 * hd + 64, :], w=[d_kT])
            p.dma("sp", ktm[:, :, :], Z["kC"][:, 64 * hd:64 * hd + 64].rearrange("(n q) d -> q n d", q=128), w=[d_ktm])
            p.dma("sp", vtm[:, :, 0:64], Z["vC"][:, 64 * hd:64 * hd + 64].rearrange("(n q) d -> q n d", q=128), w=[d_vtm])
            p.op("dve", lambda h: h.memset(vtm[:, :, 64:65], 1.0), w=[d_vtm])
            p.op("dve", lambda h: h.memset(vtm[:, :, 65:66], 0.0), w=[d_vtm])
            p.op("pool", lambda h: h.tensor_scalar(out=ktm[:, :, :], in0=ktm[:, :, :], scalar1=0.125, scalar2=None, op0=ALU.mult),
                 r=[d_ktm], w=[d_ktm])
            for d in range(2):
                p.op("dve", lambda h, d=d: h.memset(Cs[d][0][:], 0.0), w=[Cs[d][1]])
            for s_ in range(NT):
                for d in range(2):
                    tt = ORDER[d][s_]
                    tk = slice(tt * 128, (tt + 1) * 128)
                    Cst, d_Cst = Cs[d]
                    Cdc, d_Cdc = Cd[d]
                    i3 = (2 * s_ + d) % 3
                    PT, d_PT = PTs[i3]
                    vp, d_vp = vps[i3]
                    rc, d_rc = rcs[i3]
                    pst, d_pst = pp.get()
                    p.op("pe", lambda h, pst=pst, tk=tk: h.matmul(pst[:, 0:128], lhsT=kT[:, tk], rhs=qT[:, tk], start=True, stop=True),
                         r=[d_kT, d_qT], w=[d_pst])
                    p.op("dve", lambda h, pst=pst, PT=PT, d=d: h.scalar_tensor_tensor(out=PT[:], in0=pst[:, 0:128], scalar=0.125,
                                                                                   in1=cm[:, 1 - d, :], op0=ALU.mult, op1=ALU.mult),
                         r=[d_pst, d_cm], w=[d_PT])
                    p.op("pool", lambda h, vp=vp, tt=tt, d=d, hd=hd: h.tensor_scalar(out=vp[:], in0=vtm[:, tt, :],
                                                                                  scalar1=tab[:, tt, d, hd:hd + 1], scalar2=None,
                                                                                  op0=ALU.mult), r=[d_vtm, d_tab], w=[d_vp])
                    p.op("dve", lambda h, Cdc=Cdc, Cst=Cst, tt=tt, d=d, hd=hd: h.tensor_scalar(out=Cdc[:], in0=Cst[:],
                                                                                            scalar1=tab[0:64, tt, d, 8 + hd:9 + hd],
                                                                                            scalar2=None, op0=ALU.mult),
                         r=[d_Cst, d_tab], w=[d_Cdc])
                    po, d_po = pp.get()
                    p.op("pe", lambda h, po=po, PT=PT, vp=vp: h.matmul(po[:, 0:66], lhsT=PT[:], rhs=vp[:], start=True, stop=False),
                         r=[d_PT, d_vp], w=[d_po])
                    p.op("pe", lambda h, po=po, Cdc=Cdc, tk=tk: h.matmul(po[:, 0:66], lhsT=qT[:, tk], rhs=Cdc[:], start=False, stop=True),
                         r=[d_qT, d_Cdc], w=[d_po])
                    pc, d_pc = pp.get()
                    p.op("pe", lambda h, pc=pc, tt=tt, vp=vp: h.matmul(pc[0:64, 0:66], lhsT=ktm[:, tt, :], rhs=vp[:], start=True, stop=True),
                         r=[d_ktm, d_vp], w=[d_pc])
                    p.op("dve", lambda h, pc=pc, Cst=Cst, Cdc=Cdc: h.tensor_tensor(out=Cst[:], in0=pc[0:64, 0:66], in1=Cdc[:], op=ALU.add),
                         r=[d_pc, d_Cdc], w=[d_Cst])
                    p.op("act", lambda h, po=po, rc=rc: h.activation(out=rc[:, 0:1], in_=po[:, 64:65], func=AF.Abs), r=[d_po], w=[d_rc])
                    p.op("dve", lambda h, rc=rc, tt=tt, d=d, hd=hd: h.tensor_tensor(out=rc[:, 0:1], in0=rc[:, 0:1],
                                                                                 in1=tab[:, tt, d, 4 + hd:5 + hd], op=ALU.max),
                         r=[d_rc, d_tab], w=[d_rc])
                    p.op("dve", lambda h, rc=rc: h.reciprocal(out=rc[:, 1:2], in_=rc[:, 0:1]), r=[d_rc], w=[d_rc])
                    dst = acc[:, tt, 64 * hd:64 * hd + 64]
                    p.op("dve", lambda h, po=po, rc=rc, dst=dst: h.scalar_tensor_tensor(out=dst, in0=po[:, 0:64], scalar=rc[:, 1:2], in1=dst,
                                                                                        op0=ALU.mult, op1=ALU.add),
                         r=[d_po, d_rc, d_acc], w=[d_acc])
        gain, d_gain = bcast_load(p, sb, I["mlstm_norm"][l:l + 1, :], 256)
        ots = [sb.t((128, 256)) for _ in range(2)]
        xcs = [sb.t((128, 256)) for _ in range(2)]
        sqs = [sb.t((128, 256)) for _ in range(2)]
        sts = [sb.t((128, 12)) for _ in range(2)]
        for tt in range(NT):
            ot, d_ot = ots[tt % 2]
            xc, d_xc = xcs[tt % 2]
            sq, d_sq = sqs[tt % 2]
            st, d_st = sts[tt % 2]
            p.dma("sp", ot[:], Z["oC"][tt * 128:(tt + 1) * 128, :], w=[d_ot])
            p.op("act", lambda h, ot=ot: h.activation(out=ot[:], in_=ot[:], func=AF.Sigmoid), r=[d_ot], w=[d_ot])
            hv = acc[:, tt, :].rearrange("q (g e) -> q g e", e=64)
            v3 = lambda a: a[:, :].rearrange("q (g e) -> q g e", e=64)
            p.op("dve", lambda h, st=st, hv=hv: h.tensor_reduce(out=st[:, 0:4], in_=hv, axis=AX.X, op=ALU.add), r=[d_acc], w=[d_st])
            p.op("dve", lambda h, st=st: h.tensor_scalar(out=st[:, 0:4], in0=st[:, 0:4], scalar1=1.0 / 64, scalar2=None, op0=ALU.mult),
                 r=[d_st], w=[d_st])
            p.op("dve", lambda h, st=st, hv=hv, xc=xc: h.tensor_tensor(out=v3(xc), in0=hv, in1=st[:, 0:4].unsqueeze(2).to_broadcast([128, 4, 64]),
                                                                       op=ALU.subtract), r=[d_acc, d_st], w=[d_xc])
            p.op("act", lambda h, sq=sq, xc=xc: h.activation(out=sq[:], in_=xc[:], func=AF.Square), r=[d_xc], w=[d_sq])
            p.op("dve", lambda h, st=st, sq=sq: h.tensor_reduce(out=st[:, 4:8], in_=v3(sq), axis=AX.X, op=ALU.add), r=[d_sq], w=[d_st])
            rstd(p, st[:, 8:12], st[:, 4:8], st[:, 4:8], 1.0 / 64, [d_st])
            p.op("dve", lambda h, st=st, xc=xc: h.tensor_tensor(out=v3(xc), in0=v3(xc), in1=st[:, 8:12].unsqueeze(2).to_broadcast([128, 4, 64]),
                                                                op=ALU.mult), r=[d_st, d_xc], w=[d_xc])
            p.op("dve", lambda h, xc=xc: h.tensor_tensor(out=xc[:], in0=xc[:], in1=gain[:], op=ALU.mult), r=[d_gain, d_xc], w=[d_xc])
            p.op("dve", lambda h, xc=xc, ot=ot: h.tensor_tensor(out=xc[:], in0=xc[:], in1=ot[:], op=ALU.mult), r=[d_ot, d_xc], w=[d_xc])
            p.dma("sp", Z["y"][tt * 128:(tt + 1) * 128, 512:768], xc[:], r=[d_xc])


def chunk_d(p, l, I, S, Z, sbo, tab, d_tab, cm, d_cm, strict, d_strict, ones, d_ones, acc, d_acc):
    ident, d_ident = S["ident"]
    with SB(p) as sb:
        bk = [sb.ps() for _ in range(8)]
        B3 = lambda t: t[:, :].rearrange("q (h c) -> q h c", h=4)
        two = lambda shape, nm, dt=F32: [sb.t(shape, dt, nm) for _ in range(2)]
        qf = [two((64, 4, 128), "qf") for _ in range(2)]
        kf = [two((64, 4, 128), "kf") for _ in range(2)]
        ktm = [two((128, 4, 64), "ktm") for _ in range(2)]
        vtm = [two((128, 4, 64), "vtm") for _ in range(2)]
        diag, Ed, e1, e2, tmpP = two((128, 4, 128), "diag"), two((128, 4, 128), "Ed"), two((128, 4, 128), "e1"), two((128, 4, 128), "e2"), two((128, 4, 128), "tmpP")
        decT = [two((128, 4, 128), "decT") for _ in range(2)]
        decS = two((128, 4, 128), "decS")
        PQ = [two((128, 8, 128), "PQ") for _ in range(2)]
        TTt = [two((128, 4, 128), "TT") for _ in range(2)]
        Ru, Rw, kdec = two((128, 4, 64), "Ru"), two((128, 4, 64), "Rw"), two((128, 4, 64), "kdec")
        u_all, d_u = sb.t((128, 8, 64), F32, "uall")
        vnew, d_vnew = sb.t((128, 8, 64), F32, "vnew")
        wT = two((64, 4, 128), "wT")
        attnT = two((128, 4, 128), "attnT")
        Sst, d_S = sb.t((64, 8, 64), F32, "Sst")
        otmp = two((128, 4, 64), "otmp")
        p.op("dve", lambda h: h.memset(Sst[:], 0.0), w=[d_S])
        bc_col = lambda ap: ap.unsqueeze(2).to_broadcast([128, 4, 128])
        bc_c64 = lambda ap, n=128: ap.unsqueeze(2).to_broadcast([n, 4, 64])
        bc_mat = lambda ap: ap.unsqueeze(1).to_broadcast([128, 4, 128])
        for s_ in range(NT):
            par = s_ % 2
            tts = [ORDER[d][s_] for d in range(2)]
            col = lambda d, g: tab[:, tts[d], d, 4 * g:4 * g + 4]
            for d in range(2):
                tt = tts[d]
                tk = slice(tt * 128, (tt + 1) * 128)
                p.dma("sp", qf[d][par][0][:], Z["gqT"][:, tk].rearrange("(h q) t -> q h t", q=64), w=[qf[d][par][1]])
                p.dma("sp", kf[d][par][0][:], Z["gkT"][:, tk].rearrange("(h q) t -> q h t", q=64), w=[kf[d][par][1]])
                p.dma("sp", ktm[d][par][0][:], Z["gk"][tk, :].rearrange("q (h e) -> q h e", e=64), w=[ktm[d][par][1]])
                p.dma("sp", vtm[d][par][0][:], Z["gv"][tk, :].rearrange("q (h e) -> q h e", e=64), w=[vtm[d][par][1]])
            for d in range(2):
                dg, d_dg = diag[d]
                p.op("dve", lambda h, d=d, dg=dg: h.tensor_tensor(out=dg[:], in0=bc_mat(ident[:, :]), in1=bc_col(col(d, 5)), op=ALU.mult),
                     r=[d_ident, d_tab], w=[d_dg])
                pg, d_pg = bk[d]
                p.op("pe", lambda h, pg=pg, dg=dg: h.matmul(pg[:, :], lhsT=ones[:], rhs=dg[:, :, :].rearrange("q h c -> q (h c)"), start=True, stop=True),
                     r=[d_ones, d_dg], w=[d_pg])
                E_, d_E = Ed[d]
                p.op("dve", lambda h, d=d, pg=pg, E_=E_: h.tensor_tensor(out=E_[:], in0=B3(pg), in1=bc_col(col(d, 5)), op=ALU.subtract),
                     r=[d_pg, d_tab], w=[d_E])
                a1, d_a1 = e1[d]
                a2, d_a2 = e2[d]
                p.op("dve", lambda h, E_=E_, a1=a1: h.tensor_scalar(out=a1[:], in0=E_[:], scalar1=0.0, scalar2=None, op0=ALU.min), r=[d_E], w=[d_a1])
                p.op("pool", lambda h, E_=E_, a2=a2: h.tensor_scalar(out=a2[:], in0=E_[:], scalar1=0.0, scalar2=None, op0=ALU.max), r=[d_E], w=[d_a2])
                p.op("act", lambda h, a1=a1: h.activation(out=a1[:], in_=a1[:], func=AF.Exp), r=[d_a1], w=[d_a1])
                p.op("act", lambda h, a2=a2: h.activation(out=a2[:], in_=a2[:], func=AF.Exp, scale=-1.0), r=[d_a2], w=[d_a2])
                dT, d_dT = decT[d][par]
                dS, d_dS = decS[d]
                p.op("pool", lambda h, d=d, a1=a1, dT=dT: h.tensor_tensor(out=dT[:], in0=a1[:], in1=bc_mat(cm[:, 1 - d, :]), op=ALU.mult),
                     r=[d_a1, d_cm], w=[d_dT])
                p.op("pool", lambda h, d=d, a2=a2, dS=dS: h.tensor_tensor(out=dS[:], in0=a2[:], in1=bc_mat(strict[:, d, :]), op=ALU.mult),
                     r=[d_a2, d_strict], w=[d_dS])
            for d in range(2):
                kf_, d_kf = kf[d][par]
                pG, d_pG = bk[2 + d]
                for hd in range(4):
                    p.op("pe", lambda h, pG=pG, hd=hd, kf_=kf_: h.matmul(pG[:, hd * 128:(hd + 1) * 128], lhsT=kf_[:, hd, :], rhs=kf_[:, hd, :],
                                                                        start=True, stop=True), r=[d_kf], w=[d_pG])
                tp_, d_tp = tmpP[d]
                p.op("dve", lambda h, d=d, pG=pG, tp_=tp_: h.tensor_tensor(out=tp_[:], in0=B3(pG), in1=bc_col(col(d, 4)), op=ALU.mult),
                     r=[d_pG, d_tab], w=[d_tp])
                pq_, d_pq = PQ[d][0]
                p.op("pool", lambda h, d=d, tp_=tp_, pq_=pq_: h.tensor_tensor(out=pq_[:, 0:4, :], in0=tp_[:], in1=decS[d][0][:], op=ALU.mult),
                     r=[d_tp, decS[d][1]], w=[d_pq])
                pQ, d_pQ = bk[4 + d]
                for hd in range(4):
                    p.op("pe", lambda h, pQ=pQ, hd=hd, pq_=pq_: h.transpose(pQ[:, hd * 128:(hd + 1) * 128], pq_[:, hd, :], ident[:]),
                         r=[d_pq, d_ident], w=[d_pQ])
                p.op("act", lambda h, pQ=pQ, pq_=pq_: h.activation(out=pq_[:, 4:8, :], in_=B3(pQ), func=AF.Copy), r=[d_pQ], w=[d_pq])
                tt0, d_tt0 = TTt[d][0]
                p.op("dve", lambda h, pQ=pQ, tt0=tt0: h.tensor_tensor(out=tt0[:], in0=B3(pQ), in1=bc_mat(ident[:, :]), op=ALU.add),
                     r=[d_pQ, d_ident], w=[d_tt0])
            for k in range(1, 7):
                for d in range(2):
                    c_, d_c = PQ[d][(k - 1) % 2]
                    n_, d_n = PQ[d][k % 2]
                    pP, d_pP = bk[d]
                    pQ, d_pQ = bk[2 + d]
                    for hd in range(4):
                        p.op("pe", lambda h, pP=pP, hd=hd, c_=c_: h.matmul(pP[:, hd * 128:(hd + 1) * 128], lhsT=c_[:, 4 + hd, :], rhs=c_[:, hd, :],
                                                                          start=True, stop=True), r=[d_c], w=[d_pP])
                    if k < 6:
                        for hd in range(4):
                            p.op("pe", lambda h, pQ=pQ, hd=hd, c_=c_: h.matmul(pQ[:, hd * 128:(hd + 1) * 128], lhsT=c_[:, hd, :], rhs=c_[:, 4 + hd, :],
                                                                              start=True, stop=True), r=[d_c], w=[d_pQ])
                    p.op("act", lambda h, pP=pP, n_=n_: h.activation(out=n_[:, 0:4, :], in_=B3(pP), func=AF.Copy), r=[d_pP], w=[d_n])
                    if k < 6:
                        p.op("dve", lambda h, pQ=pQ, n_=n_: h.tensor_copy(out=n_[:, 4:8, :], in_=B3(pQ)), r=[d_pQ], w=[d_n])
                for d in range(2):
                    n_, d_n = PQ[d][k % 2]
                    tc_, d_tc = TTt[d][(k - 1) % 2]
                    tn_, d_tn = TTt[d][k % 2]
                    pT, d_pT = bk[4 + d]
                    for hd in range(4):
                        p.op("pe", lambda h, pT=pT, hd=hd, n_=n_, tc_=tc_: h.matmul(pT[:, hd * 128:(hd + 1) * 128], lhsT=n_[:, hd, :], rhs=tc_[:, hd, :],
                                                                                  start=True, stop=False), r=[d_n, d_tc], w=[d_pT])
                        p.op("pe", lambda h, pT=pT, hd=hd, tc_=tc_: h.matmul(pT[:, hd * 128:(hd + 1) * 128], lhsT=ident[:], rhs=tc_[:, hd, :],
                                                                            start=False, stop=True), r=[d_ident, d_tc], w=[d_pT])
                    if d == 0:
                        p.op("dve", lambda h, pT=pT, tn_=tn_: h.tensor_copy(out=tn_[:], in_=B3(pT)), r=[d_pT], w=[d_tn])
                    else:
                        p.op("act", lambda h, pT=pT, tn_=tn_: h.activation(out=tn_[:], in_=B3(pT), func=AF.Copy), r=[d_pT], w=[d_tn])
            pu, d_pu = bk[6]
            for d in range(2):
                TTf, d_TTf = TTt[d][0]
                ru, d_ru = Ru[d]
                rw, d_rw = Rw[d]
                kd, d_kd = kdec[d]
                kt_, d_kt = ktm[d][par]
                vt_, d_vt = vtm[d][par]
                p.op("pool", lambda h, d=d, ru=ru, vt_=vt_: h.tensor_tensor(out=ru[:], in0=vt_[:], in1=bc_c64(col(d, 3)), op=ALU.mult),
                     r=[d_vt, d_tab], w=[d_ru])
                p.op("pool", lambda h, d=d, rw=rw, kt_=kt_: h.tensor_tensor(out=rw[:], in0=kt_[:], in1=bc_c64(col(d, 9)), op=ALU.mult),
                     r=[d_kt, d_tab], w=[d_rw])
                p.op("pool", lambda h, d=d, kd=kd, kt_=kt_: h.tensor_tensor(out=kd[:], in0=kt_[:], in1=bc_c64(col(d, 7)), op=ALU.mult),
                     r=[d_kt, d_tab], w=[d_kd])
                for hd in range(4):
                    i = d * 4 + hd
                    p.op("pe", lambda h, i=i, hd=hd, TTf=TTf, ru=ru: h.matmul(pu[:, i * 64:(i + 1) * 64], lhsT=TTf[:, hd, :], rhs=ru[:, hd, :],
                                                                             start=True, stop=True), r=[d_TTf, d_ru], w=[d_pu])
            p.op("act", lambda h: h.activation(out=u_all[:], in_=pu[:, :].rearrange("q (i e) -> q i e", e=64), func=AF.Copy), r=[d_pu], w=[d_u])
            for d in range(2):
                TTf, d_TTf = TTt[d][0]
                rw, d_rw = Rw[d]
                pw, d_pw = bk[d]
                for hd in range(4):
                    p.op("pe", lambda h, pw=pw, hd=hd, TTf=TTf, rw=rw: h.matmul(pw[0:64, hd * 128:(hd + 1) * 128], lhsT=rw[:, hd, :], rhs=TTf[:, hd, :],
                                                                               start=True, stop=True), r=[d_TTf, d_rw], w=[d_pw])
                w_, d_w_ = wT[d]
                p.op("dve", lambda h, pw=pw, w_=w_: h.tensor_copy(out=w_[:], in_=pw[0:64, :].rearrange("q (h c) -> q h c", h=4)), r=[d_pw], w=[d_w_])
                pS, d_pS = bk[2 + d]
                kf_, d_kf = kf[d][par]
                qf_, d_qf = qf[d][par]
                for hd in range(4):
                    p.op("pe", lambda h, pS=pS, hd=hd, kf_=kf_, qf_=qf_: h.matmul(pS[:, hd * 128:(hd + 1) * 128], lhsT=kf_[:, hd, :], rhs=qf_[:, hd, :],
                                                                                start=True, stop=True), r=[d_kf, d_qf], w=[d_pS])
                at_, d_at = attnT[d]
                p.op("dve", lambda h, pS=pS, at_=at_, d=d: h.tensor_tensor(out=at_[:], in0=B3(pS), in1=decT[d][par][0][:], op=ALU.mult),
                     r=[d_pS, decT[d][par][1]], w=[d_at])
            pv, d_pv = bk[7]
            for d in range(2):
                for hd in range(4):
                    i = d * 4 + hd
                    p.op("pe", lambda h, i=i, hd=hd, d=d: h.matmul(pv[:, i * 64:(i + 1) * 64], lhsT=wT[d][0][:, hd, :], rhs=Sst[:, i, :],
                                                                  start=True, stop=True), r=[wT[d][1], d_S], w=[d_pv])
            p.op("dve", lambda h: h.tensor_tensor(out=vnew[:], in0=u_all[:], in1=pv[:, :].rearrange("q (i e) -> q i e", e=64), op=ALU.subtract),
                 r=[d_pv, d_u], w=[d_vnew])
            po1, d_po1 = bk[4]
            po2, d_po2 = bk[5]
            pn, d_pn = bk[6]
            for d in range(2):
                for hd in range(4):
                    i = d * 4 + hd
                    p.op("pe", lambda h, i=i, hd=hd, d=d: h.matmul(po1[:, i * 64:(i + 1) * 64], lhsT=attnT[d][0][:, hd, :], rhs=vnew[:, i, :],
                                                                  start=True, stop=True), r=[attnT[d][1], d_vnew], w=[d_po1])
            for d in range(2):
                for hd in range(4):
                    i = d * 4 + hd
                    p.op("pe", lambda h, i=i, hd=hd, d=d: h.matmul(po2[:, i * 64:(i + 1) * 64], lhsT=qf[d][par][0][:, hd, :], rhs=Sst[:, i, :],
                                                                  start=True, stop=True), r=[qf[d][par][1], d_S], w=[d_po2])
            for d in range(2):
                for hd in range(4):
                    i = d * 4 + hd
                    p.op("pe", lambda h, i=i, hd=hd, d=d: h.matmul(pn[0:64, i * 64:(i + 1) * 64], lhsT=kdec[d][0][:, hd, :], rhs=vnew[:, i, :],
                                                                  start=True, stop=True), r=[kdec[d][1], d_vnew], w=[d_pn])
            for d in range(2):
                dst = acc[:, tts[d], :].rearrange("q (h e) -> q h e", e=64)
                ot_, d_ot = otmp[d]
                p.op("dve", lambda h, d=d, dst=dst: h.tensor_tensor(out=dst, in0=po1[:, d * 256:(d + 1) * 256].rearrange("q (h e) -> q h e", e=64),
                                                                    in1=dst, op=ALU.add), r=[d_po1, d_acc], w=[d_acc])
                p.op("dve", lambda h, d=d, ot_=ot_: h.tensor_tensor(out=ot_[:], in0=po2[:, d * 256:(d + 1) * 256].rearrange("q (h e) -> q h e", e=64),
                                                                    in1=bc_c64(col(d, 6)), op=ALU.mult), r=[d_po2, d_tab], w=[d_ot])
                p.op("pool", lambda h, dst=dst, ot_=ot_: h.tensor_tensor(out=dst, in0=dst, in1=ot_[:], op=ALU.add), r=[d_ot, d_acc], w=[d_acc])
            for d in range(2):
                sv = Sst[:, 4 * d:4 * d + 4, :]
                egl_b = tab[0:64, tts[d], d, 32:36].unsqueeze(2).to_broadcast([64, 4, 64])
                p.op("dve", lambda h, sv=sv, egl_b=egl_b: h.tensor_tensor(out=sv, in0=sv, in1=egl_b, op=ALU.mult), r=[d_S, d_tab], w=[d_S])
                p.op("dve", lambda h, sv=sv, d=d: h.tensor_tensor(out=sv, in0=pn[0:64, d * 256:(d + 1) * 256].rearrange("q (h e) -> q h e", e=64),
                                                                  in1=sv, op=ALU.add), r=[d_pn, d_S], w=[d_S])
        gain, d_gain = sb.t((128, 256))
        for g4 in range(4):
            p.dma("sp", gain[:, 64 * g4:64 * g4 + 64], I["gdn_norm"][l:l + 1, :].partition_broadcast(128), w=[d_gain])
        zts = [sb.t((128, 256)) for _ in range(2)]
        sgs = [sb.t((128, 256)) for _ in range(2)]
        sqs = [sb.t((128, 256)) for _ in range(2)]
        sts = [sb.t((128, 8)) for _ in range(2)]
        v3 = lambda a: a[:, :].rearrange("q (g e) -> q g e", e=64)
        for tt in range(NT):
            zt, d_zt = zts[tt % 2]
            sg, d_sg = sgs[tt % 2]
            sq, d_sq = sqs[tt % 2]
            st, d_st = sts[tt % 2]
            p.dma("sp", zt[:], Z["zD"][tt * 128:(tt + 1) * 128, :], w=[d_zt])
            p.op("act", lambda h, zt=zt, sg=sg: h.activation(out=sg[:], in_=zt[:], func=AF.Sigmoid), r=[d_zt], w=[d_sg])
            p.op("dve", lambda h, zt=zt, sg=sg: h.tensor_tensor(out=sg[:], in0=sg[:], in1=zt[:], op=ALU.mult), r=[d_zt, d_sg], w=[d_sg])
            ov = acc[:, tt, :]
            p.op("act", lambda h, sq=sq, ov=ov: h.activation(out=sq[:], in_=ov, func=AF.Square), r=[d_acc], w=[d_sq])
            p.op("dve", lambda h, st=st, sq=sq: h.tensor_reduce(out=st[:, 0:4], in_=v3(sq), axis=AX.X, op=ALU.add), r=[d_sq], w=[d_st])
            rstd(p, st[:, 4:8], st[:, 0:4], st[:, 0:4], 1.0 / 64, [d_st])
            p.op("dve", lambda h, sq=sq, st=st, ov=ov: h.tensor_tensor(out=v3(sq), in0=ov.rearrange("q (g e) -> q g e", e=64),
                                                                       in1=st[:, 4:8].unsqueeze(2).to_broadcast([128, 4, 64]), op=ALU.mult),
                 r=[d_acc, d_st], w=[d_sq])
            p.op("dve", lambda h, sq=sq: h.tensor_tensor(out=sq[:], in0=sq[:], in1=gain[:], op=ALU.mult), r=[d_gain, d_sq], w=[d_sq])
            p.op("dve", lambda h, sq=sq, sg=sg: h.tensor_tensor(out=sq[:], in0=sq[:], in1=sg[:], op=ALU.mult), r=[d_sg, d_sq], w=[d_sq])
            p.dma("sp", Z["y"][tt * 128:(tt + 1) * 128, 768:1024], sq[:], r=[d_sq])


def phase_outproj(p, l, I, S, Z, xsrc):
    ident, d_ident = S["ident"]
    grow, d_grow = S["grow"]
    with SB(p) as sb:
        w, d_w = sb.t((128, 8, D), BF16, "wout")
        for k in range(8):
            p.dma("pool", w[:, k, :], I["w_out"][l, k * 128:(k + 1) * 128, :], w=[d_w])
        yts = [sb.t((128, D)) for _ in range(2)]
        xts = [sb.t((128, D)) for _ in range(2)]
        yTs = [sb.t((128, 8, 128), BF16) for _ in range(2)]
        tps = [sb.ps() for _ in range(2)]
        pos = [sb.ps() for _ in range(2)]
        tiles = range(NT) if l == 0 else range(2, NT)
        for tt in tiles:
            j = 1 if tt < 2 else 0
            rows = slice(tt * 128, (tt + 1) * 128)
            yt, d_yt = yts[tt % 2]
            xt, d_xt = xts[tt % 2]
            yT, d_yT = yTs[tt % 2]
            p.dma("sp", yt[:], Z["y"][rows, :], w=[d_yt])
            p.dma("sp", xt[:], xsrc[rows, :], w=[d_xt])
            for c in range(8):
                tp, d_tp = tps[c // 4]
                p.op("pe", lambda h, c=c, tp=tp, yt=yt: h.transpose(tp[:, (c % 4) * 128:(c % 4 + 1) * 128], yt[:, c * 128:(c + 1) * 128], ident[:]),
                     r=[d_yt, d_ident], w=[d_tp])
            for hh in range(2):
                tp, d_tp = tps[hh]
                p.op("act", lambda h, hh=hh, tp=tp, yT=yT: h.activation(out=yT[:, 4 * hh:4 * hh + 4, :],
                                                                       in_=tp[:, :].rearrange("q (c t) -> q c t", c=4), func=AF.Copy),
                     r=[d_tp], w=[d_yT])
            for hh in range(2):
                po, d_po = pos[hh]
                for k in range(8):
                    p.op("pe", lambda h, k=k, po=po, yT=yT, hh=hh: h.matmul(po[:, :], lhsT=yT[:, k, :], rhs=w[:, k, hh * 512:(hh + 1) * 512],
                                                                          start=(k == 0), stop=(k == 7)), r=[d_yT, d_w], w=[d_po])
                p.op("dve", lambda h, po=po, yt=yt, hh=hh, j=j: h.tensor_tensor(out=yt[:, hh * 512:(hh + 1) * 512], in0=po[:, :],
                                                                             in1=grow[:, 0, j, hh * 512:(hh + 1) * 512], op=ALU.mult),
                     r=[d_po, d_grow], w=[d_yt])
            p.op("dve", lambda h, xt=xt, yt=yt: h.tensor_tensor(out=xt[:], in0=xt[:], in1=yt[:], op=ALU.add), r=[d_yt, d_xt], w=[d_xt])
            p.dma("sp", Z["xres"][rows, :], xt[:], r=[d_xt])


def norm_objs(sb, S):
    junk, d_junk = sb.t((128, D))
    xs, d_xs = sb.t((128, D))
    ss, d_ss = sb.t((128, 4))
    tps = [sb.ps() for _ in range(2)]
    return (junk, d_junk, ss, d_ss, xs, d_xs, tps, S["ident"][0], S["ident"][1])


def phase_ffn_dense(p, l, I, S, Z):
    gsh, d_gsh = S["gsh"]
    grow, d_grow = S["grow"]
    NF = D_FF // 128
    with SB(p) as sb:
        wg, d_wg = sb.t((128, 8, D_FF), BF16, "wg")
        wu, d_wu = sb.t((128, 8, D_FF), BF16, "wu")
        wd, d_wd = sb.t((128, NF, D), BF16, "wd")
        for k in range(8):
            p.dma("pool", wg[:, k, :], I["ffn_w_gate"][k * 128:(k + 1) * 128, :], w=[d_wg])
            p.dma("pool", wu[:, k, :], I["ffn_w_up"][k * 128:(k + 1) * 128, :], w=[d_wu])
        for k in range(NF):
            p.dma("pool", wd[:, k, :], I["ffn_w_down"][k * 128:(k + 1) * 128, :], w=[d_wd])
        nobj = norm_objs(sb, S)
        xb, d_xb = sb.t((128, 2, D), F32, "xblk")
        xnT, d_xnT = sb.t((128, 8, 256), BF16, "xnT")
        hT, d_hT = sb.t((128, NF, 256), BF16, "hT")
        sgs = [sb.t((128, 256)) for _ in range(2)]
        pgs = [sb.ps() for _ in range(2)]
        pus = [sb.ps() for _ in range(2)]
        pos = [sb.ps() for _ in range(2)]
        ot, d_ot = sb.t((128, 512))
        blocks = [(2 * i, 2) for i in range(17)]
        n = 0
        for (t0, ntl) in blocks:
            ntok = ntl * 128
            j = 1 if t0 < 2 else 0
            for ti in range(ntl):
                tt = t0 + ti
                p.dma("sp", xb[:, ti, :], Z["xres"][tt * 128:(tt + 1) * 128, :], w=[d_xb])
            for ti in range(ntl):
                norm_tile(p, nobj, xb[:, ti, :], d_xb, gsh, d_gsh, 1, j, xnT, d_xnT, ti * 128)
            for fc in range(NF):
                pg, d_pg = pgs[fc % 2]
                pu, d_pu = pus[fc % 2]
                sg, d_sg = sgs[fc % 2]
                for k in range(8):
                    p.op("pe", lambda h, k=k, pg=pg, fc=fc: h.matmul(pg[:, 0:ntok], lhsT=wg[:, k, fc * 128:(fc + 1) * 128], rhs=xnT[:, k, 0:ntok],
                                                                    start=(k == 0), stop=(k == 7)), r=[d_wg, d_xnT], w=[d_pg])
                for k in range(8):
                    p.op("pe", lambda h, k=k, pu=pu, fc=fc: h.matmul(pu[:, 0:ntok], lhsT=wu[:, k, fc * 128:(fc + 1) * 128], rhs=xnT[:, k, 0:ntok],
                                                                    start=(k == 0), stop=(k == 7)), r=[d_wu, d_xnT], w=[d_pu])
                p.op("act", lambda h, pg=pg, sg=sg: h.activation(out=sg[:, 0:ntok], in_=pg[:, 0:ntok], func=AF.Sigmoid), r=[d_pg], w=[d_sg])
                p.op("dve", lambda h, pg=pg, sg=sg: h.tensor_tensor(out=sg[:, 0:ntok], in0=pg[:, 0:ntok], in1=sg[:, 0:ntok], op=ALU.mult),
                     r=[d_pg, d_sg], w=[d_sg])
                p.op("dve", lambda h, pu=pu, sg=sg, fc=fc: h.tensor_tensor(out=hT[:, fc, 0:ntok], in0=pu[:, 0:ntok], in1=sg[:, 0:ntok], op=ALU.mult),
                     r=[d_pu, d_sg], w=[d_hT])
            for ti in range(ntl):
                tt = t0 + ti
                for hh in range(2):
                    po, d_po = pos[n % 2]
                    n += 1
                    for fc in range(NF):
                        p.op("pe", lambda h, fc=fc, po=po, ti=ti, hh=hh: h.matmul(po[:, :], lhsT=hT[:, fc, ti * 128:(ti + 1) * 128],
                                                                                rhs=wd[:, fc, hh * 512:(hh + 1) * 512],
                                                                                start=(fc == 0), stop=(fc == NF - 1)), r=[d_hT, d_wd], w=[d_po])
                    p.op("dve", lambda h, po=po, hh=hh, j=j: h.tensor_tensor(out=ot[:], in0=po[:, :], in1=grow[:, 1, j, hh * 512:(hh + 1) * 512],
                                                                          op=ALU.mult), r=[d_po, d_grow], w=[d_ot])
                    p.op("dve", lambda h, ti=ti, hh=hh: h.tensor_tensor(out=xb[:, ti, hh * 512:(hh + 1) * 512], in0=xb[:, ti, hh * 512:(hh + 1) * 512],
                                                                         in1=ot[:], op=ALU.add), r=[d_ot, d_xb], w=[d_xb])
                p.dma("sp", Z["xres"][tt * 128:(tt + 1) * 128, :], xb[:, ti, :], r=[d_xb])


def phase_moe(p, l, I, S, Z, out):
    gsh, d_gsh = S["gsh"]
    grow, d_grow = S["grow"]
    NTM = 16
    SLAB = 512
    NSL = D_FFE // SLAB
    with SB(p) as sb:
        fl, d_fl = sb.t((128, 2))
        p.dma("sp", fl[:], I["flags"][:, :], w=[d_fl])
        xm, d_xm = sb.t((128, NTM, D), F32, "xm")
        xnT, d_xnT = sb.t((128, 8, NTM * 128), BF16, "xnTm")
        gates, d_gates = sb.t((128, NTM, NE), F32, "gates")
        rt, d_rt = sb.t((128, 8, NE), F32, "router")
        p.dma("sp", rt[:], I["moe_router"].rearrange("(k q) e -> q k e", q=128), w=[d_rt])
        with SB(p) as sb2:
            nobj = norm_objs(sb2, S)
            (junk, d_junk, ss, d_ss, xs, d_xs, tps, ident, d_ident) = nobj
            xa = [sb2.t((128, D)) for _ in range(2)]
            xnf, d_xnf = sb2.t((128, 8, 128), F32, "xnf")
            plg, d_plg = sb2.ps()
            lg, d_lg = sb2.t((128, 8))
            mx, d_mx = sb2.t((128, 8))
            wk, d_wk = sb2.t((128, 32))
            for jt in range(NTM):
                a_, d_a = xa[0]
                b_, d_b = xa[1]
                p.dma("sp", a_[:], Z["xres"][256 + jt * 128:256 + (jt + 1) * 128, :], w=[d_a])
                p.dma("sp", b_[:], Z["xres"][256 + 2048 + jt * 128:256 + 2048 + (jt + 1) * 128, :], w=[d_b])
                p.op("dve", lambda h, jt=jt: h.tensor_scalar(out=xm[:, jt, :], in0=a_[:], scalar1=fl[:, 0:1], scalar2=None, op0=ALU.mult),
                     r=[d_a, d_fl], w=[d_xm])
                p.op("dve", lambda h, jt=jt: h.scalar_tensor_tensor(out=xm[:, jt, :], in0=b_[:], scalar=fl[:, 1:2], in1=xm[:, jt, :],
                                                                    op0=ALU.mult, op1=ALU.add), r=[d_b, d_fl, d_xm], w=[d_xm])
                norm_tile(p, nobj, xm[:, jt, :], d_xm, gsh, d_gsh, 1, 0, xnT, d_xnT, jt * 128)
                for c in range(8):
                    tp, d_tp = tps[c // 4]
                    p.op("act", lambda h, c=c, tp=tp: h.activation(out=xnf[:, c, :], in_=tp[:, (c % 4) * 128:(c % 4 + 1) * 128],
                                                                   func=AF.Identity, scale=gsh[:, 2, c, 0:1], bias=gsh[:, 3, c, 0:1]),
                         r=[d_tp, d_gsh], w=[d_xnf])
                for k in range(8):
                    p.op("pe", lambda h, k=k: h.matmul(plg[:, 0:NE], lhsT=xnf[:, k, :], rhs=rt[:, k, :], start=(k == 0), stop=(k == 7)),
                         r=[d_xnf, d_rt], w=[d_plg])
                p.op("dve", lambda h: h.tensor_copy(out=lg[:], in_=plg[:, 0:NE]), r=[d_plg], w=[d_lg])
                p.op("dve", lambda h: h.max(out=mx[:], in_=lg[:]), r=[d_lg], w=[d_mx])
                p.op("dve", lambda h: h.tensor_tensor(out=wk[:, 0:1], in0=mx[:, 1:2], in1=mx[:, 0:1], op=ALU.subtract), r=[d_mx], w=[d_wk])
                p.op("act", lambda h: h.activation(out=wk[:, 1:2], in_=wk[:, 0:1], func=AF.Exp), r=[d_wk], w=[d_wk])
                p.op("dve", lambda h: h.tensor_scalar(out=wk[:, 1:2], in0=wk[:, 1:2], scalar1=1.0, scalar2=None, op0=ALU.add), r=[d_wk], w=[d_wk])
                p.op("dve", lambda h: h.reciprocal(out=wk[:, 2:3], in_=wk[:, 1:2]), r=[d_wk], w=[d_wk])
                p.op("dve", lambda h: h.tensor_scalar(out=wk[:, 3:4], in0=wk[:, 2:3], scalar1=-1.0, scalar2=1.0, op0=ALU.mult, op1=ALU.add),
                     r=[d_wk], w=[d_wk])
                p.op("dve", lambda h: h.tensor_scalar(out=wk[:, 8:16], in0=lg[:], scalar1=mx[:, 0:1], scalar2=wk[:, 2:3], op0=ALU.is_equal,
                                                      op1=ALU.mult), r=[d_lg, d_mx, d_wk], w=[d_wk])
                p.op("dve", lambda h: h.tensor_scalar(out=wk[:, 16:24], in0=lg[:], scalar1=mx[:, 1:2], scalar2=wk[:, 3:4], op0=ALU.is_equal,
                                                      op1=ALU.mult), r=[d_lg, d_mx, d_wk], w=[d_wk])
                p.op("dve", lambda h, jt=jt: h.tensor_tensor(out=gates[:, jt, :], in0=wk[:, 8:16], in1=wk[:, 16:24], op=ALU.add),
                     r=[d_wk], w=[d_gates])
        wgs = [sb.t((128, 8, SLAB), BF16, "wgs") for _ in range(2)]
        wus = [sb.t((128, 8, SLAB), BF16, "wus") for _ in range(2)]
        wds = [sb.t((128, 4, D), BF16, "wds") for _ in range(2)]
        hTs = [sb.t((128, 4, 512), BF16, "hTs") for _ in range(2)]
        sgs = [sb.t((128, 512)) for _ in range(2)]
        pgs = [sb.ps() for _ in range(2)]
        pus = [sb.ps() for _ in range(2)]
        pos = [sb.ps() for _ in range(2)]
        ns = 0
        nh = 0
        nf = 0
        no = 0
        for e in range(NE):
            for sl in range(NSL):
                wg, d_wg = wgs[ns % 2]
                wu, d_wu = wus[ns % 2]
                wd, d_wd = wds[ns % 2]
                ns += 1
                c0 = sl * SLAB
                p.dma("pool", wg[:], I["moe_w_gate"][e].rearrange("(k q) f -> q k f", q=128)[:, :, c0:c0 + SLAB], w=[d_wg])
                p.dma("pool", wu[:], I["moe_w_up"][e].rearrange("(k q) f -> q k f", q=128)[:, :, c0:c0 + SLAB], w=[d_wu])
                p.dma("pool", wd[:], I["moe_w_down"][e, c0:c0 + SLAB, :].rearrange("(k q) d -> q k d", q=128), w=[d_wd])
                for k in range(4):
                    p.op("dve", lambda h, k=k, wd=wd: h.tensor_tensor(out=wd[:, k, :], in0=wd[:, k, :], in1=grow[:, 1, 0, :], op=ALU.mult),
                         r=[d_grow, d_wd], w=[d_wd])
                for tb in range(NTM // 4):
                    hT, d_hT = hTs[nh % 2]
                    nh += 1
                    toks = slice(tb * 512, (tb + 1) * 512)
                    for fc in range(4):
                        pg, d_pg = pgs[nf % 2]
                        pu, d_pu = pus[nf % 2]
                        sg, d_sg = sgs[nf % 2]
                        nf += 1
                        for k in range(8):
                            p.op("pe", lambda h, k=k, pg=pg, fc=fc, wg=wg: h.matmul(pg[:, :], lhsT=wg[:, k, fc * 128:(fc + 1) * 128], rhs=xnT[:, k, toks],
                                                                                  start=(k == 0), stop=(k == 7)), r=[d_wg, d_xnT], w=[d_pg])
                        for k in range(8):
                            p.op("pe", lambda h, k=k, pu=pu, fc=fc, wu=wu: h.matmul(pu[:, :], lhsT=wu[:, k, fc * 128:(fc + 1) * 128], rhs=xnT[:, k, toks],
                                                                                  start=(k == 0), stop=(k == 7)), r=[d_wu, d_xnT], w=[d_pu])
                        p.op("act", lambda h, pg=pg, sg=sg: h.activation(out=sg[:], in_=pg[:, :], func=AF.Sigmoid), r=[d_pg], w=[d_sg])
                        p.op("dve", lambda h, pg=pg, sg=sg: h.tensor_tensor(out=sg[:], in0=pg[:, :], in1=sg[:], op=ALU.mult), r=[d_pg, d_sg], w=[d_sg])
                        p.op("dve", lambda h, pu=pu, sg=sg, fc=fc, hT=hT: h.tensor_tensor(out=hT[:, fc, :], in0=pu[:, :], in1=sg[:], op=ALU.mult),
                             r=[d_pu, d_sg], w=[d_hT])
                    for ti in range(4):
                        jt = tb * 4 + ti
                        for hh in range(2):
                            po, d_po = pos[no % 2]
                            no += 1
                            for fc in range(4):
                                p.op("pe", lambda h, fc=fc, po=po, ti=ti, hh=hh, hT=hT, wd=wd: h.matmul(
                                    po[:, :], lhsT=hT[:, fc, ti * 128:(ti + 1) * 128], rhs=wd[:, fc, hh * 512:(hh + 1) * 512],
                                    start=(fc == 0), stop=(fc == 3)), r=[d_hT, d_wd], w=[d_po])
                            dst = xm[:, jt, hh * 512:(hh + 1) * 512]
                            p.op("dve", lambda h, po=po, dst=dst, jt=jt, e=e: h.scalar_tensor_tensor(out=dst, in0=po[:, :], scalar=gates[:, jt, e:e + 1],
                                                                                                  in1=dst, op0=ALU.mult, op1=ALU.add),
                                 r=[d_po, d_gates, d_xm], w=[d_xm])
        for jt in range(NTM):
            p.dma("sp", out[jt * 128:(jt + 1) * 128, :], xm[:, jt, :], r=[d_xm])


_CACHE = {}


def kernel(**inputs):
    inp = {k: np.asarray(v) for k, v in inputs.items()}
    if "p" not in _CACHE:
        _CACHE["p"] = build()
    p = _CACHE["p"]
    in_maps = [host_inputs(inp, c) for c in range(8)]
    res = run_bass_kernel_spmd(p.nc, in_maps, core_ids=list(range(8)))
    out = np.zeros((4, NLAT, D), np.float32)
    for c in range(8):
        b, hh = c // 2, c % 2
        out[b, hh * 2048:(hh + 1) * 2048, :] = np.asarray(res.results[c]["out"], dtype=np.float32)
    return out
```

```python
import math
from contextlib import ExitStack
import numpy as np
import concourse.bass as bass
import concourse.mybir as mybir
from concourse.bass_utils import run_bass_kernel_spmd

F32 = mybir.dt.float32
BF16 = mybir.dt.bfloat16
AF = mybir.ActivationFunctionType
ALU = mybir.AluOpType
AX = mybir.AxisListType

D = 1024
NCTX = 256
NLAT = 4096
T = NCTX + NLAT
NT = T // 128
EPS = 1e-6
N_IN = 3360
D_FF = 2816
D_FFE = 3584
NE = 8


class Dep:
    __slots__ = ("w", "r", "excl")

    def __init__(self, excl=False):
        self.w = {}
        self.r = {}
        self.excl = excl


class Prog:
    def __init__(self):
        self.nc = bass.Bass("TRN2", target_bir_lowering=False)
        nc = self.nc
        self.h = {"pe": nc.tensor, "act": nc.scalar, "dve": nc.vector, "pool": nc.gpsimd, "sp": nc.sync}
        self.sem = {}
        self.cnt = {}
        self.semobj = {}
        self.seen = {e: {} for e in self.h}
        self.nsem = 0
        for e in self.h:
            self._newsem(e)
        self.NS = 12
        self.slots = {q: [nc.alloc_semaphore(f"dq_{q}_{i}") for i in range(self.NS)] for q in ("sp", "pool", "act")}
        self.dcnt = {q: 0 for q in self.slots}
        for q in self.slots:
            for s in self.slots[q]:
                self.semobj[id(s)] = s

    def _newsem(self, e):
        s = self.nc.alloc_semaphore(f"s_{e}_{self.nsem}")
        self.nsem += 1
        self.sem[e] = s
        self.cnt[e] = 0
        if not hasattr(self, "semobj"):
            self.semobj = {}
        self.semobj[id(s)] = s

    def _wait(self, e, tok):
        sid, val = tok
        if self.seen[e].get(sid, 0) >= val:
            return
        self.seen[e][sid] = val
        self.h[e].wait_ge(self.semobj[sid], val)

    def _deps(self, e, r, w):
        need = {}
        for d in r:
            for sid, v in d.w.items():
                need[sid] = max(need.get(sid, 0), v)
            if d.excl:
                for sid, v in d.r.items():
                    need[sid] = max(need.get(sid, 0), v)
        for d in w:
            for sid, v in d.w.items():
                need[sid] = max(need.get(sid, 0), v)
            for sid, v in d.r.items():
                need[sid] = max(need.get(sid, 0), v)
        own = id(self.sem[e])
        for sid, v in need.items():
            if e == "pe" and sid == own:
                continue
            self._wait(e, (sid, v))

    def _mark(self, tok, r, w):
        sid, v = tok
        for d in r:
            d.r[sid] = max(d.r.get(sid, 0), v)
        for d in w:
            d.w = {sid: v}
            d.r = {}

    def op(self, e, fn, r=(), w=()):
        self._deps(e, r, w)
        if self.cnt[e] >= 30000:
            self._newsem(e)
        ins = fn(self.h[e])
        self.cnt[e] += 1
        ins.then_inc(self.sem[e], 1)
        self._mark((id(self.sem[e]), self.cnt[e]), r, w)

    def dma(self, q, out, in_, r=(), w=(), **kw):
        self._deps(q, r, w)
        i = self.dcnt[q]
        self.dcnt[q] += 1
        s = self.slots[q][i % self.NS]
        val = 16 * (i // self.NS + 1)
        self._wait(q, (id(s), val - 16))
        self.h[q].dma_start(out=out, in_=in_, **kw).then_inc(s, 16)
        self._mark((id(s), val), r, w)

    def eps_ap(self, eps, n):
        assert abs(eps - EPS) < 1e-12
        return self.epst[0:n, 0:1]

    def barrier(self):
        toks = []
        for e in self.h:
            if self.cnt[e] > 0:
                toks.append((id(self.sem[e]), self.cnt[e]))
        for q in self.slots:
            n = self.dcnt[q]
            for j in range(min(n, self.NS)):
                i = n - 1 - j
                toks.append((id(self.slots[q][i % self.NS]), 16 * (i // self.NS + 1)))
        for e in self.h:
            for t in toks:
                self._wait(e, t)


class SB:
    N = 0

    def __init__(self, p):
        self.p = p
        self.es = ExitStack()
        self.n = 0

    def __enter__(self):
        self.es.__enter__()
        return self

    def __exit__(self, *a):
        self.p.barrier()
        return self.es.__exit__(*a)

    def t(self, shape, dt=F32, name="t"):
        SB.N += 1
        return self.es.enter_context(self.p.nc.sbuf_tensor(f"{name}_{SB.N}", list(shape), dt)), Dep()

    def ps(self, shape=(128, 512), dt=F32, name="ps"):
        SB.N += 1
        return self.es.enter_context(self.p.nc.psum_tensor(f"{name}_{SB.N}", list(shape), dt)), Dep(excl=True)


CA_Q, CA_K, CA_V = 0, 256, 512
CB_Q, CB_K, CB_V = 768, 1024, 1152
CC_Q, CC_K, CC_V, CC_O, CC_G = 1280, 1536, 1792, 2048, 2304
CD_QKV, CD_Z, CD_G = 2320, 3088, 3344


def dram(p, name, shape, dt, kind="Internal"):
    return p.nc.dram_tensor(name, list(shape), dt, kind=kind).ap()


def phase_mod(p, l, I, S):
    nc = p.nc
    modT, d_modT = S["modT"]
    gsh, d_gsh = S["gsh"]
    grow, d_grow = S["grow"]
    with SB(p) as sb:
        cc, d_cc = sb.t((128, 8, 2))
        sc, d_sc = sb.t((128, 8, 2))
        ones, d_ones = sb.t((128, 128))
        rep, d_rep = sb.t((128, 2, 8, 128))
        bmT, d_bmT = sb.t((128, 48))
        nrm, d_nrm = sb.t((128, 2, 8))
        wm = [sb.t((128, 8, 512)) for _ in range(2)]
        psm, d_psm = sb.ps((128, 512))
        psr = [sb.ps((128, 512)) for _ in range(2)]
        p.dma("sp", cc[:], I["cc"][:, :, :], w=[d_cc])
        p.dma("sp", bmT[:], I["bmodT"][:, l, :], w=[d_bmT])
        p.dma("sp", nrm[:, 0, :], I["norm1T"][:, l, :], w=[d_nrm])
        p.dma("sp", nrm[:, 1, :], I["norm2T"][:, l, :], w=[d_nrm])
        for j in range(2):
            for which, c0 in ((0, 2048), (1, 5120)):
                p.dma("sp", grow[:, which, j, :], I["b_mod"][l:l + 1, c0:c0 + 1024].partition_broadcast(128), w=[d_grow])
        p.op("act", lambda h: h.activation(out=sc[:], in_=cc[:], func=AF.Sigmoid), r=[d_cc], w=[d_sc])
        p.op("dve", lambda h: h.tensor_tensor(out=sc[:], in0=sc[:], in1=cc[:], op=ALU.mult), r=[d_cc, d_sc], w=[d_sc])
        p.op("dve", lambda h: h.memset(ones[:], 1.0), w=[d_ones])
        for j in range(2):
            for k in range(8):
                p.op("dve", lambda h, j=j, k=k: h.tensor_scalar(out=rep[:, j, k, :], in0=ones[:], scalar1=sc[:, k, j:j + 1],
                                                                 scalar2=None, op0=ALU.mult), r=[d_ones, d_sc], w=[d_rep])
        wsrc = I["w_mod"][l].rearrange("(k q) n -> q k n", q=128)
        for blk in range(12):
            wt, d_wt = wm[blk % 2]
            p.dma("sp", wt[:], wsrc[:, :, blk * 512:(blk + 1) * 512], w=[d_wt])
            for fc in range(4):
                for k in range(8):
                    p.op("pe", lambda h, fc=fc, k=k, wt=wt: h.matmul(psm[:, fc * 2:fc * 2 + 2], lhsT=wt[:, k, fc * 128:(fc + 1) * 128],
                                                                    rhs=sc[:, k, :], start=(k == 0), stop=(k == 7)),
                         r=[d_wt, d_sc], w=[d_psm])
            p.op("dve", lambda h, blk=blk: h.tensor_copy(out=modT[:, blk * 4:(blk + 1) * 4, :],
                                                         in_=psm[:, 0:8].rearrange("q (f j) -> q f j", j=2)),
                 r=[d_psm], w=[d_modT])
            if blk in (4, 5, 10, 11):
                which = 0 if blk < 6 else 1
                half = blk % 2
                for j in range(2):
                    pr, d_pr = psr[j]
                    for k in range(8):
                        p.op("pe", lambda h, j=j, k=k, wt=wt, pr=pr: h.matmul(pr[:, :], lhsT=rep[:, j, k, :], rhs=wt[:, k, :],
                                                                              start=(k == 0), stop=(k == 7)),
                             r=[d_wt, d_rep], w=[d_pr])
                    dst = grow[:, which, j, half * 512:(half + 1) * 512]
                    p.op("dve", lambda h, dst=dst, pr=pr: h.tensor_tensor(out=dst, in0=pr[:, :], in1=dst, op=ALU.add),
                         r=[d_pr, d_grow], w=[d_grow])
        for j in range(2):
            p.op("dve", lambda h, j=j: h.tensor_tensor(out=modT[:, :, j], in0=modT[:, :, j], in1=bmT[:], op=ALU.add),
                 r=[d_bmT, d_modT], w=[d_modT])
        for j in range(2):
            for n_i, (c_sh, c_sc) in enumerate(((0, 8), (24, 32))):
                p.op("dve", lambda h, j=j, n_i=n_i, c_sc=c_sc: h.scalar_tensor_tensor(
                    out=gsh[:, 2 * n_i, :, j], in0=modT[:, c_sc:c_sc + 8, j], scalar=1.0, in1=nrm[:, n_i, :],
                    op0=ALU.add, op1=ALU.mult), r=[d_modT, d_nrm], w=[d_gsh])
                p.op("dve", lambda h, j=j, n_i=n_i, c_sh=c_sh: h.tensor_copy(out=gsh[:, 2 * n_i + 1, :, j], in_=modT[:, c_sh:c_sh + 8, j]),
                     r=[d_modT], w=[d_gsh])


def rstd(p, out, in_, tmp, scale, deps, eps=EPS):
    p.op("act", lambda h: h.activation(out=tmp, in_=in_, func=AF.Sqrt, scale=scale, bias=p.eps_ap(eps, in_.shape[0])), r=deps + [p.d_eps], w=deps)
    p.op("dve", lambda h: h.reciprocal(out=out, in_=tmp), r=deps, w=deps)


def norm_tile(p, sb_objs, xt, d_xt, gsh, d_gsh, which, j, xnT, d_xnT, tok0):
    (junk, d_junk, ss, d_ss, xs, d_xs, tps, ident, d_ident) = sb_objs
    p.op("act", lambda h: h.activation(out=junk[:], in_=xt[:], func=AF.Square, accum_out=ss[:, 0:1]), r=[d_xt], w=[d_junk, d_ss])
    rstd(p, ss[:, 2:3], ss[:, 0:1], ss[:, 1:2], 1.0 / D, [d_ss])
    p.op("dve", lambda h: h.tensor_scalar(out=xs[:], in0=xt[:], scalar1=ss[:, 2:3], scalar2=None, op0=ALU.mult),
         r=[d_xt, d_ss], w=[d_xs])
    for c in range(8):
        tp, d_tp = tps[c // 4]
        p.op("pe", lambda h, c=c, tp=tp: h.transpose(tp[:, (c % 4) * 128:(c % 4 + 1) * 128], xs[:, c * 128:(c + 1) * 128], ident[:]),
             r=[d_xs, d_ident], w=[d_tp])
    for c in range(8):
        tp, d_tp = tps[c // 4]
        p.op("act", lambda h, c=c, tp=tp: h.activation(out=xnT[:, c, tok0:tok0 + 128], in_=tp[:, (c % 4) * 128:(c % 4 + 1) * 128],
                                                       func=AF.Identity, scale=gsh[:, 2 * which, c, j:j + 1],
                                                       bias=gsh[:, 2 * which + 1, c, j:j + 1]),
             r=[d_tp, d_gsh], w=[d_xnT])


def qk_norm_rope(p, sbo, src, d_src, ncols, dim, gain, d_gain, rope, d_rope, dst, d_dst):
    (sq, d_sq, st, d_st, t1, d_t1, t2, d_t2) = sbo
    ng = ncols // dim
    p.op("act", lambda h: h.activation(out=sq[:, 0:ncols], in_=src, func=AF.Square), r=[d_src], w=[d_sq])
    p.op("dve", lambda h: h.tensor_reduce(out=st[:, 0:ng], in_=sq[:, 0:ncols].rearrange("q (g d) -> q g d", d=dim), axis=AX.X, op=ALU.add),
         r=[d_sq], w=[d_st])
    rstd(p, st[:, 0:ng], st[:, 0:ng], st[:, 0:ng], 1.0 / dim, [d_st])
    tgt = t1 if rope is not None else dst
    d_tgt = d_t1 if rope is not None else d_dst
    p.op("dve", lambda h: h.tensor_tensor(out=tgt[:, 0:ncols].rearrange("q (g d) -> q g d", d=dim),
                                          in0=src.rearrange("q (g d) -> q g d", d=dim),
                                          in1=st[:, 0:ng].unsqueeze(2).to_broadcast([128, ng, dim]), op=ALU.mult),
         r=[d_src, d_st], w=[d_tgt])
    p.op("dve", lambda h: h.tensor_tensor(out=tgt[:, 0:ncols], in0=tgt[:, 0:ncols], in1=gain[:, 0:ncols], op=ALU.mult),
         r=[d_gain, d_tgt], w=[d_tgt])
    if rope is None:
        return
    q4 = dim // 4
    v = lambda a: a[:, 0:ncols].rearrange("q (g a s f) -> q g a s f", a=2, s=2, f=q4)
    cb = rope[:, 0, :].rearrange("q (a s f) -> q a s f", a=2, s=2).unsqueeze(1).to_broadcast([128, ng, 2, 2, q4])
    sbv = rope[:, 1, :].rearrange("q (a s f) -> q a s f", a=2, s=2)
    for s_ in range(2):
        p.op("dve", lambda h, s_=s_: h.tensor_tensor(out=v(t2)[:, :, :, s_, :], in0=v(t1)[:, :, :, 1 - s_, :],
                                                     in1=sbv[:, :, s_, :].unsqueeze(1).to_broadcast([128, ng, 2, q4]), op=ALU.mult),
             r=[d_t1, d_rope], w=[d_t2])
    p.op("dve", lambda h: h.tensor_tensor(out=v(t1), in0=v(t1), in1=cb, op=ALU.mult), r=[d_rope, d_t1], w=[d_t1])
    p.op("dve", lambda h: h.tensor_tensor(out=dst[:, 0:ncols], in0=t1[:, 0:ncols], in1=t2[:, 0:ncols], op=ALU.add),
         r=[d_t1, d_t2], w=[d_dst])


def phase_inproj(p, l, I, S, Z, xsrc):
    gsh, d_gsh = S["gsh"]
    ident, d_ident = S["ident"]
    with SB(p) as sb:
        w, d_w = sb.t((128, 8, N_IN), BF16, "win")
        for k in range(8):
            p.dma("pool", w[:, k, :], I["w_in"][l, k * 128:(k + 1) * 128, :], w=[d_w])
        gainA, d_gainA = sb.t((128, 512))
        gainB, d_gainB = sb.t((128, 384))
        for m in range(8):
            p.dma("sp", gainA[:, m * 32:(m + 1) * 32], I["diff_qk_gain"][l, 0:1, :].partition_broadcast(128), w=[d_gainA])
            p.dma("sp", gainA[:, 256 + m * 32:256 + (m + 1) * 32], I["diff_qk_gain"][l, 1:2, :].partition_broadcast(128), w=[d_gainA])
        for m in range(6):
            p.dma("sp", gainB[:, m * 64:(m + 1) * 64], I["gqa_qk_gain"][l, (0 if m < 4 else 1):(1 if m < 4 else 2), :].partition_broadcast(128),
                  w=[d_gainB])
        xts = [sb.t((128, D)) for _ in range(2)]
        junk, d_junk = sb.t((128, D))
        xs, d_xs = sb.t((128, D))
        ss, d_ss = sb.t((128, 4))
        tps = [sb.ps() for _ in range(2)]
        nobj = (junk, d_junk, ss, d_ss, xs, d_xs, tps, ident, d_ident)
        xnT, d_xnT = sb.t((128, 8, 512), BF16, "xnT")
        sq, d_sq = sb.t((128, 512))
        st, d_st = sb.t((128, 16))
        t1, d_t1 = sb.t((128, 512))
        t2, d_t2 = sb.t((128, 512))
        qko = (sq, d_sq, st, d_st, t1, d_t1, t2, d_t2)
        qkn, d_qkn = sb.t((128, 512))
        sq2, d_sq2 = sb.t((128, 384))
        st2, d_st2 = sb.t((128, 16))
        t12, d_t12 = sb.t((128, 384))
        t22, d_t22 = sb.t((128, 384))
        qko2 = (sq2, d_sq2, st2, d_st2, t12, d_t12, t22, d_t22)
        qkn2, d_qkn2 = sb.t((128, 384))
        stg, d_stg = sb.t((128, 16))
        ps_tr2, d_ps_tr2 = sb.ps()
        ropeA, d_ropeA = sb.t((128, 2, 32))
        ropeB, d_ropeB = sb.t((128, 2, 64))
        ps_tm = [sb.ps() for _ in range(2)]
        ps_fm = [sb.ps() for _ in range(2)]
        ps_tr, d_ps_tr = sb.ps()
        stA, d_stA = sb.t((128, 4, 512), BF16)
        stB, d_stB = sb.t((128, 3, 512), BF16)
        stv, d_stv = sb.t((128, 512), BF16)
        stf, d_stf = sb.t((128, 784))
        stfm = [sb.t((128, 512)) for _ in range(2)]
        blocks = [(0, 2)] + [(2 + 4 * i, 4) for i in range(8)]
        ntm = 0
        nfm = 0
        for (t0, ntl) in blocks:
            ntok = ntl * 128
            tokb = t0 * 128
            j = 1 if t0 < 2 else 0
            for ti in range(ntl):
                tt = t0 + ti
                xt, d_xt = xts[tt % 2]
                p.dma("sp", xt[:], xsrc[tt * 128:(tt + 1) * 128, :], w=[d_xt])
                norm_tile(p, nobj, xt, d_xt, gsh, d_gsh, 0, j, xnT, d_xnT, ti * 128)
            for ti in range(ntl):
                tt = t0 + ti
                tk = slice(ti * 128, (ti + 1) * 128)
                rows = slice(tt * 128, (tt + 1) * 128)
                lat = tt >= 2
                if lat:
                    p.dma("sp", ropeA[:], I["ropeA"][(tt - 2) * 128:(tt - 1) * 128, :, :], w=[d_ropeA])
                    p.dma("sp", ropeB[:], I["ropeB"][(tt - 2) * 128:(tt - 1) * 128, :, :], w=[d_ropeB])

                def tm_mm(c0, ncol):
                    nonlocal ntm
                    ps, d_ps = ps_tm[ntm % 2]
                    ntm += 1
                    for k in range(8):
                        p.op("pe", lambda h, k=k, ps=ps: h.matmul(ps[:, 0:ncol], lhsT=xnT[:, k, tk], rhs=w[:, k, c0:c0 + ncol],
                                                                  start=(k == 0), stop=(k == 7)), r=[d_xnT, d_w], w=[d_ps])
                    return ps, d_ps
                ps, d_ps = tm_mm(CA_Q, 512)
                qk_norm_rope(p, qko, ps[:, 0:512], d_ps, 512, 32, gainA, d_gainA, ropeA if lat else None, d_ropeA, qkn, d_qkn)
                def trA():
                    for c in range(4):
                        p.op("pe", lambda h, c=c: h.transpose(ps_tr[:, c * 128:(c + 1) * 128], qkn[:, c * 128:(c + 1) * 128], ident[:]),
                             r=[d_qkn, d_ident], w=[d_ps_tr])
                    p.op("act", lambda h: h.activation(out=stA[:, :, tk], in_=ps_tr[:, :].rearrange("q (c t) -> q c t", c=4), func=AF.Copy),
                         r=[d_ps_tr], w=[d_stA])
                ps, d_ps = tm_mm(CA_V, 256)
                p.op("act", lambda h, ps=ps: h.activation(out=stv[:, 0:256], in_=ps[:, 0:256], func=AF.Copy), r=[d_ps], w=[d_stv])
                p.dma("sp", Z["vA"][rows, :], stv[:, 0:256], r=[d_stv])
                ps, d_ps = tm_mm(CB_Q, 512)
                p.op("act", lambda h, ps=ps: h.activation(out=stv[:, 256:384], in_=ps[:, 384:512], func=AF.Copy), r=[d_ps], w=[d_stv])
                p.dma("sp", Z["vB"][rows, :], stv[:, 256:384], r=[d_stv])
                qk_norm_rope(p, qko2, ps[:, 0:384], d_ps, 384, 64, gainB, d_gainB, ropeB if lat else None, d_ropeB, qkn2, d_qkn2)

                def trB():
                    for c in range(3):
                        p.op("pe", lambda h, c=c: h.transpose(ps_tr2[:, c * 128:(c + 1) * 128], qkn2[:, c * 128:(c + 1) * 128], ident[:]),
                             r=[d_qkn2, d_ident], w=[d_ps_tr2])
                    p.op("act", lambda h: h.activation(out=stB[:, :, tk], in_=ps_tr2[:, 0:384].rearrange("q (c t) -> q c t", c=3), func=AF.Copy),
                         r=[d_ps_tr2], w=[d_stB])
                ps, d_ps = tm_mm(CC_V, 512)
                p.op("act", lambda h, ps=ps: h.activation(out=stf[:, 0:512], in_=ps[:, 0:512], func=AF.Copy), r=[d_ps], w=[d_stf])
                p.dma("sp", Z["vC"][rows, :], stf[:, 0:256], r=[d_stf])
                p.dma("sp", Z["oC"][rows, :], stf[:, 256:512], r=[d_stf])
                ps, d_ps = tm_mm(CD_Z, 272)
                p.op("act", lambda h, ps=ps: h.activation(out=stf[:, 512:784], in_=ps[:, 0:272], func=AF.Copy), r=[d_ps], w=[d_stf])
                p.dma("sp", Z["zD"][rows, :], stf[:, 512:768], r=[d_stf])
                p.dma("sp", Z["gD"][rows, :], stf[:, 768:784], r=[d_stf])
                ps, d_ps = tm_mm(CC_K, 256)
                p.op("act", lambda h, ps=ps: h.activation(out=stf[:, 0:256], in_=ps[:, 0:256], func=AF.Copy), r=[d_ps], w=[d_stf])
                p.dma("sp", Z["kC"][rows, :], stf[:, 0:256], r=[d_stf])
                ps, d_ps = tm_mm(CC_G, 16)
                p.op("act", lambda h, ps=ps: h.activation(out=stg[:, 0:16], in_=ps[:, 0:16], func=AF.Copy), r=[d_ps], w=[d_stg])
                p.dma("sp", Z["gC"][rows, :], stg[:, 0:16], r=[d_stg])
                trA()
                trB()
            tb = slice(tokb, tokb + ntok)
            for c in range(2):
                p.dma("sp", Z["qTa"][c * 128:(c + 1) * 128, tb], stA[:, c, 0:ntok], r=[d_stA])
                p.dma("sp", Z["kTa"][c * 128:(c + 1) * 128, tb], stA[:, 2 + c, 0:ntok], r=[d_stA])
                p.dma("sp", Z["qTb"][c * 128:(c + 1) * 128, tb], stB[:, c, 0:ntok], r=[d_stB])
            p.dma("sp", Z["kTb"][:, tb], stB[:, 2, 0:ntok], r=[d_stB])
            for ci in range(10):
                c0 = CC_Q + ci * 128 if ci < 4 else CD_QKV + (ci - 4) * 128
                ps, d_ps = ps_fm[nfm % 2]
                so, d_so = stfm[nfm % 2]
                nfm += 1
                for k in range(8):
                    p.op("pe", lambda h, k=k, ps=ps, c0=c0: h.matmul(ps[:, 0:ntok], lhsT=w[:, k, c0:c0 + 128], rhs=xnT[:, k, 0:ntok],
                                                                     start=(k == 0), stop=(k == 7)), r=[d_xnT, d_w], w=[d_ps])
                p.op("act", lambda h, ps=ps, so=so: h.activation(out=so[:, 0:ntok], in_=ps[:, 0:ntok], func=AF.Copy), r=[d_ps], w=[d_so])
                if ci < 2:
                    dst = Z["qTc"][ci * 128:(ci + 1) * 128, tb]
                elif ci < 4:
                    dst = Z["kTc"][(ci - 2) * 128:(ci - 1) * 128, tb]
                else:
                    dst = Z["qkvT"][(ci - 4) * 128:(ci - 3) * 128, tb]
                p.dma("sp", dst, so[:, 0:ntok], r=[d_so])


def rope_table(dim):
    nf = dim // 4
    t = np.arange(NLAT)
    row = (t // 64).astype(np.float32)
    col = (t % 64).astype(np.float32)
    inv = (np.float32(10000.0) ** (-np.arange(nf, dtype=np.float32) / np.float32(nf))).astype(np.float32)
    ang = np.stack([row[:, None] * inv, col[:, None] * inv], axis=1).astype(np.float32)
    c, s = np.cos(ang).astype(np.float32), np.sin(ang).astype(np.float32)
    C = np.stack([c, c], axis=2)
    Sp = np.stack([-s, s], axis=2)
    return np.ascontiguousarray(np.stack([C.reshape(NLAT, dim), Sp.reshape(NLAT, dim)], axis=1)).astype(np.float32)


IN_SPECS = {
    "xin": ([T, D], F32), "cc": ([128, 8, 2], F32), "flags": ([128, 2], F32),
    "bmodT": ([128, 2, 48], F32), "norm1T": ([128, 2, 8], F32), "norm2T": ([128, 2, 8], F32),
    "b_mod": ([2, 6 * D], F32), "w_mod": ([2, D, 6 * D], F32), "w_in": ([2, D, N_IN], F32), "w_out": ([2, D, D], F32),
    "diff_qk_gain": ([2, 2, 32], F32), "diff_lambda": ([2, 4, 32], F32), "diff_subln": ([2, 64], F32),
    "gqa_qk_gain": ([2, 2, 64], F32), "mlstm_gate_bias": ([2, 16], F32), "mlstm_norm": ([2, 256], F32),
    "gdn_convT": ([128, 2, 6, 5], F32), "gdn_a_log": ([2, 8], F32), "gdn_dt_bias": ([2, 8], F32), "gdn_norm": ([2, 64], F32),
    "ffn_w_gate": ([D, D_FF], F32), "ffn_w_up": ([D, D_FF], F32), "ffn_w_down": ([D_FF, D], F32),
    "moe_router": ([D, NE], F32), "moe_w_gate": ([NE, D, D_FFE], F32), "moe_w_up": ([NE, D, D_FFE], F32),
    "moe_w_down": ([NE, D_FFE, D], F32),
    "ropeA": ([NLAT, 2, 32], F32), "ropeB": ([NLAT, 2, 64], F32), "ident": ([128, 128], F32),
    "cmask": ([128, 2, 128], F32),
}


def host_inputs(inp, core):
    b, hh = core // 2, core % 2
    f = lambda a: np.ascontiguousarray(np.asarray(a, dtype=np.float32))
    colsT = lambda v, n: f(np.asarray(v).reshape(v.shape[0], n, 128).transpose(2, 0, 1))
    m = {}
    m["xin"] = f(np.concatenate([inp["ctx"][b], inp["x"][b]], axis=0))
    m["cc"] = f(np.stack([np.asarray(inp["c"][b]).reshape(8, 128).T, np.asarray(inp["c_ctx"]).reshape(8, 128).T], axis=2))
    fl = np.zeros((128, 2), np.float32)
    fl[:, hh] = 1.0
    m["flags"] = fl
    m["bmodT"] = colsT(inp["b_mod"], 48)
    m["norm1T"] = colsT(inp["norm1"], 8)
    m["norm2T"] = colsT(inp["norm2"], 8)
    for k in ("b_mod", "w_mod", "w_in", "w_out", "diff_qk_gain", "diff_lambda", "diff_subln", "gqa_qk_gain", "mlstm_norm",
              "gdn_norm"):
        m[k] = f(inp[k])
    m["mlstm_gate_bias"] = f(np.asarray(inp["mlstm_gate_bias"]).reshape(2, 16))
    m["gdn_a_log"] = f(np.asarray(inp["gdn_a_log"]).reshape(2, 8))
    m["gdn_dt_bias"] = f(np.asarray(inp["gdn_dt_bias"]).reshape(2, 8))
    m["gdn_convT"] = f(np.asarray(inp["gdn_conv"]).reshape(2, 5, 6, 128).transpose(3, 0, 2, 1))
    m["ffn_w_gate"] = f(inp["ffn_w_gate"][0])
    m["ffn_w_up"] = f(inp["ffn_w_up"][0])
    m["ffn_w_down"] = f(inp["ffn_w_down"][0])
    m["moe_router"] = f(inp["moe_router"][0])
    m["moe_w_gate"] = f(inp["moe_w_gate"][0])
    m["moe_w_up"] = f(inp["moe_w_up"][0])
    m["moe_w_down"] = f(inp["moe_w_down"][0])
    m["ropeA"] = rope_table(32)
    m["ropeB"] = rope_table(64)
    m["ident"] = np.eye(128, dtype=np.float32)
    i = np.arange(128)
    low = (i[:, None] >= i[None, :]).astype(np.float32)
    m["cmask"] = f(np.stack([low, low.T], axis=1))
    return m


SCRATCH = {
    "xres": ([T, D], F32),
    "qTa": ([256, T], BF16), "kTa": ([256, T], BF16), "vA": ([T, 256], BF16),
    "qTb": ([256, T], BF16), "kTb": ([128, T], BF16), "vB": ([T, 128], BF16),
    "qTc": ([256, T], F32), "kTc": ([256, T], F32), "vC": ([T, 256], F32), "oC": ([T, 256], F32), "gC": ([T, 16], F32),
    "qkvT": ([768, T], F32), "zD": ([T, 256], F32), "gD": ([T, 16], F32), "kC": ([T, 256], F32),
    "gqT": ([256, T], F32), "gkT": ([256, T], F32), "gk": ([T, 256], F32), "gv": ([T, 256], F32),
    "y": ([T, D], F32), "ym": ([NLAT // 2, 512], F32), "xm1": ([NLAT // 2, D], F32),
}


def build(debug=None, upto="all"):
    p = Prog()
    nc = p.nc
    I = {k: nc.dram_tensor(k, sh, dt, kind="ExternalInput").ap() for k, (sh, dt) in IN_SPECS.items()}
    Z = {}
    for k, (sh, dt) in SCRATCH.items():
        kind = "ExternalOutput" if (debug and k in debug) else "Internal"
        Z[k] = nc.dram_tensor("z_" + k, sh, dt, kind=kind).ap()
    out = nc.dram_tensor("out", [NLAT // 2, D], F32, kind="ExternalOutput").ap()
    with SB(p) as gsb:
        S = {"modT": gsb.t((128, 48, 2)), "gsh": gsb.t((128, 4, 8, 2)), "grow": gsb.t((128, 2, 2, D)), "ident": gsb.t((128, 128))}
        p.dma("sp", S["ident"][0][:], I["ident"][:, :], w=[S["ident"][1]])
        epst, p.d_eps = gsb.t((128, 1))
        p.epst = epst
        p.op("dve", lambda h: h.memset(epst[:], EPS), w=[p.d_eps])
        if debug and "modT" in debug:
            dbg_mod = nc.dram_tensor("z_modT", [128, 48, 2], F32, kind="ExternalOutput").ap()
            dbg_grow = nc.dram_tensor("z_grow", [128, 2, 2, D], F32, kind="ExternalOutput").ap()
        for l in range(2):
            phase_mod(p, l, I, S)
            if debug and "modT" in debug and l == 0:
                p.dma("sp", dbg_mod[:, :, :], S["modT"][0][:], r=[S["modT"][1]])
                p.dma("sp", dbg_grow[:, :, :, :], S["grow"][0][:], r=[S["grow"][1]])
            phase_inproj(p, l, I, S, Z, I["xin"] if l == 0 else Z["xres"])
            if upto == "inproj":
                break
            if "noattn" not in upto:
                phase_attn(p, l, I, S, Z, l == 0, mine=(l == 1))
            if upto == "attn":
                break
            phase_chunk(p, l, I, S, Z, do_c=("noc" not in upto), do_d=("nod" not in upto), upto=upto)
            if upto.startswith("chunk"):
                break
            phase_outproj(p, l, I, S, Z, I["xin"] if l == 0 else Z["xres"], mine=(l == 1))
            if l == 0:
                phase_ffn_dense(p, l, I, S, Z)
                if upto == "layer0":
                    break
            else:
                phase_moe(p, l, I, S, Z, out)
        p.barrier()
    return p


def bcast_load(p, sb, src_row_ap, n, name="bc"):
    t, d = sb.t((128, n), F32, name)
    p.dma("sp", t[:], src_row_ap.partition_broadcast(128), w=[d])
    return t, d


def phase_attn(p, l, I, S, Z, with_ctx, mine=False):
    ident, d_ident = S["ident"]
    lambda_init = 0.8 - 0.6 * math.exp(-0.3 * l)
    with SB(p) as sb:
        lamv, d_lamv = sb.t((128, 4, 32))
        p.dma("sp", lamv[:], I["diff_lambda"][l:l + 1, :, :].partition_broadcast(128), w=[d_lamv])
        cst, d_cst = sb.t((128, 16))
        tmp32, d_tmp32 = sb.t((128, 2, 32))
        p.op("dve", lambda h: h.tensor_tensor(out=tmp32[:], in0=lamv[:, 0:4:2, :], in1=lamv[:, 1:4:2, :], op=ALU.mult),
             r=[d_lamv], w=[d_tmp32])
        p.op("dve", lambda h: h.tensor_reduce(out=cst[:, 0:2], in_=tmp32[:], axis=AX.X, op=ALU.add), r=[d_tmp32], w=[d_cst])
        p.op("act", lambda h: h.activation(out=cst[:, 2:4], in_=cst[:, 0:2], func=AF.Exp), r=[d_cst], w=[d_cst])
        p.op("dve", lambda h: h.tensor_tensor(out=cst[:, 4:5], in0=cst[:, 3:4], in1=cst[:, 2:3], op=ALU.subtract), r=[d_cst], w=[d_cst])
        p.op("dve", lambda h: h.tensor_scalar(out=cst[:, 4:5], in0=cst[:, 4:5], scalar1=-lambda_init, scalar2=None, op0=ALU.add),
             r=[d_cst], w=[d_cst])
        gA, d_gA = sb.t((128, 2, 32))
        gB, d_gB = sb.t((128, 2, 64))
        p.dma("sp", gA[:], I["diff_qk_gain"][l:l + 1, :, :].partition_broadcast(128), w=[d_gA])
        p.dma("sp", gB[:], I["gqa_qk_gain"][l:l + 1, :, :].partition_broadcast(128), w=[d_gB])
        p.op("dve", lambda h: h.tensor_reduce(out=cst[:, 6:8], in_=gA[:], axis=AX.X, op=ALU.max, apply_absolute_value=True),
             r=[d_gA], w=[d_cst])
        p.op("dve", lambda h: h.tensor_reduce(out=cst[:, 8:10], in_=gB[:], axis=AX.X, op=ALU.max, apply_absolute_value=True),
             r=[d_gB], w=[d_cst])
        p.op("dve", lambda h: h.scalar_tensor_tensor(out=cst[:, 10:11], in0=cst[:, 6:7], scalar=-math.sqrt(32.0), in1=cst[:, 7:8],
                                                     op0=ALU.mult, op1=ALU.mult), r=[d_cst], w=[d_cst])
        p.op("dve", lambda h: h.scalar_tensor_tensor(out=cst[:, 11:12], in0=cst[:, 8:9], scalar=-8.0, in1=cst[:, 9:10],
                                                     op0=ALU.mult, op1=ALU.mult), r=[d_cst], w=[d_cst])
        subg, d_subg = bcast_load(p, sb, I["diff_subln"][l:l + 1, :], 64)
        p.op("dve", lambda h: h.tensor_scalar(out=subg[:], in0=subg[:], scalar1=1.0 - lambda_init, scalar2=None, op0=ALU.mult),
             r=[d_subg], w=[d_subg])
        kT, d_kT = sb.t((64, T), BF16, "kT")
        va, d_va = sb.t((128, NT, 65), BF16, "vaug")
        qTs = [sb.t((64, 512), BF16, "qT") for _ in range(2)]
        NSB = 2
        pTs = [sb.t((128, 1024), BF16, "pT") for _ in range(NSB)]
        ps_s = [sb.ps((128, 1024)) for _ in range(NSB)]
        ps_o = [sb.ps() for _ in range(2)]
        ps_t, d_ps_t = sb.ps()
        osb = [sb.t((65, 512), F32, "osb") for _ in range(2)]
        on = [sb.t((128, 64), F32, "on") for _ in range(2)]
        od, d_od = sb.t((128, 64))
        junk, d_junk = sb.t((128, 64))
        st, d_st = sb.t((128, 4))
        ystage, d_ys = sb.t((128, 4, 64))
        rc, d_rc = sb.t((128, 2))
        qblocks = ([(0, 256, [0, 1])] if with_ctx else []) + [(256 + 512 * i, 512, list(range(NT))) for i in range(4 if mine else 8)]
        cnt = {"s": 0, "q": 0}
        if mine:
            fl, d_fl = sb.t((128, 2))
            p.dma("sp", fl[:], I["flags"][:, :], w=[d_fl])
            qTo = [sb.t((64, 512), BF16, "qTo") for _ in range(2)]

        def run_head(kind, qsrc_rows, nmaps, scale, negB, ycol):
            for (q0, qn, kts) in qblocks:
                qT, d_qT = qTs[cnt["q"] % 2]
                cnt["q"] += 1
                if not mine:
                    p.dma("sp", qT[:, 0:qn], qsrc_rows[:, q0:q0 + qn], w=[d_qT])
                else:
                    qo, d_qo = qTo[cnt["q"] % 2]
                    p.dma("sp", qT[:, 0:qn], qsrc_rows[:, q0:q0 + qn], w=[d_qT])
                    p.dma("sp", qo[:, 0:qn], qsrc_rows[:, q0 + 2048:q0 + 2048 + qn], w=[d_qo])
                    p.op("dve", lambda h, qT=qT: h.tensor_scalar(out=qT[:, 0:qn], in0=qT[:, 0:qn], scalar1=fl[0:64, 0:1], scalar2=None, op0=ALU.mult),
                         r=[d_qT, d_fl], w=[d_qT])
                    p.op("dve", lambda h, qT=qT, qo=qo: h.scalar_tensor_tensor(out=qT[:, 0:qn], in0=qo[:, 0:qn], scalar=fl[0:64, 1:2], in1=qT[:, 0:qn],
                                                                               op0=ALU.mult, op1=ALU.add), r=[d_qo, d_fl, d_qT], w=[d_qT])
                for j in range(nmaps):
                    kr = slice(32 * j, 32 * j + 32) if kind == "A" else slice(0, 64)
                    po, d_po = ps_o[j]
                    LA = 1
                    pend = []
                    pairs = [kts[i2:i2 + 2] for i2 in range(0, len(kts), 2)]
                    for pi in range(len(pairs) + LA):
                        if pi < len(pairs):
                            pr = pairs[pi]
                            i = cnt["s"]
                            cnt["s"] += 1
                            ps, d_ps = ps_s[i % NSB]
                            pT, d_pT = pTs[i % NSB]
                            for hh, kt in enumerate(pr):
                                p.op("pe", lambda h, ps=ps, kt=kt, kr=kr, qT=qT, hh=hh: h.matmul(ps[:, hh * 512:hh * 512 + qn],
                                                                                               lhsT=kT[kr, kt * 128:(kt + 1) * 128],
                                                                                               rhs=qT[kr, 0:qn], start=True, stop=True),
                                     r=[d_kT, d_qT], w=[d_ps])
                            np_ = len(pr)
                            p.op("act", lambda h, ps=ps, pT=pT, np_=np_: h.activation(
                                out=pT[:, :].rearrange("q (b n) -> q b n", b=2)[:, 0:np_, 0:qn],
                                in_=ps[:, :].rearrange("q (b n) -> q b n", b=2)[:, 0:np_, 0:qn], func=AF.Exp, scale=scale, bias=negB),
                                 r=[d_ps, d_cst], w=[d_pT])
                            pend.append((pr, pi, pT, d_pT))
                        if pi >= LA:
                            pr2, pi2, pT2, d_pT2 = pend.pop(0)
                            for hh, kt2 in enumerate(pr2):
                                first = (pi2 == 0 and hh == 0)
                                last = (pi2 == len(pairs) - 1 and hh == len(pr2) - 1)
                                p.op("pe", lambda h, po=po, pT2=pT2, kt2=kt2, hh=hh, first=first, last=last: h.matmul(
                                    po[0:65, 0:qn], lhsT=va[:, kt2, :], rhs=pT2[:, hh * 512:hh * 512 + qn], start=first, stop=last),
                                     r=[d_va, d_pT2], w=[d_po])
                    ob, d_ob = osb[j]
                    p.op("act", lambda h, ob=ob, po=po: h.activation(out=ob[:, 0:qn], in_=po[0:65, 0:qn], func=AF.Copy), r=[d_po], w=[d_ob])
                nsub = qn // 128
                for s_ in range(nsub):
                    for j in range(nmaps):
                        ob, d_ob = osb[j]
                        p.op("pe", lambda h, ob=ob, j=j, s_=s_: h.transpose(ps_t[:, j * 128:j * 128 + 65], ob[0:65, s_ * 128:(s_ + 1) * 128],
                                                                            ident[0:65, 0:65]), r=[d_ob, d_ident], w=[d_ps_t])
                    for j in range(nmaps):
                        o_, d_o = on[j]
                        p.op("dve", lambda h, j=j: h.reciprocal(out=rc[:, j:j + 1], in_=ps_t[:, j * 128 + 64:j * 128 + 65]),
                             r=[d_ps_t], w=[d_rc])
                        p.op("dve", lambda h, o_=o_, j=j: h.tensor_scalar(out=o_[:], in0=ps_t[:, j * 128:j * 128 + 64],
                                                                          scalar1=rc[:, j:j + 1], scalar2=None,
                                                                          op0=ALU.mult), r=[d_ps_t, d_rc], w=[d_o])
                    if kind == "A":
                        p.op("dve", lambda h: h.scalar_tensor_tensor(out=od[:], in0=on[1][0][:], scalar=cst[:, 4:5], in1=on[0][0][:],
                                                                     op0=ALU.mult, op1=ALU.add), r=[on[0][1], on[1][1], d_cst], w=[d_od])
                        p.op("act", lambda h: h.activation(out=junk[:], in_=od[:], func=AF.Square, accum_out=st[:, 0:1]),
                             r=[d_od], w=[d_junk, d_st])
                        rstd(p, st[:, 2:3], st[:, 0:1], st[:, 1:2], 1.0 / 64, [d_st])
                        p.op("dve", lambda h, s_=s_: h.scalar_tensor_tensor(out=ystage[:, s_, :], in0=od[:], scalar=st[:, 2:3], in1=subg[:],
                                                                            op0=ALU.mult, op1=ALU.mult), r=[d_od, d_st, d_subg], w=[d_ys])
                    else:
                        p.op("dve", lambda h, s_=s_: h.tensor_copy(out=ystage[:, s_, :], in_=on[0][0][:]), r=[on[0][1]], w=[d_ys])
                ydst = Z["ym"][q0 - 256:q0 - 256 + qn, ycol:ycol + 64] if mine else Z["y"][q0:q0 + qn, ycol:ycol + 64]
                p.dma("sp", ydst.rearrange("(s q) d -> q s d", q=128), ystage[:, 0:nsub, :], r=[d_ys])

        def load_kv(ksrc_rows, vsrc_cols):
            p.dma("sp", kT[:, :], ksrc_rows, w=[d_kT])
            p.dma("sp", va[:, :, 0:64], vsrc_cols.rearrange("(n q) d -> q n d", q=128), w=[d_va])
            p.op("dve", lambda h: h.memset(va[:, :, 64:65], 1.0), w=[d_va])

        for hd in range(4):
            load_kv(Z["kTa"][64 * hd:64 * hd + 64, :], Z["vA"][:, 64 * hd:64 * hd + 64])
            run_head("A", Z["qTa"][64 * hd:64 * hd + 64, :], 2, 1.0 / math.sqrt(32.0), cst[:, 10:11], 64 * hd)
        for kv in range(2):
            load_kv(Z["kTb"][64 * kv:64 * kv + 64, :], Z["vB"][:, 64 * kv:64 * kv + 64])
            for g in (2 * kv, 2 * kv + 1):
                run_head("B", Z["qTb"][64 * g:64 * g + 64, :], 1, 0.125, cst[:, 11:12], 256 + 64 * g)


ORDER = [list(range(NT)), [1, 0] + list(range(NT - 1, 1, -1))]
NGC = 40


class PsumPool:
    def __init__(self, sb, nbanks=8):
        self.q = []
        banks = [sb.ps() for b in range(nbanks)]
        for k in range(4):
            for (t, d) in banks:
                self.q.append((t[:, k * 128:(k + 1) * 128], d))
        self.i = 0

    def get(self):
        r = self.q[self.i % len(self.q)]
        self.i += 1
        return r


def phase_gdn_prep(p, l, I, S, Z):
    ident, d_ident = S["ident"]
    W = 2 + 256 + 4 + 4096 + 2
    with SB(p) as sb:
        cw, d_cw = sb.t((128, 6, 5))
        p.dma("sp", cw[:], I["gdn_convT"][:, l, :, :], w=[d_cw])
        bones, d_bones = sb.t((128, 128))
        p.op("dve", lambda h: h.memset(bones[:], 0.0), w=[d_bones])
        p.op("dve", lambda h: h.memset(bones[0:64, 0:64], 1.0), w=[d_bones])
        p.op("dve", lambda h: h.memset(bones[64:128, 64:128], 1.0), w=[d_bones])
        X, d_X = sb.t((128, W))
        acc, d_acc = sb.t((128, W))
        sq, d_sq = sb.t((128, 512))
        rs, d_rs = sb.t((128, 512))
        pss = [sb.ps() for _ in range(2)]
        pst = [sb.ps() for _ in range(2)]
        tst = [sb.t((128, 128)) for _ in range(2)]
        p.op("dve", lambda h: h.memset(X[:], 0.0), w=[d_X])
        nb = 0
        for fc in range(6):
            p.dma("sp", X[:, 2:258], Z["qkvT"][fc * 128:(fc + 1) * 128, 0:256], w=[d_X])
            p.dma("sp", X[:, 262:4358], Z["qkvT"][fc * 128:(fc + 1) * 128, 256:T], w=[d_X])
            lo, hi = 2, 4358
            p.op("dve", lambda h, fc=fc: h.tensor_scalar(out=acc[:, lo:hi], in0=X[:, lo - 2:hi - 2], scalar1=cw[:, fc, 0:1], scalar2=None,
                                                         op0=ALU.mult), r=[d_X, d_cw], w=[d_acc])
            for tap in range(1, 5):
                eng = "dve"
                p.op(eng, lambda h, fc=fc, tap=tap: h.scalar_tensor_tensor(out=acc[:, lo:hi], in0=X[:, lo + tap - 2:hi + tap - 2],
                                                                             scalar=cw[:, fc, tap:tap + 1], in1=acc[:, lo:hi],
                                                                             op0=ALU.mult, op1=ALU.add), r=[d_X, d_cw, d_acc], w=[d_acc])
            p.op("act", lambda h: h.activation(out=X[:, lo:hi], in_=acc[:, lo:hi], func=AF.Sigmoid), r=[d_acc], w=[d_X])
            p.op("dve", lambda h: h.tensor_tensor(out=acc[:, lo:hi], in0=acc[:, lo:hi], in1=X[:, lo:hi], op=ALU.mult), r=[d_X, d_acc], w=[d_acc])
            segs = [(2, 256, 0)] + [(262 + 512 * i, 512, 256 + 512 * i) for i in range(8)]
            if fc < 4:
                for (c0, n, t0) in segs:
                    ps, d_ps = pss[nb % 2]
                    nb += 1
                    p.op("act", lambda h: h.activation(out=sq[:, 0:n], in_=acc[:, c0:c0 + n], func=AF.Square), r=[d_acc], w=[d_sq])
                    p.op("pe", lambda h, ps=ps: h.matmul(ps[:, 0:n], lhsT=bones[:], rhs=sq[:, 0:n], start=True, stop=True),
                         r=[d_bones, d_sq], w=[d_ps])
                    p.op("act", lambda h, ps=ps: h.activation(out=rs[:, 0:n], in_=ps[:, 0:n], func=AF.Sqrt, scale=1.0, bias=p.eps_ap(EPS, 128)),
                         r=[d_ps, p.d_eps], w=[d_rs])
                    p.op("dve", lambda h: h.reciprocal(out=rs[:, 0:n], in_=rs[:, 0:n]), r=[d_rs], w=[d_rs])
                    p.op("dve", lambda h: h.scalar_tensor_tensor(out=acc[:, c0:c0 + n], in0=acc[:, c0:c0 + n], scalar=(0.125 if fc < 2 else 1.0),
                                                                 in1=rs[:, 0:n], op0=ALU.mult, op1=ALU.mult), r=[d_acc, d_rs], w=[d_acc])
                dst = Z["gqT"] if fc < 2 else Z["gkT"]
                r0 = (fc % 2) * 128
                p.dma("sp", dst[r0:r0 + 128, 0:256], acc[:, 2:258], r=[d_acc])
                p.dma("sp", dst[r0:r0 + 128, 256:T], acc[:, 262:4358], r=[d_acc])
            if fc >= 2:
                dst = Z["gk"] if fc < 4 else Z["gv"]
                r0 = (fc % 2) * 128
                for tt in range(NT):
                    c0 = 2 + tt * 128 if tt < 2 else 262 + (tt - 2) * 128
                    ps, d_ps = pst[tt % 2]
                    ts_, d_ts = tst[tt % 2]
                    p.op("pe", lambda h, ps=ps, c0=c0: h.transpose(ps[:, 0:128], acc[:, c0:c0 + 128], ident[:]), r=[d_acc, d_ident], w=[d_ps])
                    p.op("act", lambda h, ps=ps, ts_=ts_: h.activation(out=ts_[:], in_=ps[:, 0:128], func=AF.Copy), r=[d_ps], w=[d_ts])
                    p.dma("sp", dst[tt * 128:(tt + 1) * 128, r0:r0 + 128], ts_[:], r=[d_ts])


def softplus_parts(p, z, d_z, tmp, d_tmp, n):
    p.op("dve", lambda h: h.tensor_scalar(out=tmp[:, n:2 * n], in0=z, scalar1=-1.0, scalar2=None, op0=ALU.mult), r=[d_z], w=[d_tmp])
    p.op("dve", lambda h: h.tensor_tensor(out=tmp[:, n:2 * n], in0=tmp[:, n:2 * n], in1=z, op=ALU.min), r=[d_z, d_tmp], w=[d_tmp])
    p.op("act", lambda h: h.activation(out=tmp[:, n:2 * n], in_=tmp[:, n:2 * n], func=AF.Exp), r=[d_tmp], w=[d_tmp])
    p.op("act", lambda h: h.activation(out=tmp[:, 0:n], in_=tmp[:, n:2 * n], func=AF.Ln, scale=1.0, bias=p.ones1[:, 0:1]),
         r=[d_tmp, p.d_ones1], w=[d_tmp])


def phase_gates(p, l, I, S, Z, tab, d_tab, cm, d_cm, ones, d_ones):
    ident, d_ident = S["ident"]
    with SB(p) as sb:
        pp = PsumPool(sb, 4)
        biasC, d_biasC = bcast_load(p, sb, I["mlstm_gate_bias"][l:l + 1, :], 16)
        dtb, d_dtb = bcast_load(p, sb, I["gdn_dt_bias"][l:l + 1, :], 8)
        nea, d_nea = bcast_load(p, sb, I["gdn_a_log"][l:l + 1, :], 8)
        p.op("act", lambda h: h.activation(out=nea[:], in_=nea[:], func=AF.Exp), r=[d_nea], w=[d_nea])
        p.op("dve", lambda h: h.tensor_scalar(out=nea[:], in0=nea[:], scalar1=-1.0, scalar2=None, op0=ALU.mult), r=[d_nea], w=[d_nea])
        gts = [sb.t((128, 32)) for _ in range(2)]
        for d in range(2):
            Bprev, d_B = sb.t((128, 4))
            R, d_R = sb.t((128, 4))
            p.op("dve", lambda h: h.memset(Bprev[:], 0.0), w=[d_B])
            p.op("dve", lambda h: h.memset(R[:], 0.0), w=[d_R])
            for s_, tt in enumerate(ORDER[d]):
                g, d_g = gts[s_ % 2]
                p.dma("sp", g[:, 0:16], Z["gC"][tt * 128:(tt + 1) * 128, :], w=[d_g])
                p.dma("sp", g[:, 16:32], Z["gD"][tt * 128:(tt + 1) * 128, :], w=[d_g])
                wk, d_wk = S["gwk"][s_ % 2]
                lhs_cum = cm[:, 1 - d, :]
                T_ = lambda a, b: tab[:, tt, d, a:b]
                xf = wk[:, 0:4]
                ig = wk[:, 4:8]
                p.op("dve", lambda h: h.tensor_tensor(out=xf, in0=g[:, 8 * d + 4:8 * d + 8], in1=biasC[:, 8 * d + 4:8 * d + 8], op=ALU.add),
                     r=[d_g, d_biasC], w=[d_wk])
                p.op("dve", lambda h: h.tensor_tensor(out=ig, in0=g[:, 8 * d:8 * d + 4], in1=biasC[:, 8 * d:8 * d + 4], op=ALU.add),
                     r=[d_g, d_biasC], w=[d_wk])
                softplus_parts(p, xf, d_wk, wk[:, 8:16], d_wk, 4)
                logf = wk[:, 16:20]
                p.op("dve", lambda h: h.scalar_tensor_tensor(out=logf, in0=xf, scalar=0.0, in1=wk[:, 8:12], op0=ALU.min, op1=ALU.subtract),
                     r=[d_wk], w=[d_wk])
                pcs, d_pcs = pp.get()
                ptot, d_ptot = pp.get()
                p.op("pe", lambda h: h.matmul(pcs[:, 0:4], lhsT=lhs_cum, rhs=logf, start=True, stop=True), r=[d_cm, d_wk], w=[d_pcs])
                p.op("pe", lambda h: h.matmul(ptot[:, 0:4], lhsT=ones[:], rhs=logf, start=True, stop=True), r=[d_ones, d_wk], w=[d_ptot])
                Bv = wk[:, 20:24]
                av = wk[:, 24:28]
                p.op("dve", lambda h: h.tensor_tensor(out=Bv, in0=pcs[:, 0:4], in1=Bprev[:], op=ALU.add), r=[d_pcs, d_B], w=[d_wk])
                p.op("dve", lambda h: h.tensor_tensor(out=av, in0=ig, in1=Bv, op=ALU.subtract), r=[d_wk], w=[d_wk])
                p.op("dve", lambda h: h.tensor_tensor(out=Bprev[:], in0=ptot[:, 0:4], in1=Bprev[:], op=ALU.add), r=[d_ptot, d_B], w=[d_B])
                ptr, d_ptr = pp.get()
                p.op("pe", lambda h: h.transpose(ptr[0:4, 0:128], av, ident[:]), r=[d_wk, d_ident], w=[d_ptr])
                am, d_am = S["gam4"]
                p.op("dve", lambda h: h.tensor_reduce(out=am[0:4, 0:1], in_=ptr[0:4, 0:128], axis=AX.X, op=ALU.max), r=[d_ptr], w=[d_am])
                p.op("dve", lambda h: h.tensor_scalar(out=am[0:4, 4:8], in0=ident[0:4, 0:4], scalar1=am[0:4, 0:1], scalar2=None, op0=ALU.mult),
                     r=[d_am, d_ident], w=[d_am])
                pam, d_pam = pp.get()
                p.op("pe", lambda h: h.matmul(pam[:, 0:4], lhsT=ones[0:4, :], rhs=am[0:4, 4:8], start=True, stop=True),
                     r=[d_ones, d_am], w=[d_pam])
                Mc = wk[:, 28:32]
                p.op("dve", lambda h: h.tensor_tensor(out=Mc, in0=pam[:, 0:4], in1=R[:], op=ALU.max), r=[d_pam, d_R], w=[d_wk])
                p.op("dve", lambda h: h.tensor_tensor(out=wk[:, 32:36], in0=R[:], in1=Mc, op=ALU.subtract), r=[d_R, d_wk], w=[d_wk])
                p.op("dve", lambda h: h.tensor_tensor(out=wk[:, 36:40], in0=av, in1=Mc, op=ALU.subtract), r=[d_wk], w=[d_wk])
                p.op("dve", lambda h: h.tensor_tensor(out=wk[:, 40:44], in0=Bv, in1=Mc, op=ALU.add), r=[d_wk], w=[d_wk])
                p.op("dve", lambda h: h.tensor_copy(out=R[:], in_=Mc), r=[d_wk], w=[d_R])
                p.op("act", lambda h: h.activation(out=T_(8, 12), in_=wk[:, 32:36], func=AF.Exp), r=[d_wk], w=[d_tab])
                p.op("act", lambda h: h.activation(out=T_(0, 4), in_=wk[:, 36:40], func=AF.Exp), r=[d_wk], w=[d_tab])
                p.op("act", lambda h: h.activation(out=T_(4, 8), in_=wk[:, 40:44], func=AF.Exp, scale=-1.0), r=[d_wk], w=[d_tab])
                z = wk[:, 44:48]
                p.op("dve", lambda h: h.tensor_tensor(out=z, in0=g[:, 16 + 8 * d + 4:16 + 8 * d + 8], in1=dtb[:, 4 * d:4 * d + 4], op=ALU.add),
                     r=[d_g, d_dtb], w=[d_wk])
                softplus_parts(p, z, d_wk, wk[:, 48:56], d_wk, 4)
                gg = wk[:, 56:60]
                p.op("dve", lambda h: h.scalar_tensor_tensor(out=gg, in0=z, scalar=0.0, in1=wk[:, 48:52], op0=ALU.max, op1=ALU.add),
                     r=[d_wk], w=[d_wk])
                p.op("dve", lambda h: h.tensor_tensor(out=gg, in0=gg, in1=nea[:, 4 * d:4 * d + 4], op=ALU.mult), r=[d_wk, d_nea], w=[d_wk])
                p.op("act", lambda h: h.activation(out=T_(12, 16), in_=g[:, 16 + 8 * d:16 + 8 * d + 4], func=AF.Sigmoid), r=[d_g], w=[d_tab])
                p.op("dve", lambda h: h.tensor_scalar(out=T_(16, 20), in0=T_(12, 16), scalar1=-1.0, scalar2=None, op0=ALU.mult),
                     r=[d_tab], w=[d_tab])
                pgm, d_pgm = pp.get()
                pgl, d_pgl = pp.get()
                p.op("pe", lambda h: h.matmul(pgm[:, 0:4], lhsT=lhs_cum, rhs=gg, start=True, stop=True), r=[d_cm, d_wk], w=[d_pgm])
                p.op("pe", lambda h: h.matmul(pgl[:, 0:4], lhsT=ones[:], rhs=gg, start=True, stop=True), r=[d_ones, d_wk], w=[d_pgl])
                p.op("dve", lambda h: h.tensor_copy(out=T_(20, 24), in_=pgm[:, 0:4]), r=[d_pgm], w=[d_tab])
                p.op("dve", lambda h: h.tensor_tensor(out=wk[:, 60:64], in0=pgl[:, 0:4], in1=T_(20, 24), op=ALU.subtract),
                     r=[d_pgl, d_tab], w=[d_wk])
                p.op("act", lambda h: h.activation(out=T_(24, 28), in_=T_(20, 24), func=AF.Exp), r=[d_tab], w=[d_tab])
                p.op("act", lambda h: h.activation(out=T_(28, 32), in_=wk[:, 60:64], func=AF.Exp), r=[d_wk], w=[d_tab])
                p.op("act", lambda h: h.activation(out=T_(32, 36), in_=pgl[:, 0:4], func=AF.Exp), r=[d_pgl], w=[d_tab])
                p.op("dve", lambda h: h.tensor_tensor(out=T_(36, 40), in0=T_(12, 16), in1=T_(24, 28), op=ALU.mult), r=[d_tab], w=[d_tab])


def phase_chunk(p, l, I, S, Z, do_c=True, do_d=True, upto=""):
    ident, d_ident = S["ident"]
    phase_gdn_prep(p, l, I, S, Z)
    if "stopprep" in upto:
        return
    with SB(p) as sb:
        tab, d_tab = sb.t((128, NT, 2, NGC), F32, "gtab")
        cm, d_cm = sb.t((128, 2, 128), F32, "cmask")
        p.dma("sp", cm[:], I["cmask"][:, :, :], w=[d_cm])
        ones, d_ones = sb.t((128, 128))
        p.op("dve", lambda h: h.memset(ones[:], 1.0), w=[d_ones])
        ones1, p.d_ones1 = sb.t((128, 1))
        p.ones1 = ones1
        p.op("dve", lambda h: h.memset(ones1[:], 1.0), w=[p.d_ones1])
        S["gwk"] = [sb.t((128, 64)) for _ in range(2)]
        S["gam4"] = sb.t((8, 8))
        strict, d_strict = sb.t((128, 2, 128))
        for d in range(2):
            p.op("dve", lambda h, d=d: h.tensor_tensor(out=strict[:, d, :], in0=cm[:, d, :], in1=ident[:], op=ALU.subtract),
                 r=[d_cm, d_ident], w=[d_strict])
        phase_gates(p, l, I, S, Z, tab, d_tab, cm, d_cm, ones, d_ones)
        if "stopgates" in upto:
            return
        acc, d_acc = sb.t((128, NT, 256), F32, "hacc")
        if do_c:
            p.op("dve", lambda h: h.memset(acc[:], 0.0), w=[d_acc])
            chunk_c(p, l, I, S, Z, sb, tab, d_tab, cm, d_cm, acc, d_acc)
            p.barrier()
        if do_d:
            p.op("dve", lambda h: h.memset(acc[:], 0.0), w=[d_acc])
            chunk_d(p, l, I, S, Z, sb, tab, d_tab, cm, d_cm, strict, d_strict, ones, d_ones, acc, d_acc)


def chunk_c(p, l, I, S, Z, sbo, tab, d_tab, cm, d_cm, acc, d_acc):
    ident, d_ident = S["ident"]
    with SB(p) as sb:
        two = lambda shape, nm: [sb.t(shape, F32, nm) for _ in range(2)]
        ps_st = [sb.ps() for _ in range(2)]
        ps_o = [sb.ps() for _ in range(2)]
        ps_c = [sb.ps() for _ in range(2)]
        qf = [two((64, 4, 128), "cqf") for _ in range(2)]
        kf = [two((64, 4, 128), "ckf") for _ in range(2)]
        ktm = [two((128, 4, 64), "cktm") for _ in range(2)]
        vaug = [two((128, 4, 66), "cvaug") for _ in range(2)]
        for d in range(2):
            for q_ in range(2):
                va_, d_va_ = vaug[d][q_]
                p.op("dve", lambda h, va_=va_: h.memset(va_[:, :, 64:65], 1.0), w=[d_va_])
                p.op("dve", lambda h, va_=va_: h.memset(va_[:, :, 65:66], 0.0), w=[d_va_])
        ks, PT, vp, htmp = two((128, 4, 64), "cks"), two((128, 4, 128), "cPT"), two((128, 4, 66), "cvp"), two((128, 4, 64), "chtmp")
        rc = two((128, 8), "crc")
        Cst, d_Cst = sb.t((64, 8, 66), F32, "cCst")
        Cd, d_Cd = sb.t((64, 8, 66), F32, "cCd")
        p.op("dve", lambda h: h.memset(Cst[:], 0.0), w=[d_Cst])
        B3 = lambda t: t[:, :].rearrange("q (h c) -> q h c", h=4)
        O3 = lambda t: t[:, 0:264].rearrange("q (h c) -> q h c", h=4)
        for s_ in range(NT):
            par = s_ % 2
            tts = [ORDER[d][s_] for d in range(2)]
            for d in range(2):
                tk = slice(tts[d] * 128, (tts[d] + 1) * 128)
                p.dma("sp", qf[d][par][0][:], Z["qTc"][:, tk].rearrange("(h q) t -> q h t", q=64), w=[qf[d][par][1]])
                p.dma("sp", kf[d][par][0][:], Z["kTc"][:, tk].rearrange("(h q) t -> q h t", q=64), w=[kf[d][par][1]])
                p.dma("sp", ktm[d][par][0][:], Z["kC"][tk, :].rearrange("q (h e) -> q h e", e=64), w=[ktm[d][par][1]])
                p.dma("sp", vaug[d][par][0][:, :, 0:64], Z["vC"][tk, :].rearrange("q (h e) -> q h e", e=64), w=[vaug[d][par][1]])
            for d in range(2):
                tt = tts[d]
                qf_, d_qf = qf[d][par]
                kf_, d_kf = kf[d][par]
                pst, d_pst = ps_st[d]
                for hd in range(4):
                    p.op("pe", lambda h, pst=pst, hd=hd, kf_=kf_, qf_=qf_: h.matmul(pst[:, hd * 128:(hd + 1) * 128], lhsT=kf_[:, hd, :], rhs=qf_[:, hd, :],
                                                                                  start=True, stop=True), r=[d_kf, d_qf], w=[d_pst])
                PT_, d_PT = PT[d]
                p.op("dve", lambda h, pst=pst, PT_=PT_, d=d: h.scalar_tensor_tensor(out=PT_[:], in0=B3(pst), scalar=0.125,
                                                                                 in1=cm[:, 1 - d, :].unsqueeze(1).to_broadcast([128, 4, 128]),
                                                                                 op0=ALU.mult, op1=ALU.mult), r=[d_pst, d_cm], w=[d_PT])
                vp_, d_vp = vp[d]
                va_, d_va_ = vaug[d][par]
                p.op("pool", lambda h, vp_=vp_, va_=va_, tt=tt, d=d: h.tensor_tensor(out=vp_[:], in0=va_[:],
                                                                                  in1=tab[:, tt, d, 0:4].unsqueeze(2).to_broadcast([128, 4, 66]),
                                                                                  op=ALU.mult), r=[d_va_, d_tab], w=[d_vp])
                ks_, d_ks = ks[d]
                kt_, d_kt = ktm[d][par]
                p.op("pool", lambda h, ks_=ks_, kt_=kt_: h.tensor_scalar(out=ks_[:], in0=kt_[:], scalar1=0.125, scalar2=None, op0=ALU.mult),
                     r=[d_kt], w=[d_ks])
                p.op("dve", lambda h, tt=tt, d=d: h.tensor_tensor(out=Cd[:, 4 * d:4 * d + 4, :], in0=Cst[:, 4 * d:4 * d + 4, :],
                                                                  in1=tab[0:64, tt, d, 8:12].unsqueeze(2).to_broadcast([64, 4, 66]), op=ALU.mult),
                     r=[d_Cst, d_tab], w=[d_Cd])
            for d in range(2):
                qf_, d_qf = qf[d][par]
                po, d_po = ps_o[d]
                pc, d_pc = ps_c[d]
                PT_, d_PT = PT[d]
                vp_, d_vp = vp[d]
                ks_, d_ks = ks[d]
                for hd in range(4):
                    i = 4 * d + hd
                    p.op("pe", lambda h, po=po, hd=hd, PT_=PT_, vp_=vp_: h.matmul(po[:, hd * 66:(hd + 1) * 66], lhsT=PT_[:, hd, :], rhs=vp_[:, hd, :],
                                                                                start=True, stop=False), r=[d_PT, d_vp], w=[d_po])
                    p.op("pe", lambda h, po=po, hd=hd, i=i, qf_=qf_: h.matmul(po[:, hd * 66:(hd + 1) * 66], lhsT=qf_[:, hd, :], rhs=Cd[:, i, :],
                                                                             start=False, stop=True), r=[d_qf, d_Cd], w=[d_po])
                for hd in range(4):
                    p.op("pe", lambda h, pc=pc, hd=hd, ks_=ks_, vp_=vp_: h.matmul(pc[0:64, hd * 66:(hd + 1) * 66], lhsT=ks_[:, hd, :], rhs=vp_[:, hd, :],
                                                                                start=True, stop=True), r=[d_ks, d_vp], w=[d_pc])
                p.op("dve", lambda h, pc=pc, d=d: h.tensor_tensor(out=Cst[:, 4 * d:4 * d + 4, :],
                                                                  in0=pc[0:64, 0:264].rearrange("q (h c) -> q h c", h=4),
                                                                  in1=Cd[:, 4 * d:4 * d + 4, :], op=ALU.add), r=[d_pc, d_Cd], w=[d_Cst])
            for d in range(2):
                tt = tts[d]
                po, d_po = ps_o[d]
                rc_, d_rc = rc[d]
                ht_, d_ht = htmp[d]
                p.op("act", lambda h, po=po, rc_=rc_: h.activation(out=rc_[:, 0:4], in_=O3(po)[:, :, 64], func=AF.Abs), r=[d_po], w=[d_rc])
                p.op("dve", lambda h, rc_=rc_, tt=tt, d=d: h.tensor_tensor(out=rc_[:, 0:4], in0=rc_[:, 0:4], in1=tab[:, tt, d, 4:8], op=ALU.max),
                     r=[d_rc, d_tab], w=[d_rc])
                p.op("dve", lambda h, rc_=rc_: h.reciprocal(out=rc_[:, 4:8], in_=rc_[:, 0:4]), r=[d_rc], w=[d_rc])
                p.op("dve", lambda h, po=po, rc_=rc_, ht_=ht_: h.tensor_tensor(out=ht_[:], in0=O3(po)[:, :, 0:64],
                                                                               in1=rc_[:, 4:8].unsqueeze(2).to_broadcast([128, 4, 64]), op=ALU.mult),
                     r=[d_po, d_rc], w=[d_ht])
                dst = acc[:, tt, :].rearrange("q (h e) -> q h e", e=64)
                p.op("pool", lambda h, dst=dst, ht_=ht_: h.tensor_tensor(out=dst, in0=dst, in1=ht_[:], op=ALU.add), r=[d_ht, d_acc], w=[d_acc])
        gain, d_gain = bcast_load(p, sb, I["mlstm_norm"][l:l + 1, :], 256)
        ots = [sb.t((128, 256)) for _ in range(2)]
        xcs = [sb.t((128, 256)) for _ in range(2)]
        sqs = [sb.t((128, 256)) for _ in range(2)]
        sts = [sb.t((128, 12)) for _ in range(2)]
        for tt in range(NT):
            ot, d_ot = ots[tt % 2]
            xc, d_xc = xcs[tt % 2]
            sq, d_sq = sqs[tt % 2]
            st, d_st = sts[tt % 2]
            p.dma("sp", ot[:], Z["oC"][tt * 128:(tt + 1) * 128, :], w=[d_ot])
            p.op("act", lambda h, ot=ot: h.activation(out=ot[:], in_=ot[:], func=AF.Sigmoid), r=[d_ot], w=[d_ot])
            hv = acc[:, tt, :].rearrange("q (g e) -> q g e", e=64)
            v3 = lambda a: a[:, :].rearrange("q (g e) -> q g e", e=64)
            p.op("dve", lambda h, st=st, hv=hv: h.tensor_reduce(out=st[:, 0:4], in_=hv, axis=AX.X, op=ALU.add), r=[d_acc], w=[d_st])
            p.op("dve", lambda h, st=st: h.tensor_scalar(out=st[:, 0:4], in0=st[:, 0:4], scalar1=1.0 / 64, scalar2=None, op0=ALU.mult),
                 r=[d_st], w=[d_st])
            p.op("dve", lambda h, st=st, hv=hv, xc=xc: h.tensor_tensor(out=v3(xc), in0=hv, in1=st[:, 0:4].unsqueeze(2).to_broadcast([128, 4, 64]),
                                                                       op=ALU.subtract), r=[d_acc, d_st], w=[d_xc])
            p.op("act", lambda h, sq=sq, xc=xc: h.activation(out=sq[:], in_=xc[:], func=AF.Square), r=[d_xc], w=[d_sq])
            p.op("dve", lambda h, st=st, sq=sq: h.tensor_reduce(out=st[:, 4:8], in_=v3(sq), axis=AX.X, op=ALU.add), r=[d_sq], w=[d_st])
            rstd(p, st[:, 8:12], st[:, 4:8], st[:, 4:8], 1.0 / 64, [d_st])
            p.op("dve", lambda h, st=st, xc=xc: h.tensor_tensor(out=v3(xc), in0=v3(xc), in1=st[:, 8:12].unsqueeze(2).to_broadcast([128, 4, 64]),
                                                                op=ALU.mult), r=[d_st, d_xc], w=[d_xc])
            p.op("dve", lambda h, xc=xc: h.tensor_tensor(out=xc[:], in0=xc[:], in1=gain[:], op=ALU.mult), r=[d_gain, d_xc], w=[d_xc])
            p.op("dve", lambda h, xc=xc, ot=ot: h.tensor_tensor(out=xc[:], in0=xc[:], in1=ot[:], op=ALU.mult), r=[d_ot, d_xc], w=[d_xc])
            p.dma("sp", Z["y"][tt * 128:(tt + 1) * 128, 512:768], xc[:], r=[d_xc])


def chunk_d(p, l, I, S, Z, sbo, tab, d_tab, cm, d_cm, strict, d_strict, ones, d_ones, acc, d_acc):
    ident, d_ident = S["ident"]
    with SB(p) as sb:
        bk = [sb.ps() for _ in range(8)]
        B3 = lambda t: t[:, :].rearrange("q (h c) -> q h c", h=4)
        two = lambda shape, nm, dt=F32: [sb.t(shape, dt, nm) for _ in range(2)]
        qf = [two((64, 4, 128), "qf") for _ in range(2)]
        kf = [two((64, 4, 128), "kf") for _ in range(2)]
        ktm = [two((128, 4, 64), "ktm") for _ in range(2)]
        vtm = [two((128, 4, 64), "vtm") for _ in range(2)]
        diag, Ed, e1, e2, tmpP = two((128, 4, 128), "diag"), two((128, 4, 128), "Ed"), two((128, 4, 128), "e1"), two((128, 4, 128), "e2"), two((128, 4, 128), "tmpP")
        decT = [two((128, 4, 128), "decT") for _ in range(2)]
        decS = two((128, 4, 128), "decS")
        PQ = [two((128, 8, 128), "PQ") for _ in range(2)]
        TTt = [two((128, 4, 128), "TT") for _ in range(2)]
        Ru, Rw, kdec = two((128, 4, 64), "Ru"), two((128, 4, 64), "Rw"), two((128, 4, 64), "kdec")
        u_all, d_u = sb.t((128, 8, 64), F32, "uall")
        vnew, d_vnew = sb.t((128, 8, 64), F32, "vnew")
        wT = two((64, 4, 128), "wT")
        attnT = two((128, 4, 128), "attnT")
        Sst, d_S = sb.t((64, 8, 64), F32, "Sst")
        otmp = two((128, 4, 64), "otmp")
        p.op("dve", lambda h: h.memset(Sst[:], 0.0), w=[d_S])
        bc_col = lambda ap: ap.unsqueeze(2).to_broadcast([128, 4, 128])
        bc_c64 = lambda ap, n=128: ap.unsqueeze(2).to_broadcast([n, 4, 64])
        bc_mat = lambda ap: ap.unsqueeze(1).to_broadcast([128, 4, 128])
        for s_ in range(NT):
            par = s_ % 2
            tts = [ORDER[d][s_] for d in range(2)]
            col = lambda d, g: tab[:, tts[d], d, 4 * g:4 * g + 4]
            for d in range(2):
                tt = tts[d]
                tk = slice(tt * 128, (tt + 1) * 128)
                p.dma("sp", qf[d][par][0][:], Z["gqT"][:, tk].rearrange("(h q) t -> q h t", q=64), w=[qf[d][par][1]])
                p.dma("sp", kf[d][par][0][:], Z["gkT"][:, tk].rearrange("(h q) t -> q h t", q=64), w=[kf[d][par][1]])
                p.dma("sp", ktm[d][par][0][:], Z["gk"][tk, :].rearrange("q (h e) -> q h e", e=64), w=[ktm[d][par][1]])
                p.dma("sp", vtm[d][par][0][:], Z["gv"][tk, :].rearrange("q (h e) -> q h e", e=64), w=[vtm[d][par][1]])
            for d in range(2):
                dg, d_dg = diag[d]
                p.op("dve", lambda h, d=d, dg=dg: h.tensor_tensor(out=dg[:], in0=bc_mat(ident[:, :]), in1=bc_col(col(d, 5)), op=ALU.mult),
                     r=[d_ident, d_tab], w=[d_dg])
                pg, d_pg = bk[d]
                p.op("pe", lambda h, pg=pg, dg=dg: h.matmul(pg[:, :], lhsT=ones[:], rhs=dg[:, :, :].rearrange("q h c -> q (h c)"), start=True, stop=True),
                     r=[d_ones, d_dg], w=[d_pg])
                E_, d_E = Ed[d]
                p.op("dve", lambda h, d=d, pg=pg, E_=E_: h.tensor_tensor(out=E_[:], in0=B3(pg), in1=bc_col(col(d, 5)), op=ALU.subtract),
                     r=[d_pg, d_tab], w=[d_E])
                a1, d_a1 = e1[d]
                a2, d_a2 = e2[d]
                p.op("dve", lambda h, E_=E_, a1=a1: h.tensor_scalar(out=a1[:], in0=E_[:], scalar1=0.0, scalar2=None, op0=ALU.min), r=[d_E], w=[d_a1])
                p.op("pool", lambda h, E_=E_, a2=a2: h.tensor_scalar(out=a2[:], in0=E_[:], scalar1=0.0, scalar2=None, op0=ALU.max), r=[d_E], w=[d_a2])
                p.op("act", lambda h, a1=a1: h.activation(out=a1[:], in_=a1[:], func=AF.Exp), r=[d_a1], w=[d_a1])
                p.op("act", lambda h, a2=a2: h.activation(out=a2[:], in_=a2[:], func=AF.Exp, scale=-1.0), r=[d_a2], w=[d_a2])
                dT, d_dT = decT[d][par]
                dS, d_dS = decS[d]
                p.op("pool", lambda h, d=d, a1=a1, dT=dT: h.tensor_tensor(out=dT[:], in0=a1[:], in1=bc_mat(cm[:, 1 - d, :]), op=ALU.mult),
                     r=[d_a1, d_cm], w=[d_dT])
                p.op("pool", lambda h, d=d, a2=a2, dS=dS: h.tensor_tensor(out=dS[:], in0=a2[:], in1=bc_mat(strict[:, d, :]), op=ALU.mult),
                     r=[d_a2, d_strict], w=[d_dS])
            for d in range(2):
                kf_, d_kf = kf[d][par]
                pG, d_pG = bk[2 + d]
                for hd in range(4):
                    p.op("pe", lambda h, pG=pG, hd=hd, kf_=kf_: h.matmul(pG[:, hd * 128:(hd + 1) * 128], lhsT=kf_[:, hd, :], rhs=kf_[:, hd, :],
                                                                        start=True, stop=True), r=[d_kf], w=[d_pG])
                tp_, d_tp = tmpP[d]
                p.op("dve", lambda h, d=d, pG=pG, tp_=tp_: h.tensor_tensor(out=tp_[:], in0=B3(pG), in1=bc_col(col(d, 4)), op=ALU.mult),
                     r=[d_pG, d_tab], w=[d_tp])
                pq_, d_pq = PQ[d][0]
                p.op("pool", lambda h, d=d, tp_=tp_, pq_=pq_: h.tensor_tensor(out=pq_[:, 0:4, :], in0=tp_[:], in1=decS[d][0][:], op=ALU.mult),
                     r=[d_tp, decS[d][1]], w=[d_pq])
                pQ, d_pQ = bk[4 + d]
                for hd in range(4):
                    p.op("pe", lambda h, pQ=pQ, hd=hd, pq_=pq_: h.transpose(pQ[:, hd * 128:(hd + 1) * 128], pq_[:, hd, :], ident[:]),
                         r=[d_pq, d_ident], w=[d_pQ])
                p.op("act", lambda h, pQ=pQ, pq_=pq_: h.activation(out=pq_[:, 4:8, :], in_=B3(pQ), func=AF.Copy), r=[d_pQ], w=[d_pq])
                tt0, d_tt0 = TTt[d][0]
                p.op("dve", lambda h, pQ=pQ, tt0=tt0: h.tensor_tensor(out=tt0[:], in0=B3(pQ), in1=bc_mat(ident[:, :]), op=ALU.add),
                     r=[d_pQ, d_ident], w=[d_tt0])
            for k in range(1, 7):
                for d in range(2):
                    c_, d_c = PQ[d][(k - 1) % 2]
                    n_, d_n = PQ[d][k % 2]
                    pP, d_pP = bk[d]
                    pQ, d_pQ = bk[2 + d]
                    for hd in range(4):
                        p.op("pe", lambda h, pP=pP, hd=hd, c_=c_: h.matmul(pP[:, hd * 128:(hd + 1) * 128], lhsT=c_[:, 4 + hd, :], rhs=c_[:, hd, :],
                                                                          start=True, stop=True), r=[d_c], w=[d_pP])
                    if k < 6:
                        for hd in range(4):
                            p.op("pe", lambda h, pQ=pQ, hd=hd, c_=c_: h.matmul(pQ[:, hd * 128:(hd + 1) * 128], lhsT=c_[:, hd, :], rhs=c_[:, 4 + hd, :],
                                                                              start=True, stop=True), r=[d_c], w=[d_pQ])
                    p.op("act", lambda h, pP=pP, n_=n_: h.activation(out=n_[:, 0:4, :], in_=B3(pP), func=AF.Copy), r=[d_pP], w=[d_n])
                    if k < 6:
                        p.op("dve", lambda h, pQ=pQ, n_=n_: h.tensor_copy(out=n_[:, 4:8, :], in_=B3(pQ)), r=[d_pQ], w=[d_n])
                for d in range(2):
                    n_, d_n = PQ[d][k % 2]
                    tc_, d_tc = TTt[d][(k - 1) % 2]
                    tn_, d_tn = TTt[d][k % 2]
                    pT, d_pT = bk[4 + d]
                    for hd in range(4):
                        p.op("pe", lambda h, pT=pT, hd=hd, n_=n_, tc_=tc_: h.matmul(pT[:, hd * 128:(hd + 1) * 128], lhsT=n_[:, hd, :], rhs=tc_[:, hd, :],
                                                                                  start=True, stop=False), r=[d_n, d_tc], w=[d_pT])
                        p.op("pe", lambda h, pT=pT, hd=hd, tc_=tc_: h.matmul(pT[:, hd * 128:(hd + 1) * 128], lhsT=ident[:], rhs=tc_[:, hd, :],
                                                                            start=False, stop=True), r=[d_ident, d_tc], w=[d_pT])
                    if d == 0:
                        p.op("dve", lambda h, pT=pT, tn_=tn_: h.tensor_copy(out=tn_[:], in_=B3(pT)), r=[d_pT], w=[d_tn])
                    else:
                        p.op("act", lambda h, pT=pT, tn_=tn_: h.activation(out=tn_[:], in_=B3(pT), func=AF.Copy), r=[d_pT], w=[d_tn])
            pu, d_pu = bk[6]
            for d in range(2):
                TTf, d_TTf = TTt[d][0]
                ru, d_ru = Ru[d]
                rw, d_rw = Rw[d]
                kd, d_kd = kdec[d]
                kt_, d_kt = ktm[d][par]
                vt_, d_vt = vtm[d][par]
                p.op("pool", lambda h, d=d, ru=ru, vt_=vt_: h.tensor_tensor(out=ru[:], in0=vt_[:], in1=bc_c64(col(d, 3)), op=ALU.mult),
                     r=[d_vt, d_tab], w=[d_ru])
                p.op("pool", lambda h, d=d, rw=rw, kt_=kt_: h.tensor_tensor(out=rw[:], in0=kt_[:], in1=bc_c64(col(d, 9)), op=ALU.mult),
                     r=[d_kt, d_tab], w=[d_rw])
                p.op("pool", lambda h, d=d, kd=kd, kt_=kt_: h.tensor_tensor(out=kd[:], in0=kt_[:], in1=bc_c64(col(d, 7)), op=ALU.mult),
                     r=[d_kt, d_tab], w=[d_kd])
                for hd in range(4):
                    i = d * 4 + hd
                    p.op("pe", lambda h, i=i, hd=hd, TTf=TTf, ru=ru: h.matmul(pu[:, i * 64:(i + 1) * 64], lhsT=TTf[:, hd, :], rhs=ru[:, hd, :],
                                                                             start=True, stop=True), r=[d_TTf, d_ru], w=[d_pu])
            p.op("act", lambda h: h.activation(out=u_all[:], in_=pu[:, :].rearrange("q (i e) -> q i e", e=64), func=AF.Copy), r=[d_pu], w=[d_u])
            for d in range(2):
                TTf, d_TTf = TTt[d][0]
                rw, d_rw = Rw[d]
                pw, d_pw = bk[d]
                for hd in range(4):
                    p.op("pe", lambda h, pw=pw, hd=hd, TTf=TTf, rw=rw: h.matmul(pw[0:64, hd * 128:(hd + 1) * 128], lhsT=rw[:, hd, :], rhs=TTf[:, hd, :],
                                                                               start=True, stop=True), r=[d_TTf, d_rw], w=[d_pw])
                w_, d_w_ = wT[d]
                p.op("dve", lambda h, pw=pw, w_=w_: h.tensor_copy(out=w_[:], in_=pw[0:64, :].rearrange("q (h c) -> q h c", h=4)), r=[d_pw], w=[d_w_])
                pS, d_pS = bk[2 + d]
                kf_, d_kf = kf[d][par]
                qf_, d_qf = qf[d][par]
                for hd in range(4):
                    p.op("pe", lambda h, pS=pS, hd=hd, kf_=kf_, qf_=qf_: h.matmul(pS[:, hd * 128:(hd + 1) * 128], lhsT=kf_[:, hd, :], rhs=qf_[:, hd, :],
                                                                                start=True, stop=True), r=[d_kf, d_qf], w=[d_pS])
                at_, d_at = attnT[d]
                p.op("dve", lambda h, pS=pS, at_=at_, d=d: h.tensor_tensor(out=at_[:], in0=B3(pS), in1=decT[d][par][0][:], op=ALU.mult),
                     r=[d_pS, decT[d][par][1]], w=[d_at])
            pv, d_pv = bk[7]
            for d in range(2):
                for hd in range(4):
                    i = d * 4 + hd
                    p.op("pe", lambda h, i=i, hd=hd, d=d: h.matmul(pv[:, i * 64:(i + 1) * 64], lhsT=wT[d][0][:, hd, :], rhs=Sst[:, i, :],
                                                                  start=True, stop=True), r=[wT[d][1], d_S], w=[d_pv])
            p.op("dve", lambda h: h.tensor_tensor(out=vnew[:], in0=u_all[:], in1=pv[:, :].rearrange("q (i e) -> q i e", e=64), op=ALU.subtract),
                 r=[d_pv, d_u], w=[d_vnew])
            po1, d_po1 = bk[4]
            po2, d_po2 = bk[5]
            pn, d_pn = bk[6]
            for d in range(2):
                for hd in range(4):
                    i = d * 4 + hd
                    p.op("pe", lambda h, i=i, hd=hd, d=d: h.matmul(po1[:, i * 64:(i + 1) * 64], lhsT=attnT[d][0][:, hd, :], rhs=vnew[:, i, :],
                                                                  start=True, stop=True), r=[attnT[d][1], d_vnew], w=[d_po1])
            for d in range(2):
                for hd in range(4):
                    i = d * 4 + hd
                    p.op("pe", lambda h, i=i, hd=hd, d=d: h.matmul(po2[:, i * 64:(i + 1) * 64], lhsT=qf[d][par][0][:, hd, :], rhs=Sst[:, i, :],
                                                                  start=True, stop=True), r=[qf[d][par][1], d_S], w=[d_po2])
            for d in range(2):
                for hd in range(4):
                    i = d * 4 + hd
                    p.op("pe", lambda h, i=i, hd=hd, d=d: h.matmul(pn[0:64, i * 64:(i + 1) * 64], lhsT=kdec[d][0][:, hd, :], rhs=vnew[:, i, :],
                                                                  start=True, stop=True), r=[kdec[d][1], d_vnew], w=[d_pn])
            for d in range(2):
                dst = acc[:, tts[d], :].rearrange("q (h e) -> q h e", e=64)
                ot_, d_ot = otmp[d]
                p.op("dve", lambda h, d=d, dst=dst: h.tensor_tensor(out=dst, in0=po1[:, d * 256:(d + 1) * 256].rearrange("q (h e) -> q h e", e=64),
                                                                    in1=dst, op=ALU.add), r=[d_po1, d_acc], w=[d_acc])
                p.op("dve", lambda h, d=d, ot_=ot_: h.tensor_tensor(out=ot_[:], in0=po2[:, d * 256:(d + 1) * 256].rearrange("q (h e) -> q h e", e=64),
                                                                    in1=bc_c64(col(d, 6)), op=ALU.mult), r=[d_po2, d_tab], w=[d_ot])
                p.op("pool", lambda h, dst=dst, ot_=ot_: h.tensor_tensor(out=dst, in0=dst, in1=ot_[:], op=ALU.add), r=[d_ot, d_acc], w=[d_acc])
            for d in range(2):
                sv = Sst[:, 4 * d:4 * d + 4, :]
                egl_b = tab[0:64, tts[d], d, 32:36].unsqueeze(2).to_broadcast([64, 4, 64])
                p.op("dve", lambda h, sv=sv, egl_b=egl_b: h.tensor_tensor(out=sv, in0=sv, in1=egl_b, op=ALU.mult), r=[d_S, d_tab], w=[d_S])
                p.op("dve", lambda h, sv=sv, d=d: h.tensor_tensor(out=sv, in0=pn[0:64, d * 256:(d + 1) * 256].rearrange("q (h e) -> q h e", e=64),
                                                                  in1=sv, op=ALU.add), r=[d_pn, d_S], w=[d_S])
        gain, d_gain = sb.t((128, 256))
        for g4 in range(4):
            p.dma("sp", gain[:, 64 * g4:64 * g4 + 64], I["gdn_norm"][l:l + 1, :].partition_broadcast(128), w=[d_gain])
        zts = [sb.t((128, 256)) for _ in range(2)]
        sgs = [sb.t((128, 256)) for _ in range(2)]
        sqs = [sb.t((128, 256)) for _ in range(2)]
        sts = [sb.t((128, 8)) for _ in range(2)]
        v3 = lambda a: a[:, :].rearrange("q (g e) -> q g e", e=64)
        for tt in range(NT):
            zt, d_zt = zts[tt % 2]
            sg, d_sg = sgs[tt % 2]
            sq, d_sq = sqs[tt % 2]
            st, d_st = sts[tt % 2]
            p.dma("sp", zt[:], Z["zD"][tt * 128:(tt + 1) * 128, :], w=[d_zt])
            p.op("act", lambda h, zt=zt, sg=sg: h.activation(out=sg[:], in_=zt[:], func=AF.Sigmoid), r=[d_zt], w=[d_sg])
            p.op("dve", lambda h, zt=zt, sg=sg: h.tensor_tensor(out=sg[:], in0=sg[:], in1=zt[:], op=ALU.mult), r=[d_zt, d_sg], w=[d_sg])
            ov = acc[:, tt, :]
            p.op("act", lambda h, sq=sq, ov=ov: h.activation(out=sq[:], in_=ov, func=AF.Square), r=[d_acc], w=[d_sq])
            p.op("dve", lambda h, st=st, sq=sq: h.tensor_reduce(out=st[:, 0:4], in_=v3(sq), axis=AX.X, op=ALU.add), r=[d_sq], w=[d_st])
            rstd(p, st[:, 4:8], st[:, 0:4], st[:, 0:4], 1.0 / 64, [d_st])
            p.op("dve", lambda h, sq=sq, st=st, ov=ov: h.tensor_tensor(out=v3(sq), in0=ov.rearrange("q (g e) -> q g e", e=64),
                                                                       in1=st[:, 4:8].unsqueeze(2).to_broadcast([128, 4, 64]), op=ALU.mult),
                 r=[d_acc, d_st], w=[d_sq])
            p.op("dve", lambda h, sq=sq: h.tensor_tensor(out=sq[:], in0=sq[:], in1=gain[:], op=ALU.mult), r=[d_gain, d_sq], w=[d_sq])
            p.op("dve", lambda h, sq=sq, sg=sg: h.tensor_tensor(out=sq[:], in0=sq[:], in1=sg[:], op=ALU.mult), r=[d_sg, d_sq], w=[d_sq])
            p.dma("sp", Z["y"][tt * 128:(tt + 1) * 128, 768:1024], sq[:], r=[d_sq])


def phase_outproj(p, l, I, S, Z, xsrc, mine=False):
    ident, d_ident = S["ident"]
    grow, d_grow = S["grow"]
    with SB(p) as sb:
        w, d_w = sb.t((128, 8, D), BF16, "wout")
        for k in range(8):
            p.dma("pool", w[:, k, :], I["w_out"][l, k * 128:(k + 1) * 128, :], w=[d_w])
        yts = [sb.t((128, D)) for _ in range(2)]
        xts = [sb.t((128, D)) for _ in range(2)]
        yTs = [sb.t((128, 8, 128), BF16) for _ in range(2)]
        tps = [sb.ps() for _ in range(2)]
        pos = [sb.ps() for _ in range(2)]
        tiles = range(NT) if not mine else range(16)
        if mine:
            fl, d_fl = sb.t((128, 2))
            p.dma("sp", fl[:], I["flags"][:, :], w=[d_fl])
            ob_, d_ob_ = sb.t((128, D))
        for tt in tiles:
            j = 1 if (tt < 2 and not mine) else 0
            rows = slice(tt * 128, (tt + 1) * 128)
            yt, d_yt = yts[tt % 2]
            xt, d_xt = xts[tt % 2]
            yT, d_yT = yTs[tt % 2]
            if not mine:
                p.dma("sp", yt[:], Z["y"][rows, :], w=[d_yt])
                p.dma("sp", xt[:], xsrc[rows, :], w=[d_xt])
            else:
                ra = slice(256 + tt * 128, 256 + (tt + 1) * 128)
                rb = slice(256 + 2048 + tt * 128, 256 + 2048 + (tt + 1) * 128)
                p.dma("sp", yt[:, 0:512], Z["ym"][rows, :], w=[d_yt])
                p.dma("sp", yt[:, 512:1024], Z["y"][ra, 512:1024], w=[d_yt])
                p.dma("sp", ob_[:, 0:512], Z["y"][rb, 512:1024], w=[d_ob_])
                p.op("dve", lambda h, yt=yt: h.tensor_scalar(out=yt[:, 512:1024], in0=yt[:, 512:1024], scalar1=fl[:, 0:1], scalar2=None, op0=ALU.mult),
                     r=[d_yt, d_fl], w=[d_yt])
                p.op("dve", lambda h, yt=yt: h.scalar_tensor_tensor(out=yt[:, 512:1024], in0=ob_[:, 0:512], scalar=fl[:, 1:2], in1=yt[:, 512:1024],
                                                                    op0=ALU.mult, op1=ALU.add), r=[d_ob_, d_fl, d_yt], w=[d_yt])
                p.dma("sp", xt[:], xsrc[ra, :], w=[d_xt])
                p.dma("sp", ob_[:], xsrc[rb, :], w=[d_ob_])
                p.op("dve", lambda h, xt=xt: h.tensor_scalar(out=xt[:], in0=xt[:], scalar1=fl[:, 0:1], scalar2=None, op0=ALU.mult),
                     r=[d_xt, d_fl], w=[d_xt])
                p.op("dve", lambda h, xt=xt: h.scalar_tensor_tensor(out=xt[:], in0=ob_[:], scalar=fl[:, 1:2], in1=xt[:],
                                                                    op0=ALU.mult, op1=ALU.add), r=[d_ob_, d_fl, d_xt], w=[d_xt])
            for c in range(8):
                tp, d_tp = tps[c // 4]
                p.op("pe", lambda h, c=c, tp=tp, yt=yt: h.transpose(tp[:, (c % 4) * 128:(c % 4 + 1) * 128], yt[:, c * 128:(c + 1) * 128], ident[:]),
                     r=[d_yt, d_ident], w=[d_tp])
            for hh in range(2):
                tp, d_tp = tps[hh]
                p.op("act", lambda h, hh=hh, tp=tp, yT=yT: h.activation(out=yT[:, 4 * hh:4 * hh + 4, :],
                                                                       in_=tp[:, :].rearrange("q (c t) -> q c t", c=4), func=AF.Copy),
                     r=[d_tp], w=[d_yT])
            for hh in range(2):
                po, d_po = pos[hh]
                for k in range(8):
                    p.op("pe", lambda h, k=k, po=po, yT=yT, hh=hh: h.matmul(po[:, :], lhsT=yT[:, k, :], rhs=w[:, k, hh * 512:(hh + 1) * 512],
                                                                          start=(k == 0), stop=(k == 7)), r=[d_yT, d_w], w=[d_po])
                p.op("dve", lambda h, po=po, yt=yt, hh=hh, j=j: h.tensor_tensor(out=yt[:, hh * 512:(hh + 1) * 512], in0=po[:, :],
                                                                             in1=grow[:, 0, j, hh * 512:(hh + 1) * 512], op=ALU.mult),
                     r=[d_po, d_grow], w=[d_yt])
            p.op("dve", lambda h, xt=xt, yt=yt: h.tensor_tensor(out=xt[:], in0=xt[:], in1=yt[:], op=ALU.add), r=[d_yt, d_xt], w=[d_xt])
            p.dma("sp", (Z["xm1"] if mine else Z["xres"])[rows, :], xt[:], r=[d_xt])


def norm_objs(sb, S):
    junk, d_junk = sb.t((128, D))
    xs, d_xs = sb.t((128, D))
    ss, d_ss = sb.t((128, 4))
    tps = [sb.ps() for _ in range(2)]
    return (junk, d_junk, ss, d_ss, xs, d_xs, tps, S["ident"][0], S["ident"][1])


def phase_ffn_dense(p, l, I, S, Z):
    gsh, d_gsh = S["gsh"]
    grow, d_grow = S["grow"]
    NF = D_FF // 128
    with SB(p) as sb:
        wg, d_wg = sb.t((128, 8, D_FF), BF16, "wg")
        wu, d_wu = sb.t((128, 8, D_FF), BF16, "wu")
        wd, d_wd = sb.t((128, NF, D), BF16, "wd")
        for k in range(8):
            p.dma("pool", wg[:, k, :], I["ffn_w_gate"][k * 128:(k + 1) * 128, :], w=[d_wg])
            p.dma("pool", wu[:, k, :], I["ffn_w_up"][k * 128:(k + 1) * 128, :], w=[d_wu])
        for k in range(NF):
            p.dma("pool", wd[:, k, :], I["ffn_w_down"][k * 128:(k + 1) * 128, :], w=[d_wd])
        nobj = norm_objs(sb, S)
        xb, d_xb = sb.t((128, 2, D), F32, "xblk")
        xnT, d_xnT = sb.t((128, 8, 256), BF16, "xnT")
        hT, d_hT = sb.t((128, NF, 256), BF16, "hT")
        sgs = [sb.t((128, 256)) for _ in range(2)]
        pgs = [sb.ps() for _ in range(2)]
        pus = [sb.ps() for _ in range(2)]
        pos = [sb.ps() for _ in range(2)]
        ot, d_ot = sb.t((128, 512))
        blocks = [(2 * i, 2) for i in range(17)]
        n = 0
        for (t0, ntl) in blocks:
            ntok = ntl * 128
            j = 1 if t0 < 2 else 0
            for ti in range(ntl):
                tt = t0 + ti
                p.dma("sp", xb[:, ti, :], Z["xres"][tt * 128:(tt + 1) * 128, :], w=[d_xb])
            for ti in range(ntl):
                norm_tile(p, nobj, xb[:, ti, :], d_xb, gsh, d_gsh, 1, j, xnT, d_xnT, ti * 128)
            for fc in range(NF):
                pg, d_pg = pgs[fc % 2]
                pu, d_pu = pus[fc % 2]
                sg, d_sg = sgs[fc % 2]
                for k in range(8):
                    p.op("pe", lambda h, k=k, pg=pg, fc=fc: h.matmul(pg[:, 0:ntok], lhsT=wg[:, k, fc * 128:(fc + 1) * 128], rhs=xnT[:, k, 0:ntok],
                                                                    start=(k == 0), stop=(k == 7)), r=[d_wg, d_xnT], w=[d_pg])
                for k in range(8):
                    p.op("pe", lambda h, k=k, pu=pu, fc=fc: h.matmul(pu[:, 0:ntok], lhsT=wu[:, k, fc * 128:(fc + 1) * 128], rhs=xnT[:, k, 0:ntok],
                                                                    start=(k == 0), stop=(k == 7)), r=[d_wu, d_xnT], w=[d_pu])
                p.op("act", lambda h, pg=pg, sg=sg: h.activation(out=sg[:, 0:ntok], in_=pg[:, 0:ntok], func=AF.Sigmoid), r=[d_pg], w=[d_sg])
                p.op("dve", lambda h, pg=pg, sg=sg: h.tensor_tensor(out=sg[:, 0:ntok], in0=pg[:, 0:ntok], in1=sg[:, 0:ntok], op=ALU.mult),
                     r=[d_pg, d_sg], w=[d_sg])
                p.op("dve", lambda h, pu=pu, sg=sg, fc=fc: h.tensor_tensor(out=hT[:, fc, 0:ntok], in0=pu[:, 0:ntok], in1=sg[:, 0:ntok], op=ALU.mult),
                     r=[d_pu, d_sg], w=[d_hT])
            for ti in range(ntl):
                tt = t0 + ti
                for hh in range(2):
                    po, d_po = pos[n % 2]
                    n += 1
                    for fc in range(NF):
                        p.op("pe", lambda h, fc=fc, po=po, ti=ti, hh=hh: h.matmul(po[:, :], lhsT=hT[:, fc, ti * 128:(ti + 1) * 128],
                                                                                rhs=wd[:, fc, hh * 512:(hh + 1) * 512],
                                                                                start=(fc == 0), stop=(fc == NF - 1)), r=[d_hT, d_wd], w=[d_po])
                    p.op("dve", lambda h, po=po, hh=hh, j=j: h.tensor_tensor(out=ot[:], in0=po[:, :], in1=grow[:, 1, j, hh * 512:(hh + 1) * 512],
                                                                          op=ALU.mult), r=[d_po, d_grow], w=[d_ot])
                    p.op("dve", lambda h, ti=ti, hh=hh: h.tensor_tensor(out=xb[:, ti, hh * 512:(hh + 1) * 512], in0=xb[:, ti, hh * 512:(hh + 1) * 512],
                                                                         in1=ot[:], op=ALU.add), r=[d_ot, d_xb], w=[d_xb])
                p.dma("sp", Z["xres"][tt * 128:(tt + 1) * 128, :], xb[:, ti, :], r=[d_xb])


def phase_moe(p, l, I, S, Z, out):
    gsh, d_gsh = S["gsh"]
    grow, d_grow = S["grow"]
    NTM = 16
    SLAB = 512
    NSL = D_FFE // SLAB
    with SB(p) as sb:
        fl, d_fl = sb.t((128, 2))
        p.dma("sp", fl[:], I["flags"][:, :], w=[d_fl])
        xm, d_xm = sb.t((128, NTM, D), F32, "xm")
        xnT, d_xnT = sb.t((128, 8, NTM * 128), BF16, "xnTm")
        gates, d_gates = sb.t((128, NTM, NE), F32, "gates")
        rt, d_rt = sb.t((128, 8, NE), F32, "router")
        p.dma("sp", rt[:], I["moe_router"].rearrange("(k q) e -> q k e", q=128), w=[d_rt])
        with SB(p) as sb2:
            nobj = norm_objs(sb2, S)
            (junk, d_junk, ss, d_ss, xs, d_xs, tps, ident, d_ident) = nobj
            xa = [sb2.t((128, D)) for _ in range(2)]
            xnf, d_xnf = sb2.t((128, 8, 128), F32, "xnf")
            plg, d_plg = sb2.ps()
            lg, d_lg = sb2.t((128, 8))
            mx, d_mx = sb2.t((128, 8))
            wk, d_wk = sb2.t((128, 32))
            for jt in range(NTM):
                a_, d_a = xa[0]
                b_, d_b = xa[1]
                p.dma("sp", xm[:, jt, :], Z["xm1"][jt * 128:(jt + 1) * 128, :], w=[d_xm])
                norm_tile(p, nobj, xm[:, jt, :], d_xm, gsh, d_gsh, 1, 0, xnT, d_xnT, jt * 128)
                for c in range(8):
                    tp, d_tp = tps[c // 4]
                    p.op("act", lambda h, c=c, tp=tp: h.activation(out=xnf[:, c, :], in_=tp[:, (c % 4) * 128:(c % 4 + 1) * 128],
                                                                   func=AF.Identity, scale=gsh[:, 2, c, 0:1], bias=gsh[:, 3, c, 0:1]),
                         r=[d_tp, d_gsh], w=[d_xnf])
                for k in range(8):
                    p.op("pe", lambda h, k=k: h.matmul(plg[:, 0:NE], lhsT=xnf[:, k, :], rhs=rt[:, k, :], start=(k == 0), stop=(k == 7)),
                         r=[d_xnf, d_rt], w=[d_plg])
                p.op("dve", lambda h: h.tensor_copy(out=lg[:], in_=plg[:, 0:NE]), r=[d_plg], w=[d_lg])
                p.op("dve", lambda h: h.max(out=mx[:], in_=lg[:]), r=[d_lg], w=[d_mx])
                p.op("dve", lambda h: h.tensor_tensor(out=wk[:, 0:1], in0=mx[:, 1:2], in1=mx[:, 0:1], op=ALU.subtract), r=[d_mx], w=[d_wk])
                p.op("act", lambda h: h.activation(out=wk[:, 1:2], in_=wk[:, 0:1], func=AF.Exp), r=[d_wk], w=[d_wk])
                p.op("dve", lambda h: h.tensor_scalar(out=wk[:, 1:2], in0=wk[:, 1:2], scalar1=1.0, scalar2=None, op0=ALU.add), r=[d_wk], w=[d_wk])
                p.op("dve", lambda h: h.reciprocal(out=wk[:, 2:3], in_=wk[:, 1:2]), r=[d_wk], w=[d_wk])
                p.op("dve", lambda h: h.tensor_scalar(out=wk[:, 3:4], in0=wk[:, 2:3], scalar1=-1.0, scalar2=1.0, op0=ALU.mult, op1=ALU.add),
                     r=[d_wk], w=[d_wk])
                p.op("dve", lambda h: h.tensor_scalar(out=wk[:, 8:16], in0=lg[:], scalar1=mx[:, 0:1], scalar2=wk[:, 2:3], op0=ALU.is_equal,
                                                      op1=ALU.mult), r=[d_lg, d_mx, d_wk], w=[d_wk])
                p.op("dve", lambda h: h.tensor_scalar(out=wk[:, 16:24], in0=lg[:], scalar1=mx[:, 1:2], scalar2=wk[:, 3:4], op0=ALU.is_equal,
                                                      op1=ALU.mult), r=[d_lg, d_mx, d_wk], w=[d_wk])
                p.op("dve", lambda h, jt=jt: h.tensor_tensor(out=gates[:, jt, :], in0=wk[:, 8:16], in1=wk[:, 16:24], op=ALU.add),
                     r=[d_wk], w=[d_gates])
        wgs = [sb.t((128, 8, SLAB), BF16, "wgs") for _ in range(2)]
        wus = [sb.t((128, 8, SLAB), BF16, "wus") for _ in range(2)]
        wds = [sb.t((128, 4, D), BF16, "wds") for _ in range(2)]
        hTs = [sb.t((128, 4, 512), BF16, "hTs") for _ in range(2)]
        sgs = [sb.t((128, 512)) for _ in range(2)]
        pgs = [sb.ps() for _ in range(2)]
        pus = [sb.ps() for _ in range(2)]
        pos = [sb.ps() for _ in range(2)]
        ns = 0
        nh = 0
        nf = 0
        no = 0
        for e in range(NE):
            for sl in range(NSL):
                wg, d_wg = wgs[ns % 2]
                wu, d_wu = wus[ns % 2]
                wd, d_wd = wds[ns % 2]
                ns += 1
                c0 = sl * SLAB
                p.dma("pool", wg[:], I["moe_w_gate"][e].rearrange("(k q) f -> q k f", q=128)[:, :, c0:c0 + SLAB], w=[d_wg])
                p.dma("pool", wu[:], I["moe_w_up"][e].rearrange("(k q) f -> q k f", q=128)[:, :, c0:c0 + SLAB], w=[d_wu])
                p.dma("pool", wd[:], I["moe_w_down"][e, c0:c0 + SLAB, :].rearrange("(k q) d -> q k d", q=128), w=[d_wd])
                for k in range(4):
                    p.op("dve", lambda h, k=k, wd=wd: h.tensor_tensor(out=wd[:, k, :], in0=wd[:, k, :], in1=grow[:, 1, 0, :], op=ALU.mult),
                         r=[d_grow, d_wd], w=[d_wd])
                for tb in range(NTM // 4):
                    hT, d_hT = hTs[nh % 2]
                    nh += 1
                    toks = slice(tb * 512, (tb + 1) * 512)
                    for fc in range(4):
                        pg, d_pg = pgs[nf % 2]
                        pu, d_pu = pus[nf % 2]
                        sg, d_sg = sgs[nf % 2]
                        nf += 1
                        for k in range(8):
                            p.op("pe", lambda h, k=k, pg=pg, fc=fc, wg=wg: h.matmul(pg[:, :], lhsT=wg[:, k, fc * 128:(fc + 1) * 128], rhs=xnT[:, k, toks],
                                                                                  start=(k == 0), stop=(k == 7)), r=[d_wg, d_xnT], w=[d_pg])
                        for k in range(8):
                            p.op("pe", lambda h, k=k, pu=pu, fc=fc, wu=wu: h.matmul(pu[:, :], lhsT=wu[:, k, fc * 128:(fc + 1) * 128], rhs=xnT[:, k, toks],
                                                                                  start=(k == 0), stop=(k == 7)), r=[d_wu, d_xnT], w=[d_pu])
                        p.op("act", lambda h, pg=pg, sg=sg: h.activation(out=sg[:], in_=pg[:, :], func=AF.Sigmoid), r=[d_pg], w=[d_sg])
                        p.op("dve", lambda h, pg=pg, sg=sg: h.tensor_tensor(out=sg[:], in0=pg[:, :], in1=sg[:], op=ALU.mult), r=[d_pg, d_sg], w=[d_sg])
                        p.op("dve", lambda h, pu=pu, sg=sg, fc=fc, hT=hT: h.tensor_tensor(out=hT[:, fc, :], in0=pu[:, :], in1=sg[:], op=ALU.mult),
                             r=[d_pu, d_sg], w=[d_hT])
                    for ti in range(4):
                        jt = tb * 4 + ti
                        for hh in range(2):
                            po, d_po = pos[no % 2]
                            no += 1
                            for fc in range(4):
                                p.op("pe", lambda h, fc=fc, po=po, ti=ti, hh=hh, hT=hT, wd=wd: h.matmul(
                                    po[:, :], lhsT=hT[:, fc, ti * 128:(ti + 1) * 128], rhs=wd[:, fc, hh * 512:(hh + 1) * 512],
                                    start=(fc == 0), stop=(fc == 3)), r=[d_hT, d_wd], w=[d_po])
                            dst = xm[:, jt, hh * 512:(hh + 1) * 512]
                            p.op("dve", lambda h, po=po, dst=dst, jt=jt, e=e: h.scalar_tensor_tensor(out=dst, in0=po[:, :], scalar=gates[:, jt, e:e + 1],
                                                                                                  in1=dst, op0=ALU.mult, op1=ALU.add),
                                 r=[d_po, d_gates, d_xm], w=[d_xm])
        for jt in range(NTM):
            p.dma("sp", out[jt * 128:(jt + 1) * 128, :], xm[:, jt, :], r=[d_xm])


_CACHE = {}


def kernel(**inputs):
    inp = {k: np.asarray(v) for k, v in inputs.items()}
    if "p" not in _CACHE:
        _CACHE["p"] = build()
    p = _CACHE["p"]
    in_maps = [host_inputs(inp, c) for c in range(8)]
    res = run_bass_kernel_spmd(p.nc, in_maps, core_ids=list(range(8)))
    out = np.zeros((4, NLAT, D), np.float32)
    for c in range(8):
        b, hh = c // 2, c % 2
        out[b, hh * 2048:(hh + 1) * 2048, :] = np.asarray(res.results[c]["out"], dtype=np.float32)
    return out
```

```python
import math
from contextlib import ExitStack
import numpy as np
import concourse.bass as bass
import concourse.mybir as mybir
from concourse.bass_utils import run_bass_kernel_spmd

F32 = mybir.dt.float32
BF16 = mybir.dt.bfloat16
AF = mybir.ActivationFunctionType
ALU = mybir.AluOpType
AX = mybir.AxisListType

D = 1024
NCTX = 256
NLAT = 4096
T = NCTX + NLAT
NT = T // 128
EPS = 1e-6
N_IN = 3360
D_FF = 2816
D_FFE = 3584
NE = 8


class Dep:
    __slots__ = ("w", "r", "excl")

    def __init__(self, excl=False):
        self.w = {}
        self.r = {}
        self.excl = excl


class Prog:
    def __init__(self):
        self.nc = bass.Bass("TRN2", target_bir_lowering=False)
        nc = self.nc
        self.h = {"pe": nc.tensor, "act": nc.scalar, "dve": nc.vector, "pool": nc.gpsimd, "sp": nc.sync}
        self.sem = {}
        self.cnt = {}
        self.semobj = {}
        self.seen = {e: {} for e in self.h}
        self.nsem = 0
        for e in self.h:
            self._newsem(e)
        self.NS = 12
        self.slots = {q: [nc.alloc_semaphore(f"dq_{q}_{i}") for i in range(self.NS)] for q in ("sp", "pool", "act")}
        self.dcnt = {q: 0 for q in self.slots}
        for q in self.slots:
            for s in self.slots[q]:
                self.semobj[id(s)] = s

    def _newsem(self, e):
        s = self.nc.alloc_semaphore(f"s_{e}_{self.nsem}")
        self.nsem += 1
        self.sem[e] = s
        self.cnt[e] = 0
        if not hasattr(self, "semobj"):
            self.semobj = {}
        self.semobj[id(s)] = s

    def _wait(self, e, tok):
        sid, val = tok
        if self.seen[e].get(sid, 0) >= val:
            return
        self.seen[e][sid] = val
        self.h[e].wait_ge(self.semobj[sid], val)

    def _deps(self, e, r, w):
        need = {}
        for d in r:
            for sid, v in d.w.items():
                need[sid] = max(need.get(sid, 0), v)
            if d.excl:
                for sid, v in d.r.items():
                    need[sid] = max(need.get(sid, 0), v)
        for d in w:
            for sid, v in d.w.items():
                need[sid] = max(need.get(sid, 0), v)
            for sid, v in d.r.items():
                need[sid] = max(need.get(sid, 0), v)
        own = id(self.sem[e])
        for sid, v in need.items():
            if e == "pe" and sid == own:
                continue
            self._wait(e, (sid, v))

    def _mark(self, tok, r, w):
        sid, v = tok
        for d in r:
            d.r[sid] = max(d.r.get(sid, 0), v)
        for d in w:
            d.w = {sid: v}
            d.r = {}

    def op(self, e, fn, r=(), w=()):
        self._deps(e, r, w)
        if self.cnt[e] >= 30000:
            self._newsem(e)
        ins = fn(self.h[e])
        self.cnt[e] += 1
        ins.then_inc(self.sem[e], 1)
        self._mark((id(self.sem[e]), self.cnt[e]), r, w)

    def dma(self, q, out, in_, r=(), w=(), **kw):
        self._deps(q, r, w)
        i = self.dcnt[q]
        self.dcnt[q] += 1
        s = self.slots[q][i % self.NS]
        val = 16 * (i // self.NS + 1)
        self._wait(q, (id(s), val - 16))
        self.h[q].dma_start(out=out, in_=in_, **kw).then_inc(s, 16)
        self._mark((id(s), val), r, w)

    def eps_ap(self, eps, n):
        assert abs(eps - EPS) < 1e-12
        return self.epst[0:n, 0:1]

    def barrier(self):
        toks = []
        for e in self.h:
            if self.cnt[e] > 0:
                toks.append((id(self.sem[e]), self.cnt[e]))
        for q in self.slots:
            n = self.dcnt[q]
            for j in range(min(n, self.NS)):
                i = n - 1 - j
                toks.append((id(self.slots[q][i % self.NS]), 16 * (i // self.NS + 1)))
        for e in self.h:
            for t in toks:
                self._wait(e, t)


class SB:
    N = 0

    def __init__(self, p):
        self.p = p
        self.es = ExitStack()
        self.n = 0

    def __enter__(self):
        self.es.__enter__()
        return self

    def __exit__(self, *a):
        self.p.barrier()
        return self.es.__exit__(*a)

    def t(self, shape, dt=F32, name="t"):
        SB.N += 1
        return self.es.enter_context(self.p.nc.sbuf_tensor(f"{name}_{SB.N}", list(shape), dt)), Dep()

    def ps(self, shape=(128, 512), dt=F32, name="ps"):
        SB.N += 1
        return self.es.enter_context(self.p.nc.psum_tensor(f"{name}_{SB.N}", list(shape), dt)), Dep(excl=True)


CA_Q, CA_K, CA_V = 0, 256, 512
CB_Q, CB_K, CB_V = 768, 1024, 1152
CC_Q, CC_K, CC_V, CC_O, CC_G = 1280, 1536, 1792, 2048, 2304
CD_QKV, CD_Z, CD_G = 2320, 3088, 3344


def dram(p, name, shape, dt, kind="Internal"):
    return p.nc.dram_tensor(name, list(shape), dt, kind=kind).ap()


def phase_mod(p, l, I, S):
    nc = p.nc
    modT, d_modT = S["modT"]
    gsh, d_gsh = S["gsh"]
    grow, d_grow = S["grow"]
    with SB(p) as sb:
        cc, d_cc = sb.t((128, 8, 2))
        sc, d_sc = sb.t((128, 8, 2))
        ones, d_ones = sb.t((128, 128))
        rep, d_rep = sb.t((128, 2, 8, 128))
        bmT, d_bmT = sb.t((128, 48))
        nrm, d_nrm = sb.t((128, 2, 8))
        wm = [sb.t((128, 8, 512)) for _ in range(2)]
        psm, d_psm = sb.ps((128, 512))
        psr = [sb.ps((128, 512)) for _ in range(2)]
        p.dma("sp", cc[:], I["cc"][:, :, :], w=[d_cc])
        p.dma("sp", bmT[:], I["bmodT"][:, l, :], w=[d_bmT])
        p.dma("sp", nrm[:, 0, :], I["norm1T"][:, l, :], w=[d_nrm])
        p.dma("sp", nrm[:, 1, :], I["norm2T"][:, l, :], w=[d_nrm])
        for j in range(2):
            for which, c0 in ((0, 2048), (1, 5120)):
                p.dma("sp", grow[:, which, j, :], I["b_mod"][l:l + 1, c0:c0 + 1024].partition_broadcast(128), w=[d_grow])
        p.op("act", lambda h: h.activation(out=sc[:], in_=cc[:], func=AF.Sigmoid), r=[d_cc], w=[d_sc])
        p.op("dve", lambda h: h.tensor_tensor(out=sc[:], in0=sc[:], in1=cc[:], op=ALU.mult), r=[d_cc, d_sc], w=[d_sc])
        p.op("dve", lambda h: h.memset(ones[:], 1.0), w=[d_ones])
        for j in range(2):
            for k in range(8):
                p.op("dve", lambda h, j=j, k=k: h.tensor_scalar(out=rep[:, j, k, :], in0=ones[:], scalar1=sc[:, k, j:j + 1],
                                                                 scalar2=None, op0=ALU.mult), r=[d_ones, d_sc], w=[d_rep])
        wsrc = I["w_mod"][l].rearrange("(k q) n -> q k n", q=128)
        for blk in range(12):
            wt, d_wt = wm[blk % 2]
            p.dma("sp", wt[:], wsrc[:, :, blk * 512:(blk + 1) * 512], w=[d_wt])
            for fc in range(4):
                for k in range(8):
                    p.op("pe", lambda h, fc=fc, k=k, wt=wt: h.matmul(psm[:, fc * 2:fc * 2 + 2], lhsT=wt[:, k, fc * 128:(fc + 1) * 128],
                                                                    rhs=sc[:, k, :], start=(k == 0), stop=(k == 7)),
                         r=[d_wt, d_sc], w=[d_psm])
            p.op("dve", lambda h, blk=blk: h.tensor_copy(out=modT[:, blk * 4:(blk + 1) * 4, :],
                                                         in_=psm[:, 0:8].rearrange("q (f j) -> q f j", j=2)),
                 r=[d_psm], w=[d_modT])
            if blk in (4, 5, 10, 11):
                which = 0 if blk < 6 else 1
                half = blk % 2
                for j in range(2):
                    pr, d_pr = psr[j]
                    for k in range(8):
                        p.op("pe", lambda h, j=j, k=k, wt=wt, pr=pr: h.matmul(pr[:, :], lhsT=rep[:, j, k, :], rhs=wt[:, k, :],
                                                                              start=(k == 0), stop=(k == 7)),
                             r=[d_wt, d_rep], w=[d_pr])
                    dst = grow[:, which, j, half * 512:(half + 1) * 512]
                    p.op("dve", lambda h, dst=dst, pr=pr: h.tensor_tensor(out=dst, in0=pr[:, :], in1=dst, op=ALU.add),
                         r=[d_pr, d_grow], w=[d_grow])
        for j in range(2):
            p.op("dve", lambda h, j=j: h.tensor_tensor(out=modT[:, :, j], in0=modT[:, :, j], in1=bmT[:], op=ALU.add),
                 r=[d_bmT, d_modT], w=[d_modT])
        for j in range(2):
            for n_i, (c_sh, c_sc) in enumerate(((0, 8), (24, 32))):
                p.op("dve", lambda h, j=j, n_i=n_i, c_sc=c_sc: h.scalar_tensor_tensor(
                    out=gsh[:, 2 * n_i, :, j], in0=modT[:, c_sc:c_sc + 8, j], scalar=1.0, in1=nrm[:, n_i, :],
                    op0=ALU.add, op1=ALU.mult), r=[d_modT, d_nrm], w=[d_gsh])
                p.op("dve", lambda h, j=j, n_i=n_i, c_sh=c_sh: h.tensor_copy(out=gsh[:, 2 * n_i + 1, :, j], in_=modT[:, c_sh:c_sh + 8, j]),
                     r=[d_modT], w=[d_gsh])


def rstd(p, out, in_, tmp, scale, deps, eps=EPS):
    p.op("act", lambda h: h.activation(out=tmp, in_=in_, func=AF.Sqrt, scale=scale, bias=p.eps_ap(eps, in_.shape[0])), r=deps + [p.d_eps], w=deps)
    p.op("dve", lambda h: h.reciprocal(out=out, in_=tmp), r=deps, w=deps)


def norm_tile(p, sb_objs, xt, d_xt, gsh, d_gsh, which, j, xnT, d_xnT, tok0):
    (junk, d_junk, ss, d_ss, xs, d_xs, tps, ident, d_ident) = sb_objs
    p.op("act", lambda h: h.activation(out=junk[:], in_=xt[:], func=AF.Square, accum_out=ss[:, 0:1]), r=[d_xt], w=[d_junk, d_ss])
    rstd(p, ss[:, 2:3], ss[:, 0:1], ss[:, 1:2], 1.0 / D, [d_ss])
    p.op("dve", lambda h: h.tensor_scalar(out=xs[:], in0=xt[:], scalar1=ss[:, 2:3], scalar2=None, op0=ALU.mult),
         r=[d_xt, d_ss], w=[d_xs])
    for c in range(8):
        tp, d_tp = tps[c // 4]
        p.op("pe", lambda h, c=c, tp=tp: h.transpose(tp[:, (c % 4) * 128:(c % 4 + 1) * 128], xs[:, c * 128:(c + 1) * 128], ident[:]),
             r=[d_xs, d_ident], w=[d_tp])
    for c in range(8):
        tp, d_tp = tps[c // 4]
        p.op("act", lambda h, c=c, tp=tp: h.activation(out=xnT[:, c, tok0:tok0 + 128], in_=tp[:, (c % 4) * 128:(c % 4 + 1) * 128],
                                                       func=AF.Identity, scale=gsh[:, 2 * which, c, j:j + 1],
                                                       bias=gsh[:, 2 * which + 1, c, j:j + 1]),
             r=[d_tp, d_gsh], w=[d_xnT])


def qk_norm_rope(p, sbo, src, d_src, ncols, dim, gain, d_gain, rope, d_rope, dst, d_dst):
    (sq, d_sq, st, d_st, t1, d_t1, t2, d_t2) = sbo
    ng = ncols // dim
    p.op("act", lambda h: h.activation(out=sq[:, 0:ncols], in_=src, func=AF.Square), r=[d_src], w=[d_sq])
    p.op("dve", lambda h: h.tensor_reduce(out=st[:, 0:ng], in_=sq[:, 0:ncols].rearrange("q (g d) -> q g d", d=dim), axis=AX.X, op=ALU.add),
         r=[d_sq], w=[d_st])
    rstd(p, st[:, 0:ng], st[:, 0:ng], st[:, 0:ng], 1.0 / dim, [d_st])
    tgt = t1 if rope is not None else dst
    d_tgt = d_t1 if rope is not None else d_dst
    p.op("dve", lambda h: h.tensor_tensor(out=tgt[:, 0:ncols].rearrange("q (g d) -> q g d", d=dim),
                                          in0=src.rearrange("q (g d) -> q g d", d=dim),
                                          in1=st[:, 0:ng].unsqueeze(2).to_broadcast([128, ng, dim]), op=ALU.mult),
         r=[d_src, d_st], w=[d_tgt])
    p.op("dve", lambda h: h.tensor_tensor(out=tgt[:, 0:ncols], in0=tgt[:, 0:ncols], in1=gain[:, 0:ncols], op=ALU.mult),
         r=[d_gain, d_tgt], w=[d_tgt])
    if rope is None:
        return
    q4 = dim // 4
    v = lambda a: a[:, 0:ncols].rearrange("q (g a s f) -> q g a s f", a=2, s=2, f=q4)
    cb = rope[:, 0, :].rearrange("q (a s f) -> q a s f", a=2, s=2).unsqueeze(1).to_broadcast([128, ng, 2, 2, q4])
    sbv = rope[:, 1, :].rearrange("q (a s f) -> q a s f", a=2, s=2)
    for s_ in range(2):
        p.op("dve", lambda h, s_=s_: h.tensor_tensor(out=v(t2)[:, :, :, s_, :], in0=v(t1)[:, :, :, 1 - s_, :],
                                                     in1=sbv[:, :, s_, :].unsqueeze(1).to_broadcast([128, ng, 2, q4]), op=ALU.mult),
             r=[d_t1, d_rope], w=[d_t2])
    p.op("dve", lambda h: h.tensor_tensor(out=v(t1), in0=v(t1), in1=cb, op=ALU.mult), r=[d_rope, d_t1], w=[d_t1])
    p.op("dve", lambda h: h.tensor_tensor(out=dst[:, 0:ncols], in0=t1[:, 0:ncols], in1=t2[:, 0:ncols], op=ALU.add),
         r=[d_t1, d_t2], w=[d_dst])


def phase_inproj(p, l, I, S, Z, xsrc):
    gsh, d_gsh = S["gsh"]
    ident, d_ident = S["ident"]
    with SB(p) as sb:
        w, d_w = sb.t((128, 8, N_IN), BF16, "win")
        for k in range(8):
            p.dma("pool", w[:, k, :], I["w_in"][l, k * 128:(k + 1) * 128, :], w=[d_w])
        gainA, d_gainA = sb.t((128, 512))
        gainB, d_gainB = sb.t((128, 384))
        for m in range(8):
            p.dma("sp", gainA[:, m * 32:(m + 1) * 32], I["diff_qk_gain"][l, 0:1, :].partition_broadcast(128), w=[d_gainA])
            p.dma("sp", gainA[:, 256 + m * 32:256 + (m + 1) * 32], I["diff_qk_gain"][l, 1:2, :].partition_broadcast(128), w=[d_gainA])
        for m in range(6):
            p.dma("sp", gainB[:, m * 64:(m + 1) * 64], I["gqa_qk_gain"][l, (0 if m < 4 else 1):(1 if m < 4 else 2), :].partition_broadcast(128),
                  w=[d_gainB])
        xts = [sb.t((128, D)) for _ in range(2)]
        junk, d_junk = sb.t((128, D))
        xs, d_xs = sb.t((128, D))
        ss, d_ss = sb.t((128, 4))
        tps = [sb.ps() for _ in range(2)]
        nobj = (junk, d_junk, ss, d_ss, xs, d_xs, tps, ident, d_ident)
        xnT, d_xnT = sb.t((128, 8, 512), BF16, "xnT")
        sq, d_sq = sb.t((128, 512))
        st, d_st = sb.t((128, 16))
        t1, d_t1 = sb.t((128, 512))
        t2, d_t2 = sb.t((128, 512))
        qko = (sq, d_sq, st, d_st, t1, d_t1, t2, d_t2)
        qkn, d_qkn = sb.t((128, 512))
        sq2, d_sq2 = sb.t((128, 384))
        st2, d_st2 = sb.t((128, 16))
        t12, d_t12 = sb.t((128, 384))
        t22, d_t22 = sb.t((128, 384))
        qko2 = (sq2, d_sq2, st2, d_st2, t12, d_t12, t22, d_t22)
        qkn2, d_qkn2 = sb.t((128, 384))
        stg, d_stg = sb.t((128, 16))
        ps_tr2, d_ps_tr2 = sb.ps()
        ropeA, d_ropeA = sb.t((128, 2, 32))
        ropeB, d_ropeB = sb.t((128, 2, 64))
        ps_tm = [sb.ps() for _ in range(2)]
        ps_fm = [sb.ps() for _ in range(2)]
        ps_tr, d_ps_tr = sb.ps()
        stA, d_stA = sb.t((128, 4, 512), BF16)
        stB, d_stB = sb.t((128, 3, 512), BF16)
        stv, d_stv = sb.t((128, 512), BF16)
        stf, d_stf = sb.t((128, 784))
        stfm = [sb.t((128, 512)) for _ in range(2)]
        blocks = [(0, 2)] + [(2 + 4 * i, 4) for i in range(8)]
        ntm = 0
        nfm = 0
        for (t0, ntl) in blocks:
            ntok = ntl * 128
            tokb = t0 * 128
            j = 1 if t0 < 2 else 0
            for ti in range(ntl):
                tt = t0 + ti
                xt, d_xt = xts[tt % 2]
                p.dma("sp", xt[:], xsrc[tt * 128:(tt + 1) * 128, :], w=[d_xt])
                norm_tile(p, nobj, xt, d_xt, gsh, d_gsh, 0, j, xnT, d_xnT, ti * 128)
            for ti in range(ntl):
                tt = t0 + ti
                tk = slice(ti * 128, (ti + 1) * 128)
                rows = slice(tt * 128, (tt + 1) * 128)
                lat = tt >= 2
                if lat:
                    p.dma("sp", ropeA[:], I["ropeA"][(tt - 2) * 128:(tt - 1) * 128, :, :], w=[d_ropeA])
                    p.dma("sp", ropeB[:], I["ropeB"][(tt - 2) * 128:(tt - 1) * 128, :, :], w=[d_ropeB])

                def tm_mm(c0, ncol):
                    nonlocal ntm
                    ps, d_ps = ps_tm[ntm % 2]
                    ntm += 1
                    for k in range(8):
                        p.op("pe", lambda h, k=k, ps=ps: h.matmul(ps[:, 0:ncol], lhsT=xnT[:, k, tk], rhs=w[:, k, c0:c0 + ncol],
                                                                  start=(k == 0), stop=(k == 7)), r=[d_xnT, d_w], w=[d_ps])
                    return ps, d_ps
                ps, d_ps = tm_mm(CA_Q, 512)
                qk_norm_rope(p, qko, ps[:, 0:512], d_ps, 512, 32, gainA, d_gainA, ropeA if lat else None, d_ropeA, qkn, d_qkn)
                def trA():
                    for c in range(4):
                        p.op("pe", lambda h, c=c: h.transpose(ps_tr[:, c * 128:(c + 1) * 128], qkn[:, c * 128:(c + 1) * 128], ident[:]),
                             r=[d_qkn, d_ident], w=[d_ps_tr])
                    p.op("act", lambda h: h.activation(out=stA[:, :, tk], in_=ps_tr[:, :].rearrange("q (c t) -> q c t", c=4), func=AF.Copy),
                         r=[d_ps_tr], w=[d_stA])
                ps, d_ps = tm_mm(CA_V, 256)
                p.op("act", lambda h, ps=ps: h.activation(out=stv[:, 0:256], in_=ps[:, 0:256], func=AF.Copy), r=[d_ps], w=[d_stv])
                p.dma("sp", Z["vA"][rows, :], stv[:, 0:256], r=[d_stv])
                ps, d_ps = tm_mm(CB_Q, 512)
                p.op("act", lambda h, ps=ps: h.activation(out=stv[:, 256:384], in_=ps[:, 384:512], func=AF.Copy), r=[d_ps], w=[d_stv])
                p.dma("sp", Z["vB"][rows, :], stv[:, 256:384], r=[d_stv])
                qk_norm_rope(p, qko2, ps[:, 0:384], d_ps, 384, 64, gainB, d_gainB, ropeB if lat else None, d_ropeB, qkn2, d_qkn2)

                def trB():
                    for c in range(3):
                        p.op("pe", lambda h, c=c: h.transpose(ps_tr2[:, c * 128:(c + 1) * 128], qkn2[:, c * 128:(c + 1) * 128], ident[:]),
                             r=[d_qkn2, d_ident], w=[d_ps_tr2])
                    p.op("act", lambda h: h.activation(out=stB[:, :, tk], in_=ps_tr2[:, 0:384].rearrange("q (c t) -> q c t", c=3), func=AF.Copy),
                         r=[d_ps_tr2], w=[d_stB])
                ps, d_ps = tm_mm(CC_V, 512)
                p.op("act", lambda h, ps=ps: h.activation(out=stf[:, 0:512], in_=ps[:, 0:512], func=AF.Copy), r=[d_ps], w=[d_stf])
                p.dma("sp", Z["vC"][rows, :], stf[:, 0:256], r=[d_stf])
                p.dma("sp", Z["oC"][rows, :], stf[:, 256:512], r=[d_stf])
                ps, d_ps = tm_mm(CD_Z, 272)
                p.op("act", lambda h, ps=ps: h.activation(out=stf[:, 512:784], in_=ps[:, 0:272], func=AF.Copy), r=[d_ps], w=[d_stf])
                p.dma("sp", Z["zD"][rows, :], stf[:, 512:768], r=[d_stf])
                p.dma("sp", Z["gD"][rows, :], stf[:, 768:784], r=[d_stf])
                ps, d_ps = tm_mm(CC_K, 256)
                p.op("act", lambda h, ps=ps: h.activation(out=stf[:, 0:256], in_=ps[:, 0:256], func=AF.Copy), r=[d_ps], w=[d_stf])
                p.dma("sp", Z["kC"][rows, :], stf[:, 0:256], r=[d_stf])
                ps, d_ps = tm_mm(CC_G, 16)
                p.op("act", lambda h, ps=ps: h.activation(out=stg[:, 0:16], in_=ps[:, 0:16], func=AF.Copy), r=[d_ps], w=[d_stg])
                p.dma("sp", Z["gC"][rows, :], stg[:, 0:16], r=[d_stg])
                trA()
                trB()
            tb = slice(tokb, tokb + ntok)
            for c in range(2):
                p.dma("sp", Z["qTa"][c * 128:(c + 1) * 128, tb], stA[:, c, 0:ntok], r=[d_stA])
                p.dma("sp", Z["kTa"][c * 128:(c + 1) * 128, tb], stA[:, 2 + c, 0:ntok], r=[d_stA])
                p.dma("sp", Z["qTb"][c * 128:(c + 1) * 128, tb], stB[:, c, 0:ntok], r=[d_stB])
            p.dma("sp", Z["kTb"][:, tb], stB[:, 2, 0:ntok], r=[d_stB])
            for ci in range(10):
                c0 = CC_Q + ci * 128 if ci < 4 else CD_QKV + (ci - 4) * 128
                ps, d_ps = ps_fm[nfm % 2]
                so, d_so = stfm[nfm % 2]
                nfm += 1
                for k in range(8):
                    p.op("pe", lambda h, k=k, ps=ps, c0=c0: h.matmul(ps[:, 0:ntok], lhsT=w[:, k, c0:c0 + 128], rhs=xnT[:, k, 0:ntok],
                                                                     start=(k == 0), stop=(k == 7)), r=[d_xnT, d_w], w=[d_ps])
                p.op("act", lambda h, ps=ps, so=so: h.activation(out=so[:, 0:ntok], in_=ps[:, 0:ntok], func=AF.Copy), r=[d_ps], w=[d_so])
                if ci < 2:
                    dst = Z["qTc"][ci * 128:(ci + 1) * 128, tb]
                elif ci < 4:
                    dst = Z["kTc"][(ci - 2) * 128:(ci - 1) * 128, tb]
                else:
                    dst = Z["qkvT"][(ci - 4) * 128:(ci - 3) * 128, tb]
                p.dma("sp", dst, so[:, 0:ntok], r=[d_so])


def rope_table(dim):
    nf = dim // 4
    t = np.arange(NLAT)
    row = (t // 64).astype(np.float32)
    col = (t % 64).astype(np.float32)
    inv = (np.float32(10000.0) ** (-np.arange(nf, dtype=np.float32) / np.float32(nf))).astype(np.float32)
    ang = np.stack([row[:, None] * inv, col[:, None] * inv], axis=1).astype(np.float32)
    c, s = np.cos(ang).astype(np.float32), np.sin(ang).astype(np.float32)
    C = np.stack([c, c], axis=2)
    Sp = np.stack([-s, s], axis=2)
    return np.ascontiguousarray(np.stack([C.reshape(NLAT, dim), Sp.reshape(NLAT, dim)], axis=1)).astype(np.float32)


IN_SPECS = {
    "xin": ([T, D], F32), "cc": ([128, 8, 2], F32), "flags": ([128, 2], F32),
    "bmodT": ([128, 2, 48], F32), "norm1T": ([128, 2, 8], F32), "norm2T": ([128, 2, 8], F32),
    "b_mod": ([2, 6 * D], F32), "w_mod": ([2, D, 6 * D], F32), "w_in": ([2, D, N_IN], F32), "w_out": ([2, D, D], F32),
    "diff_qk_gain": ([2, 2, 32], F32), "diff_lambda": ([2, 4, 32], F32), "diff_subln": ([2, 64], F32),
    "gqa_qk_gain": ([2, 2, 64], F32), "mlstm_gate_bias": ([2, 16], F32), "mlstm_norm": ([2, 256], F32),
    "gdn_convT": ([128, 2, 6, 5], F32), "gdn_a_log": ([2, 8], F32), "gdn_dt_bias": ([2, 8], F32), "gdn_norm": ([2, 64], F32),
    "ffn_w_gate": ([D, D_FF], F32), "ffn_w_up": ([D, D_FF], F32), "ffn_w_down": ([D_FF, D], F32),
    "moe_router": ([D, NE], F32), "moe_w_gate": ([NE, D, D_FFE], F32), "moe_w_up": ([NE, D, D_FFE], F32),
    "moe_w_down": ([NE, D_FFE, D], F32),
    "ropeA": ([NLAT, 2, 32], F32), "ropeB": ([NLAT, 2, 64], F32), "ident": ([128, 128], F32),
    "cmask": ([128, 2, 128], F32),
}


def host_inputs(inp, core):
    b, hh = core // 2, core % 2
    f = lambda a: np.ascontiguousarray(np.asarray(a, dtype=np.float32))
    colsT = lambda v, n: f(np.asarray(v).reshape(v.shape[0], n, 128).transpose(2, 0, 1))
    m = {}
    m["xin"] = f(np.concatenate([inp["ctx"][b], inp["x"][b]], axis=0))
    m["cc"] = f(np.stack([np.asarray(inp["c"][b]).reshape(8, 128).T, np.asarray(inp["c_ctx"]).reshape(8, 128).T], axis=2))
    fl = np.zeros((128, 2), np.float32)
    fl[:, hh] = 1.0
    m["flags"] = fl
    m["bmodT"] = colsT(inp["b_mod"], 48)
    m["norm1T"] = colsT(inp["norm1"], 8)
    m["norm2T"] = colsT(inp["norm2"], 8)
    for k in ("b_mod", "w_mod", "w_in", "w_out", "diff_qk_gain", "diff_lambda", "diff_subln", "gqa_qk_gain", "mlstm_norm",
              "gdn_norm"):
        m[k] = f(inp[k])
    m["mlstm_gate_bias"] = f(np.asarray(inp["mlstm_gate_bias"]).reshape(2, 16))
    m["gdn_a_log"] = f(np.asarray(inp["gdn_a_log"]).reshape(2, 8))
    m["gdn_dt_bias"] = f(np.asarray(inp["gdn_dt_bias"]).reshape(2, 8))
    m["gdn_convT"] = f(np.asarray(inp["gdn_conv"]).reshape(2, 5, 6, 128).transpose(3, 0, 2, 1))
    m["ffn_w_gate"] = f(inp["ffn_w_gate"][0])
    m["ffn_w_up"] = f(inp["ffn_w_up"][0])
    m["ffn_w_down"] = f(inp["ffn_w_down"][0])
    m["moe_router"] = f(inp["moe_router"][0])
    m["moe_w_gate"] = f(inp["moe_w_gate"][0])
    m["moe_w_up"] = f(inp["moe_w_up"][0])
    m["moe_w_down"] = f(inp["moe_w_down"][0])
    m["ropeA"] = rope_table(32)
    m["ropeB"] = rope_table(64)
    m["ident"] = np.eye(128, dtype=np.float32)
    i = np.arange(128)
    low = (i[:, None] >= i[None, :]).astype(np.float32)
    m["cmask"] = f(np.stack([low, low.T], axis=1))
    return m


SCRATCH = {
    "xres": ([T, D], F32),
    "qTa": ([256, T], BF16), "kTa": ([256, T], BF16), "vA": ([T, 256], BF16),
    "qTb": ([256, T], BF16), "kTb": ([128, T], BF16), "vB": ([T, 128], BF16),
    "qTc": ([256, T], F32), "kTc": ([256, T], F32), "vC": ([T, 256], F32), "oC": ([T, 256], F32), "gC": ([T, 16], F32),
    "qkvT": ([768, T], F32), "zD": ([T, 256], F32), "gD": ([T, 16], F32), "kC": ([T, 256], F32),
    "gqT": ([256, T], F32), "gkT": ([256, T], F32), "gk": ([T, 256], F32), "gv": ([T, 256], F32),
    "y": ([T, D], F32), "ym": ([NLAT // 2, 512], F32), "xm1": ([NLAT // 2, D], F32),
}


def build(debug=None, upto="all"):
    p = Prog()
    nc = p.nc
    I = {k: nc.dram_tensor(k, sh, dt, kind="ExternalInput").ap() for k, (sh, dt) in IN_SPECS.items()}
    Z = {}
    for k, (sh, dt) in SCRATCH.items():
        kind = "ExternalOutput" if (debug and k in debug) else "Internal"
        Z[k] = nc.dram_tensor("z_" + k, sh, dt, kind=kind).ap()
    out = nc.dram_tensor("out", [NLAT // 2, D], F32, kind="ExternalOutput").ap()
    with SB(p) as gsb:
        S = {"modT": gsb.t((128, 48, 2)), "gsh": gsb.t((128, 4, 8, 2)), "grow": gsb.t((128, 2, 2, D)), "ident": gsb.t((128, 128))}
        p.dma("sp", S["ident"][0][:], I["ident"][:, :], w=[S["ident"][1]])
        epst, p.d_eps = gsb.t((128, 1))
        p.epst = epst
        p.op("dve", lambda h: h.memset(epst[:], EPS), w=[p.d_eps])
        if debug and "modT" in debug:
            dbg_mod = nc.dram_tensor("z_modT", [128, 48, 2], F32, kind="ExternalOutput").ap()
            dbg_grow = nc.dram_tensor("z_grow", [128, 2, 2, D], F32, kind="ExternalOutput").ap()
        for l in range(2):
            phase_mod(p, l, I, S)
            if debug and "modT" in debug and l == 0:
                p.dma("sp", dbg_mod[:, :, :], S["modT"][0][:], r=[S["modT"][1]])
                p.dma("sp", dbg_grow[:, :, :, :], S["grow"][0][:], r=[S["grow"][1]])
            phase_inproj(p, l, I, S, Z, I["xin"] if l == 0 else Z["xres"])
            if upto == "inproj":
                break
            if "noattn" not in upto:
                phase_attn(p, l, I, S, Z, l == 0, mine=(l == 1))
            if upto == "attn":
                break
            phase_chunk(p, l, I, S, Z, do_c=("noc" not in upto), do_d=("nod" not in upto), upto=upto)
            if upto.startswith("chunk"):
                break
            phase_outproj(p, l, I, S, Z, I["xin"] if l == 0 else Z["xres"], mine=(l == 1))
            if l == 0:
                phase_ffn_dense(p, l, I, S, Z)
                if upto == "layer0":
                    break
            else:
                phase_moe(p, l, I, S, Z, out)
        p.barrier()
    return p


def bcast_load(p, sb, src_row_ap, n, name="bc"):
    t, d = sb.t((128, n), F32, name)
    p.dma("sp", t[:], src_row_ap.partition_broadcast(128), w=[d])
    return t, d


def phase_attn(p, l, I, S, Z, with_ctx, mine=False):
    ident, d_ident = S["ident"]
    lambda_init = 0.8 - 0.6 * math.exp(-0.3 * l)
    with SB(p) as sb:
        lamv, d_lamv = sb.t((128, 4, 32))
        p.dma("sp", lamv[:], I["diff_lambda"][l:l + 1, :, :].partition_broadcast(128), w=[d_lamv])
        cst, d_cst = sb.t((128, 16))
        tmp32, d_tmp32 = sb.t((128, 2, 32))
        p.op("dve", lambda h: h.tensor_tensor(out=tmp32[:], in0=lamv[:, 0:4:2, :], in1=lamv[:, 1:4:2, :], op=ALU.mult),
             r=[d_lamv], w=[d_tmp32])
        p.op("dve", lambda h: h.tensor_reduce(out=cst[:, 0:2], in_=tmp32[:], axis=AX.X, op=ALU.add), r=[d_tmp32], w=[d_cst])
        p.op("act", lambda h: h.activation(out=cst[:, 2:4], in_=cst[:, 0:2], func=AF.Exp), r=[d_cst], w=[d_cst])
        p.op("dve", lambda h: h.tensor_tensor(out=cst[:, 4:5], in0=cst[:, 3:4], in1=cst[:, 2:3], op=ALU.subtract), r=[d_cst], w=[d_cst])
        p.op("dve", lambda h: h.tensor_scalar(out=cst[:, 4:5], in0=cst[:, 4:5], scalar1=-lambda_init, scalar2=None, op0=ALU.add),
             r=[d_cst], w=[d_cst])
        gA, d_gA = sb.t((128, 2, 32))
        gB, d_gB = sb.t((128, 2, 64))
        p.dma("sp", gA[:], I["diff_qk_gain"][l:l + 1, :, :].partition_broadcast(128), w=[d_gA])
        p.dma("sp", gB[:], I["gqa_qk_gain"][l:l + 1, :, :].partition_broadcast(128), w=[d_gB])
        p.op("dve", lambda h: h.tensor_reduce(out=cst[:, 6:8], in_=gA[:], axis=AX.X, op=ALU.max, apply_absolute_value=True),
             r=[d_gA], w=[d_cst])
        p.op("dve", lambda h: h.tensor_reduce(out=cst[:, 8:10], in_=gB[:], axis=AX.X, op=ALU.max, apply_absolute_value=True),
             r=[d_gB], w=[d_cst])
        p.op("dve", lambda h: h.scalar_tensor_tensor(out=cst[:, 10:11], in0=cst[:, 6:7], scalar=-math.sqrt(32.0), in1=cst[:, 7:8],
                                                     op0=ALU.mult, op1=ALU.mult), r=[d_cst], w=[d_cst])
        p.op("dve", lambda h: h.scalar_tensor_tensor(out=cst[:, 11:12], in0=cst[:, 8:9], scalar=-8.0, in1=cst[:, 9:10],
                                                     op0=ALU.mult, op1=ALU.mult), r=[d_cst], w=[d_cst])
        subg, d_subg = bcast_load(p, sb, I["diff_subln"][l:l + 1, :], 64)
        p.op("dve", lambda h: h.tensor_scalar(out=subg[:], in0=subg[:], scalar1=1.0 - lambda_init, scalar2=None, op0=ALU.mult),
             r=[d_subg], w=[d_subg])
        kT, d_kT = sb.t((64, T), BF16, "kT")
        va, d_va = sb.t((128, NT, 65), BF16, "vaug")
        qTs = [sb.t((64, 512), BF16, "qT") for _ in range(2)]
        NSB = 2
        pTs = [sb.t((128, 1024), BF16, "pT") for _ in range(NSB)]
        ps_s = [sb.ps((128, 1024)) for _ in range(NSB)]
        ps_o = [sb.ps() for _ in range(2)]
        ps_t, d_ps_t = sb.ps()
        osb = [sb.t((65, 512), F32, "osb") for _ in range(2)]
        on = [sb.t((128, 64), F32, "on") for _ in range(2)]
        od, d_od = sb.t((128, 64))
        junk, d_junk = sb.t((128, 64))
        st, d_st = sb.t((128, 4))
        ystage, d_ys = sb.t((128, 4, 64))
        rc, d_rc = sb.t((128, 2))
        qblocks = ([(0, 256, [0, 1])] if with_ctx else []) + [(256 + 512 * i, 512, list(range(NT))) for i in range(4 if mine else 8)]
        cnt = {"s": 0, "q": 0}
        if mine:
            fl, d_fl = sb.t((128, 2))
            p.dma("sp", fl[:], I["flags"][:, :], w=[d_fl])
            qTo = [sb.t((64, 512), BF16, "qTo") for _ in range(2)]

        def run_head(kind, qsrc_rows, nmaps, scale, negB, ycol):
            for (q0, qn, kts) in qblocks:
                qT, d_qT = qTs[cnt["q"] % 2]
                cnt["q"] += 1
                if not mine:
                    p.dma("sp", qT[:, 0:qn], qsrc_rows[:, q0:q0 + qn], w=[d_qT])
                else:
                    qo, d_qo = qTo[cnt["q"] % 2]
                    p.dma("sp", qT[:, 0:qn], qsrc_rows[:, q0:q0 + qn], w=[d_qT])
                    p.dma("sp", qo[:, 0:qn], qsrc_rows[:, q0 + 2048:q0 + 2048 + qn], w=[d_qo])
                    p.op("dve", lambda h, qT=qT: h.tensor_scalar(out=qT[:, 0:qn], in0=qT[:, 0:qn], scalar1=fl[0:64, 0:1], scalar2=None, op0=ALU.mult),
                         r=[d_qT, d_fl], w=[d_qT])
                    p.op("dve", lambda h, qT=qT, qo=qo: h.scalar_tensor_tensor(out=qT[:, 0:qn], in0=qo[:, 0:qn], scalar=fl[0:64, 1:2], in1=qT[:, 0:qn],
                                                                               op0=ALU.mult, op1=ALU.add), r=[d_qo, d_fl, d_qT], w=[d_qT])
                for j in range(nmaps):
                    kr = slice(32 * j, 32 * j + 32) if kind == "A" else slice(0, 64)
                    po, d_po = ps_o[j]
                    LA = 1
                    pend = []
                    pairs = [kts[i2:i2 + 2] for i2 in range(0, len(kts), 2)]
                    for pi in range(len(pairs) + LA):
                        if pi < len(pairs):
                            pr = pairs[pi]
                            i = cnt["s"]
                            cnt["s"] += 1
                            ps, d_ps = ps_s[i % NSB]
                            pT, d_pT = pTs[i % NSB]
                            for hh, kt in enumerate(pr):
                                p.op("pe", lambda h, ps=ps, kt=kt, kr=kr, qT=qT, hh=hh: h.matmul(ps[:, hh * 512:hh * 512 + qn],
                                                                                               lhsT=kT[kr, kt * 128:(kt + 1) * 128],
                                                                                               rhs=qT[kr, 0:qn], start=True, stop=True),
                                     r=[d_kT, d_qT], w=[d_ps])
                            np_ = len(pr)
                            p.op("act", lambda h, ps=ps, pT=pT, np_=np_: h.activation(
                                out=pT[:, :].rearrange("q (b n) -> q b n", b=2)[:, 0:np_, 0:qn],
                                in_=ps[:, :].rearrange("q (b n) -> q b n", b=2)[:, 0:np_, 0:qn], func=AF.Exp, scale=scale, bias=negB),
                                 r=[d_ps, d_cst], w=[d_pT])
                            pend.append((pr, pi, pT, d_pT))
                        if pi >= LA:
                            pr2, pi2, pT2, d_pT2 = pend.pop(0)
                            for hh, kt2 in enumerate(pr2):
                                first = (pi2 == 0 and hh == 0)
                                last = (pi2 == len(pairs) - 1 and hh == len(pr2) - 1)
                                p.op("pe", lambda h, po=po, pT2=pT2, kt2=kt2, hh=hh, first=first, last=last: h.matmul(
                                    po[0:65, 0:qn], lhsT=va[:, kt2, :], rhs=pT2[:, hh * 512:hh * 512 + qn], start=first, stop=last),
                                     r=[d_va, d_pT2], w=[d_po])
                    ob, d_ob = osb[j]
                    p.op("act", lambda h, ob=ob, po=po: h.activation(out=ob[:, 0:qn], in_=po[0:65, 0:qn], func=AF.Copy), r=[d_po], w=[d_ob])
                nsub = qn // 128
                for s_ in range(nsub):
                    for j in range(nmaps):
                        ob, d_ob = osb[j]
                        p.op("pe", lambda h, ob=ob, j=j, s_=s_: h.transpose(ps_t[:, j * 128:j * 128 + 65], ob[0:65, s_ * 128:(s_ + 1) * 128],
                                                                            ident[0:65, 0:65]), r=[d_ob, d_ident], w=[d_ps_t])
                    for j in range(nmaps):
                        o_, d_o = on[j]
                        p.op("dve", lambda h, j=j: h.reciprocal(out=rc[:, j:j + 1], in_=ps_t[:, j * 128 + 64:j * 128 + 65]),
                             r=[d_ps_t], w=[d_rc])
                        p.op("dve", lambda h, o_=o_, j=j: h.tensor_scalar(out=o_[:], in0=ps_t[:, j * 128:j * 128 + 64],
                                                                          scalar1=rc[:, j:j + 1], scalar2=None,
                                                                          op0=ALU.mult), r=[d_ps_t, d_rc], w=[d_o])
                    if kind == "A":
                        p.op("dve", lambda h: h.scalar_tensor_tensor(out=od[:], in0=on[1][0][:], scalar=cst[:, 4:5], in1=on[0][0][:],
                                                                     op0=ALU.mult, op1=ALU.add), r=[on[0][1], on[1][1], d_cst], w=[d_od])
                        p.op("act", lambda h: h.activation(out=junk[:], in_=od[:], func=AF.Square, accum_out=st[:, 0:1]),
                             r=[d_od], w=[d_junk, d_st])
                        rstd(p, st[:, 2:3], st[:, 0:1], st[:, 1:2], 1.0 / 64, [d_st])
                        p.op("dve", lambda h, s_=s_: h.scalar_tensor_tensor(out=ystage[:, s_, :], in0=od[:], scalar=st[:, 2:3], in1=subg[:],
                                                                            op0=ALU.mult, op1=ALU.mult), r=[d_od, d_st, d_subg], w=[d_ys])
                    else:
                        p.op("dve", lambda h, s_=s_: h.tensor_copy(out=ystage[:, s_, :], in_=on[0][0][:]), r=[on[0][1]], w=[d_ys])
                ydst = Z["ym"][q0 - 256:q0 - 256 + qn, ycol:ycol + 64] if mine else Z["y"][q0:q0 + qn, ycol:ycol + 64]
                p.dma("sp", ydst.rearrange("(s q) d -> q s d", q=128), ystage[:, 0:nsub, :], r=[d_ys])

        def load_kv(ksrc_rows, vsrc_cols):
            p.dma("sp", kT[:, :], ksrc_rows, w=[d_kT])
            p.dma("sp", va[:, :, 0:64], vsrc_cols.rearrange("(n q) d -> q n d", q=128), w=[d_va])
            p.op("dve", lambda h: h.memset(va[:, :, 64:65], 1.0), w=[d_va])

        for hd in range(4):
            load_kv(Z["kTa"][64 * hd:64 * hd + 64, :], Z["vA"][:, 64 * hd:64 * hd + 64])
            run_head("A", Z["qTa"][64 * hd:64 * hd + 64, :], 2, 1.0 / math.sqrt(32.0), cst[:, 10:11], 64 * hd)
        for kv in range(2):
            load_kv(Z["kTb"][64 * kv:64 * kv + 64, :], Z["vB"][:, 64 * kv:64 * kv + 64])
            for g in (2 * kv, 2 * kv + 1):
                run_head("B", Z["qTb"][64 * g:64 * g + 64, :], 1, 0.125, cst[:, 11:12], 256 + 64 * g)


ORDER = [list(range(NT)), [1, 0] + list(range(NT - 1, 1, -1))]
NGC = 40


class PsumPool:
    def __init__(self, sb, nbanks=8):
        self.q = []
        banks = [sb.ps() for b in range(nbanks)]
        for k in range(4):
            for (t, d) in banks:
                self.q.append((t[:, k * 128:(k + 1) * 128], d))
        self.i = 0

    def get(self):
        r = self.q[self.i % len(self.q)]
        self.i += 1
        return r


def phase_gdn_prep(p, l, I, S, Z):
    ident, d_ident = S["ident"]
    W = 2 + 256 + 4 + 4096 + 2
    with SB(p) as sb:
        cw, d_cw = sb.t((128, 6, 5))
        p.dma("sp", cw[:], I["gdn_convT"][:, l, :, :], w=[d_cw])
        bones, d_bones = sb.t((128, 128))
        p.op("dve", lambda h: h.memset(bones[:], 0.0), w=[d_bones])
        p.op("dve", lambda h: h.memset(bones[0:64, 0:64], 1.0), w=[d_bones])
        p.op("dve", lambda h: h.memset(bones[64:128, 64:128], 1.0), w=[d_bones])
        X, d_X = sb.t((128, W))
        acc, d_acc = sb.t((128, W))
        sq, d_sq = sb.t((128, 512))
        rs, d_rs = sb.t((128, 512))
        pss = [sb.ps() for _ in range(2)]
        pst = [sb.ps() for _ in range(2)]
        tst = [sb.t((128, 128)) for _ in range(2)]
        p.op("dve", lambda h: h.memset(X[:], 0.0), w=[d_X])
        nb = 0
        for fc in range(6):
            p.dma("sp", X[:, 2:258], Z["qkvT"][fc * 128:(fc + 1) * 128, 0:256], w=[d_X])
            p.dma("sp", X[:, 262:4358], Z["qkvT"][fc * 128:(fc + 1) * 128, 256:T], w=[d_X])
            lo, hi = 2, 4358
            p.op("dve", lambda h, fc=fc: h.tensor_scalar(out=acc[:, lo:hi], in0=X[:, lo - 2:hi - 2], scalar1=cw[:, fc, 0:1], scalar2=None,
                                                         op0=ALU.mult), r=[d_X, d_cw], w=[d_acc])
            for tap in range(1, 5):
                eng = "dve"
                p.op(eng, lambda h, fc=fc, tap=tap: h.scalar_tensor_tensor(out=acc[:, lo:hi], in0=X[:, lo + tap - 2:hi + tap - 2],
                                                                             scalar=cw[:, fc, tap:tap + 1], in1=acc[:, lo:hi],
                                                                             op0=ALU.mult, op1=ALU.add), r=[d_X, d_cw, d_acc], w=[d_acc])
            p.op("act", lambda h: h.activation(out=X[:, lo:hi], in_=acc[:, lo:hi], func=AF.Sigmoid), r=[d_acc], w=[d_X])
            p.op("dve", lambda h: h.tensor_tensor(out=acc[:, lo:hi], in0=acc[:, lo:hi], in1=X[:, lo:hi], op=ALU.mult), r=[d_X, d_acc], w=[d_acc])
            segs = [(2, 256, 0)] + [(262 + 512 * i, 512, 256 + 512 * i) for i in range(8)]
            if fc < 4:
                for (c0, n, t0) in segs:
                    ps, d_ps = pss[nb % 2]
                    nb += 1
                    p.op("act", lambda h: h.activation(out=sq[:, 0:n], in_=acc[:, c0:c0 + n], func=AF.Square), r=[d_acc], w=[d_sq])
                    p.op("pe", lambda h, ps=ps: h.matmul(ps[:, 0:n], lhsT=bones[:], rhs=sq[:, 0:n], start=True, stop=True),
                         r=[d_bones, d_sq], w=[d_ps])
                    p.op("act", lambda h, ps=ps: h.activation(out=rs[:, 0:n], in_=ps[:, 0:n], func=AF.Sqrt, scale=1.0, bias=p.eps_ap(EPS, 128)),
                         r=[d_ps, p.d_eps], w=[d_rs])
                    p.op("dve", lambda h: h.reciprocal(out=rs[:, 0:n], in_=rs[:, 0:n]), r=[d_rs], w=[d_rs])
                    p.op("dve", lambda h: h.scalar_tensor_tensor(out=acc[:, c0:c0 + n], in0=acc[:, c0:c0 + n], scalar=(0.125 if fc < 2 else 1.0),
                                                                 in1=rs[:, 0:n], op0=ALU.mult, op1=ALU.mult), r=[d_acc, d_rs], w=[d_acc])
                dst = Z["gqT"] if fc < 2 else Z["gkT"]
                r0 = (fc % 2) * 128
                p.dma("sp", dst[r0:r0 + 128, 0:256], acc[:, 2:258], r=[d_acc])
                p.dma("sp", dst[r0:r0 + 128, 256:T], acc[:, 262:4358], r=[d_acc])
            if fc >= 2:
                dst = Z["gk"] if fc < 4 else Z["gv"]
                r0 = (fc % 2) * 128
                for tt in range(NT):
                    c0 = 2 + tt * 128 if tt < 2 else 262 + (tt - 2) * 128
                    ps, d_ps = pst[tt % 2]
                    ts_, d_ts = tst[tt % 2]
                    p.op("pe", lambda h, ps=ps, c0=c0: h.transpose(ps[:, 0:128], acc[:, c0:c0 + 128], ident[:]), r=[d_acc, d_ident], w=[d_ps])
                    p.op("act", lambda h, ps=ps, ts_=ts_: h.activation(out=ts_[:], in_=ps[:, 0:128], func=AF.Copy), r=[d_ps], w=[d_ts])
                    p.dma("sp", dst[tt * 128:(tt + 1) * 128, r0:r0 + 128], ts_[:], r=[d_ts])


def softplus_parts(p, z, d_z, tmp, d_tmp, n):
    p.op("dve", lambda h: h.tensor_scalar(out=tmp[:, n:2 * n], in0=z, scalar1=-1.0, scalar2=None, op0=ALU.mult), r=[d_z], w=[d_tmp])
    p.op("dve", lambda h: h.tensor_tensor(out=tmp[:, n:2 * n], in0=tmp[:, n:2 * n], in1=z, op=ALU.min), r=[d_z, d_tmp], w=[d_tmp])
    p.op("act", lambda h: h.activation(out=tmp[:, n:2 * n], in_=tmp[:, n:2 * n], func=AF.Exp), r=[d_tmp], w=[d_tmp])
    p.op("act", lambda h: h.activation(out=tmp[:, 0:n], in_=tmp[:, n:2 * n], func=AF.Ln, scale=1.0, bias=p.ones1[:, 0:1]),
         r=[d_tmp, p.d_ones1], w=[d_tmp])


def phase_gates(p, l, I, S, Z, tab, d_tab, cm, d_cm, ones, d_ones):
    ident, d_ident = S["ident"]
    with SB(p) as sb:
        pp = PsumPool(sb, 4)
        biasC, d_biasC = bcast_load(p, sb, I["mlstm_gate_bias"][l:l + 1, :], 16)
        dtb, d_dtb = bcast_load(p, sb, I["gdn_dt_bias"][l:l + 1, :], 8)
        nea, d_nea = bcast_load(p, sb, I["gdn_a_log"][l:l + 1, :], 8)
        p.op("act", lambda h: h.activation(out=nea[:], in_=nea[:], func=AF.Exp), r=[d_nea], w=[d_nea])
        p.op("dve", lambda h: h.tensor_scalar(out=nea[:], in0=nea[:], scalar1=-1.0, scalar2=None, op0=ALU.mult), r=[d_nea], w=[d_nea])
        gts = [sb.t((128, 32)) for _ in range(2)]
        for d in range(2):
            Bprev, d_B = sb.t((128, 4))
            R, d_R = sb.t((128, 4))
            p.op("dve", lambda h: h.memset(Bprev[:], 0.0), w=[d_B])
            p.op("dve", lambda h: h.memset(R[:], 0.0), w=[d_R])
            for s_, tt in enumerate(ORDER[d]):
                g, d_g = gts[s_ % 2]
                p.dma("sp", g[:, 0:16], Z["gC"][tt * 128:(tt + 1) * 128, :], w=[d_g])
                p.dma("sp", g[:, 16:32], Z["gD"][tt * 128:(tt + 1) * 128, :], w=[d_g])
                wk, d_wk = S["gwk"][s_ % 2]
                lhs_cum = cm[:, 1 - d, :]
                T_ = lambda a, b: tab[:, tt, d, a:b]
                xf = wk[:, 0:4]
                ig = wk[:, 4:8]
                p.op("dve", lambda h: h.tensor_tensor(out=xf, in0=g[:, 8 * d + 4:8 * d + 8], in1=biasC[:, 8 * d + 4:8 * d + 8], op=ALU.add),
                     r=[d_g, d_biasC], w=[d_wk])
                p.op("dve", lambda h: h.tensor_tensor(out=ig, in0=g[:, 8 * d:8 * d + 4], in1=biasC[:, 8 * d:8 * d + 4], op=ALU.add),
                     r=[d_g, d_biasC], w=[d_wk])
                softplus_parts(p, xf, d_wk, wk[:, 8:16], d_wk, 4)
                logf = wk[:, 16:20]
                p.op("dve", lambda h: h.scalar_tensor_tensor(out=logf, in0=xf, scalar=0.0, in1=wk[:, 8:12], op0=ALU.min, op1=ALU.subtract),
                     r=[d_wk], w=[d_wk])
                pcs, d_pcs = pp.get()
                ptot, d_ptot = pp.get()
                p.op("pe", lambda h: h.matmul(pcs[:, 0:4], lhsT=lhs_cum, rhs=logf, start=True, stop=True), r=[d_cm, d_wk], w=[d_pcs])
                p.op("pe", lambda h: h.matmul(ptot[:, 0:4], lhsT=ones[:], rhs=logf, start=True, stop=True), r=[d_ones, d_wk], w=[d_ptot])
                Bv = wk[:, 20:24]
                av = wk[:, 24:28]
                p.op("dve", lambda h: h.tensor_tensor(out=Bv, in0=pcs[:, 0:4], in1=Bprev[:], op=ALU.add), r=[d_pcs, d_B], w=[d_wk])
                p.op("dve", lambda h: h.tensor_tensor(out=av, in0=ig, in1=Bv, op=ALU.subtract), r=[d_wk], w=[d_wk])
                p.op("dve", lambda h: h.tensor_tensor(out=Bprev[:], in0=ptot[:, 0:4], in1=Bprev[:], op=ALU.add), r=[d_ptot, d_B], w=[d_B])
                ptr, d_ptr = pp.get()
                p.op("pe", lambda h: h.transpose(ptr[0:4, 0:128], av, ident[:]), r=[d_wk, d_ident], w=[d_ptr])
                am, d_am = S["gam4"]
                p.op("dve", lambda h: h.tensor_reduce(out=am[0:4, 0:1], in_=ptr[0:4, 0:128], axis=AX.X, op=ALU.max), r=[d_ptr], w=[d_am])
                p.op("dve", lambda h: h.tensor_scalar(out=am[0:4, 4:8], in0=ident[0:4, 0:4], scalar1=am[0:4, 0:1], scalar2=None, op0=ALU.mult),
                     r=[d_am, d_ident], w=[d_am])
                pam, d_pam = pp.get()
                p.op("pe", lambda h: h.matmul(pam[:, 0:4], lhsT=ones[0:4, :], rhs=am[0:4, 4:8], start=True, stop=True),
                     r=[d_ones, d_am], w=[d_pam])
                Mc = wk[:, 28:32]
                p.op("dve", lambda h: h.tensor_tensor(out=Mc, in0=pam[:, 0:4], in1=R[:], op=ALU.max), r=[d_pam, d_R], w=[d_wk])
                p.op("dve", lambda h: h.tensor_tensor(out=wk[:, 32:36], in0=R[:], in1=Mc, op=ALU.subtract), r=[d_R, d_wk], w=[d_wk])
                p.op("dve", lambda h: h.tensor_tensor(out=wk[:, 36:40], in0=av, in1=Mc, op=ALU.subtract), r=[d_wk], w=[d_wk])
                p.op("dve", lambda h: h.tensor_tensor(out=wk[:, 40:44], in0=Bv, in1=Mc, op=ALU.add), r=[d_wk], w=[d_wk])
                p.op("dve", lambda h: h.tensor_copy(out=R[:], in_=Mc), r=[d_wk], w=[d_R])
                p.op("act", lambda h: h.activation(out=T_(8, 12), in_=wk[:, 32:36], func=AF.Exp), r=[d_wk], w=[d_tab])
                p.op("act", lambda h: h.activation(out=T_(0, 4), in_=wk[:, 36:40], func=AF.Exp), r=[d_wk], w=[d_tab])
                p.op("act", lambda h: h.activation(out=T_(4, 8), in_=wk[:, 40:44], func=AF.Exp, scale=-1.0), r=[d_wk], w=[d_tab])
                z = wk[:, 44:48]
                p.op("dve", lambda h: h.tensor_tensor(out=z, in0=g[:, 16 + 8 * d + 4:16 + 8 * d + 8], in1=dtb[:, 4 * d:4 * d + 4], op=ALU.add),
                     r=[d_g, d_dtb], w=[d_wk])
                softplus_parts(p, z, d_wk, wk[:, 48:56], d_wk, 4)
                gg = wk[:, 56:60]
                p.op("dve", lambda h: h.scalar_tensor_tensor(out=gg, in0=z, scalar=0.0, in1=wk[:, 48:52], op0=ALU.max, op1=ALU.add),
                     r=[d_wk], w=[d_wk])
                p.op("dve", lambda h: h.tensor_tensor(out=gg, in0=gg, in1=nea[:, 4 * d:4 * d + 4], op=ALU.mult), r=[d_wk, d_nea], w=[d_wk])
                p.op("act", lambda h: h.activation(out=T_(12, 16), in_=g[:, 16 + 8 * d:16 + 8 * d + 4], func=AF.Sigmoid), r=[d_g], w=[d_tab])
                p.op("dve", lambda h: h.tensor_scalar(out=T_(16, 20), in0=T_(12, 16), scalar1=-1.0, scalar2=None, op0=ALU.mult),
                     r=[d_tab], w=[d_tab])
                pgm, d_pgm = pp.get()
                pgl, d_pgl = pp.get()
                p.op("pe", lambda h: h.matmul(pgm[:, 0:4], lhsT=lhs_cum, rhs=gg, start=True, stop=True), r=[d_cm, d_wk], w=[d_pgm])
                p.op("pe", lambda h: h.matmul(pgl[:, 0:4], lhsT=ones[:], rhs=gg, start=True, stop=True), r=[d_ones, d_wk], w=[d_pgl])
                p.op("dve", lambda h: h.tensor_copy(out=T_(20, 24), in_=pgm[:, 0:4]), r=[d_pgm], w=[d_tab])
                p.op("dve", lambda h: h.tensor_tensor(out=wk[:, 60:64], in0=pgl[:, 0:4], in1=T_(20, 24), op=ALU.subtract),
                     r=[d_pgl, d_tab], w=[d_wk])
                p.op("act", lambda h: h.activation(out=T_(24, 28), in_=T_(20, 24), func=AF.Exp), r=[d_tab], w=[d_tab])
                p.op("act", lambda h: h.activation(out=T_(28, 32), in_=wk[:, 60:64], func=AF.Exp), r=[d_wk], w=[d_tab])
                p.op("act", lambda h: h.activation(out=T_(32, 36), in_=pgl[:, 0:4], func=AF.Exp), r=[d_pgl], w=[d_tab])
                p.op("dve", lambda h: h.tensor_tensor(out=T_(36, 40), in0=T_(12, 16), in1=T_(24, 28), op=ALU.mult), r=[d_tab], w=[d_tab])


def phase_chunk(p, l, I, S, Z, do_c=True, do_d=True, upto=""):
    ident, d_ident = S["ident"]
    phase_gdn_prep(p, l, I, S, Z)
    if "stopprep" in upto:
        return
    with SB(p) as sb:
        tab, d_tab = sb.t((128, NT, 2, NGC), F32, "gtab")
        cm, d_cm = sb.t((128, 2, 128), F32, "cmask")
        p.dma("sp", cm[:], I["cmask"][:, :, :], w=[d_cm])
        ones, d_ones = sb.t((128, 128))
        p.op("dve", lambda h: h.memset(ones[:], 1.0), w=[d_ones])
        ones1, p.d_ones1 = sb.t((128, 1))
        p.ones1 = ones1
        p.op("dve", lambda h: h.memset(ones1[:], 1.0), w=[p.d_ones1])
        S["gwk"] = [sb.t((128, 64)) for _ in range(2)]
        S["gam4"] = sb.t((8, 8))
        strict, d_strict = sb.t((128, 2, 128))
        for d in range(2):
            p.op("dve", lambda h, d=d: h.tensor_tensor(out=strict[:, d, :], in0=cm[:, d, :], in1=ident[:], op=ALU.subtract),
                 r=[d_cm, d_ident], w=[d_strict])
        phase_gates(p, l, I, S, Z, tab, d_tab, cm, d_cm, ones, d_ones)
        if "stopgates" in upto:
            return
        acc, d_acc = sb.t((128, NT, 256), F32, "hacc")
        if do_c:
            p.op("dve", lambda h: h.memset(acc[:], 0.0), w=[d_acc])
            chunk_c(p, l, I, S, Z, sb, tab, d_tab, cm, d_cm, acc, d_acc)
            p.barrier()
        if do_d:
            p.op("dve", lambda h: h.memset(acc[:], 0.0), w=[d_acc])
            chunk_d(p, l, I, S, Z, sb, tab, d_tab, cm, d_cm, strict, d_strict, ones, d_ones, acc, d_acc)


def chunk_c(p, l, I, S, Z, sbo, tab, d_tab, cm, d_cm, acc, d_acc):
    ident, d_ident = S["ident"]
    with SB(p) as sb:
        two = lambda shape, nm: [sb.t(shape, F32, nm) for _ in range(2)]
        ps_st = [sb.ps() for _ in range(2)]
        ps_o = [sb.ps() for _ in range(2)]
        ps_c = [sb.ps() for _ in range(2)]
        qf = [two((64, 4, 128), "cqf") for _ in range(2)]
        kf = [two((64, 4, 128), "ckf") for _ in range(2)]
        ktm = [two((128, 4, 64), "cktm") for _ in range(2)]
        vaug = [two((128, 4, 66), "cvaug") for _ in range(2)]
        for d in range(2):
            for q_ in range(2):
                va_, d_va_ = vaug[d][q_]
                p.op("dve", lambda h, va_=va_: h.memset(va_[:, :, 64:65], 1.0), w=[d_va_])
                p.op("dve", lambda h, va_=va_: h.memset(va_[:, :, 65:66], 0.0), w=[d_va_])
        ks, PT, vp, htmp = two((128, 4, 64), "cks"), two((128, 4, 128), "cPT"), two((128, 4, 66), "cvp"), two((128, 4, 64), "chtmp")
        rc = two((128, 8), "crc")
        Cst, d_Cst = sb.t((64, 8, 66), F32, "cCst")
        Cd, d_Cd = sb.t((64, 8, 66), F32, "cCd")
        p.op("dve", lambda h: h.memset(Cst[:], 0.0), w=[d_Cst])
        B3 = lambda t: t[:, :].rearrange("q (h c) -> q h c", h=4)
        O3 = lambda t: t[:, 0:264].rearrange("q (h c) -> q h c", h=4)
        for s_ in range(NT):
            par = s_ % 2
            tts = [ORDER[d][s_] for d in range(2)]
            for d in range(2):
                tk = slice(tts[d] * 128, (tts[d] + 1) * 128)
                p.dma("sp", qf[d][par][0][:], Z["qTc"][:, tk].rearrange("(h q) t -> q h t", q=64), w=[qf[d][par][1]])
                p.dma("sp", kf[d][par][0][:], Z["kTc"][:, tk].rearrange("(h q) t -> q h t", q=64), w=[kf[d][par][1]])
                p.dma("sp", ktm[d][par][0][:], Z["kC"][tk, :].rearrange("q (h e) -> q h e", e=64), w=[ktm[d][par][1]])
                p.dma("sp", vaug[d][par][0][:, :, 0:64], Z["vC"][tk, :].rearrange("q (h e) -> q h e", e=64), w=[vaug[d][par][1]])
            for d in range(2):
                tt = tts[d]
                qf_, d_qf = qf[d][par]
                kf_, d_kf = kf[d][par]
                pst, d_pst = ps_st[d]
                for hd in range(4):
                    p.op("pe", lambda h, pst=pst, hd=hd, kf_=kf_, qf_=qf_: h.matmul(pst[:, hd * 128:(hd + 1) * 128], lhsT=kf_[:, hd, :], rhs=qf_[:, hd, :],
                                                                                  start=True, stop=True), r=[d_kf, d_qf], w=[d_pst])
                PT_, d_PT = PT[d]
                p.op("dve", lambda h, pst=pst, PT_=PT_, d=d: h.scalar_tensor_tensor(out=PT_[:], in0=B3(pst), scalar=0.125,
                                                                                 in1=cm[:, 1 - d, :].unsqueeze(1).to_broadcast([128, 4, 128]),
                                                                                 op0=ALU.mult, op1=ALU.mult), r=[d_pst, d_cm], w=[d_PT])
                vp_, d_vp = vp[d]
                va_, d_va_ = vaug[d][par]
                p.op("pool", lambda h, vp_=vp_, va_=va_, tt=tt, d=d: h.tensor_tensor(out=vp_[:], in0=va_[:],
                                                                                  in1=tab[:, tt, d, 0:4].unsqueeze(2).to_broadcast([128, 4, 66]),
                                                                                  op=ALU.mult), r=[d_va_, d_tab], w=[d_vp])
                ks_, d_ks = ks[d]
                kt_, d_kt = ktm[d][par]
                p.op("pool", lambda h, ks_=ks_, kt_=kt_: h.tensor_scalar(out=ks_[:], in0=kt_[:], scalar1=0.125, scalar2=None, op0=ALU.mult),
                     r=[d_kt], w=[d_ks])
                p.op("dve", lambda h, tt=tt, d=d: h.tensor_tensor(out=Cd[:, 4 * d:4 * d + 4, :], in0=Cst[:, 4 * d:4 * d + 4, :],
                                                                  in1=tab[0:64, tt, d, 8:12].unsqueeze(2).to_broadcast([64, 4, 66]), op=ALU.mult),
                     r=[d_Cst, d_tab], w=[d_Cd])
            for d in range(2):
                qf_, d_qf = qf[d][par]
                po, d_po = ps_o[d]
                pc, d_pc = ps_c[d]
                PT_, d_PT = PT[d]
                vp_, d_vp = vp[d]
                ks_, d_ks = ks[d]
                for hd in range(4):
                    i = 4 * d + hd
                    p.op("pe", lambda h, po=po, hd=hd, PT_=PT_, vp_=vp_: h.matmul(po[:, hd * 66:(hd + 1) * 66], lhsT=PT_[:, hd, :], rhs=vp_[:, hd, :],
                                                                                start=True, stop=False), r=[d_PT, d_vp], w=[d_po])
                    p.op("pe", lambda h, po=po, hd=hd, i=i, qf_=qf_: h.matmul(po[:, hd * 66:(hd + 1) * 66], lhsT=qf_[:, hd, :], rhs=Cd[:, i, :],
                                                                             start=False, stop=True), r=[d_qf, d_Cd], w=[d_po])
                for hd in range(4):
                    p.op("pe", lambda h, pc=pc, hd=hd, ks_=ks_, vp_=vp_: h.matmul(pc[0:64, hd * 66:(hd + 1) * 66], lhsT=ks_[:, hd, :], rhs=vp_[:, hd, :],
                                                                                start=True, stop=True), r=[d_ks, d_vp], w=[d_pc])
                p.op("dve", lambda h, pc=pc, d=d: h.tensor_tensor(out=Cst[:, 4 * d:4 * d + 4, :],
                                                                  in0=pc[0:64, 0:264].rearrange("q (h c) -> q h c", h=4),
                                                                  in1=Cd[:, 4 * d:4 * d + 4, :], op=ALU.add), r=[d_pc, d_Cd], w=[d_Cst])
            for d in range(2):
                tt = tts[d]
                po, d_po = ps_o[d]
                rc_, d_rc = rc[d]
                ht_, d_ht = htmp[d]
                p.op("act", lambda h, po=po, rc_=rc_: h.activation(out=rc_[:, 0:4], in_=O3(po)[:, :, 64], func=AF.Abs), r=[d_po], w=[d_rc])
                p.op("dve", lambda h, rc_=rc_, tt=tt, d=d: h.tensor_tensor(out=rc_[:, 0:4], in0=rc_[:, 0:4], in1=tab[:, tt, d, 4:8], op=ALU.max),
                     r=[d_rc, d_tab], w=[d_rc])
                p.op("dve", lambda h, rc_=rc_: h.reciprocal(out=rc_[:, 4:8], in_=rc_[:, 0:4]), r=[d_rc], w=[d_rc])
                p.op("dve", lambda h, po=po, rc_=rc_, ht_=ht_: h.tensor_tensor(out=ht_[:], in0=O3(po)[:, :, 0:64],
                                                                               in1=rc_[:, 4:8].unsqueeze(2).to_broadcast([128, 4, 64]), op=ALU.mult),
                     r=[d_po, d_rc], w=[d_ht])
                dst = acc[:, tt, :].rearrange("q (h e) -> q h e", e=64)
                p.op("pool", lambda h, dst=dst, ht_=ht_: h.tensor_tensor(out=dst, in0=dst, in1=ht_[:], op=ALU.add), r=[d_ht, d_acc], w=[d_acc])
        gain, d_gain = bcast_load(p, sb, I["mlstm_norm"][l:l + 1, :], 256)
        ots = [sb.t((128, 256)) for _ in range(2)]
        xcs = [sb.t((128, 256)) for _ in range(2)]
        sqs = [sb.t((128, 256)) for _ in range(2)]
        sts = [sb.t((128, 12)) for _ in range(2)]
        for tt in range(NT):
            ot, d_ot = ots[tt % 2]
            xc, d_xc = xcs[tt % 2]
            sq, d_sq = sqs[tt % 2]
            st, d_st = sts[tt % 2]
            p.dma("sp", ot[:], Z["oC"][tt * 128:(tt + 1) * 128, :], w=[d_ot])
            p.op("act", lambda h, ot=ot: h.activation(out=ot[:], in_=ot[:], func=AF.Sigmoid), r=[d_ot], w=[d_ot])
            hv = acc[:, tt, :].rearrange("q (g e) -> q g e", e=64)
            v3 = lambda a: a[:, :].rearrange("q (g e) -> q g e", e=64)
            p.op("dve", lambda h, st=st, hv=hv: h.tensor_reduce(out=st[:, 0:4], in_=hv, axis=AX.X, op=ALU.add), r=[d_acc], w=[d_st])
            p.op("dve", lambda h, st=st: h.tensor_scalar(out=st[:, 0:4], in0=st[:, 0:4], scalar1=1.0 / 64, scalar2=None, op0=ALU.mult),
                 r=[d_st], w=[d_st])
            p.op("dve", lambda h, st=st, hv=hv, xc=xc: h.tensor_tensor(out=v3(xc), in0=hv, in1=st[:, 0:4].unsqueeze(2).to_broadcast([128, 4, 64]),
                                                                       op=ALU.subtract), r=[d_acc, d_st], w=[d_xc])
            p.op("act", lambda h, sq=sq, xc=xc: h.activation(out=sq[:], in_=xc[:], func=AF.Square), r=[d_xc], w=[d_sq])
            p.op("dve", lambda h, st=st, sq=sq: h.tensor_reduce(out=st[:, 4:8], in_=v3(sq), axis=AX.X, op=ALU.add), r=[d_sq], w=[d_st])
            rstd(p, st[:, 8:12], st[:, 4:8], st[:, 4:8], 1.0 / 64, [d_st])
            p.op("dve", lambda h, st=st, xc=xc: h.tensor_tensor(out=v3(xc), in0=v3(xc), in1=st[:, 8:12].unsqueeze(2).to_broadcast([128, 4, 64]),
                                                                op=ALU.mult), r=[d_st, d_xc], w=[d_xc])
            p.op("dve", lambda h, xc=xc: h.tensor_tensor(out=xc[:], in0=xc[:], in1=gain[:], op=ALU.mult), r=[d_gain, d_xc], w=[d_xc])
            p.op("dve", lambda h, xc=xc, ot=ot: h.tensor_tensor(out=xc[:], in0=xc[:], in1=ot[:], op=ALU.mult), r=[d_ot, d_xc], w=[d_xc])
            p.dma("sp", Z["y"][tt * 128:(tt + 1) * 128, 512:768], xc[:], r=[d_xc])


def chunk_d(p, l, I, S, Z, sbo, tab, d_tab, cm, d_cm, strict, d_strict, ones, d_ones, acc, d_acc):
    ident, d_ident = S["ident"]
    with SB(p) as sb:
        bk = [sb.ps() for _ in range(8)]
        B3 = lambda t: t[:, :].rearrange("q (h c) -> q h c", h=4)
        two = lambda shape, nm, dt=F32: [sb.t(shape, dt, nm) for _ in range(2)]
        qf = [two((64, 4, 128), "qf") for _ in range(2)]
        kf = [two((64, 4, 128), "kf") for _ in range(2)]
        ktm = [two((128, 4, 64), "ktm") for _ in range(2)]
        vtm = [two((128, 4, 64), "vtm") for _ in range(2)]
        diag, Ed, e1, e2, tmpP = two((128, 4, 128), "diag"), two((128, 4, 128), "Ed"), two((128, 4, 128), "e1"), two((128, 4, 128), "e2"), two((128, 4, 128), "tmpP")
        decT = [two((128, 4, 128), "decT") for _ in range(2)]
        decS = two((128, 4, 128), "decS")
        PQ = [two((128, 8, 128), "PQ") for _ in range(2)]
        TTt = [two((128, 4, 128), "TT") for _ in range(2)]
        PI = [two((128, 4, 128), "PI") for _ in range(2)]
        Ru, Rw, kdec = two((128, 4, 64), "Ru"), two((128, 4, 64), "Rw"), two((128, 4, 64), "kdec")
        u_all, d_u = sb.t((128, 8, 64), F32, "uall")
        vnew, d_vnew = sb.t((128, 8, 64), F32, "vnew")
        wT = two((64, 4, 128), "wT")
        attnT = two((128, 4, 128), "attnT")
        Sst, d_S = sb.t((64, 8, 64), F32, "Sst")
        otmp = two((128, 4, 64), "otmp")
        p.op("dve", lambda h: h.memset(Sst[:], 0.0), w=[d_S])
        bc_col = lambda ap: ap.unsqueeze(2).to_broadcast([128, 4, 128])
        bc_c64 = lambda ap, n=128: ap.unsqueeze(2).to_broadcast([n, 4, 64])
        bc_mat = lambda ap: ap.unsqueeze(1).to_broadcast([128, 4, 128])
        for s_ in range(NT):
            par = s_ % 2
            tts = [ORDER[d][s_] for d in range(2)]
            col = lambda d, g: tab[:, tts[d], d, 4 * g:4 * g + 4]
            for d in range(2):
                tt = tts[d]
                tk = slice(tt * 128, (tt + 1) * 128)
                p.dma("sp", qf[d][par][0][:], Z["gqT"][:, tk].rearrange("(h q) t -> q h t", q=64), w=[qf[d][par][1]])
                p.dma("sp", kf[d][par][0][:], Z["gkT"][:, tk].rearrange("(h q) t -> q h t", q=64), w=[kf[d][par][1]])
                p.dma("sp", ktm[d][par][0][:], Z["gk"][tk, :].rearrange("q (h e) -> q h e", e=64), w=[ktm[d][par][1]])
                p.dma("sp", vtm[d][par][0][:], Z["gv"][tk, :].rearrange("q (h e) -> q h e", e=64), w=[vtm[d][par][1]])
            for d in range(2):
                dg, d_dg = diag[d]
                p.op("dve", lambda h, d=d, dg=dg: h.tensor_tensor(out=dg[:], in0=bc_mat(ident[:, :]), in1=bc_col(col(d, 5)), op=ALU.mult),
                     r=[d_ident, d_tab], w=[d_dg])
                pg, d_pg = bk[d]
                p.op("pe", lambda h, pg=pg, dg=dg: h.matmul(pg[:, :], lhsT=ones[:], rhs=dg[:, :, :].rearrange("q h c -> q (h c)"), start=True, stop=True),
                     r=[d_ones, d_dg], w=[d_pg])
                E_, d_E = Ed[d]
                p.op("dve", lambda h, d=d, pg=pg, E_=E_: h.tensor_tensor(out=E_[:], in0=B3(pg), in1=bc_col(col(d, 5)), op=ALU.subtract),
                     r=[d_pg, d_tab], w=[d_E])
                a1, d_a1 = e1[d]
                a2, d_a2 = e2[d]
                p.op("dve", lambda h, E_=E_, a1=a1: h.tensor_scalar(out=a1[:], in0=E_[:], scalar1=0.0, scalar2=None, op0=ALU.min), r=[d_E], w=[d_a1])
                p.op("pool", lambda h, E_=E_, a2=a2: h.tensor_scalar(out=a2[:], in0=E_[:], scalar1=0.0, scalar2=None, op0=ALU.max), r=[d_E], w=[d_a2])
                p.op("act", lambda h, a1=a1: h.activation(out=a1[:], in_=a1[:], func=AF.Exp), r=[d_a1], w=[d_a1])
                p.op("act", lambda h, a2=a2: h.activation(out=a2[:], in_=a2[:], func=AF.Exp, scale=-1.0), r=[d_a2], w=[d_a2])
                dT, d_dT = decT[d][par]
                dS, d_dS = decS[d]
                p.op("pool", lambda h, d=d, a1=a1, dT=dT: h.tensor_tensor(out=dT[:], in0=a1[:], in1=bc_mat(cm[:, 1 - d, :]), op=ALU.mult),
                     r=[d_a1, d_cm], w=[d_dT])
                p.op("pool", lambda h, d=d, a2=a2, dS=dS: h.tensor_tensor(out=dS[:], in0=a2[:], in1=bc_mat(strict[:, d, :]), op=ALU.mult),
                     r=[d_a2, d_strict], w=[d_dS])
            for d in range(2):
                kf_, d_kf = kf[d][par]
                pG, d_pG = bk[2 + d]
                for hd in range(4):
                    p.op("pe", lambda h, pG=pG, hd=hd, kf_=kf_: h.matmul(pG[:, hd * 128:(hd + 1) * 128], lhsT=kf_[:, hd, :], rhs=kf_[:, hd, :],
                                                                        start=True, stop=True), r=[d_kf], w=[d_pG])
                tp_, d_tp = tmpP[d]
                p.op("dve", lambda h, d=d, pG=pG, tp_=tp_: h.tensor_tensor(out=tp_[:], in0=B3(pG), in1=bc_col(col(d, 4)), op=ALU.mult),
                     r=[d_pG, d_tab], w=[d_tp])
                pq_, d_pq = PQ[d][0]
                p.op("pool", lambda h, d=d, tp_=tp_, pq_=pq_: h.tensor_tensor(out=pq_[:, 0:4, :], in0=tp_[:], in1=decS[d][0][:], op=ALU.mult),
                     r=[d_tp, decS[d][1]], w=[d_pq])
                pQ, d_pQ = bk[4 + d]
                for hd in range(4):
                    p.op("pe", lambda h, pQ=pQ, hd=hd, pq_=pq_: h.transpose(pQ[:, hd * 128:(hd + 1) * 128], pq_[:, hd, :], ident[:]),
                         r=[d_pq, d_ident], w=[d_pQ])
                p.op("act", lambda h, pQ=pQ, pq_=pq_: h.activation(out=pq_[:, 4:8, :], in_=B3(pQ), func=AF.Copy), r=[d_pQ], w=[d_pq])
                tt0, d_tt0 = TTt[d][0]
                p.op("dve", lambda h, pQ=pQ, tt0=tt0: h.tensor_tensor(out=tt0[:], in0=B3(pQ), in1=bc_mat(ident[:, :]), op=ALU.add),
                     r=[d_pQ, d_ident], w=[d_tt0])
            for k in range(1, 7):
                for d in range(2):
                    c_, d_c = PQ[d][(k - 1) % 2]
                    n_, d_n = PQ[d][k % 2]
                    pP, d_pP = bk[d]
                    pQ, d_pQ = bk[2 + d]
                    for hd in range(4):
                        p.op("pe", lambda h, pP=pP, hd=hd, c_=c_: h.matmul(pP[:, hd * 128:(hd + 1) * 128], lhsT=c_[:, 4 + hd, :], rhs=c_[:, hd, :],
                                                                          start=True, stop=True), r=[d_c], w=[d_pP])
                    if k < 6:
                        for hd in range(4):
                            p.op("pe", lambda h, pQ=pQ, hd=hd, c_=c_: h.matmul(pQ[:, hd * 128:(hd + 1) * 128], lhsT=c_[:, hd, :], rhs=c_[:, 4 + hd, :],
                                                                              start=True, stop=True), r=[d_c], w=[d_pQ])
                    p.op("act", lambda h, pP=pP, n_=n_: h.activation(out=n_[:, 0:4, :], in_=B3(pP), func=AF.Copy), r=[d_pP], w=[d_n])
                    pi_, d_pi = PI[d][k % 2]
                    p.op("dve", lambda h, pP=pP, pi_=pi_: h.tensor_tensor(out=pi_[:], in0=B3(pP), in1=bc_mat(ident[:, :]), op=ALU.add),
                         r=[d_pP, d_ident], w=[d_pi])
                    if k < 6:
                        p.op("dve", lambda h, pQ=pQ, n_=n_: h.tensor_copy(out=n_[:, 4:8, :], in_=B3(pQ)), r=[d_pQ], w=[d_n])
                for d in range(2):
                    n_, d_n = PQ[d][k % 2]
                    tc_, d_tc = TTt[d][(k - 1) % 2]
                    tn_, d_tn = TTt[d][k % 2]
                    pT, d_pT = bk[4 + d]
                    pi_, d_pi = PI[d][k % 2]
                    for hd in range(4):
                        p.op("pe", lambda h, pT=pT, hd=hd, pi_=pi_, tc_=tc_: h.matmul(pT[:, hd * 128:(hd + 1) * 128], lhsT=pi_[:, hd, :], rhs=tc_[:, hd, :],
                                                                                    start=True, stop=True), r=[d_pi, d_tc], w=[d_pT])
                    if d == 0:
                        p.op("dve", lambda h, pT=pT, tn_=tn_: h.tensor_copy(out=tn_[:], in_=B3(pT)), r=[d_pT], w=[d_tn])
                    else:
                        p.op("act", lambda h, pT=pT, tn_=tn_: h.activation(out=tn_[:], in_=B3(pT), func=AF.Copy), r=[d_pT], w=[d_tn])
            pu, d_pu = bk[6]
            for d in range(2):
                TTf, d_TTf = TTt[d][0]
                ru, d_ru = Ru[d]
                rw, d_rw = Rw[d]
                kd, d_kd = kdec[d]
                kt_, d_kt = ktm[d][par]
                vt_, d_vt = vtm[d][par]
                p.op("pool", lambda h, d=d, ru=ru, vt_=vt_: h.tensor_tensor(out=ru[:], in0=vt_[:], in1=bc_c64(col(d, 3)), op=ALU.mult),
                     r=[d_vt, d_tab], w=[d_ru])
                p.op("pool", lambda h, d=d, rw=rw, kt_=kt_: h.tensor_tensor(out=rw[:], in0=kt_[:], in1=bc_c64(col(d, 9)), op=ALU.mult),
                     r=[d_kt, d_tab], w=[d_rw])
                p.op("pool", lambda h, d=d, kd=kd, kt_=kt_: h.tensor_tensor(out=kd[:], in0=kt_[:], in1=bc_c64(col(d, 7)), op=ALU.mult),
                     r=[d_kt, d_tab], w=[d_kd])
                for hd in range(4):
                    i = d * 4 + hd
                    p.op("pe", lambda h, i=i, hd=hd, TTf=TTf, ru=ru: h.matmul(pu[:, i * 64:(i + 1) * 64], lhsT=TTf[:, hd, :], rhs=ru[:, hd, :],
                                                                             start=True, stop=True), r=[d_TTf, d_ru], w=[d_pu])
            p.op("act", lambda h: h.activation(out=u_all[:], in_=pu[:, :].rearrange("q (i e) -> q i e", e=64), func=AF.Copy), r=[d_pu], w=[d_u])
            for d in range(2):
                TTf, d_TTf = TTt[d][0]
                rw, d_rw = Rw[d]
                pw, d_pw = bk[d]
                for hd in range(4):
                    p.op("pe", lambda h, pw=pw, hd=hd, TTf=TTf, rw=rw: h.matmul(pw[0:64, hd * 128:(hd + 1) * 128], lhsT=rw[:, hd, :], rhs=TTf[:, hd, :],
                                                                               start=True, stop=True), r=[d_TTf, d_rw], w=[d_pw])
                w_, d_w_ = wT[d]
                p.op("dve", lambda h, pw=pw, w_=w_: h.tensor_copy(out=w_[:], in_=pw[0:64, :].rearrange("q (h c) -> q h c", h=4)), r=[d_pw], w=[d_w_])
                pS, d_pS = bk[2 + d]
                kf_, d_kf = kf[d][par]
                qf_, d_qf = qf[d][par]
                for hd in range(4):
                    p.op("pe", lambda h, pS=pS, hd=hd, kf_=kf_, qf_=qf_: h.matmul(pS[:, hd * 128:(hd + 1) * 128], lhsT=kf_[:, hd, :], rhs=qf_[:, hd, :],
                                                                                start=True, stop=True), r=[d_kf, d_qf], w=[d_pS])
                at_, d_at = attnT[d]
                p.op("dve", lambda h, pS=pS, at_=at_, d=d: h.tensor_tensor(out=at_[:], in0=B3(pS), in1=decT[d][par][0][:], op=ALU.mult),
                     r=[d_pS, decT[d][par][1]], w=[d_at])
            pv, d_pv = bk[7]
            for d in range(2):
                for hd in range(4):
                    i = d * 4 + hd
                    p.op("pe", lambda h, i=i, hd=hd, d=d: h.matmul(pv[:, i * 64:(i + 1) * 64], lhsT=wT[d][0][:, hd, :], rhs=Sst[:, i, :],
                                                                  start=True, stop=True), r=[wT[d][1], d_S], w=[d_pv])
            p.op("dve", lambda h: h.tensor_tensor(out=vnew[:], in0=u_all[:], in1=pv[:, :].rearrange("q (i e) -> q i e", e=64), op=ALU.subtract),
                 r=[d_pv, d_u], w=[d_vnew])
            po1, d_po1 = bk[4]
            po2, d_po2 = bk[5]
            pn, d_pn = bk[6]
            for d in range(2):
                for hd in range(4):
                    i = d * 4 + hd
                    p.op("pe", lambda h, i=i, hd=hd, d=d: h.matmul(po1[:, i * 64:(i + 1) * 64], lhsT=attnT[d][0][:, hd, :], rhs=vnew[:, i, :],
                                                                  start=True, stop=True), r=[attnT[d][1], d_vnew], w=[d_po1])
            for d in range(2):
                for hd in range(4):
                    i = d * 4 + hd
                    p.op("pe", lambda h, i=i, hd=hd, d=d: h.matmul(po2[:, i * 64:(i + 1) * 64], lhsT=qf[d][par][0][:, hd, :], rhs=Sst[:, i, :],
                                                                  start=True, stop=True), r=[qf[d][par][1], d_S], w=[d_po2])
            for d in range(2):
                for hd in range(4):
                    i = d * 4 + hd
                    p.op("pe", lambda h, i=i, hd=hd, d=d: h.matmul(pn[0:64, i * 64:(i + 1) * 64], lhsT=kdec[d][0][:, hd, :], rhs=vnew[:, i, :],
                                                                  start=True, stop=True), r=[kdec[d][1], d_vnew], w=[d_pn])
            for d in range(2):
                dst = acc[:, tts[d], :].rearrange("q (h e) -> q h e", e=64)
                ot_, d_ot = otmp[d]
                p.op("dve", lambda h, d=d, dst=dst: h.tensor_tensor(out=dst, in0=po1[:, d * 256:(d + 1) * 256].rearrange("q (h e) -> q h e", e=64),
                                                                    in1=dst, op=ALU.add), r=[d_po1, d_acc], w=[d_acc])
                p.op("dve", lambda h, d=d, ot_=ot_: h.tensor_tensor(out=ot_[:], in0=po2[:, d * 256:(d + 1) * 256].rearrange("q (h e) -> q h e", e=64),
                                                                    in1=bc_c64(col(d, 6)), op=ALU.mult), r=[d_po2, d_tab], w=[d_ot])
                p.op("pool", lambda h, dst=dst, ot_=ot_: h.tensor_tensor(out=dst, in0=dst, in1=ot_[:], op=ALU.add), r=[d_ot, d_acc], w=[d_acc])
            for d in range(2):
                sv = Sst[:, 4 * d:4 * d + 4, :]
                egl_b = tab[0:64, tts[d], d, 32:36].unsqueeze(2).to_broadcast([64, 4, 64])
                p.op("dve", lambda h, sv=sv, egl_b=egl_b: h.tensor_tensor(out=sv, in0=sv, in1=egl_b, op=ALU.mult), r=[d_S, d_tab], w=[d_S])
                p.op("dve", lambda h, sv=sv, d=d: h.tensor_tensor(out=sv, in0=pn[0:64, d * 256:(d + 1) * 256].rearrange("q (h e) -> q h e", e=64),
                                                                  in1=sv, op=ALU.add), r=[d_pn, d_S], w=[d_S])
        gain, d_gain = sb.t((128, 256))
        for g4 in range(4):
            p.dma("sp", gain[:, 64 * g4:64 * g4 + 64], I["gdn_norm"][l:l + 1, :].partition_broadcast(128), w=[d_gain])
        zts = [sb.t((128, 256)) for _ in range(2)]
        sgs = [sb.t((128, 256)) for _ in range(2)]
        sqs = [sb.t((128, 256)) for _ in range(2)]
        sts = [sb.t((128, 8)) for _ in range(2)]
        v3 = lambda a: a[:, :].rearrange("q (g e) -> q g e", e=64)
        for tt in range(NT):
            zt, d_zt = zts[tt % 2]
            sg, d_sg = sgs[tt % 2]
            sq, d_sq = sqs[tt % 2]
            st, d_st = sts[tt % 2]
            p.dma("sp", zt[:], Z["zD"][tt * 128:(tt + 1) * 128, :], w=[d_zt])
            p.op("act", lambda h, zt=zt, sg=sg: h.activation(out=sg[:], in_=zt[:], func=AF.Sigmoid), r=[d_zt], w=[d_sg])
            p.op("dve", lambda h, zt=zt, sg=sg: h.tensor_tensor(out=sg[:], in0=sg[:], in1=zt[:], op=ALU.mult), r=[d_zt, d_sg], w=[d_sg])
            ov = acc[:, tt, :]
            p.op("act", lambda h, sq=sq, ov=ov: h.activation(out=sq[:], in_=ov, func=AF.Square), r=[d_acc], w=[d_sq])
            p.op("dve", lambda h, st=st, sq=sq: h.tensor_reduce(out=st[:, 0:4], in_=v3(sq), axis=AX.X, op=ALU.add), r=[d_sq], w=[d_st])
            rstd(p, st[:, 4:8], st[:, 0:4], st[:, 0:4], 1.0 / 64, [d_st])
            p.op("dve", lambda h, sq=sq, st=st, ov=ov: h.tensor_tensor(out=v3(sq), in0=ov.rearrange("q (g e) -> q g e", e=64),
                                                                       in1=st[:, 4:8].unsqueeze(2).to_broadcast([128, 4, 64]), op=ALU.mult),
                 r=[d_acc, d_st], w=[d_sq])
            p.op("dve", lambda h, sq=sq: h.tensor_tensor(out=sq[:], in0=sq[:], in1=gain[:], op=ALU.mult), r=[d_gain, d_sq], w=[d_sq])
            p.op("dve", lambda h, sq=sq, sg=sg: h.tensor_tensor(out=sq[:], in0=sq[:], in1=sg[:], op=ALU.mult), r=[d_sg, d_sq], w=[d_sq])
            p.dma("sp", Z["y"][tt * 128:(tt + 1) * 128, 768:1024], sq[:], r=[d_sq])


def phase_outproj(p, l, I, S, Z, xsrc, mine=False):
    ident, d_ident = S["ident"]
    grow, d_grow = S["grow"]
    with SB(p) as sb:
        w, d_w = sb.t((128, 8, D), BF16, "wout")
        for k in range(8):
            p.dma("pool", w[:, k, :], I["w_out"][l, k * 128:(k + 1) * 128, :], w=[d_w])
        yts = [sb.t((128, D)) for _ in range(2)]
        xts = [sb.t((128, D)) for _ in range(2)]
        yTs = [sb.t((128, 8, 128), BF16) for _ in range(2)]
        tps = [sb.ps() for _ in range(2)]
        pos = [sb.ps() for _ in range(2)]
        tiles = range(NT) if not mine else range(16)
        if mine:
            fl, d_fl = sb.t((128, 2))
            p.dma("sp", fl[:], I["flags"][:, :], w=[d_fl])
            ob_, d_ob_ = sb.t((128, D))
        for tt in tiles:
            j = 1 if (tt < 2 and not mine) else 0
            rows = slice(tt * 128, (tt + 1) * 128)
            yt, d_yt = yts[tt % 2]
            xt, d_xt = xts[tt % 2]
            yT, d_yT = yTs[tt % 2]
            if not mine:
                p.dma("sp", yt[:], Z["y"][rows, :], w=[d_yt])
                p.dma("sp", xt[:], xsrc[rows, :], w=[d_xt])
            else:
                ra = slice(256 + tt * 128, 256 + (tt + 1) * 128)
                rb = slice(256 + 2048 + tt * 128, 256 + 2048 + (tt + 1) * 128)
                p.dma("sp", yt[:, 0:512], Z["ym"][rows, :], w=[d_yt])
                p.dma("sp", yt[:, 512:1024], Z["y"][ra, 512:1024], w=[d_yt])
                p.dma("sp", ob_[:, 0:512], Z["y"][rb, 512:1024], w=[d_ob_])
                p.op("dve", lambda h, yt=yt: h.tensor_scalar(out=yt[:, 512:1024], in0=yt[:, 512:1024], scalar1=fl[:, 0:1], scalar2=None, op0=ALU.mult),
                     r=[d_yt, d_fl], w=[d_yt])
                p.op("dve", lambda h, yt=yt: h.scalar_tensor_tensor(out=yt[:, 512:1024], in0=ob_[:, 0:512], scalar=fl[:, 1:2], in1=yt[:, 512:1024],
                                                                    op0=ALU.mult, op1=ALU.add), r=[d_ob_, d_fl, d_yt], w=[d_yt])
                p.dma("sp", xt[:], xsrc[ra, :], w=[d_xt])
                p.dma("sp", ob_[:], xsrc[rb, :], w=[d_ob_])
                p.op("dve", lambda h, xt=xt: h.tensor_scalar(out=xt[:], in0=xt[:], scalar1=fl[:, 0:1], scalar2=None, op0=ALU.mult),
                     r=[d_xt, d_fl], w=[d_xt])
                p.op("dve", lambda h, xt=xt: h.scalar_tensor_tensor(out=xt[:], in0=ob_[:], scalar=fl[:, 1:2], in1=xt[:],
                                                                    op0=ALU.mult, op1=ALU.add), r=[d_ob_, d_fl, d_xt], w=[d_xt])
            for c in range(8):
                tp, d_tp = tps[c // 4]
                p.op("pe", lambda h, c=c, tp=tp, yt=yt: h.transpose(tp[:, (c % 4) * 128:(c % 4 + 1) * 128], yt[:, c * 128:(c + 1) * 128], ident[:]),
                     r=[d_yt, d_ident], w=[d_tp])
            for hh in range(2):
                tp, d_tp = tps[hh]
                p.op("act", lambda h, hh=hh, tp=tp, yT=yT: h.activation(out=yT[:, 4 * hh:4 * hh + 4, :],
                                                                       in_=tp[:, :].rearrange("q (c t) -> q c t", c=4), func=AF.Copy),
                     r=[d_tp], w=[d_yT])
            for hh in range(2):
                po, d_po = pos[hh]
                for k in range(8):
                    p.op("pe", lambda h, k=k, po=po, yT=yT, hh=hh: h.matmul(po[:, :], lhsT=yT[:, k, :], rhs=w[:, k, hh * 512:(hh + 1) * 512],
                                                                          start=(k == 0), stop=(k == 7)), r=[d_yT, d_w], w=[d_po])
                p.op("dve", lambda h, po=po, yt=yt, hh=hh, j=j: h.tensor_tensor(out=yt[:, hh * 512:(hh + 1) * 512], in0=po[:, :],
                                                                             in1=grow[:, 0, j, hh * 512:(hh + 1) * 512], op=ALU.mult),
                     r=[d_po, d_grow], w=[d_yt])
            p.op("dve", lambda h, xt=xt, yt=yt: h.tensor_tensor(out=xt[:], in0=xt[:], in1=yt[:], op=ALU.add), r=[d_yt, d_xt], w=[d_xt])
            p.dma("sp", (Z["xm1"] if mine else Z["xres"])[rows, :], xt[:], r=[d_xt])


def norm_objs(sb, S):
    junk, d_junk = sb.t((128, D))
    xs, d_xs = sb.t((128, D))
    ss, d_ss = sb.t((128, 4))
    tps = [sb.ps() for _ in range(2)]
    return (junk, d_junk, ss, d_ss, xs, d_xs, tps, S["ident"][0], S["ident"][1])


def phase_ffn_dense(p, l, I, S, Z):
    gsh, d_gsh = S["gsh"]
    grow, d_grow = S["grow"]
    NF = D_FF // 128
    with SB(p) as sb:
        wg, d_wg = sb.t((128, 8, D_FF), BF16, "wg")
        wu, d_wu = sb.t((128, 8, D_FF), BF16, "wu")
        wd, d_wd = sb.t((128, NF, D), BF16, "wd")
        for k in range(8):
            p.dma("pool", wg[:, k, :], I["ffn_w_gate"][k * 128:(k + 1) * 128, :], w=[d_wg])
            p.dma("pool", wu[:, k, :], I["ffn_w_up"][k * 128:(k + 1) * 128, :], w=[d_wu])
        for k in range(NF):
            p.dma("pool", wd[:, k, :], I["ffn_w_down"][k * 128:(k + 1) * 128, :], w=[d_wd])
        nobj = norm_objs(sb, S)
        xb, d_xb = sb.t((128, 2, D), F32, "xblk")
        xnT, d_xnT = sb.t((128, 8, 256), BF16, "xnT")
        hT, d_hT = sb.t((128, NF, 256), BF16, "hT")
        sgs = [sb.t((128, 256)) for _ in range(2)]
        pgs = [sb.ps() for _ in range(2)]
        pus = [sb.ps() for _ in range(2)]
        pos = [sb.ps() for _ in range(2)]
        ot, d_ot = sb.t((128, 512))
        blocks = [(2 * i, 2) for i in range(17)]
        n = 0
        for (t0, ntl) in blocks:
            ntok = ntl * 128
            j = 1 if t0 < 2 else 0
            for ti in range(ntl):
                tt = t0 + ti
                p.dma("sp", xb[:, ti, :], Z["xres"][tt * 128:(tt + 1) * 128, :], w=[d_xb])
            for ti in range(ntl):
                norm_tile(p, nobj, xb[:, ti, :], d_xb, gsh, d_gsh, 1, j, xnT, d_xnT, ti * 128)
            for fc in range(NF):
                pg, d_pg = pgs[fc % 2]
                pu, d_pu = pus[fc % 2]
                sg, d_sg = sgs[fc % 2]
                for k in range(8):
                    p.op("pe", lambda h, k=k, pg=pg, fc=fc: h.matmul(pg[:, 0:ntok], lhsT=wg[:, k, fc * 128:(fc + 1) * 128], rhs=xnT[:, k, 0:ntok],
                                                                    start=(k == 0), stop=(k == 7)), r=[d_wg, d_xnT], w=[d_pg])
                for k in range(8):
                    p.op("pe", lambda h, k=k, pu=pu, fc=fc: h.matmul(pu[:, 0:ntok], lhsT=wu[:, k, fc * 128:(fc + 1) * 128], rhs=xnT[:, k, 0:ntok],
                                                                    start=(k == 0), stop=(k == 7)), r=[d_wu, d_xnT], w=[d_pu])
                p.op("act", lambda h, pg=pg, sg=sg: h.activation(out=sg[:, 0:ntok], in_=pg[:, 0:ntok], func=AF.Sigmoid), r=[d_pg], w=[d_sg])
                p.op("dve", lambda h, pg=pg, sg=sg: h.tensor_tensor(out=sg[:, 0:ntok], in0=pg[:, 0:ntok], in1=sg[:, 0:ntok], op=ALU.mult),
                     r=[d_pg, d_sg], w=[d_sg])
                p.op("dve", lambda h, pu=pu, sg=sg, fc=fc: h.tensor_tensor(out=hT[:, fc, 0:ntok], in0=pu[:, 0:ntok], in1=sg[:, 0:ntok], op=ALU.mult),
                     r=[d_pu, d_sg], w=[d_hT])
            for ti in range(ntl):
                tt = t0 + ti
                for hh in range(2):
                    po, d_po = pos[n % 2]
                    n += 1
                    for fc in range(NF):
                        p.op("pe", lambda h, fc=fc, po=po, ti=ti, hh=hh: h.matmul(po[:, :], lhsT=hT[:, fc, ti * 128:(ti + 1) * 128],
                                                                                rhs=wd[:, fc, hh * 512:(hh + 1) * 512],
                                                                                start=(fc == 0), stop=(fc == NF - 1)), r=[d_hT, d_wd], w=[d_po])
                    p.op("dve", lambda h, po=po, hh=hh, j=j: h.tensor_tensor(out=ot[:], in0=po[:, :], in1=grow[:, 1, j, hh * 512:(hh + 1) * 512],
                                                                          op=ALU.mult), r=[d_po, d_grow], w=[d_ot])
                    p.op("dve", lambda h, ti=ti, hh=hh: h.tensor_tensor(out=xb[:, ti, hh * 512:(hh + 1) * 512], in0=xb[:, ti, hh * 512:(hh + 1) * 512],
                                                                         in1=ot[:], op=ALU.add), r=[d_ot, d_xb], w=[d_xb])
                p.dma("sp", Z["xres"][tt * 128:(tt + 1) * 128, :], xb[:, ti, :], r=[d_xb])


def phase_moe(p, l, I, S, Z, out):
    gsh, d_gsh = S["gsh"]
    grow, d_grow = S["grow"]
    NTM = 16
    SLAB = 512
    NSL = D_FFE // SLAB
    with SB(p) as sb:
        fl, d_fl = sb.t((128, 2))
        p.dma("sp", fl[:], I["flags"][:, :], w=[d_fl])
        xm, d_xm = sb.t((128, NTM, D), F32, "xm")
        xnT, d_xnT = sb.t((128, 8, NTM * 128), BF16, "xnTm")
        gates, d_gates = sb.t((128, NTM, NE), F32, "gates")
        rt, d_rt = sb.t((128, 8, NE), F32, "router")
        p.dma("sp", rt[:], I["moe_router"].rearrange("(k q) e -> q k e", q=128), w=[d_rt])
        with SB(p) as sb2:
            nobj = norm_objs(sb2, S)
            (junk, d_junk, ss, d_ss, xs, d_xs, tps, ident, d_ident) = nobj
            xa = [sb2.t((128, D)) for _ in range(2)]
            xnf, d_xnf = sb2.t((128, 8, 128), F32, "xnf")
            plg, d_plg = sb2.ps()
            lg, d_lg = sb2.t((128, 8))
            mx, d_mx = sb2.t((128, 8))
            wk, d_wk = sb2.t((128, 32))
            for jt in range(NTM):
                a_, d_a = xa[0]
                b_, d_b = xa[1]
                p.dma("sp", xm[:, jt, :], Z["xm1"][jt * 128:(jt + 1) * 128, :], w=[d_xm])
                norm_tile(p, nobj, xm[:, jt, :], d_xm, gsh, d_gsh, 1, 0, xnT, d_xnT, jt * 128)
                for c in range(8):
                    tp, d_tp = tps[c // 4]
                    p.op("act", lambda h, c=c, tp=tp: h.activation(out=xnf[:, c, :], in_=tp[:, (c % 4) * 128:(c % 4 + 1) * 128],
                                                                   func=AF.Identity, scale=gsh[:, 2, c, 0:1], bias=gsh[:, 3, c, 0:1]),
                         r=[d_tp, d_gsh], w=[d_xnf])
                for k in range(8):
                    p.op("pe", lambda h, k=k: h.matmul(plg[:, 0:NE], lhsT=xnf[:, k, :], rhs=rt[:, k, :], start=(k == 0), stop=(k == 7)),
                         r=[d_xnf, d_rt], w=[d_plg])
                p.op("dve", lambda h: h.tensor_copy(out=lg[:], in_=plg[:, 0:NE]), r=[d_plg], w=[d_lg])
                p.op("dve", lambda h: h.max(out=mx[:], in_=lg[:]), r=[d_lg], w=[d_mx])
                p.op("dve", lambda h: h.tensor_tensor(out=wk[:, 0:1], in0=mx[:, 1:2], in1=mx[:, 0:1], op=ALU.subtract), r=[d_mx], w=[d_wk])
                p.op("act", lambda h: h.activation(out=wk[:, 1:2], in_=wk[:, 0:1], func=AF.Exp), r=[d_wk], w=[d_wk])
                p.op("dve", lambda h: h.tensor_scalar(out=wk[:, 1:2], in0=wk[:, 1:2], scalar1=1.0, scalar2=None, op0=ALU.add), r=[d_wk], w=[d_wk])
                p.op("dve", lambda h: h.reciprocal(out=wk[:, 2:3], in_=wk[:, 1:2]), r=[d_wk], w=[d_wk])
                p.op("dve", lambda h: h.tensor_scalar(out=wk[:, 3:4], in0=wk[:, 2:3], scalar1=-1.0, scalar2=1.0, op0=ALU.mult, op1=ALU.add),
                     r=[d_wk], w=[d_wk])
                p.op("dve", lambda h: h.tensor_scalar(out=wk[:, 8:16], in0=lg[:], scalar1=mx[:, 0:1], scalar2=wk[:, 2:3], op0=ALU.is_equal,
                                                      op1=ALU.mult), r=[d_lg, d_mx, d_wk], w=[d_wk])
                p.op("dve", lambda h: h.tensor_scalar(out=wk[:, 16:24], in0=lg[:], scalar1=mx[:, 1:2], scalar2=wk[:, 3:4], op0=ALU.is_equal,
                                                      op1=ALU.mult), r=[d_lg, d_mx, d_wk], w=[d_wk])
                p.op("dve", lambda h, jt=jt: h.tensor_tensor(out=gates[:, jt, :], in0=wk[:, 8:16], in1=wk[:, 16:24], op=ALU.add),
                     r=[d_wk], w=[d_gates])
        wgs = [sb.t((128, 8, SLAB), BF16, "wgs") for _ in range(2)]
        wus = [sb.t((128, 8, SLAB), BF16, "wus") for _ in range(2)]
        wds = [sb.t((128, 4, D), BF16, "wds") for _ in range(2)]
        hTs = [sb.t((128, 4, 512), BF16, "hTs") for _ in range(2)]
        sgs = [sb.t((128, 512)) for _ in range(2)]
        pgs = [sb.ps() for _ in range(2)]
        pus = [sb.ps() for _ in range(2)]
        pos = [sb.ps() for _ in range(2)]
        ns = 0
        nh = 0
        nf = 0
        no = 0
        for e in range(NE):
            for sl in range(NSL):
                wg, d_wg = wgs[ns % 2]
                wu, d_wu = wus[ns % 2]
                wd, d_wd = wds[ns % 2]
                ns += 1
                c0 = sl * SLAB
                p.dma("pool", wg[:], I["moe_w_gate"][e].rearrange("(k q) f -> q k f", q=128)[:, :, c0:c0 + SLAB], w=[d_wg])
                p.dma("pool", wu[:], I["moe_w_up"][e].rearrange("(k q) f -> q k f", q=128)[:, :, c0:c0 + SLAB], w=[d_wu])
                p.dma("pool", wd[:], I["moe_w_down"][e, c0:c0 + SLAB, :].rearrange("(k q) d -> q k d", q=128), w=[d_wd])
                for k in range(4):
                    p.op("dve", lambda h, k=k, wd=wd: h.tensor_tensor(out=wd[:, k, :], in0=wd[:, k, :], in1=grow[:, 1, 0, :], op=ALU.mult),
                         r=[d_grow, d_wd], w=[d_wd])
                for tb in range(NTM // 4):
                    hT, d_hT = hTs[nh % 2]
                    nh += 1
                    toks = slice(tb * 512, (tb + 1) * 512)
                    for fc in range(4):
                        pg, d_pg = pgs[nf % 2]
                        pu, d_pu = pus[nf % 2]
                        sg, d_sg = sgs[nf % 2]
                        nf += 1
                        for k in range(8):
                            p.op("pe", lambda h, k=k, pg=pg, fc=fc, wg=wg: h.matmul(pg[:, :], lhsT=wg[:, k, fc * 128:(fc + 1) * 128], rhs=xnT[:, k, toks],
                                                                                  start=(k == 0), stop=(k == 7)), r=[d_wg, d_xnT], w=[d_pg])
                        for k in range(8):
                            p.op("pe", lambda h, k=k, pu=pu, fc=fc, wu=wu: h.matmul(pu[:, :], lhsT=wu[:, k, fc * 128:(fc + 1) * 128], rhs=xnT[:, k, toks],
                                                                                  start=(k == 0), stop=(k == 7)), r=[d_wu, d_xnT], w=[d_pu])
                        p.op("act", lambda h, pg=pg, sg=sg: h.activation(out=sg[:], in_=pg[:, :], func=AF.Sigmoid), r=[d_pg], w=[d_sg])
                        p.op("dve", lambda h, pg=pg, sg=sg: h.tensor_tensor(out=sg[:], in0=pg[:, :], in1=sg[:], op=ALU.mult), r=[d_pg, d_sg], w=[d_sg])
                        p.op("dve", lambda h, pu=pu, sg=sg, fc=fc, hT=hT: h.tensor_tensor(out=hT[:, fc, :], in0=pu[:, :], in1=sg[:], op=ALU.mult),
                             r=[d_pu, d_sg], w=[d_hT])
                    for ti in range(4):
                        jt = tb * 4 + ti
                        for hh in range(2):
                            po, d_po = pos[no % 2]
                            no += 1
                            for fc in range(4):
                                p.op("pe", lambda h, fc=fc, po=po, ti=ti, hh=hh, hT=hT, wd=wd: h.matmul(
                                    po[:, :], lhsT=hT[:, fc, ti * 128:(ti + 1) * 128], rhs=wd[:, fc, hh * 512:(hh + 1) * 512],
                                    start=(fc == 0), stop=(fc == 3)), r=[d_hT, d_wd], w=[d_po])
                            dst = xm[:, jt, hh * 512:(hh + 1) * 512]
                            p.op("dve", lambda h, po=po, dst=dst, jt=jt, e=e: h.scalar_tensor_tensor(out=dst, in0=po[:, :], scalar=gates[:, jt, e:e + 1],
                                                                                                  in1=dst, op0=ALU.mult, op1=ALU.add),
                                 r=[d_po, d_gates, d_xm], w=[d_xm])
        for jt in range(NTM):
            p.dma("sp", out[jt * 128:(jt + 1) * 128, :], xm[:, jt, :], r=[d_xm])


_CACHE = {}


def kernel(**inputs):
    inp = {k: np.asarray(v) for k, v in inputs.items()}
    if "p" not in _CACHE:
        _CACHE["p"] = build()
    p = _CACHE["p"]
    in_maps = [host_inputs(inp, c) for c in range(8)]
    res = run_bass_kernel_spmd(p.nc, in_maps, core_ids=list(range(8)))
    out = np.zeros((4, NLAT, D), np.float32)
    for c in range(8):
        b, hh = c // 2, c % 2
        out[b, hh * 2048:(hh + 1) * 2048, :] = np.asarray(res.results[c]["out"], dtype=np.float32)
    return out
```

```python
import math
from contextlib import ExitStack
import numpy as np
import concourse.bass as bass
import concourse.mybir as mybir
from concourse.bass_utils import run_bass_kernel_spmd

F32 = mybir.dt.float32
BF16 = mybir.dt.bfloat16
AF = mybir.ActivationFunctionType
ALU = mybir.AluOpType
AX = mybir.AxisListType

D = 1024
NCTX = 256
NLAT = 4096
T = NCTX + NLAT
NT = T // 128
EPS = 1e-6
N_IN = 3360
D_FF = 2816
D_FFE = 3584
NE = 8


class Dep:
    __slots__ = ("w", "r", "excl")

    def __init__(self, excl=False):
        self.w = {}
        self.r = {}
        self.excl = excl


class Prog:
    def __init__(self):
        self.nc = bass.Bass("TRN2", target_bir_lowering=False)
        nc = self.nc
        self.h = {"pe": nc.tensor, "act": nc.scalar, "dve": nc.vector, "pool": nc.gpsimd, "sp": nc.sync}
        self.sem = {}
        self.cnt = {}
        self.semobj = {}
        self.seen = {e: {} for e in self.h}
        self.nsem = 0
        for e in self.h:
            self._newsem(e)
        self.NS = 12
        self.slots = {q: [nc.alloc_semaphore(f"dq_{q}_{i}") for i in range(self.NS)] for q in ("sp", "pool", "act")}
        self.dcnt = {q: 0 for q in self.slots}
        for q in self.slots:
            for s in self.slots[q]:
                self.semobj[id(s)] = s

    def _newsem(self, e):
        s = self.nc.alloc_semaphore(f"s_{e}_{self.nsem}")
        self.nsem += 1
        self.sem[e] = s
        self.cnt[e] = 0
        if not hasattr(self, "semobj"):
            self.semobj = {}
        self.semobj[id(s)] = s

    def _wait(self, e, tok):
        sid, val = tok
        if self.seen[e].get(sid, 0) >= val:
            return
        self.seen[e][sid] = val
        self.h[e].wait_ge(self.semobj[sid], val)

    def _deps(self, e, r, w):
        need = {}
        for d in r:
            for sid, v in d.w.items():
                need[sid] = max(need.get(sid, 0), v)
            if d.excl:
                for sid, v in d.r.items():
                    need[sid] = max(need.get(sid, 0), v)
        for d in w:
            for sid, v in d.w.items():
                need[sid] = max(need.get(sid, 0), v)
            for sid, v in d.r.items():
                need[sid] = max(need.get(sid, 0), v)
        own = id(self.sem[e])
        for sid, v in need.items():
            if e == "pe" and sid == own:
                continue
            self._wait(e, (sid, v))

    def _mark(self, tok, r, w):
        sid, v = tok
        for d in r:
            d.r[sid] = max(d.r.get(sid, 0), v)
        for d in w:
            d.w = {sid: v}
            d.r = {}

    def op(self, e, fn, r=(), w=()):
        self._deps(e, r, w)
        if self.cnt[e] >= 30000:
            self._newsem(e)
        ins = fn(self.h[e])
        self.cnt[e] += 1
        ins.then_inc(self.sem[e], 1)
        self._mark((id(self.sem[e]), self.cnt[e]), r, w)

    def dma(self, q, out, in_, r=(), w=(), **kw):
        self._deps(q, r, w)
        i = self.dcnt[q]
        self.dcnt[q] += 1
        s = self.slots[q][i % self.NS]
        val = 16 * (i // self.NS + 1)
        self._wait(q, (id(s), val - 16))
        self.h[q].dma_start(out=out, in_=in_, **kw).then_inc(s, 16)
        self._mark((id(s), val), r, w)

    def eps_ap(self, eps, n):
        assert abs(eps - EPS) < 1e-12
        return self.epst[0:n, 0:1]

    def barrier(self):
        toks = []
        for e in self.h:
            if self.cnt[e] > 0:
                toks.append((id(self.sem[e]), self.cnt[e]))
        for q in self.slots:
            n = self.dcnt[q]
            for j in range(min(n, self.NS)):
                i = n - 1 - j
                toks.append((id(self.slots[q][i % self.NS]), 16 * (i // self.NS + 1)))
        for e in self.h:
            for t in toks:
                self._wait(e, t)


class SB:
    N = 0

    def __init__(self, p):
        self.p = p
        self.es = ExitStack()
        self.n = 0

    def __enter__(self):
        self.es.__enter__()
        return self

    def __exit__(self, *a):
        self.p.barrier()
        return self.es.__exit__(*a)

    def t(self, shape, dt=F32, name="t"):
        SB.N += 1
        return self.es.enter_context(self.p.nc.sbuf_tensor(f"{name}_{SB.N}", list(shape), dt)), Dep()

    def ps(self, shape=(128, 512), dt=F32, name="ps"):
        SB.N += 1
        return self.es.enter_context(self.p.nc.psum_tensor(f"{name}_{SB.N}", list(shape), dt)), Dep(excl=True)


CA_Q, CA_K, CA_V = 0, 256, 512
CB_Q, CB_K, CB_V = 768, 1024, 1152
CC_Q, CC_K, CC_V, CC_O, CC_G = 1280, 1536, 1792, 2048, 2304
CD_QKV, CD_Z, CD_G = 2320, 3088, 3344


def dram(p, name, shape, dt, kind="Internal"):
    return p.nc.dram_tensor(name, list(shape), dt, kind=kind).ap()


def phase_mod(p, l, I, S):
    nc = p.nc
    modT, d_modT = S["modT"]
    gsh, d_gsh = S["gsh"]
    grow, d_grow = S["grow"]
    with SB(p) as sb:
        cc, d_cc = sb.t((128, 8, 2))
        sc, d_sc = sb.t((128, 8, 2))
        ones, d_ones = sb.t((128, 128))
        rep, d_rep = sb.t((128, 2, 8, 128))
        bmT, d_bmT = sb.t((128, 48))
        nrm, d_nrm = sb.t((128, 2, 8))
        wm = [sb.t((128, 8, 512)) for _ in range(2)]
        psm, d_psm = sb.ps((128, 512))
        psr = [sb.ps((128, 512)) for _ in range(2)]
        p.dma("sp", cc[:], I["cc"][:, :, :], w=[d_cc])
        p.dma("sp", bmT[:], I["bmodT"][:, l, :], w=[d_bmT])
        p.dma("sp", nrm[:, 0, :], I["norm1T"][:, l, :], w=[d_nrm])
        p.dma("sp", nrm[:, 1, :], I["norm2T"][:, l, :], w=[d_nrm])
        for j in range(2):
            for which, c0 in ((0, 2048), (1, 5120)):
                p.dma("sp", grow[:, which, j, :], I["b_mod"][l:l + 1, c0:c0 + 1024].partition_broadcast(128), w=[d_grow])
        p.op("act", lambda h: h.activation(out=sc[:], in_=cc[:], func=AF.Sigmoid), r=[d_cc], w=[d_sc])
        p.op("dve", lambda h: h.tensor_tensor(out=sc[:], in0=sc[:], in1=cc[:], op=ALU.mult), r=[d_cc, d_sc], w=[d_sc])
        p.op("dve", lambda h: h.memset(ones[:], 1.0), w=[d_ones])
        for j in range(2):
            for k in range(8):
                p.op("dve", lambda h, j=j, k=k: h.tensor_scalar(out=rep[:, j, k, :], in0=ones[:], scalar1=sc[:, k, j:j + 1],
                                                                 scalar2=None, op0=ALU.mult), r=[d_ones, d_sc], w=[d_rep])
        wsrc = I["w_mod"][l].rearrange("(k q) n -> q k n", q=128)
        for blk in range(12):
            wt, d_wt = wm[blk % 2]
            p.dma("sp", wt[:], wsrc[:, :, blk * 512:(blk + 1) * 512], w=[d_wt])
            for fc in range(4):
                for k in range(8):
                    p.op("pe", lambda h, fc=fc, k=k, wt=wt: h.matmul(psm[:, fc * 2:fc * 2 + 2], lhsT=wt[:, k, fc * 128:(fc + 1) * 128],
                                                                    rhs=sc[:, k, :], start=(k == 0), stop=(k == 7)),
                         r=[d_wt, d_sc], w=[d_psm])
            p.op("dve", lambda h, blk=blk: h.tensor_copy(out=modT[:, blk * 4:(blk + 1) * 4, :],
                                                         in_=psm[:, 0:8].rearrange("q (f j) -> q f j", j=2)),
                 r=[d_psm], w=[d_modT])
            if blk in (4, 5, 10, 11):
                which = 0 if blk < 6 else 1
                half = blk % 2
                for j in range(2):
                    pr, d_pr = psr[j]
                    for k in range(8):
                        p.op("pe", lambda h, j=j, k=k, wt=wt, pr=pr: h.matmul(pr[:, :], lhsT=rep[:, j, k, :], rhs=wt[:, k, :],
                                                                              start=(k == 0), stop=(k == 7)),
                             r=[d_wt, d_rep], w=[d_pr])
                    dst = grow[:, which, j, half * 512:(half + 1) * 512]
                    p.op("dve", lambda h, dst=dst, pr=pr: h.tensor_tensor(out=dst, in0=pr[:, :], in1=dst, op=ALU.add),
                         r=[d_pr, d_grow], w=[d_grow])
        for j in range(2):
            p.op("dve", lambda h, j=j: h.tensor_tensor(out=modT[:, :, j], in0=modT[:, :, j], in1=bmT[:], op=ALU.add),
                 r=[d_bmT, d_modT], w=[d_modT])
        for j in range(2):
            for n_i, (c_sh, c_sc) in enumerate(((0, 8), (24, 32))):
                p.op("dve", lambda h, j=j, n_i=n_i, c_sc=c_sc: h.scalar_tensor_tensor(
                    out=gsh[:, 2 * n_i, :, j], in0=modT[:, c_sc:c_sc + 8, j], scalar=1.0, in1=nrm[:, n_i, :],
                    op0=ALU.add, op1=ALU.mult), r=[d_modT, d_nrm], w=[d_gsh])
                p.op("dve", lambda h, j=j, n_i=n_i, c_sh=c_sh: h.tensor_copy(out=gsh[:, 2 * n_i + 1, :, j], in_=modT[:, c_sh:c_sh + 8, j]),
                     r=[d_modT], w=[d_gsh])


def rstd(p, out, in_, tmp, scale, deps, eps=EPS):
    p.op("act", lambda h: h.activation(out=tmp, in_=in_, func=AF.Sqrt, scale=scale, bias=p.eps_ap(eps, in_.shape[0])), r=deps + [p.d_eps], w=deps)
    p.op("dve", lambda h: h.reciprocal(out=out, in_=tmp), r=deps, w=deps)


def norm_tile(p, sb_objs, xt, d_xt, gsh, d_gsh, which, j, xnT, d_xnT, tok0):
    (junk, d_junk, ss, d_ss, xs, d_xs, tps, ident, d_ident) = sb_objs
    p.op("act", lambda h: h.activation(out=junk[:], in_=xt[:], func=AF.Square, accum_out=ss[:, 0:1]), r=[d_xt], w=[d_junk, d_ss])
    rstd(p, ss[:, 2:3], ss[:, 0:1], ss[:, 1:2], 1.0 / D, [d_ss])
    p.op("dve", lambda h: h.tensor_scalar(out=xs[:], in0=xt[:], scalar1=ss[:, 2:3], scalar2=None, op0=ALU.mult),
         r=[d_xt, d_ss], w=[d_xs])
    for c in range(8):
        tp, d_tp = tps[c // 4]
        p.op("pe", lambda h, c=c, tp=tp: h.transpose(tp[:, (c % 4) * 128:(c % 4 + 1) * 128], xs[:, c * 128:(c + 1) * 128], ident[:]),
             r=[d_xs, d_ident], w=[d_tp])
    for c in range(8):
        tp, d_tp = tps[c // 4]
        p.op("act", lambda h, c=c, tp=tp: h.activation(out=xnT[:, c, tok0:tok0 + 128], in_=tp[:, (c % 4) * 128:(c % 4 + 1) * 128],
                                                       func=AF.Identity, scale=gsh[:, 2 * which, c, j:j + 1],
                                                       bias=gsh[:, 2 * which + 1, c, j:j + 1]),
             r=[d_tp, d_gsh], w=[d_xnT])


def qk_norm_rope(p, sbo, src, d_src, ncols, dim, gain, d_gain, rope, d_rope, dst, d_dst):
    (sq, d_sq, st, d_st, t1, d_t1, t2, d_t2) = sbo
    ng = ncols // dim
    p.op("act", lambda h: h.activation(out=sq[:, 0:ncols], in_=src, func=AF.Square), r=[d_src], w=[d_sq])
    p.op("dve", lambda h: h.tensor_reduce(out=st[:, 0:ng], in_=sq[:, 0:ncols].rearrange("q (g d) -> q g d", d=dim), axis=AX.X, op=ALU.add),
         r=[d_sq], w=[d_st])
    rstd(p, st[:, 0:ng], st[:, 0:ng], st[:, 0:ng], 1.0 / dim, [d_st])
    tgt = t1 if rope is not None else dst
    d_tgt = d_t1 if rope is not None else d_dst
    p.op("dve", lambda h: h.tensor_tensor(out=tgt[:, 0:ncols].rearrange("q (g d) -> q g d", d=dim),
                                          in0=src.rearrange("q (g d) -> q g d", d=dim),
                                          in1=st[:, 0:ng].unsqueeze(2).to_broadcast([128, ng, dim]), op=ALU.mult),
         r=[d_src, d_st], w=[d_tgt])
    p.op("dve", lambda h: h.tensor_tensor(out=tgt[:, 0:ncols], in0=tgt[:, 0:ncols], in1=gain[:, 0:ncols], op=ALU.mult),
         r=[d_gain, d_tgt], w=[d_tgt])
    if rope is None:
        return
    q4 = dim // 4
    v = lambda a: a[:, 0:ncols].rearrange("q (g a s f) -> q g a s f", a=2, s=2, f=q4)
    cb = rope[:, 0, :].rearrange("q (a s f) -> q a s f", a=2, s=2).unsqueeze(1).to_broadcast([128, ng, 2, 2, q4])
    sbv = rope[:, 1, :].rearrange("q (a s f) -> q a s f", a=2, s=2)
    for s_ in range(2):
        p.op("dve", lambda h, s_=s_: h.tensor_tensor(out=v(t2)[:, :, :, s_, :], in0=v(t1)[:, :, :, 1 - s_, :],
                                                     in1=sbv[:, :, s_, :].unsqueeze(1).to_broadcast([128, ng, 2, q4]), op=ALU.mult),
             r=[d_t1, d_rope], w=[d_t2])
    p.op("dve", lambda h: h.tensor_tensor(out=v(t1), in0=v(t1), in1=cb, op=ALU.mult), r=[d_rope, d_t1], w=[d_t1])
    p.op("dve", lambda h: h.tensor_tensor(out=dst[:, 0:ncols], in0=t1[:, 0:ncols], in1=t2[:, 0:ncols], op=ALU.add),
         r=[d_t1, d_t2], w=[d_dst])


def phase_inproj(p, l, I, S, Z, xsrc):
    gsh, d_gsh = S["gsh"]
    ident, d_ident = S["ident"]
    with SB(p) as sb:
        w, d_w = sb.t((128, 8, N_IN), BF16, "win")
        for k in range(8):
            p.dma("pool", w[:, k, :], I["w_in"][l, k * 128:(k + 1) * 128, :], w=[d_w])
        gainA, d_gainA = sb.t((128, 512))
        gainB, d_gainB = sb.t((128, 384))
        for m in range(8):
            p.dma("sp", gainA[:, m * 32:(m + 1) * 32], I["diff_qk_gain"][l, 0:1, :].partition_broadcast(128), w=[d_gainA])
            p.dma("sp", gainA[:, 256 + m * 32:256 + (m + 1) * 32], I["diff_qk_gain"][l, 1:2, :].partition_broadcast(128), w=[d_gainA])
        for m in range(6):
            p.dma("sp", gainB[:, m * 64:(m + 1) * 64], I["gqa_qk_gain"][l, (0 if m < 4 else 1):(1 if m < 4 else 2), :].partition_broadcast(128),
                  w=[d_gainB])
        xts = [sb.t((128, D)) for _ in range(2)]
        junk, d_junk = sb.t((128, D))
        xs, d_xs = sb.t((128, D))
        ss, d_ss = sb.t((128, 4))
        tps = [sb.ps() for _ in range(2)]
        nobj = (junk, d_junk, ss, d_ss, xs, d_xs, tps, ident, d_ident)
        xnT, d_xnT = sb.t((128, 8, 512), BF16, "xnT")
        sq, d_sq = sb.t((128, 512))
        st, d_st = sb.t((128, 16))
        t1, d_t1 = sb.t((128, 512))
        t2, d_t2 = sb.t((128, 512))
        qko = (sq, d_sq, st, d_st, t1, d_t1, t2, d_t2)
        qkn, d_qkn = sb.t((128, 512))
        sq2, d_sq2 = sb.t((128, 384))
        st2, d_st2 = sb.t((128, 16))
        t12, d_t12 = sb.t((128, 384))
        t22, d_t22 = sb.t((128, 384))
        qko2 = (sq2, d_sq2, st2, d_st2, t12, d_t12, t22, d_t22)
        qkn2, d_qkn2 = sb.t((128, 384))
        stg, d_stg = sb.t((128, 16))
        ps_tr2, d_ps_tr2 = sb.ps()
        ropeA, d_ropeA = sb.t((128, 2, 32))
        ropeB, d_ropeB = sb.t((128, 2, 64))
        ps_tm = [sb.ps() for _ in range(2)]
        ps_fm = [sb.ps() for _ in range(2)]
        ps_tr, d_ps_tr = sb.ps()
        stA, d_stA = sb.t((128, 4, 512), BF16)
        stB, d_stB = sb.t((128, 3, 512), BF16)
        stv, d_stv = sb.t((128, 512), BF16)
        stf, d_stf = sb.t((128, 784))
        stfm = [sb.t((128, 512)) for _ in range(2)]
        blocks = [(0, 2)] + [(2 + 4 * i, 4) for i in range(8)]
        ntm = 0
        nfm = 0
        for (t0, ntl) in blocks:
            ntok = ntl * 128
            tokb = t0 * 128
            j = 1 if t0 < 2 else 0
            for ti in range(ntl):
                tt = t0 + ti
                xt, d_xt = xts[tt % 2]
                p.dma("sp", xt[:], xsrc[tt * 128:(tt + 1) * 128, :], w=[d_xt])
                norm_tile(p, nobj, xt, d_xt, gsh, d_gsh, 0, j, xnT, d_xnT, ti * 128)
            for ti in range(ntl):
                tt = t0 + ti
                tk = slice(ti * 128, (ti + 1) * 128)
                rows = slice(tt * 128, (tt + 1) * 128)
                lat = tt >= 2
                if lat:
                    p.dma("sp", ropeA[:], I["ropeA"][(tt - 2) * 128:(tt - 1) * 128, :, :], w=[d_ropeA])
                    p.dma("sp", ropeB[:], I["ropeB"][(tt - 2) * 128:(tt - 1) * 128, :, :], w=[d_ropeB])

                def tm_mm(c0, ncol):
                    nonlocal ntm
                    ps, d_ps = ps_tm[ntm % 2]
                    ntm += 1
                    for k in range(8):
                        p.op("pe", lambda h, k=k, ps=ps: h.matmul(ps[:, 0:ncol], lhsT=xnT[:, k, tk], rhs=w[:, k, c0:c0 + ncol],
                                                                  start=(k == 0), stop=(k == 7)), r=[d_xnT, d_w], w=[d_ps])
                    return ps, d_ps
                ps, d_ps = tm_mm(CA_Q, 512)
                qk_norm_rope(p, qko, ps[:, 0:512], d_ps, 512, 32, gainA, d_gainA, ropeA if lat else None, d_ropeA, qkn, d_qkn)
                def trA():
                    for c in range(4):
                        p.op("pe", lambda h, c=c: h.transpose(ps_tr[:, c * 128:(c + 1) * 128], qkn[:, c * 128:(c + 1) * 128], ident[:]),
                             r=[d_qkn, d_ident], w=[d_ps_tr])
                    p.op("act", lambda h: h.activation(out=stA[:, :, tk], in_=ps_tr[:, :].rearrange("q (c t) -> q c t", c=4), func=AF.Copy),
                         r=[d_ps_tr], w=[d_stA])
                ps, d_ps = tm_mm(CA_V, 256)
                p.op("act", lambda h, ps=ps: h.activation(out=stv[:, 0:256], in_=ps[:, 0:256], func=AF.Copy), r=[d_ps], w=[d_stv])
                p.dma("sp", Z["vA"][rows, :], stv[:, 0:256], r=[d_stv])
                ps, d_ps = tm_mm(CB_Q, 512)
                p.op("act", lambda h, ps=ps: h.activation(out=stv[:, 256:384], in_=ps[:, 384:512], func=AF.Copy), r=[d_ps], w=[d_stv])
                p.dma("sp", Z["vB"][rows, :], stv[:, 256:384], r=[d_stv])
                qk_norm_rope(p, qko2, ps[:, 0:384], d_ps, 384, 64, gainB, d_gainB, ropeB if lat else None, d_ropeB, qkn2, d_qkn2)

                def trB():
                    for c in range(3):
                        p.op("pe", lambda h, c=c: h.transpose(ps_tr2[:, c * 128:(c + 1) * 128], qkn2[:, c * 128:(c + 1) * 128], ident[:]),
                             r=[d_qkn2, d_ident], w=[d_ps_tr2])
                    p.op("act", lambda h: h.activation(out=stB[:, :, tk], in_=ps_tr2[:, 0:384].rearrange("q (c t) -> q c t", c=3), func=AF.Copy),
                         r=[d_ps_tr2], w=[d_stB])
                ps, d_ps = tm_mm(CC_V, 512)
                p.op("act", lambda h, ps=ps: h.activation(out=stf[:, 0:512], in_=ps[:, 0:512], func=AF.Copy), r=[d_ps], w=[d_stf])
                p.dma("sp", Z["vC"][rows, :], stf[:, 0:256], r=[d_stf])
                p.dma("sp", Z["oC"][rows, :], stf[:, 256:512], r=[d_stf])
                ps, d_ps = tm_mm(CD_Z, 272)
                p.op("act", lambda h, ps=ps: h.activation(out=stf[:, 512:784], in_=ps[:, 0:272], func=AF.Copy), r=[d_ps], w=[d_stf])
                p.dma("sp", Z["zD"][rows, :], stf[:, 512:768], r=[d_stf])
                p.dma("sp", Z["gD"][rows, :], stf[:, 768:784], r=[d_stf])
                ps, d_ps = tm_mm(CC_K, 256)
                p.op("act", lambda h, ps=ps: h.activation(out=stf[:, 0:256], in_=ps[:, 0:256], func=AF.Copy), r=[d_ps], w=[d_stf])
                p.dma("sp", Z["kC"][rows, :], stf[:, 0:256], r=[d_stf])
                ps, d_ps = tm_mm(CC_G, 16)
                p.op("act", lambda h, ps=ps: h.activation(out=stg[:, 0:16], in_=ps[:, 0:16], func=AF.Copy), r=[d_ps], w=[d_stg])
                p.dma("sp", Z["gC"][rows, :], stg[:, 0:16], r=[d_stg])
                trA()
                trB()
            tb = slice(tokb, tokb + ntok)
            for c in range(2):
                p.dma("sp", Z["qTa"][c * 128:(c + 1) * 128, tb], stA[:, c, 0:ntok], r=[d_stA])
                p.dma("sp", Z["kTa"][c * 128:(c + 1) * 128, tb], stA[:, 2 + c, 0:ntok], r=[d_stA])
                p.dma("sp", Z["qTb"][c * 128:(c + 1) * 128, tb], stB[:, c, 0:ntok], r=[d_stB])
            p.dma("sp", Z["kTb"][:, tb], stB[:, 2, 0:ntok], r=[d_stB])
            for ci in range(10):
                c0 = CC_Q + ci * 128 if ci < 4 else CD_QKV + (ci - 4) * 128
                ps, d_ps = ps_fm[nfm % 2]
                so, d_so = stfm[nfm % 2]
                nfm += 1
                for k in range(8):
                    p.op("pe", lambda h, k=k, ps=ps, c0=c0: h.matmul(ps[:, 0:ntok], lhsT=w[:, k, c0:c0 + 128], rhs=xnT[:, k, 0:ntok],
                                                                     start=(k == 0), stop=(k == 7)), r=[d_xnT, d_w], w=[d_ps])
                p.op("act", lambda h, ps=ps, so=so: h.activation(out=so[:, 0:ntok], in_=ps[:, 0:ntok], func=AF.Copy), r=[d_ps], w=[d_so])
                if ci < 2:
                    dst = Z["qTc"][ci * 128:(ci + 1) * 128, tb]
                elif ci < 4:
                    dst = Z["kTc"][(ci - 2) * 128:(ci - 1) * 128, tb]
                else:
                    dst = Z["qkvT"][(ci - 4) * 128:(ci - 3) * 128, tb]
                p.dma("sp", dst, so[:, 0:ntok], r=[d_so])


def rope_table(dim):
    nf = dim // 4
    t = np.arange(NLAT)
    row = (t // 64).astype(np.float32)
    col = (t % 64).astype(np.float32)
    inv = (np.float32(10000.0) ** (-np.arange(nf, dtype=np.float32) / np.float32(nf))).astype(np.float32)
    ang = np.stack([row[:, None] * inv, col[:, None] * inv], axis=1).astype(np.float32)
    c, s = np.cos(ang).astype(np.float32), np.sin(ang).astype(np.float32)
    C = np.stack([c, c], axis=2)
    Sp = np.stack([-s, s], axis=2)
    return np.ascontiguousarray(np.stack([C.reshape(NLAT, dim), Sp.reshape(NLAT, dim)], axis=1)).astype(np.float32)


IN_SPECS = {
    "xin": ([T, D], F32), "cc": ([128, 8, 2], F32), "flags": ([128, 2], F32),
    "bmodT": ([128, 2, 48], F32), "norm1T": ([128, 2, 8], F32), "norm2T": ([128, 2, 8], F32),
    "b_mod": ([2, 6 * D], F32), "w_mod": ([2, D, 6 * D], F32), "w_in": ([2, D, N_IN], F32), "w_out": ([2, D, D], F32),
    "diff_qk_gain": ([2, 2, 32], F32), "diff_lambda": ([2, 4, 32], F32), "diff_subln": ([2, 64], F32),
    "gqa_qk_gain": ([2, 2, 64], F32), "mlstm_gate_bias": ([2, 16], F32), "mlstm_norm": ([2, 256], F32),
    "gdn_convT": ([128, 2, 6, 5], F32), "gdn_a_log": ([2, 8], F32), "gdn_dt_bias": ([2, 8], F32), "gdn_norm": ([2, 64], F32),
    "ffn_w_gate": ([D, D_FF], F32), "ffn_w_up": ([D, D_FF], F32), "ffn_w_down": ([D_FF, D], F32),
    "moe_router": ([D, NE], F32), "moe_w_gate": ([NE, D, D_FFE], F32), "moe_w_up": ([NE, D, D_FFE], F32),
    "moe_w_down": ([NE, D_FFE, D], F32),
    "ropeA": ([NLAT, 2, 32], F32), "ropeB": ([NLAT, 2, 64], F32), "ident": ([128, 128], F32),
    "cmask": ([128, 2, 128], F32),
}


def host_inputs(inp, core):
    b, hh = core // 2, core % 2
    f = lambda a: np.ascontiguousarray(np.asarray(a, dtype=np.float32))
    colsT = lambda v, n: f(np.asarray(v).reshape(v.shape[0], n, 128).transpose(2, 0, 1))
    m = {}
    m["xin"] = f(np.concatenate([inp["ctx"][b], inp["x"][b]], axis=0))
    m["cc"] = f(np.stack([np.asarray(inp["c"][b]).reshape(8, 128).T, np.asarray(inp["c_ctx"]).reshape(8, 128).T], axis=2))
    fl = np.zeros((128, 2), np.float32)
    fl[:, hh] = 1.0
    m["flags"] = fl
    m["bmodT"] = colsT(inp["b_mod"], 48)
    m["norm1T"] = colsT(inp["norm1"], 8)
    m["norm2T"] = colsT(inp["norm2"], 8)
    for k in ("b_mod", "w_mod", "w_in", "w_out", "diff_qk_gain", "diff_lambda", "diff_subln", "gqa_qk_gain", "mlstm_norm",
              "gdn_norm"):
        m[k] = f(inp[k])
    m["mlstm_gate_bias"] = f(np.asarray(inp["mlstm_gate_bias"]).reshape(2, 16))
    m["gdn_a_log"] = f(np.asarray(inp["gdn_a_log"]).reshape(2, 8))
    m["gdn_dt_bias"] = f(np.asarray(inp["gdn_dt_bias"]).reshape(2, 8))
    m["gdn_convT"] = f(np.asarray(inp["gdn_conv"]).reshape(2, 5, 6, 128).transpose(3, 0, 2, 1))
    m["ffn_w_gate"] = f(inp["ffn_w_gate"][0])
    m["ffn_w_up"] = f(inp["ffn_w_up"][0])
    m["ffn_w_down"] = f(inp["ffn_w_down"][0])
    m["moe_router"] = f(inp["moe_router"][0])
    m["moe_w_gate"] = f(inp["moe_w_gate"][0])
    m["moe_w_up"] = f(inp["moe_w_up"][0])
    m["moe_w_down"] = f(inp["moe_w_down"][0])
    m["ropeA"] = rope_table(32)
    m["ropeB"] = rope_table(64)
    m["ident"] = np.eye(128, dtype=np.float32)
    i = np.arange(128)
    low = (i[:, None] >= i[None, :]).astype(np.float32)
    m["cmask"] = f(np.stack([low, low.T], axis=1))
    return m


SCRATCH = {
    "xres": ([T, D], F32),
    "qTa": ([256, T], BF16), "kTa": ([256, T], BF16), "vA": ([T, 256], BF16),
    "qTb": ([256, T], BF16), "kTb": ([128, T], BF16), "vB": ([T, 128], BF16),
    "qTc": ([256, T], F32), "kTc": ([256, T], F32), "vC": ([T, 256], F32), "oC": ([T, 256], F32), "gC": ([T, 16], F32),
    "qkvT": ([768, T], F32), "zD": ([T, 256], F32), "gD": ([T, 16], F32), "kC": ([T, 256], F32),
    "gqT": ([256, T], F32), "gkT": ([256, T], F32), "gk": ([T, 256], F32), "gv": ([T, 256], F32),
    "y": ([T, D], F32), "ym": ([NLAT // 2, 512], F32), "xm1": ([NLAT // 2, D], F32),
}


def build(debug=None, upto="all"):
    p = Prog()
    nc = p.nc
    I = {k: nc.dram_tensor(k, sh, dt, kind="ExternalInput").ap() for k, (sh, dt) in IN_SPECS.items()}
    Z = {}
    for k, (sh, dt) in SCRATCH.items():
        kind = "ExternalOutput" if (debug and k in debug) else "Internal"
        Z[k] = nc.dram_tensor("z_" + k, sh, dt, kind=kind).ap()
    out = nc.dram_tensor("out", [NLAT // 2, D], F32, kind="ExternalOutput").ap()
    with SB(p) as gsb:
        S = {"modT": gsb.t((128, 48, 2)), "gsh": gsb.t((128, 4, 8, 2)), "grow": gsb.t((128, 2, 2, D)), "ident": gsb.t((128, 128))}
        p.dma("sp", S["ident"][0][:], I["ident"][:, :], w=[S["ident"][1]])
        epst, p.d_eps = gsb.t((128, 1))
        p.epst = epst
        p.op("dve", lambda h: h.memset(epst[:], EPS), w=[p.d_eps])
        if debug and "modT" in debug:
            dbg_mod = nc.dram_tensor("z_modT", [128, 48, 2], F32, kind="ExternalOutput").ap()
            dbg_grow = nc.dram_tensor("z_grow", [128, 2, 2, D], F32, kind="ExternalOutput").ap()
        for l in range(2):
            phase_mod(p, l, I, S)
            if debug and "modT" in debug and l == 0:
                p.dma("sp", dbg_mod[:, :, :], S["modT"][0][:], r=[S["modT"][1]])
                p.dma("sp", dbg_grow[:, :, :, :], S["grow"][0][:], r=[S["grow"][1]])
            phase_inproj(p, l, I, S, Z, I["xin"] if l == 0 else Z["xres"])
            if upto == "inproj":
                break
            if "noattn" not in upto:
                phase_attn(p, l, I, S, Z, l == 0, mine=(l == 1))
            if upto == "attn":
                break
            phase_chunk(p, l, I, S, Z, do_c=("noc" not in upto), do_d=("nod" not in upto), upto=upto)
            if upto.startswith("chunk"):
                break
            phase_outproj(p, l, I, S, Z, I["xin"] if l == 0 else Z["xres"], mine=(l == 1))
            if l == 0:
                phase_ffn_dense(p, l, I, S, Z)
                if upto == "layer0":
                    break
            else:
                phase_moe(p, l, I, S, Z, out)
        p.barrier()
    return p


def bcast_load(p, sb, src_row_ap, n, name="bc"):
    t, d = sb.t((128, n), F32, name)
    p.dma("sp", t[:], src_row_ap.partition_broadcast(128), w=[d])
    return t, d


def phase_attn(p, l, I, S, Z, with_ctx, mine=False):
    ident, d_ident = S["ident"]
    lambda_init = 0.8 - 0.6 * math.exp(-0.3 * l)
    with SB(p) as sb:
        lamv, d_lamv = sb.t((128, 4, 32))
        p.dma("sp", lamv[:], I["diff_lambda"][l:l + 1, :, :].partition_broadcast(128), w=[d_lamv])
        cst, d_cst = sb.t((128, 16))
        tmp32, d_tmp32 = sb.t((128, 2, 32))
        p.op("dve", lambda h: h.tensor_tensor(out=tmp32[:], in0=lamv[:, 0:4:2, :], in1=lamv[:, 1:4:2, :], op=ALU.mult),
             r=[d_lamv], w=[d_tmp32])
        p.op("dve", lambda h: h.tensor_reduce(out=cst[:, 0:2], in_=tmp32[:], axis=AX.X, op=ALU.add), r=[d_tmp32], w=[d_cst])
        p.op("act", lambda h: h.activation(out=cst[:, 2:4], in_=cst[:, 0:2], func=AF.Exp), r=[d_cst], w=[d_cst])
        p.op("dve", lambda h: h.tensor_tensor(out=cst[:, 4:5], in0=cst[:, 3:4], in1=cst[:, 2:3], op=ALU.subtract), r=[d_cst], w=[d_cst])
        p.op("dve", lambda h: h.tensor_scalar(out=cst[:, 4:5], in0=cst[:, 4:5], scalar1=-lambda_init, scalar2=None, op0=ALU.add),
             r=[d_cst], w=[d_cst])
        gA, d_gA = sb.t((128, 2, 32))
        gB, d_gB = sb.t((128, 2, 64))
        p.dma("sp", gA[:], I["diff_qk_gain"][l:l + 1, :, :].partition_broadcast(128), w=[d_gA])
        p.dma("sp", gB[:], I["gqa_qk_gain"][l:l + 1, :, :].partition_broadcast(128), w=[d_gB])
        p.op("dve", lambda h: h.tensor_reduce(out=cst[:, 6:8], in_=gA[:], axis=AX.X, op=ALU.max, apply_absolute_value=True),
             r=[d_gA], w=[d_cst])
        p.op("dve", lambda h: h.tensor_reduce(out=cst[:, 8:10], in_=gB[:], axis=AX.X, op=ALU.max, apply_absolute_value=True),
             r=[d_gB], w=[d_cst])
        p.op("dve", lambda h: h.scalar_tensor_tensor(out=cst[:, 10:11], in0=cst[:, 6:7], scalar=-math.sqrt(32.0), in1=cst[:, 7:8],
                                                     op0=ALU.mult, op1=ALU.mult), r=[d_cst], w=[d_cst])
        p.op("dve", lambda h: h.scalar_tensor_tensor(out=cst[:, 11:12], in0=cst[:, 8:9], scalar=-8.0, in1=cst[:, 9:10],
                                                     op0=ALU.mult, op1=ALU.mult), r=[d_cst], w=[d_cst])
        subg, d_subg = bcast_load(p, sb, I["diff_subln"][l:l + 1, :], 64)
        p.op("dve", lambda h: h.tensor_scalar(out=subg[:], in0=subg[:], scalar1=1.0 - lambda_init, scalar2=None, op0=ALU.mult),
             r=[d_subg], w=[d_subg])
        kT, d_kT = sb.t((64, T), BF16, "kT")
        va, d_va = sb.t((128, NT, 65), BF16, "vaug")
        qTs = [sb.t((64, 512), BF16, "qT") for _ in range(2)]
        NSB = 2
        pTs = [sb.t((128, 1024), BF16, "pT") for _ in range(NSB)]
        ps_s = [sb.ps((128, 1024)) for _ in range(NSB)]
        ps_o = [sb.ps() for _ in range(2)]
        ps_t, d_ps_t = sb.ps()
        osb = [sb.t((65, 512), F32, "osb") for _ in range(2)]
        on = [sb.t((128, 64), F32, "on") for _ in range(2)]
        od, d_od = sb.t((128, 64))
        junk, d_junk = sb.t((128, 64))
        st, d_st = sb.t((128, 4))
        ystage, d_ys = sb.t((128, 4, 64))
        rc, d_rc = sb.t((128, 2))
        qblocks = ([(0, 256, [0, 1])] if with_ctx else []) + [(256 + 512 * i, 512, list(range(NT))) for i in range(4 if mine else 8)]
        cnt = {"s": 0, "q": 0}
        if mine:
            fl, d_fl = sb.t((128, 2))
            p.dma("sp", fl[:], I["flags"][:, :], w=[d_fl])
            qTo = [sb.t((64, 512), BF16, "qTo") for _ in range(2)]

        def run_head(kind, qsrc_rows, nmaps, scale, negB, ycol):
            for (q0, qn, kts) in qblocks:
                qT, d_qT = qTs[cnt["q"] % 2]
                cnt["q"] += 1
                if not mine:
                    p.dma("sp", qT[:, 0:qn], qsrc_rows[:, q0:q0 + qn], w=[d_qT])
                else:
                    qo, d_qo = qTo[cnt["q"] % 2]
                    p.dma("sp", qT[:, 0:qn], qsrc_rows[:, q0:q0 + qn], w=[d_qT])
                    p.dma("sp", qo[:, 0:qn], qsrc_rows[:, q0 + 2048:q0 + 2048 + qn], w=[d_qo])
                    p.op("dve", lambda h, qT=qT: h.tensor_scalar(out=qT[:, 0:qn], in0=qT[:, 0:qn], scalar1=fl[0:64, 0:1], scalar2=None, op0=ALU.mult),
                         r=[d_qT, d_fl], w=[d_qT])
                    p.op("dve", lambda h, qT=qT, qo=qo: h.scalar_tensor_tensor(out=qT[:, 0:qn], in0=qo[:, 0:qn], scalar=fl[0:64, 1:2], in1=qT[:, 0:qn],
                                                                               op0=ALU.mult, op1=ALU.add), r=[d_qo, d_fl, d_qT], w=[d_qT])
                for j in range(nmaps):
                    kr = slice(32 * j, 32 * j + 32) if kind == "A" else slice(0, 64)
                    po, d_po = ps_o[j]
                    LA = 1
                    pend = []
                    pairs = [kts[i2:i2 + 2] for i2 in range(0, len(kts), 2)]
                    for pi in range(len(pairs) + LA):
                        if pi < len(pairs):
                            pr = pairs[pi]
                            i = cnt["s"]
                            cnt["s"] += 1
                            ps, d_ps = ps_s[i % NSB]
                            pT, d_pT = pTs[i % NSB]
                            for hh, kt in enumerate(pr):
                                p.op("pe", lambda h, ps=ps, kt=kt, kr=kr, qT=qT, hh=hh: h.matmul(ps[:, hh * 512:hh * 512 + qn],
                                                                                               lhsT=kT[kr, kt * 128:(kt + 1) * 128],
                                                                                               rhs=qT[kr, 0:qn], start=True, stop=True),
                                     r=[d_kT, d_qT], w=[d_ps])
                            np_ = len(pr)
                            p.op("act", lambda h, ps=ps, pT=pT, np_=np_: h.activation(
                                out=pT[:, :].rearrange("q (b n) -> q b n", b=2)[:, 0:np_, 0:qn],
                                in_=ps[:, :].rearrange("q (b n) -> q b n", b=2)[:, 0:np_, 0:qn], func=AF.Exp, scale=scale, bias=negB),
                                 r=[d_ps, d_cst], w=[d_pT])
                            pend.append((pr, pi, pT, d_pT))
                        if pi >= LA:
                            pr2, pi2, pT2, d_pT2 = pend.pop(0)
                            for hh, kt2 in enumerate(pr2):
                                first = (pi2 == 0 and hh == 0)
                                last = (pi2 == len(pairs) - 1 and hh == len(pr2) - 1)
                                p.op("pe", lambda h, po=po, pT2=pT2, kt2=kt2, hh=hh, first=first, last=last: h.matmul(
                                    po[0:65, 0:qn], lhsT=va[:, kt2, :], rhs=pT2[:, hh * 512:hh * 512 + qn], start=first, stop=last),
                                     r=[d_va, d_pT2], w=[d_po])
                    ob, d_ob = osb[j]
                    p.op("act", lambda h, ob=ob, po=po: h.activation(out=ob[:, 0:qn], in_=po[0:65, 0:qn], func=AF.Copy), r=[d_po], w=[d_ob])
                nsub = qn // 128
                for s_ in range(nsub):
                    for j in range(nmaps):
                        ob, d_ob = osb[j]
                        p.op("pe", lambda h, ob=ob, j=j, s_=s_: h.transpose(ps_t[:, j * 128:j * 128 + 65], ob[0:65, s_ * 128:(s_ + 1) * 128],
                                                                            ident[0:65, 0:65]), r=[d_ob, d_ident], w=[d_ps_t])
                    for j in range(nmaps):
                        o_, d_o = on[j]
                        p.op("dve", lambda h, j=j: h.reciprocal(out=rc[:, j:j + 1], in_=ps_t[:, j * 128 + 64:j * 128 + 65]),
                             r=[d_ps_t], w=[d_rc])
                        p.op("dve", lambda h, o_=o_, j=j: h.tensor_scalar(out=o_[:], in0=ps_t[:, j * 128:j * 128 + 64],
                                                                          scalar1=rc[:, j:j + 1], scalar2=None,
                                                                          op0=ALU.mult), r=[d_ps_t, d_rc], w=[d_o])
                    if kind == "A":
                        p.op("dve", lambda h: h.scalar_tensor_tensor(out=od[:], in0=on[1][0][:], scalar=cst[:, 4:5], in1=on[0][0][:],
                                                                     op0=ALU.mult, op1=ALU.add), r=[on[0][1], on[1][1], d_cst], w=[d_od])
                        p.op("act", lambda h: h.activation(out=junk[:], in_=od[:], func=AF.Square, accum_out=st[:, 0:1]),
                             r=[d_od], w=[d_junk, d_st])
                        rstd(p, st[:, 2:3], st[:, 0:1], st[:, 1:2], 1.0 / 64, [d_st])
                        p.op("dve", lambda h, s_=s_: h.scalar_tensor_tensor(out=ystage[:, s_, :], in0=od[:], scalar=st[:, 2:3], in1=subg[:],
                                                                            op0=ALU.mult, op1=ALU.mult), r=[d_od, d_st, d_subg], w=[d_ys])
                    else:
                        p.op("dve", lambda h, s_=s_: h.tensor_copy(out=ystage[:, s_, :], in_=on[0][0][:]), r=[on[0][1]], w=[d_ys])
                ydst = Z["ym"][q0 - 256:q0 - 256 + qn, ycol:ycol + 64] if mine else Z["y"][q0:q0 + qn, ycol:ycol + 64]
                p.dma("sp", ydst.rearrange("(s q) d -> q s d", q=128), ystage[:, 0:nsub, :], r=[d_ys])

        def load_kv(ksrc_rows, vsrc_cols):
            p.dma("sp", kT[:, :], ksrc_rows, w=[d_kT])
            p.dma("sp", va[:, :, 0:64], vsrc_cols.rearrange("(n q) d -> q n d", q=128), w=[d_va])
            p.op("dve", lambda h: h.memset(va[:, :, 64:65], 1.0), w=[d_va])

        for hd in range(4):
            load_kv(Z["kTa"][64 * hd:64 * hd + 64, :], Z["vA"][:, 64 * hd:64 * hd + 64])
            run_head("A", Z["qTa"][64 * hd:64 * hd + 64, :], 2, 1.0 / math.sqrt(32.0), cst[:, 10:11], 64 * hd)
        for kv in range(2):
            load_kv(Z["kTb"][64 * kv:64 * kv + 64, :], Z["vB"][:, 64 * kv:64 * kv + 64])
            for g in (2 * kv, 2 * kv + 1):
                run_head("B", Z["qTb"][64 * g:64 * g + 64, :], 1, 0.125, cst[:, 11:12], 256 + 64 * g)


ORDER = [list(range(NT)), [1, 0] + list(range(NT - 1, 1, -1))]
NGC = 40


class PsumPool:
    def __init__(self, sb, nbanks=8):
        self.q = []
        banks = [sb.ps() for b in range(nbanks)]
        for k in range(4):
            for (t, d) in banks:
                self.q.append((t[:, k * 128:(k + 1) * 128], d))
        self.i = 0

    def get(self):
        r = self.q[self.i % len(self.q)]
        self.i += 1
        return r


def phase_gdn_prep(p, l, I, S, Z):
    ident, d_ident = S["ident"]
    W = 2 + 256 + 4 + 4096 + 2
    with SB(p) as sb:
        cw, d_cw = sb.t((128, 6, 5))
        p.dma("sp", cw[:], I["gdn_convT"][:, l, :, :], w=[d_cw])
        bones, d_bones = sb.t((128, 128))
        p.op("dve", lambda h: h.memset(bones[:], 0.0), w=[d_bones])
        p.op("dve", lambda h: h.memset(bones[0:64, 0:64], 1.0), w=[d_bones])
        p.op("dve", lambda h: h.memset(bones[64:128, 64:128], 1.0), w=[d_bones])
        X, d_X = sb.t((128, W))
        acc, d_acc = sb.t((128, W))
        sq, d_sq = sb.t((128, 512))
        rs, d_rs = sb.t((128, 512))
        pss = [sb.ps() for _ in range(2)]
        pst = [sb.ps() for _ in range(2)]
        tst = [sb.t((128, 128)) for _ in range(2)]
        p.op("dve", lambda h: h.memset(X[:], 0.0), w=[d_X])
        nb = 0
        for fc in range(6):
            p.dma("sp", X[:, 2:258], Z["qkvT"][fc * 128:(fc + 1) * 128, 0:256], w=[d_X])
            p.dma("sp", X[:, 262:4358], Z["qkvT"][fc * 128:(fc + 1) * 128, 256:T], w=[d_X])
            lo, hi = 2, 4358
            p.op("dve", lambda h, fc=fc: h.tensor_scalar(out=acc[:, lo:hi], in0=X[:, lo - 2:hi - 2], scalar1=cw[:, fc, 0:1], scalar2=None,
                                                         op0=ALU.mult), r=[d_X, d_cw], w=[d_acc])
            for tap in range(1, 5):
                eng = "dve"
                p.op(eng, lambda h, fc=fc, tap=tap: h.scalar_tensor_tensor(out=acc[:, lo:hi], in0=X[:, lo + tap - 2:hi + tap - 2],
                                                                             scalar=cw[:, fc, tap:tap + 1], in1=acc[:, lo:hi],
                                                                             op0=ALU.mult, op1=ALU.add), r=[d_X, d_cw, d_acc], w=[d_acc])
            p.op("act", lambda h: h.activation(out=X[:, lo:hi], in_=acc[:, lo:hi], func=AF.Sigmoid), r=[d_acc], w=[d_X])
            p.op("dve", lambda h: h.tensor_tensor(out=acc[:, lo:hi], in0=acc[:, lo:hi], in1=X[:, lo:hi], op=ALU.mult), r=[d_X, d_acc], w=[d_acc])
            segs = [(2, 256, 0)] + [(262 + 512 * i, 512, 256 + 512 * i) for i in range(8)]
            if fc < 4:
                for (c0, n, t0) in segs:
                    ps, d_ps = pss[nb % 2]
                    nb += 1
                    p.op("act", lambda h: h.activation(out=sq[:, 0:n], in_=acc[:, c0:c0 + n], func=AF.Square), r=[d_acc], w=[d_sq])
                    p.op("pe", lambda h, ps=ps: h.matmul(ps[:, 0:n], lhsT=bones[:], rhs=sq[:, 0:n], start=True, stop=True),
                         r=[d_bones, d_sq], w=[d_ps])
                    p.op("act", lambda h, ps=ps: h.activation(out=rs[:, 0:n], in_=ps[:, 0:n], func=AF.Sqrt, scale=1.0, bias=p.eps_ap(EPS, 128)),
                         r=[d_ps, p.d_eps], w=[d_rs])
                    p.op("dve", lambda h: h.reciprocal(out=rs[:, 0:n], in_=rs[:, 0:n]), r=[d_rs], w=[d_rs])
                    p.op("dve", lambda h: h.scalar_tensor_tensor(out=acc[:, c0:c0 + n], in0=acc[:, c0:c0 + n], scalar=(0.125 if fc < 2 else 1.0),
                                                                 in1=rs[:, 0:n], op0=ALU.mult, op1=ALU.mult), r=[d_acc, d_rs], w=[d_acc])
                dst = Z["gqT"] if fc < 2 else Z["gkT"]
                r0 = (fc % 2) * 128
                p.dma("sp", dst[r0:r0 + 128, 0:256], acc[:, 2:258], r=[d_acc])
                p.dma("sp", dst[r0:r0 + 128, 256:T], acc[:, 262:4358], r=[d_acc])
            if fc >= 2:
                dst = Z["gk"] if fc < 4 else Z["gv"]
                r0 = (fc % 2) * 128
                for tt in range(NT):
                    c0 = 2 + tt * 128 if tt < 2 else 262 + (tt - 2) * 128
                    ps, d_ps = pst[tt % 2]
                    ts_, d_ts = tst[tt % 2]
                    p.op("pe", lambda h, ps=ps, c0=c0: h.transpose(ps[:, 0:128], acc[:, c0:c0 + 128], ident[:]), r=[d_acc, d_ident], w=[d_ps])
                    p.op("act", lambda h, ps=ps, ts_=ts_: h.activation(out=ts_[:], in_=ps[:, 0:128], func=AF.Copy), r=[d_ps], w=[d_ts])
                    p.dma("sp", dst[tt * 128:(tt + 1) * 128, r0:r0 + 128], ts_[:], r=[d_ts])


def softplus_parts(p, z, d_z, tmp, d_tmp, n):
    p.op("dve", lambda h: h.tensor_scalar(out=tmp[:, n:2 * n], in0=z, scalar1=-1.0, scalar2=None, op0=ALU.mult), r=[d_z], w=[d_tmp])
    p.op("dve", lambda h: h.tensor_tensor(out=tmp[:, n:2 * n], in0=tmp[:, n:2 * n], in1=z, op=ALU.min), r=[d_z, d_tmp], w=[d_tmp])
    p.op("act", lambda h: h.activation(out=tmp[:, n:2 * n], in_=tmp[:, n:2 * n], func=AF.Exp), r=[d_tmp], w=[d_tmp])
    p.op("act", lambda h: h.activation(out=tmp[:, 0:n], in_=tmp[:, n:2 * n], func=AF.Ln, scale=1.0, bias=p.ones1[:, 0:1]),
         r=[d_tmp, p.d_ones1], w=[d_tmp])


def phase_gates(p, l, I, S, Z, tab, d_tab, cm, d_cm, ones, d_ones):
    ident, d_ident = S["ident"]
    with SB(p) as sb:
        pp = PsumPool(sb, 4)
        biasC, d_biasC = bcast_load(p, sb, I["mlstm_gate_bias"][l:l + 1, :], 16)
        dtb, d_dtb = bcast_load(p, sb, I["gdn_dt_bias"][l:l + 1, :], 8)
        nea, d_nea = bcast_load(p, sb, I["gdn_a_log"][l:l + 1, :], 8)
        p.op("act", lambda h: h.activation(out=nea[:], in_=nea[:], func=AF.Exp), r=[d_nea], w=[d_nea])
        p.op("dve", lambda h: h.tensor_scalar(out=nea[:], in0=nea[:], scalar1=-1.0, scalar2=None, op0=ALU.mult), r=[d_nea], w=[d_nea])
        gts = [sb.t((128, 32)) for _ in range(2)]
        for d in range(2):
            Bprev, d_B = sb.t((128, 4))
            R, d_R = sb.t((128, 4))
            p.op("dve", lambda h: h.memset(Bprev[:], 0.0), w=[d_B])
            p.op("dve", lambda h: h.memset(R[:], 0.0), w=[d_R])
            for s_, tt in enumerate(ORDER[d]):
                g, d_g = gts[s_ % 2]
                p.dma("sp", g[:, 0:16], Z["gC"][tt * 128:(tt + 1) * 128, :], w=[d_g])
                p.dma("sp", g[:, 16:32], Z["gD"][tt * 128:(tt + 1) * 128, :], w=[d_g])
                wk, d_wk = S["gwk"][s_ % 2]
                lhs_cum = cm[:, 1 - d, :]
                T_ = lambda a, b: tab[:, tt, d, a:b]
                xf = wk[:, 0:4]
                ig = wk[:, 4:8]
                p.op("dve", lambda h: h.tensor_tensor(out=xf, in0=g[:, 8 * d + 4:8 * d + 8], in1=biasC[:, 8 * d + 4:8 * d + 8], op=ALU.add),
                     r=[d_g, d_biasC], w=[d_wk])
                p.op("dve", lambda h: h.tensor_tensor(out=ig, in0=g[:, 8 * d:8 * d + 4], in1=biasC[:, 8 * d:8 * d + 4], op=ALU.add),
                     r=[d_g, d_biasC], w=[d_wk])
                softplus_parts(p, xf, d_wk, wk[:, 8:16], d_wk, 4)
                logf = wk[:, 16:20]
                p.op("dve", lambda h: h.scalar_tensor_tensor(out=logf, in0=xf, scalar=0.0, in1=wk[:, 8:12], op0=ALU.min, op1=ALU.subtract),
                     r=[d_wk], w=[d_wk])
                pcs, d_pcs = pp.get()
                ptot, d_ptot = pp.get()
                p.op("pe", lambda h: h.matmul(pcs[:, 0:4], lhsT=lhs_cum, rhs=logf, start=True, stop=True), r=[d_cm, d_wk], w=[d_pcs])
                p.op("pe", lambda h: h.matmul(ptot[:, 0:4], lhsT=ones[:], rhs=logf, start=True, stop=True), r=[d_ones, d_wk], w=[d_ptot])
                Bv = wk[:, 20:24]
                av = wk[:, 24:28]
                p.op("dve", lambda h: h.tensor_tensor(out=Bv, in0=pcs[:, 0:4], in1=Bprev[:], op=ALU.add), r=[d_pcs, d_B], w=[d_wk])
                p.op("dve", lambda h: h.tensor_tensor(out=av, in0=ig, in1=Bv, op=ALU.subtract), r=[d_wk], w=[d_wk])
                p.op("dve", lambda h: h.tensor_tensor(out=Bprev[:], in0=ptot[:, 0:4], in1=Bprev[:], op=ALU.add), r=[d_ptot, d_B], w=[d_B])
                ptr, d_ptr = pp.get()
                p.op("pe", lambda h: h.transpose(ptr[0:4, 0:128], av, ident[:]), r=[d_wk, d_ident], w=[d_ptr])
                am, d_am = S["gam4"]
                p.op("dve", lambda h: h.tensor_reduce(out=am[0:4, 0:1], in_=ptr[0:4, 0:128], axis=AX.X, op=ALU.max), r=[d_ptr], w=[d_am])
                p.op("dve", lambda h: h.tensor_scalar(out=am[0:4, 4:8], in0=ident[0:4, 0:4], scalar1=am[0:4, 0:1], scalar2=None, op0=ALU.mult),
                     r=[d_am, d_ident], w=[d_am])
                pam, d_pam = pp.get()
                p.op("pe", lambda h: h.matmul(pam[:, 0:4], lhsT=ones[0:4, :], rhs=am[0:4, 4:8], start=True, stop=True),
                     r=[d_ones, d_am], w=[d_pam])
                Mc = wk[:, 28:32]
                p.op("dve", lambda h: h.tensor_tensor(out=Mc, in0=pam[:, 0:4], in1=R[:], op=ALU.max), r=[d_pam, d_R], w=[d_wk])
                p.op("dve", lambda h: h.tensor_tensor(out=wk[:, 32:36], in0=R[:], in1=Mc, op=ALU.subtract), r=[d_R, d_wk], w=[d_wk])
                p.op("dve", lambda h: h.tensor_tensor(out=wk[:, 36:40], in0=av, in1=Mc, op=ALU.subtract), r=[d_wk], w=[d_wk])
                p.op("dve", lambda h: h.tensor_tensor(out=wk[:, 40:44], in0=Bv, in1=Mc, op=ALU.add), r=[d_wk], w=[d_wk])
                p.op("dve", lambda h: h.tensor_copy(out=R[:], in_=Mc), r=[d_wk], w=[d_R])
                p.op("act", lambda h: h.activation(out=T_(8, 12), in_=wk[:, 32:36], func=AF.Exp), r=[d_wk], w=[d_tab])
                p.op("act", lambda h: h.activation(out=T_(0, 4), in_=wk[:, 36:40], func=AF.Exp), r=[d_wk], w=[d_tab])
                p.op("act", lambda h: h.activation(out=T_(4, 8), in_=wk[:, 40:44], func=AF.Exp, scale=-1.0), r=[d_wk], w=[d_tab])
                z = wk[:, 44:48]
                p.op("dve", lambda h: h.tensor_tensor(out=z, in0=g[:, 16 + 8 * d + 4:16 + 8 * d + 8], in1=dtb[:, 4 * d:4 * d + 4], op=ALU.add),
                     r=[d_g, d_dtb], w=[d_wk])
                softplus_parts(p, z, d_wk, wk[:, 48:56], d_wk, 4)
                gg = wk[:, 56:60]
                p.op("dve", lambda h: h.scalar_tensor_tensor(out=gg, in0=z, scalar=0.0, in1=wk[:, 48:52], op0=ALU.max, op1=ALU.add),
                     r=[d_wk], w=[d_wk])
                p.op("dve", lambda h: h.tensor_tensor(out=gg, in0=gg, in1=nea[:, 4 * d:4 * d + 4], op=ALU.mult), r=[d_wk, d_nea], w=[d_wk])
                p.op("act", lambda h: h.activation(out=T_(12, 16), in_=g[:, 16 + 8 * d:16 + 8 * d + 4], func=AF.Exp, scale=-1.0), r=[d_g], w=[d_tab])
                p.op("dve", lambda h: h.tensor_scalar(out=T_(12, 16), in0=T_(12, 16), scalar1=1.0, scalar2=None, op0=ALU.add), r=[d_tab], w=[d_tab])
                p.op("dve", lambda h: h.reciprocal(out=T_(12, 16), in_=T_(12, 16)), r=[d_tab], w=[d_tab])
                p.op("dve", lambda h: h.tensor_scalar(out=T_(16, 20), in0=T_(12, 16), scalar1=-1.0, scalar2=None, op0=ALU.mult),
                     r=[d_tab], w=[d_tab])
                pgm, d_pgm = pp.get()
                pgl, d_pgl = pp.get()
                p.op("pe", lambda h: h.matmul(pgm[:, 0:4], lhsT=lhs_cum, rhs=gg, start=True, stop=True), r=[d_cm, d_wk], w=[d_pgm])
                p.op("pe", lambda h: h.matmul(pgl[:, 0:4], lhsT=ones[:], rhs=gg, start=True, stop=True), r=[d_ones, d_wk], w=[d_pgl])
                p.op("dve", lambda h: h.tensor_copy(out=T_(20, 24), in_=pgm[:, 0:4]), r=[d_pgm], w=[d_tab])
                p.op("dve", lambda h: h.tensor_tensor(out=wk[:, 60:64], in0=pgl[:, 0:4], in1=T_(20, 24), op=ALU.subtract),
                     r=[d_pgl, d_tab], w=[d_wk])
                p.op("act", lambda h: h.activation(out=T_(24, 28), in_=T_(20, 24), func=AF.Exp), r=[d_tab], w=[d_tab])
                p.op("act", lambda h: h.activation(out=T_(28, 32), in_=wk[:, 60:64], func=AF.Exp), r=[d_wk], w=[d_tab])
                p.op("act", lambda h: h.activation(out=T_(32, 36), in_=pgl[:, 0:4], func=AF.Exp), r=[d_pgl], w=[d_tab])
                p.op("dve", lambda h: h.tensor_tensor(out=T_(36, 40), in0=T_(12, 16), in1=T_(24, 28), op=ALU.mult), r=[d_tab], w=[d_tab])


def phase_chunk(p, l, I, S, Z, do_c=True, do_d=True, upto=""):
    ident, d_ident = S["ident"]
    phase_gdn_prep(p, l, I, S, Z)
    if "stopprep" in upto:
        return
    with SB(p) as sb:
        tab, d_tab = sb.t((128, NT, 2, NGC), F32, "gtab")
        cm, d_cm = sb.t((128, 2, 128), F32, "cmask")
        p.dma("sp", cm[:], I["cmask"][:, :, :], w=[d_cm])
        ones, d_ones = sb.t((128, 128))
        p.op("dve", lambda h: h.memset(ones[:], 1.0), w=[d_ones])
        ones1, p.d_ones1 = sb.t((128, 1))
        p.ones1 = ones1
        p.op("dve", lambda h: h.memset(ones1[:], 1.0), w=[p.d_ones1])
        S["gwk"] = [sb.t((128, 64)) for _ in range(2)]
        S["gam4"] = sb.t((8, 8))
        strict, d_strict = sb.t((128, 2, 128))
        for d in range(2):
            p.op("dve", lambda h, d=d: h.tensor_tensor(out=strict[:, d, :], in0=cm[:, d, :], in1=ident[:], op=ALU.subtract),
                 r=[d_cm, d_ident], w=[d_strict])
        phase_gates(p, l, I, S, Z, tab, d_tab, cm, d_cm, ones, d_ones)
        if "stopgates" in upto:
            return
        acc, d_acc = sb.t((128, NT, 256), F32, "hacc")
        if do_c:
            p.op("dve", lambda h: h.memset(acc[:], 0.0), w=[d_acc])
            chunk_c(p, l, I, S, Z, sb, tab, d_tab, cm, d_cm, acc, d_acc)
            p.barrier()
        if do_d:
            p.op("dve", lambda h: h.memset(acc[:], 0.0), w=[d_acc])
            chunk_d(p, l, I, S, Z, sb, tab, d_tab, cm, d_cm, strict, d_strict, ones, d_ones, acc, d_acc)


def chunk_c(p, l, I, S, Z, sbo, tab, d_tab, cm, d_cm, acc, d_acc):
    ident, d_ident = S["ident"]
    with SB(p) as sb:
        two = lambda shape, nm: [sb.t(shape, F32, nm) for _ in range(2)]
        ps_st = [sb.ps() for _ in range(2)]
        ps_o = [sb.ps() for _ in range(2)]
        ps_c = [sb.ps() for _ in range(2)]
        qf = [two((64, 4, 128), "cqf") for _ in range(2)]
        kf = [two((64, 4, 128), "ckf") for _ in range(2)]
        ktm = [two((128, 4, 64), "cktm") for _ in range(2)]
        vaug = [two((128, 4, 66), "cvaug") for _ in range(2)]
        for d in range(2):
            for q_ in range(2):
                va_, d_va_ = vaug[d][q_]
                p.op("dve", lambda h, va_=va_: h.memset(va_[:, :, 64:65], 1.0), w=[d_va_])
                p.op("dve", lambda h, va_=va_: h.memset(va_[:, :, 65:66], 0.0), w=[d_va_])
        ks, PT, vp, htmp = two((128, 4, 64), "cks"), two((128, 4, 128), "cPT"), two((128, 4, 66), "cvp"), two((128, 4, 64), "chtmp")
        rc = two((128, 8), "crc")
        Cst, d_Cst = sb.t((64, 8, 66), F32, "cCst")
        Cd, d_Cd = sb.t((64, 8, 66), F32, "cCd")
        p.op("dve", lambda h: h.memset(Cst[:], 0.0), w=[d_Cst])
        B3 = lambda t: t[:, :].rearrange("q (h c) -> q h c", h=4)
        O3 = lambda t: t[:, 0:264].rearrange("q (h c) -> q h c", h=4)
        for s_ in range(NT):
            par = s_ % 2
            tts = [ORDER[d][s_] for d in range(2)]
            for d in range(2):
                tk = slice(tts[d] * 128, (tts[d] + 1) * 128)
                p.dma("sp", qf[d][par][0][:], Z["qTc"][:, tk].rearrange("(h q) t -> q h t", q=64), w=[qf[d][par][1]])
                p.dma("sp", kf[d][par][0][:], Z["kTc"][:, tk].rearrange("(h q) t -> q h t", q=64), w=[kf[d][par][1]])
                p.dma("sp", ktm[d][par][0][:], Z["kC"][tk, :].rearrange("q (h e) -> q h e", e=64), w=[ktm[d][par][1]])
                p.dma("sp", vaug[d][par][0][:, :, 0:64], Z["vC"][tk, :].rearrange("q (h e) -> q h e", e=64), w=[vaug[d][par][1]])
            for d in range(2):
                tt = tts[d]
                qf_, d_qf = qf[d][par]
                kf_, d_kf = kf[d][par]
                pst, d_pst = ps_st[d]
                for hd in range(4):
                    p.op("pe", lambda h, pst=pst, hd=hd, kf_=kf_, qf_=qf_: h.matmul(pst[:, hd * 128:(hd + 1) * 128], lhsT=kf_[:, hd, :], rhs=qf_[:, hd, :],
                                                                                  start=True, stop=True), r=[d_kf, d_qf], w=[d_pst])
                PT_, d_PT = PT[d]
                p.op("dve", lambda h, pst=pst, PT_=PT_, d=d: h.scalar_tensor_tensor(out=PT_[:], in0=B3(pst), scalar=0.125,
                                                                                 in1=cm[:, 1 - d, :].unsqueeze(1).to_broadcast([128, 4, 128]),
                                                                                 op0=ALU.mult, op1=ALU.mult), r=[d_pst, d_cm], w=[d_PT])
                vp_, d_vp = vp[d]
                va_, d_va_ = vaug[d][par]
                p.op("pool", lambda h, vp_=vp_, va_=va_, tt=tt, d=d: h.tensor_tensor(out=vp_[:], in0=va_[:],
                                                                                  in1=tab[:, tt, d, 0:4].unsqueeze(2).to_broadcast([128, 4, 66]),
                                                                                  op=ALU.mult), r=[d_va_, d_tab], w=[d_vp])
                ks_, d_ks = ks[d]
                kt_, d_kt = ktm[d][par]
                p.op("pool", lambda h, ks_=ks_, kt_=kt_: h.tensor_scalar(out=ks_[:], in0=kt_[:], scalar1=0.125, scalar2=None, op0=ALU.mult),
                     r=[d_kt], w=[d_ks])
                p.op("dve", lambda h, tt=tt, d=d: h.tensor_tensor(out=Cd[:, 4 * d:4 * d + 4, :], in0=Cst[:, 4 * d:4 * d + 4, :],
                                                                  in1=tab[0:64, tt, d, 8:12].unsqueeze(2).to_broadcast([64, 4, 66]), op=ALU.mult),
                     r=[d_Cst, d_tab], w=[d_Cd])
            for d in range(2):
                qf_, d_qf = qf[d][par]
                po, d_po = ps_o[d]
                pc, d_pc = ps_c[d]
                PT_, d_PT = PT[d]
                vp_, d_vp = vp[d]
                ks_, d_ks = ks[d]
                for hd in range(4):
                    i = 4 * d + hd
                    p.op("pe", lambda h, po=po, hd=hd, PT_=PT_, vp_=vp_: h.matmul(po[:, hd * 66:(hd + 1) * 66], lhsT=PT_[:, hd, :], rhs=vp_[:, hd, :],
                                                                                start=True, stop=False), r=[d_PT, d_vp], w=[d_po])
                    p.op("pe", lambda h, po=po, hd=hd, i=i, qf_=qf_: h.matmul(po[:, hd * 66:(hd + 1) * 66], lhsT=qf_[:, hd, :], rhs=Cd[:, i, :],
                                                                             start=False, stop=True), r=[d_qf, d_Cd], w=[d_po])
                for hd in range(4):
                    p.op("pe", lambda h, pc=pc, hd=hd, ks_=ks_, vp_=vp_: h.matmul(pc[0:64, hd * 66:(hd + 1) * 66], lhsT=ks_[:, hd, :], rhs=vp_[:, hd, :],
                                                                                start=True, stop=True), r=[d_ks, d_vp], w=[d_pc])
                p.op("dve", lambda h, pc=pc, d=d: h.tensor_tensor(out=Cst[:, 4 * d:4 * d + 4, :],
                                                                  in0=pc[0:64, 0:264].rearrange("q (h c) -> q h c", h=4),
                                                                  in1=Cd[:, 4 * d:4 * d + 4, :], op=ALU.add), r=[d_pc, d_Cd], w=[d_Cst])
            for d in range(2):
                tt = tts[d]
                po, d_po = ps_o[d]
                rc_, d_rc = rc[d]
                ht_, d_ht = htmp[d]
                p.op("act", lambda h, po=po, rc_=rc_: h.activation(out=rc_[:, 0:4], in_=O3(po)[:, :, 64], func=AF.Abs), r=[d_po], w=[d_rc])
                p.op("dve", lambda h, rc_=rc_, tt=tt, d=d: h.tensor_tensor(out=rc_[:, 0:4], in0=rc_[:, 0:4], in1=tab[:, tt, d, 4:8], op=ALU.max),
                     r=[d_rc, d_tab], w=[d_rc])
                p.op("dve", lambda h, rc_=rc_: h.reciprocal(out=rc_[:, 4:8], in_=rc_[:, 0:4]), r=[d_rc], w=[d_rc])
                p.op("dve", lambda h, po=po, rc_=rc_, ht_=ht_: h.tensor_tensor(out=ht_[:], in0=O3(po)[:, :, 0:64],
                                                                               in1=rc_[:, 4:8].unsqueeze(2).to_broadcast([128, 4, 64]), op=ALU.mult),
                     r=[d_po, d_rc], w=[d_ht])
                dst = acc[:, tt, :].rearrange("q (h e) -> q h e", e=64)
                p.op("pool", lambda h, dst=dst, ht_=ht_: h.tensor_tensor(out=dst, in0=dst, in1=ht_[:], op=ALU.add), r=[d_ht, d_acc], w=[d_acc])
        gain, d_gain = bcast_load(p, sb, I["mlstm_norm"][l:l + 1, :], 256)
        ots = [sb.t((128, 256)) for _ in range(2)]
        xcs = [sb.t((128, 256)) for _ in range(2)]
        sqs = [sb.t((128, 256)) for _ in range(2)]
        sts = [sb.t((128, 12)) for _ in range(2)]
        for tt in range(NT):
            ot, d_ot = ots[tt % 2]
            xc, d_xc = xcs[tt % 2]
            sq, d_sq = sqs[tt % 2]
            st, d_st = sts[tt % 2]
            p.dma("sp", ot[:], Z["oC"][tt * 128:(tt + 1) * 128, :], w=[d_ot])
            p.op("act", lambda h, ot=ot: h.activation(out=ot[:], in_=ot[:], func=AF.Sigmoid), r=[d_ot], w=[d_ot])
            hv = acc[:, tt, :].rearrange("q (g e) -> q g e", e=64)
            v3 = lambda a: a[:, :].rearrange("q (g e) -> q g e", e=64)
            p.op("dve", lambda h, st=st, hv=hv: h.tensor_reduce(out=st[:, 0:4], in_=hv, axis=AX.X, op=ALU.add), r=[d_acc], w=[d_st])
            p.op("dve", lambda h, st=st: h.tensor_scalar(out=st[:, 0:4], in0=st[:, 0:4], scalar1=1.0 / 64, scalar2=None, op0=ALU.mult),
                 r=[d_st], w=[d_st])
            p.op("dve", lambda h, st=st, hv=hv, xc=xc: h.tensor_tensor(out=v3(xc), in0=hv, in1=st[:, 0:4].unsqueeze(2).to_broadcast([128, 4, 64]),
                                                                       op=ALU.subtract), r=[d_acc, d_st], w=[d_xc])
            p.op("act", lambda h, sq=sq, xc=xc: h.activation(out=sq[:], in_=xc[:], func=AF.Square), r=[d_xc], w=[d_sq])
            p.op("dve", lambda h, st=st, sq=sq: h.tensor_reduce(out=st[:, 4:8], in_=v3(sq), axis=AX.X, op=ALU.add), r=[d_sq], w=[d_st])
            rstd(p, st[:, 8:12], st[:, 4:8], st[:, 4:8], 1.0 / 64, [d_st])
            p.op("dve", lambda h, st=st, xc=xc: h.tensor_tensor(out=v3(xc), in0=v3(xc), in1=st[:, 8:12].unsqueeze(2).to_broadcast([128, 4, 64]),
                                                                op=ALU.mult), r=[d_st, d_xc], w=[d_xc])
            p.op("dve", lambda h, xc=xc: h.tensor_tensor(out=xc[:], in0=xc[:], in1=gain[:], op=ALU.mult), r=[d_gain, d_xc], w=[d_xc])
            p.op("dve", lambda h, xc=xc, ot=ot: h.tensor_tensor(out=xc[:], in0=xc[:], in1=ot[:], op=ALU.mult), r=[d_ot, d_xc], w=[d_xc])
            p.dma("sp", Z["y"][tt * 128:(tt + 1) * 128, 512:768], xc[:], r=[d_xc])


def chunk_d(p, l, I, S, Z, sbo, tab, d_tab, cm, d_cm, strict, d_strict, ones, d_ones, acc, d_acc):
    ident, d_ident = S["ident"]
    with SB(p) as sb:
        bk = [sb.ps() for _ in range(8)]
        B3 = lambda t: t[:, :].rearrange("q (h c) -> q h c", h=4)
        two = lambda shape, nm, dt=F32: [sb.t(shape, dt, nm) for _ in range(2)]
        qf = [two((64, 4, 128), "qf") for _ in range(2)]
        kf = [two((64, 4, 128), "kf") for _ in range(2)]
        ktm = [two((128, 4, 64), "ktm") for _ in range(2)]
        vtm = [two((128, 4, 64), "vtm") for _ in range(2)]
        diag, Ed, e1, e2, tmpP = two((128, 4, 128), "diag"), two((128, 4, 128), "Ed"), two((128, 4, 128), "e1"), two((128, 4, 128), "e2"), two((128, 4, 128), "tmpP")
        decT = [two((128, 4, 128), "decT") for _ in range(2)]
        decS = two((128, 4, 128), "decS")
        PQ = [two((128, 8, 128), "PQ") for _ in range(2)]
        TTt = [two((128, 4, 128), "TT") for _ in range(2)]
        PI = [two((128, 4, 128), "PI") for _ in range(2)]
        Ru, Rw, kdec = two((128, 4, 64), "Ru"), two((128, 4, 64), "Rw"), two((128, 4, 64), "kdec")
        u_all, d_u = sb.t((128, 8, 64), F32, "uall")
        vnew, d_vnew = sb.t((128, 8, 64), F32, "vnew")
        wT = two((64, 4, 128), "wT")
        attnT = two((128, 4, 128), "attnT")
        Sst, d_S = sb.t((64, 8, 64), F32, "Sst")
        otmp = two((128, 4, 64), "otmp")
        p.op("dve", lambda h: h.memset(Sst[:], 0.0), w=[d_S])
        bc_col = lambda ap: ap.unsqueeze(2).to_broadcast([128, 4, 128])
        bc_c64 = lambda ap, n=128: ap.unsqueeze(2).to_broadcast([n, 4, 64])
        bc_mat = lambda ap: ap.unsqueeze(1).to_broadcast([128, 4, 128])
        for s_ in range(NT):
            par = s_ % 2
            tts = [ORDER[d][s_] for d in range(2)]
            col = lambda d, g: tab[:, tts[d], d, 4 * g:4 * g + 4]
            for d in range(2):
                tt = tts[d]
                tk = slice(tt * 128, (tt + 1) * 128)
                p.dma("sp", qf[d][par][0][:], Z["gqT"][:, tk].rearrange("(h q) t -> q h t", q=64), w=[qf[d][par][1]])
                p.dma("sp", kf[d][par][0][:], Z["gkT"][:, tk].rearrange("(h q) t -> q h t", q=64), w=[kf[d][par][1]])
                p.dma("sp", ktm[d][par][0][:], Z["gk"][tk, :].rearrange("q (h e) -> q h e", e=64), w=[ktm[d][par][1]])
                p.dma("sp", vtm[d][par][0][:], Z["gv"][tk, :].rearrange("q (h e) -> q h e", e=64), w=[vtm[d][par][1]])
            for d in range(2):
                dg, d_dg = diag[d]
                p.op("dve", lambda h, d=d, dg=dg: h.tensor_tensor(out=dg[:], in0=bc_mat(ident[:, :]), in1=bc_col(col(d, 5)), op=ALU.mult),
                     r=[d_ident, d_tab], w=[d_dg])
                pg, d_pg = bk[d]
                p.op("pe", lambda h, pg=pg, dg=dg: h.matmul(pg[:, :], lhsT=ones[:], rhs=dg[:, :, :].rearrange("q h c -> q (h c)"), start=True, stop=True),
                     r=[d_ones, d_dg], w=[d_pg])
                E_, d_E = Ed[d]
                p.op("dve", lambda h, d=d, pg=pg, E_=E_: h.tensor_tensor(out=E_[:], in0=B3(pg), in1=bc_col(col(d, 5)), op=ALU.subtract),
                     r=[d_pg, d_tab], w=[d_E])
                a1, d_a1 = e1[d]
                a2, d_a2 = e2[d]
                p.op("dve", lambda h, E_=E_, a1=a1: h.tensor_scalar(out=a1[:], in0=E_[:], scalar1=0.0, scalar2=None, op0=ALU.min), r=[d_E], w=[d_a1])
                p.op("pool", lambda h, E_=E_, a2=a2: h.tensor_scalar(out=a2[:], in0=E_[:], scalar1=0.0, scalar2=None, op0=ALU.max), r=[d_E], w=[d_a2])
                p.op("act", lambda h, a1=a1: h.activation(out=a1[:], in_=a1[:], func=AF.Exp), r=[d_a1], w=[d_a1])
                p.op("act", lambda h, a2=a2: h.activation(out=a2[:], in_=a2[:], func=AF.Exp, scale=-1.0), r=[d_a2], w=[d_a2])
                dT, d_dT = decT[d][par]
                dS, d_dS = decS[d]
                p.op("pool", lambda h, d=d, a1=a1, dT=dT: h.tensor_tensor(out=dT[:], in0=a1[:], in1=bc_mat(cm[:, 1 - d, :]), op=ALU.mult),
                     r=[d_a1, d_cm], w=[d_dT])
                p.op("pool", lambda h, d=d, a2=a2, dS=dS: h.tensor_tensor(out=dS[:], in0=a2[:], in1=bc_mat(strict[:, d, :]), op=ALU.mult),
                     r=[d_a2, d_strict], w=[d_dS])
            for d in range(2):
                kf_, d_kf = kf[d][par]
                pG, d_pG = bk[2 + d]
                for hd in range(4):
                    p.op("pe", lambda h, pG=pG, hd=hd, kf_=kf_: h.matmul(pG[:, hd * 128:(hd + 1) * 128], lhsT=kf_[:, hd, :], rhs=kf_[:, hd, :],
                                                                        start=True, stop=True), r=[d_kf], w=[d_pG])
                tp_, d_tp = tmpP[d]
                p.op("dve", lambda h, d=d, pG=pG, tp_=tp_: h.tensor_tensor(out=tp_[:], in0=B3(pG), in1=bc_col(col(d, 4)), op=ALU.mult),
                     r=[d_pG, d_tab], w=[d_tp])
                pq_, d_pq = PQ[d][0]
                p.op("pool", lambda h, d=d, tp_=tp_, pq_=pq_: h.tensor_tensor(out=pq_[:, 0:4, :], in0=tp_[:], in1=decS[d][0][:], op=ALU.mult),
                     r=[d_tp, decS[d][1]], w=[d_pq])
                pQ, d_pQ = bk[4 + d]
                for hd in range(4):
                    p.op("pe", lambda h, pQ=pQ, hd=hd, pq_=pq_: h.transpose(pQ[:, hd * 128:(hd + 1) * 128], pq_[:, hd, :], ident[:]),
                         r=[d_pq, d_ident], w=[d_pQ])
                p.op("act", lambda h, pQ=pQ, pq_=pq_: h.activation(out=pq_[:, 4:8, :], in_=B3(pQ), func=AF.Copy), r=[d_pQ], w=[d_pq])
                tt0, d_tt0 = TTt[d][0]
                p.op("dve", lambda h, pQ=pQ, tt0=tt0: h.tensor_tensor(out=tt0[:], in0=B3(pQ), in1=bc_mat(ident[:, :]), op=ALU.add),
                     r=[d_pQ, d_ident], w=[d_tt0])
            for k in range(1, 7):
                for d in range(2):
                    c_, d_c = PQ[d][(k - 1) % 2]
                    n_, d_n = PQ[d][k % 2]
                    pP, d_pP = bk[d]
                    pQ, d_pQ = bk[2 + d]
                    for hd in range(4):
                        p.op("pe", lambda h, pP=pP, hd=hd, c_=c_: h.matmul(pP[:, hd * 128:(hd + 1) * 128], lhsT=c_[:, 4 + hd, :], rhs=c_[:, hd, :],
                                                                          start=True, stop=True), r=[d_c], w=[d_pP])
                    if k < 6:
                        for hd in range(4):
                            p.op("pe", lambda h, pQ=pQ, hd=hd, c_=c_: h.matmul(pQ[:, hd * 128:(hd + 1) * 128], lhsT=c_[:, hd, :], rhs=c_[:, 4 + hd, :],
                                                                              start=True, stop=True), r=[d_c], w=[d_pQ])
                    p.op("act", lambda h, pP=pP, n_=n_: h.activation(out=n_[:, 0:4, :], in_=B3(pP), func=AF.Copy), r=[d_pP], w=[d_n])
                    pi_, d_pi = PI[d][k % 2]
                    p.op("dve", lambda h, pP=pP, pi_=pi_: h.tensor_tensor(out=pi_[:], in0=B3(pP), in1=bc_mat(ident[:, :]), op=ALU.add),
                         r=[d_pP, d_ident], w=[d_pi])
                    if k < 6:
                        p.op("dve", lambda h, pQ=pQ, n_=n_: h.tensor_copy(out=n_[:, 4:8, :], in_=B3(pQ)), r=[d_pQ], w=[d_n])
                for d in range(2):
                    n_, d_n = PQ[d][k % 2]
                    tc_, d_tc = TTt[d][(k - 1) % 2]
                    tn_, d_tn = TTt[d][k % 2]
                    pT, d_pT = bk[4 + d]
                    pi_, d_pi = PI[d][k % 2]
                    for hd in range(4):
                        p.op("pe", lambda h, pT=pT, hd=hd, pi_=pi_, tc_=tc_: h.matmul(pT[:, hd * 128:(hd + 1) * 128], lhsT=pi_[:, hd, :], rhs=tc_[:, hd, :],
                                                                                    start=True, stop=True), r=[d_pi, d_tc], w=[d_pT])
                    if d == 0:
                        p.op("dve", lambda h, pT=pT, tn_=tn_: h.tensor_copy(out=tn_[:], in_=B3(pT)), r=[d_pT], w=[d_tn])
                    else:
                        p.op("act", lambda h, pT=pT, tn_=tn_: h.activation(out=tn_[:], in_=B3(pT), func=AF.Copy), r=[d_pT], w=[d_tn])
            pu, d_pu = bk[6]
            for d in range(2):
                TTf, d_TTf = TTt[d][0]
                ru, d_ru = Ru[d]
                rw, d_rw = Rw[d]
                kd, d_kd = kdec[d]
                kt_, d_kt = ktm[d][par]
                vt_, d_vt = vtm[d][par]
                p.op("pool", lambda h, d=d, ru=ru, vt_=vt_: h.tensor_tensor(out=ru[:], in0=vt_[:], in1=bc_c64(col(d, 3)), op=ALU.mult),
                     r=[d_vt, d_tab], w=[d_ru])
                p.op("pool", lambda h, d=d, rw=rw, kt_=kt_: h.tensor_tensor(out=rw[:], in0=kt_[:], in1=bc_c64(col(d, 9)), op=ALU.mult),
                     r=[d_kt, d_tab], w=[d_rw])
                p.op("pool", lambda h, d=d, kd=kd, kt_=kt_: h.tensor_tensor(out=kd[:], in0=kt_[:], in1=bc_c64(col(d, 7)), op=ALU.mult),
                     r=[d_kt, d_tab], w=[d_kd])
                for hd in range(4):
                    i = d * 4 + hd
                    p.op("pe", lambda h, i=i, hd=hd, TTf=TTf, ru=ru: h.matmul(pu[:, i * 64:(i + 1) * 64], lhsT=TTf[:, hd, :], rhs=ru[:, hd, :],
                                                                             start=True, stop=True), r=[d_TTf, d_ru], w=[d_pu])
            p.op("act", lambda h: h.activation(out=u_all[:], in_=pu[:, :].rearrange("q (i e) -> q i e", e=64), func=AF.Copy), r=[d_pu], w=[d_u])
            for d in range(2):
                TTf, d_TTf = TTt[d][0]
                rw, d_rw = Rw[d]
                pw, d_pw = bk[d]
                for hd in range(4):
                    p.op("pe", lambda h, pw=pw, hd=hd, TTf=TTf, rw=rw: h.matmul(pw[0:64, hd * 128:(hd + 1) * 128], lhsT=rw[:, hd, :], rhs=TTf[:, hd, :],
                                                                               start=True, stop=True), r=[d_TTf, d_rw], w=[d_pw])
                w_, d_w_ = wT[d]
                p.op("dve", lambda h, pw=pw, w_=w_: h.tensor_copy(out=w_[:], in_=pw[0:64, :].rearrange("q (h c) -> q h c", h=4)), r=[d_pw], w=[d_w_])
                pS, d_pS = bk[2 + d]
                kf_, d_kf = kf[d][par]
                qf_, d_qf = qf[d][par]
                for hd in range(4):
                    p.op("pe", lambda h, pS=pS, hd=hd, kf_=kf_, qf_=qf_: h.matmul(pS[:, hd * 128:(hd + 1) * 128], lhsT=kf_[:, hd, :], rhs=qf_[:, hd, :],
                                                                                start=True, stop=True), r=[d_kf, d_qf], w=[d_pS])
                at_, d_at = attnT[d]
                p.op("dve", lambda h, pS=pS, at_=at_, d=d: h.tensor_tensor(out=at_[:], in0=B3(pS), in1=decT[d][par][0][:], op=ALU.mult),
                     r=[d_pS, decT[d][par][1]], w=[d_at])
            pv, d_pv = bk[7]
            for d in range(2):
                for hd in range(4):
                    i = d * 4 + hd
                    p.op("pe", lambda h, i=i, hd=hd, d=d: h.matmul(pv[:, i * 64:(i + 1) * 64], lhsT=wT[d][0][:, hd, :], rhs=Sst[:, i, :],
                                                                  start=True, stop=True), r=[wT[d][1], d_S], w=[d_pv])
            p.op("dve", lambda h: h.tensor_tensor(out=vnew[:], in0=u_all[:], in1=pv[:, :].rearrange("q (i e) -> q i e", e=64), op=ALU.subtract),
                 r=[d_pv, d_u], w=[d_vnew])
            po1, d_po1 = bk[4]
            po2, d_po2 = bk[5]
            pn, d_pn = bk[6]
            for d in range(2):
                for hd in range(4):
                    i = d * 4 + hd
                    p.op("pe", lambda h, i=i, hd=hd, d=d: h.matmul(po1[:, i * 64:(i + 1) * 64], lhsT=attnT[d][0][:, hd, :], rhs=vnew[:, i, :],
                                                                  start=True, stop=True), r=[attnT[d][1], d_vnew], w=[d_po1])
            for d in range(2):
                for hd in range(4):
                    i = d * 4 + hd
                    p.op("pe", lambda h, i=i, hd=hd, d=d: h.matmul(po2[:, i * 64:(i + 1) * 64], lhsT=qf[d][par][0][:, hd, :], rhs=Sst[:, i, :],
                                                                  start=True, stop=True), r=[qf[d][par][1], d_S], w=[d_po2])
            for d in range(2):
                for hd in range(4):
                    i = d * 4 + hd
                    p.op("pe", lambda h, i=i, hd=hd, d=d: h.matmul(pn[0:64, i * 64:(i + 1) * 64], lhsT=kdec[d][0][:, hd, :], rhs=vnew[:, i, :],
                                                                  start=True, stop=True), r=[kdec[d][1], d_vnew], w=[d_pn])
            for d in range(2):
                dst = acc[:, tts[d], :].rearrange("q (h e) -> q h e", e=64)
                ot_, d_ot = otmp[d]
                p.op("dve", lambda h, d=d, dst=dst: h.tensor_tensor(out=dst, in0=po1[:, d * 256:(d + 1) * 256].rearrange("q (h e) -> q h e", e=64),
                                                                    in1=dst, op=ALU.add), r=[d_po1, d_acc], w=[d_acc])
                p.op("dve", lambda h, d=d, ot_=ot_: h.tensor_tensor(out=ot_[:], in0=po2[:, d * 256:(d + 1) * 256].rearrange("q (h e) -> q h e", e=64),
                                                                    in1=bc_c64(col(d, 6)), op=ALU.mult), r=[d_po2, d_tab], w=[d_ot])
                p.op("pool", lambda h, dst=dst, ot_=ot_: h.tensor_tensor(out=dst, in0=dst, in1=ot_[:], op=ALU.add), r=[d_ot, d_acc], w=[d_acc])
            for d in range(2):
                sv = Sst[:, 4 * d:4 * d + 4, :]
                egl_b = tab[0:64, tts[d], d, 32:36].unsqueeze(2).to_broadcast([64, 4, 64])
                p.op("dve", lambda h, sv=sv, egl_b=egl_b: h.tensor_tensor(out=sv, in0=sv, in1=egl_b, op=ALU.mult), r=[d_S, d_tab], w=[d_S])
                p.op("dve", lambda h, sv=sv, d=d: h.tensor_tensor(out=sv, in0=pn[0:64, d * 256:(d + 1) * 256].rearrange("q (h e) -> q h e", e=64),
                                                                  in1=sv, op=ALU.add), r=[d_pn, d_S], w=[d_S])
        gain, d_gain = sb.t((128, 256))
        for g4 in range(4):
            p.dma("sp", gain[:, 64 * g4:64 * g4 + 64], I["gdn_norm"][l:l + 1, :].partition_broadcast(128), w=[d_gain])
        zts = [sb.t((128, 256)) for _ in range(2)]
        sgs = [sb.t((128, 256)) for _ in range(2)]
        sqs = [sb.t((128, 256)) for _ in range(2)]
        sts = [sb.t((128, 8)) for _ in range(2)]
        v3 = lambda a: a[:, :].rearrange("q (g e) -> q g e", e=64)
        for tt in range(NT):
            zt, d_zt = zts[tt % 2]
            sg, d_sg = sgs[tt % 2]
            sq, d_sq = sqs[tt % 2]
            st, d_st = sts[tt % 2]
            p.dma("sp", zt[:], Z["zD"][tt * 128:(tt + 1) * 128, :], w=[d_zt])
            p.op("act", lambda h, zt=zt, sg=sg: h.activation(out=sg[:], in_=zt[:], func=AF.Sigmoid), r=[d_zt], w=[d_sg])
            p.op("dve", lambda h, zt=zt, sg=sg: h.tensor_tensor(out=sg[:], in0=sg[:], in1=zt[:], op=ALU.mult), r=[d_zt, d_sg], w=[d_sg])
            ov = acc[:, tt, :]
            p.op("act", lambda h, sq=sq, ov=ov: h.activation(out=sq[:], in_=ov, func=AF.Square), r=[d_acc], w=[d_sq])
            p.op("dve", lambda h, st=st, sq=sq: h.tensor_reduce(out=st[:, 0:4], in_=v3(sq), axis=AX.X, op=ALU.add), r=[d_sq], w=[d_st])
            rstd(p, st[:, 4:8], st[:, 0:4], st[:, 0:4], 1.0 / 64, [d_st])
            p.op("dve", lambda h, sq=sq, st=st, ov=ov: h.tensor_tensor(out=v3(sq), in0=ov.rearrange("q (g e) -> q g e", e=64),
                                                                       in1=st[:, 4:8].unsqueeze(2).to_broadcast([128, 4, 64]), op=ALU.mult),
                 r=[d_acc, d_st], w=[d_sq])
            p.op("dve", lambda h, sq=sq: h.tensor_tensor(out=sq[:], in0=sq[:], in1=gain[:], op=ALU.mult), r=[d_gain, d_sq], w=[d_sq])
            p.op("dve", lambda h, sq=sq, sg=sg: h.tensor_tensor(out=sq[:], in0=sq[:], in1=sg[:], op=ALU.mult), r=[d_sg, d_sq], w=[d_sq])
            p.dma("sp", Z["y"][tt * 128:(tt + 1) * 128, 768:1024], sq[:], r=[d_sq])


def phase_outproj(p, l, I, S, Z, xsrc, mine=False):
    ident, d_ident = S["ident"]
    grow, d_grow = S["grow"]
    with SB(p) as sb:
        w, d_w = sb.t((128, 8, D), BF16, "wout")
        for k in range(8):
            p.dma("pool", w[:, k, :], I["w_out"][l, k * 128:(k + 1) * 128, :], w=[d_w])
        yts = [sb.t((128, D)) for _ in range(2)]
        xts = [sb.t((128, D)) for _ in range(2)]
        yTs = [sb.t((128, 8, 128), BF16) for _ in range(2)]
        tps = [sb.ps() for _ in range(2)]
        pos = [sb.ps() for _ in range(2)]
        tiles = range(NT) if not mine else range(16)
        if mine:
            fl, d_fl = sb.t((128, 2))
            p.dma("sp", fl[:], I["flags"][:, :], w=[d_fl])
            ob_, d_ob_ = sb.t((128, D))
        for tt in tiles:
            j = 1 if (tt < 2 and not mine) else 0
            rows = slice(tt * 128, (tt + 1) * 128)
            yt, d_yt = yts[tt % 2]
            xt, d_xt = xts[tt % 2]
            yT, d_yT = yTs[tt % 2]
            if not mine:
                p.dma("sp", yt[:], Z["y"][rows, :], w=[d_yt])
                p.dma("sp", xt[:], xsrc[rows, :], w=[d_xt])
            else:
                ra = slice(256 + tt * 128, 256 + (tt + 1) * 128)
                rb = slice(256 + 2048 + tt * 128, 256 + 2048 + (tt + 1) * 128)
                p.dma("sp", yt[:, 0:512], Z["ym"][rows, :], w=[d_yt])
                p.dma("sp", yt[:, 512:1024], Z["y"][ra, 512:1024], w=[d_yt])
                p.dma("sp", ob_[:, 0:512], Z["y"][rb, 512:1024], w=[d_ob_])
                p.op("dve", lambda h, yt=yt: h.tensor_scalar(out=yt[:, 512:1024], in0=yt[:, 512:1024], scalar1=fl[:, 0:1], scalar2=None, op0=ALU.mult),
                     r=[d_yt, d_fl], w=[d_yt])
                p.op("dve", lambda h, yt=yt: h.scalar_tensor_tensor(out=yt[:, 512:1024], in0=ob_[:, 0:512], scalar=fl[:, 1:2], in1=yt[:, 512:1024],
                                                                    op0=ALU.mult, op1=ALU.add), r=[d_ob_, d_fl, d_yt], w=[d_yt])
                p.dma("sp", xt[:], xsrc[ra, :], w=[d_xt])
                p.dma("sp", ob_[:], xsrc[rb, :], w=[d_ob_])
                p.op("dve", lambda h, xt=xt: h.tensor_scalar(out=xt[:], in0=xt[:], scalar1=fl[:, 0:1], scalar2=None, op0=ALU.mult),
                     r=[d_xt, d_fl], w=[d_xt])
                p.op("dve", lambda h, xt=xt: h.scalar_tensor_tensor(out=xt[:], in0=ob_[:], scalar=fl[:, 1:2], in1=xt[:],
                                                                    op0=ALU.mult, op1=ALU.add), r=[d_ob_, d_fl, d_xt], w=[d_xt])
            for c in range(8):
                tp, d_tp = tps[c // 4]
                p.op("pe", lambda h, c=c, tp=tp, yt=yt: h.transpose(tp[:, (c % 4) * 128:(c % 4 + 1) * 128], yt[:, c * 128:(c + 1) * 128], ident[:]),
                     r=[d_yt, d_ident], w=[d_tp])
            for hh in range(2):
                tp, d_tp = tps[hh]
                p.op("act", lambda h, hh=hh, tp=tp, yT=yT: h.activation(out=yT[:, 4 * hh:4 * hh + 4, :],
                                                                       in_=tp[:, :].rearrange("q (c t) -> q c t", c=4), func=AF.Copy),
                     r=[d_tp], w=[d_yT])
            for hh in range(2):
                po, d_po = pos[hh]
                for k in range(8):
                    p.op("pe", lambda h, k=k, po=po, yT=yT, hh=hh: h.matmul(po[:, :], lhsT=yT[:, k, :], rhs=w[:, k, hh * 512:(hh + 1) * 512],
                                                                          start=(k == 0), stop=(k == 7)), r=[d_yT, d_w], w=[d_po])
                p.op("dve", lambda h, po=po, yt=yt, hh=hh, j=j: h.tensor_tensor(out=yt[:, hh * 512:(hh + 1) * 512], in0=po[:, :],
                                                                             in1=grow[:, 0, j, hh * 512:(hh + 1) * 512], op=ALU.mult),
                     r=[d_po, d_grow], w=[d_yt])
            p.op("dve", lambda h, xt=xt, yt=yt: h.tensor_tensor(out=xt[:], in0=xt[:], in1=yt[:], op=ALU.add), r=[d_yt, d_xt], w=[d_xt])
            p.dma("sp", (Z["xm1"] if mine else Z["xres"])[rows, :], xt[:], r=[d_xt])


def norm_objs(sb, S):
    junk, d_junk = sb.t((128, D))
    xs, d_xs = sb.t((128, D))
    ss, d_ss = sb.t((128, 4))
    tps = [sb.ps() for _ in range(2)]
    return (junk, d_junk, ss, d_ss, xs, d_xs, tps, S["ident"][0], S["ident"][1])


def phase_ffn_dense(p, l, I, S, Z):
    gsh, d_gsh = S["gsh"]
    grow, d_grow = S["grow"]
    NF = D_FF // 128
    with SB(p) as sb:
        wg, d_wg = sb.t((128, 8, D_FF), BF16, "wg")
        wu, d_wu = sb.t((128, 8, D_FF), BF16, "wu")
        wd, d_wd = sb.t((128, NF, D), BF16, "wd")
        for k in range(8):
            p.dma("pool", wg[:, k, :], I["ffn_w_gate"][k * 128:(k + 1) * 128, :], w=[d_wg])
            p.dma("pool", wu[:, k, :], I["ffn_w_up"][k * 128:(k + 1) * 128, :], w=[d_wu])
        for k in range(NF):
            p.dma("pool", wd[:, k, :], I["ffn_w_down"][k * 128:(k + 1) * 128, :], w=[d_wd])
        nobj = norm_objs(sb, S)
        xb, d_xb = sb.t((128, 2, D), F32, "xblk")
        xnT, d_xnT = sb.t((128, 8, 256), BF16, "xnT")
        hT, d_hT = sb.t((128, NF, 256), BF16, "hT")
        sgs = [sb.t((128, 256)) for _ in range(2)]
        pgs = [sb.ps() for _ in range(2)]
        pus = [sb.ps() for _ in range(2)]
        pos = [sb.ps() for _ in range(2)]
        ot, d_ot = sb.t((128, 512))
        blocks = [(2 * i, 2) for i in range(17)]
        n = 0
        for (t0, ntl) in blocks:
            ntok = ntl * 128
            j = 1 if t0 < 2 else 0
            for ti in range(ntl):
                tt = t0 + ti
                p.dma("sp", xb[:, ti, :], Z["xres"][tt * 128:(tt + 1) * 128, :], w=[d_xb])
            for ti in range(ntl):
                norm_tile(p, nobj, xb[:, ti, :], d_xb, gsh, d_gsh, 1, j, xnT, d_xnT, ti * 128)
            for fc in range(NF):
                pg, d_pg = pgs[fc % 2]
                pu, d_pu = pus[fc % 2]
                sg, d_sg = sgs[fc % 2]
                for k in range(8):
                    p.op("pe", lambda h, k=k, pg=pg, fc=fc: h.matmul(pg[:, 0:ntok], lhsT=wg[:, k, fc * 128:(fc + 1) * 128], rhs=xnT[:, k, 0:ntok],
                                                                    start=(k == 0), stop=(k == 7)), r=[d_wg, d_xnT], w=[d_pg])
                for k in range(8):
                    p.op("pe", lambda h, k=k, pu=pu, fc=fc: h.matmul(pu[:, 0:ntok], lhsT=wu[:, k, fc * 128:(fc + 1) * 128], rhs=xnT[:, k, 0:ntok],
                                                                    start=(k == 0), stop=(k == 7)), r=[d_wu, d_xnT], w=[d_pu])
                p.op("act", lambda h, pg=pg, sg=sg: h.activation(out=sg[:, 0:ntok], in_=pg[:, 0:ntok], func=AF.Sigmoid), r=[d_pg], w=[d_sg])
                p.op("dve", lambda h, pg=pg, sg=sg: h.tensor_tensor(out=sg[:, 0:ntok], in0=pg[:, 0:ntok], in1=sg[:, 0:ntok], op=ALU.mult),
                     r=[d_pg, d_sg], w=[d_sg])
                p.op("dve", lambda h, pu=pu, sg=sg, fc=fc: h.tensor_tensor(out=hT[:, fc, 0:ntok], in0=pu[:, 0:ntok], in1=sg[:, 0:ntok], op=ALU.mult),
                     r=[d_pu, d_sg], w=[d_hT])
            for ti in range(ntl):
                tt = t0 + ti
                for hh in range(2):
                    po, d_po = pos[n % 2]
                    n += 1
                    for fc in range(NF):
                        p.op("pe", lambda h, fc=fc, po=po, ti=ti, hh=hh: h.matmul(po[:, :], lhsT=hT[:, fc, ti * 128:(ti + 1) * 128],
                                                                                rhs=wd[:, fc, hh * 512:(hh + 1) * 512],
                                                                                start=(fc == 0), stop=(fc == NF - 1)), r=[d_hT, d_wd], w=[d_po])
                    p.op("dve", lambda h, po=po, hh=hh, j=j: h.tensor_tensor(out=ot[:], in0=po[:, :], in1=grow[:, 1, j, hh * 512:(hh + 1) * 512],
                                                                          op=ALU.mult), r=[d_po, d_grow], w=[d_ot])
                    p.op("dve", lambda h, ti=ti, hh=hh: h.tensor_tensor(out=xb[:, ti, hh * 512:(hh + 1) * 512], in0=xb[:, ti, hh * 512:(hh + 1) * 512],
                                                                         in1=ot[:], op=ALU.add), r=[d_ot, d_xb], w=[d_xb])
                p.dma("sp", Z["xres"][tt * 128:(tt + 1) * 128, :], xb[:, ti, :], r=[d_xb])


def phase_moe(p, l, I, S, Z, out):
    gsh, d_gsh = S["gsh"]
    grow, d_grow = S["grow"]
    NTM = 16
    SLAB = 512
    NSL = D_FFE // SLAB
    with SB(p) as sb:
        fl, d_fl = sb.t((128, 2))
        p.dma("sp", fl[:], I["flags"][:, :], w=[d_fl])
        xm, d_xm = sb.t((128, NTM, D), F32, "xm")
        xnT, d_xnT = sb.t((128, 8, NTM * 128), BF16, "xnTm")
        gates, d_gates = sb.t((128, NTM, NE), F32, "gates")
        rt, d_rt = sb.t((128, 8, NE), F32, "router")
        p.dma("sp", rt[:], I["moe_router"].rearrange("(k q) e -> q k e", q=128), w=[d_rt])
        with SB(p) as sb2:
            nobj = norm_objs(sb2, S)
            (junk, d_junk, ss, d_ss, xs, d_xs, tps, ident, d_ident) = nobj
            xa = [sb2.t((128, D)) for _ in range(2)]
            xnf, d_xnf = sb2.t((128, 8, 128), F32, "xnf")
            plg, d_plg = sb2.ps()
            lg, d_lg = sb2.t((128, 8))
            mx, d_mx = sb2.t((128, 8))
            wk, d_wk = sb2.t((128, 32))
            for jt in range(NTM):
                a_, d_a = xa[0]
                b_, d_b = xa[1]
                p.dma("sp", xm[:, jt, :], Z["xm1"][jt * 128:(jt + 1) * 128, :], w=[d_xm])
                norm_tile(p, nobj, xm[:, jt, :], d_xm, gsh, d_gsh, 1, 0, xnT, d_xnT, jt * 128)
                for c in range(8):
                    tp, d_tp = tps[c // 4]
                    p.op("act", lambda h, c=c, tp=tp: h.activation(out=xnf[:, c, :], in_=tp[:, (c % 4) * 128:(c % 4 + 1) * 128],
                                                                   func=AF.Identity, scale=gsh[:, 2, c, 0:1], bias=gsh[:, 3, c, 0:1]),
                         r=[d_tp, d_gsh], w=[d_xnf])
                for k in range(8):
                    p.op("pe", lambda h, k=k: h.matmul(plg[:, 0:NE], lhsT=xnf[:, k, :], rhs=rt[:, k, :], start=(k == 0), stop=(k == 7)),
                         r=[d_xnf, d_rt], w=[d_plg])
                p.op("dve", lambda h: h.tensor_copy(out=lg[:], in_=plg[:, 0:NE]), r=[d_plg], w=[d_lg])
                p.op("dve", lambda h: h.max(out=mx[:], in_=lg[:]), r=[d_lg], w=[d_mx])
                p.op("dve", lambda h: h.tensor_tensor(out=wk[:, 0:1], in0=mx[:, 1:2], in1=mx[:, 0:1], op=ALU.subtract), r=[d_mx], w=[d_wk])
                p.op("act", lambda h: h.activation(out=wk[:, 1:2], in_=wk[:, 0:1], func=AF.Exp), r=[d_wk], w=[d_wk])
                p.op("dve", lambda h: h.tensor_scalar(out=wk[:, 1:2], in0=wk[:, 1:2], scalar1=1.0, scalar2=None, op0=ALU.add), r=[d_wk], w=[d_wk])
                p.op("dve", lambda h: h.reciprocal(out=wk[:, 2:3], in_=wk[:, 1:2]), r=[d_wk], w=[d_wk])
                p.op("dve", lambda h: h.tensor_scalar(out=wk[:, 3:4], in0=wk[:, 2:3], scalar1=-1.0, scalar2=1.0, op0=ALU.mult, op1=ALU.add),
                     r=[d_wk], w=[d_wk])
                p.op("dve", lambda h: h.tensor_scalar(out=wk[:, 8:16], in0=lg[:], scalar1=mx[:, 0:1], scalar2=wk[:, 2:3], op0=ALU.is_equal,
                                                      op1=ALU.mult), r=[d_lg, d_mx, d_wk], w=[d_wk])
                p.op("dve", lambda h: h.tensor_scalar(out=wk[:, 16:24], in0=lg[:], scalar1=mx[:, 1:2], scalar2=wk[:, 3:4], op0=ALU.is_equal,
                                                      op1=ALU.mult), r=[d_lg, d_mx, d_wk], w=[d_wk])
                p.op("dve", lambda h, jt=jt: h.tensor_tensor(out=gates[:, jt, :], in0=wk[:, 8:16], in1=wk[:, 16:24], op=ALU.add),
                     r=[d_wk], w=[d_gates])
        wgs = [sb.t((128, 8, SLAB), BF16, "wgs") for _ in range(2)]
        wus = [sb.t((128, 8, SLAB), BF16, "wus") for _ in range(2)]
        wds = [sb.t((128, 4, D), BF16, "wds") for _ in range(2)]
        hTs = [sb.t((128, 4, 512), BF16, "hTs") for _ in range(2)]
        sgs = [sb.t((128, 512)) for _ in range(2)]
        pgs = [sb.ps() for _ in range(2)]
        pus = [sb.ps() for _ in range(2)]
        pos = [sb.ps() for _ in range(2)]
        ns = 0
        nh = 0
        nf = 0
        no = 0
        for e in range(NE):
            for sl in range(NSL):
                wg, d_wg = wgs[ns % 2]
                wu, d_wu = wus[ns % 2]
                wd, d_wd = wds[ns % 2]
                ns += 1
                c0 = sl * SLAB
                p.dma("pool", wg[:], I["moe_w_gate"][e].rearrange("(k q) f -> q k f", q=128)[:, :, c0:c0 + SLAB], w=[d_wg])
                p.dma("pool", wu[:], I["moe_w_up"][e].rearrange("(k q) f -> q k f", q=128)[:, :, c0:c0 + SLAB], w=[d_wu])
                p.dma("pool", wd[:], I["moe_w_down"][e, c0:c0 + SLAB, :].rearrange("(k q) d -> q k d", q=128), w=[d_wd])
                for k in range(4):
                    p.op("dve", lambda h, k=k, wd=wd: h.tensor_tensor(out=wd[:, k, :], in0=wd[:, k, :], in1=grow[:, 1, 0, :], op=ALU.mult),
                         r=[d_grow, d_wd], w=[d_wd])
                for tb in range(NTM // 4):
                    hT, d_hT = hTs[nh % 2]
                    nh += 1
                    toks = slice(tb * 512, (tb + 1) * 512)
                    for fc in range(4):
                        pg, d_pg = pgs[nf % 2]
                        pu, d_pu = pus[nf % 2]
                        sg, d_sg = sgs[nf % 2]
                        nf += 1
                        for k in range(8):
                            p.op("pe", lambda h, k=k, pg=pg, fc=fc, wg=wg: h.matmul(pg[:, :], lhsT=wg[:, k, fc * 128:(fc + 1) * 128], rhs=xnT[:, k, toks],
                                                                                  start=(k == 0), stop=(k == 7)), r=[d_wg, d_xnT], w=[d_pg])
                        for k in range(8):
                            p.op("pe", lambda h, k=k, pu=pu, fc=fc, wu=wu: h.matmul(pu[:, :], lhsT=wu[:, k, fc * 128:(fc + 1) * 128], rhs=xnT[:, k, toks],
                                                                                  start=(k == 0), stop=(k == 7)), r=[d_wu, d_xnT], w=[d_pu])
                        p.op("act", lambda h, pg=pg, sg=sg: h.activation(out=sg[:], in_=pg[:, :], func=AF.Sigmoid), r=[d_pg], w=[d_sg])
                        p.op("dve", lambda h, pg=pg, sg=sg: h.tensor_tensor(out=sg[:], in0=pg[:, :], in1=sg[:], op=ALU.mult), r=[d_pg, d_sg], w=[d_sg])
                        p.op("dve", lambda h, pu=pu, sg=sg, fc=fc, hT=hT: h.tensor_tensor(out=hT[:, fc, :], in0=pu[:, :], in1=sg[:], op=ALU.mult),
                             r=[d_pu, d_sg], w=[d_hT])
                    for ti in range(4):
                        jt = tb * 4 + ti
                        for hh in range(2):
                            po, d_po = pos[no % 2]
                            no += 1
                            for fc in range(4):
                                p.op("pe", lambda h, fc=fc, po=po, ti=ti, hh=hh, hT=hT, wd=wd: h.matmul(
                                    po[:, :], lhsT=hT[:, fc, ti * 128:(ti + 1) * 128], rhs=wd[:, fc, hh * 512:(hh + 1) * 512],
                                    start=(fc == 0), stop=(fc == 3)), r=[d_hT, d_wd], w=[d_po])
                            dst = xm[:, jt, hh * 512:(hh + 1) * 512]
                            p.op("dve", lambda h, po=po, dst=dst, jt=jt, e=e: h.scalar_tensor_tensor(out=dst, in0=po[:, :], scalar=gates[:, jt, e:e + 1],
                                                                                                  in1=dst, op0=ALU.mult, op1=ALU.add),
                                 r=[d_po, d_gates, d_xm], w=[d_xm])
        for jt in range(NTM):
            p.dma("sp", out[jt * 128:(jt + 1) * 128, :], xm[:, jt, :], r=[d_xm])


_CACHE = {}


def kernel(**inputs):
    inp = {k: np.asarray(v) for k, v in inputs.items()}
    if "p" not in _CACHE:
        _CACHE["p"] = build()
    p = _CACHE["p"]
    in_maps = [host_inputs(inp, c) for c in range(8)]
    res = run_bass_kernel_spmd(p.nc, in_maps, core_ids=list(range(8)))
    out = np.zeros((4, NLAT, D), np.float32)
    for c in range(8):
        b, hh = c // 2, c % 2
        out[b, hh * 2048:(hh + 1) * 2048, :] = np.asarray(res.results[c]["out"], dtype=np.float32)
    return out
```
